# Optimizing a Trainium2 kernel written in Bass

```python
import jax, jax.numpy as jnp
from jax import lax
import numpy as np

D_MODEL = 1024
BATCH = 8
SEQ = 4096
DEPTH = 1

MIX_WIDTH = D_MODEL
GMLP_WIDTH = MIX_WIDTH // 2
RWKV_WIDTH = MIX_WIDTH - GMLP_WIDTH
HEAD_DIM = 64
GMLP_HEADS = GMLP_WIDTH // HEAD_DIM
RWKV_HEADS = RWKV_WIDTH // HEAD_DIM
CHUNK = 128
DECAY_LORA = 32
ICLR_LORA = 32
GATE_LORA = 96
RWKV_PROJ = 3 * RWKV_WIDTH + DECAY_LORA + ICLR_LORA + GATE_LORA
IN_WIDTH = 2 * GMLP_WIDTH + RWKV_PROJ
N_GROUPS = 4
EXPERTS_PER_GROUP = 8
N_EXPERTS = N_GROUPS * EXPERTS_PER_GROUP
TOP_K = 2
EXPERT_HIDDEN = 512
MOE_BLOCK = 128
RMS_EPS = 1e-6
LN_EPS = 1e-5
GN_EPS = 64e-5

kernel_name = "hybrid_gmlp_rwkv7_hmoe_block"


def _rmsnorm(x, g):
    x32 = x.astype(jnp.float32)
    y = x32 * lax.rsqrt(jnp.mean(x32 * x32, axis=-1, keepdims=True) + RMS_EPS)
    return y.astype(x.dtype) * g


def _modulate(h, shift, scale):
    return h * (1 + scale[:, None, :]) + shift[:, None, :]


def _token_shift(z):
    return jnp.pad(z, ((0, 0), (1, 0), (0, 0)))[:, :-1]


def _gmlp_mixer(zu, zv, ln_w, ln_b, ws, bs):
    B, S, _ = zu.shape
    u = jax.nn.gelu(zu)
    v = jax.nn.gelu(zv)
    v32 = v.astype(jnp.float32)
    m = jnp.mean(v32, axis=-1, keepdims=True)
    var = jnp.mean(jnp.square(v32 - m), axis=-1, keepdims=True)
    v = ((v32 - m) * lax.rsqrt(var + LN_EPS)).astype(zu.dtype) * ln_w + ln_b
    vc = v.reshape(B, S // CHUNK, CHUNK, GMLP_HEADS, HEAD_DIM)
    w_causal = jnp.tril(ws)
    mixed = jnp.einsum('hts,bcshd->bcthd', w_causal, vc) + bs.T[None, None, :, :, None]
    return u * mixed.reshape(B, S, GMLP_WIDTH)


def _wkv7_scan(r, w, k, v, a, b):
    Bn, S, H, N = r.shape
    seq = tuple(jnp.moveaxis(t, 1, 0) for t in (r, w, k, v, a, b))

    def step(state, inp):
        r_t, w_t, k_t, v_t, a_t, b_t = inp
        sa = jnp.einsum('bhvk,bhk->bhv', state, a_t)
        state = (state * w_t[:, :, None, :] + sa[..., None] * b_t[:, :, None, :]
                 + v_t[..., None] * k_t[:, :, None, :])
        return state, jnp.einsum('bhvk,bhk->bhv', state, r_t)

    s0 = jnp.zeros((Bn, H, N, N), jnp.float32)
    _, y = lax.scan(step, s0, seq)
    return jnp.moveaxis(y, 0, 1)


def _rwkv7_mixer(z, mu, w0, w2, a0, a2, g2, k_k, k_a, r_k, gn_w, gn_b):
    B, S, _ = z.shape
    f32 = jnp.float32
    z = z + (_token_shift(z) - z) * mu
    W = RWKV_WIDTH
    r, k, v, xw, xa, xg = jnp.split(
        z, [W, 2 * W, 3 * W, 3 * W + DECAY_LORA, 3 * W + DECAY_LORA + ICLR_LORA], axis=-1)
    w_log = -jax.nn.softplus(-(w0 + jnp.tanh(xw) @ w2).astype(f32)) - 0.5
    decay = jnp.exp(-jnp.exp(w_log))
    a = jax.nn.sigmoid(a0 + xa @ a2)
    g = jax.nn.sigmoid(xg) @ g2
    hv = lambda t: t.reshape(B, S, RWKV_HEADS, HEAD_DIM)
    kk = hv(k * k_k).astype(f32)
    kk = kk / jnp.maximum(jnp.sqrt(jnp.sum(kk * kk, axis=-1, keepdims=True)), 1e-12)
    k = k * (1 + (a - 1) * k_a)
    r4, k4, v4, a4 = hv(r), hv(k), hv(v), hv(a)
    y = _wkv7_scan(r4.astype(f32), hv(decay), k4.astype(f32), v4.astype(f32),
                   -kk, kk * a4.astype(f32))
    m = jnp.mean(y, axis=-1, keepdims=True)
    var = jnp.mean(jnp.square(y - m), axis=-1, keepdims=True)
    y = ((y - m) * lax.rsqrt(var + GN_EPS)).reshape(B, S, W).astype(z.dtype) * gn_w + gn_b
    bonus = jnp.sum(r4 * k4 * r_k, axis=-1, keepdims=True) * v4
    return (y + bonus.reshape(B, S, W)) * g


def _hier_moe(h, rg_w, rg_b, re_w, re_b, w_gate, w_up, w_down):
    B, S, D = h.shape
    N = B * S
    f32 = jnp.float32
    ht = h.reshape(N, D)
    group_prob = jax.nn.softmax((ht @ rg_w + rg_b).astype(f32), axis=-1)
    g_p, g_idx = lax.top_k(group_prob, 1)
    exp_logits = (ht @ re_w + re_b).astype(f32).reshape(N, N_GROUPS, EXPERTS_PER_GROUP)
    sel = jnp.take_along_axis(exp_logits, g_idx[:, :, None], axis=1)[:, 0]
    e_p, e_idx = lax.top_k(jax.nn.softmax(sel, axis=-1), TOP_K)
    e_p = e_p / jnp.sum(e_p, axis=-1, keepdims=True)
    weights = g_p * e_p
    expert = g_idx * EXPERTS_PER_GROUP + e_idx
    A = N * TOP_K
    flat_e = expert.reshape(A)
    flat_w = weights.reshape(A)
    flat_tok = jnp.repeat(jnp.arange(N, dtype=jnp.int32), TOP_K)
    order = jnp.argsort(flat_e)
    se, stok, sw = flat_e[order], flat_tok[order], flat_w[order]
    counts = jnp.bincount(flat_e, length=N_EXPERTS)
    padded = (counts + MOE_BLOCK - 1) // MOE_BLOCK * MOE_BLOCK
    pad_end = jnp.cumsum(padded)
    pad_start = pad_end - padded
    start = jnp.cumsum(counts) - counts
    dest = pad_start[se] + jnp.arange(A) - start[se]
    n_blocks = -(-A // MOE_BLOCK) + N_EXPERTS
    P = n_blocks * MOE_BLOCK
    tok_buf = jnp.full((P,), N, jnp.int32).at[dest].set(stok)
    w_buf = jnp.zeros((P,), f32).at[dest].set(sw)
    block_e = jnp.minimum(
        jnp.searchsorted(pad_end, jnp.arange(n_blocks) * MOE_BLOCK, side='right'), N_EXPERTS - 1)
    x_pad = jnp.concatenate([ht, jnp.zeros((1, D), ht.dtype)], axis=0)
    xb = x_pad[tok_buf].reshape(n_blocks, MOE_BLOCK, D)

    def expert_block(args):
        xbk, e = args
        return (jax.nn.silu(xbk @ w_gate[e]) * (xbk @ w_up[e])) @ w_down[e]

    yb = lax.map(expert_block, (xb, block_e)).reshape(P, D)
    y = jnp.zeros((N + 1, D), yb.dtype).at[tok_buf].add(yb * w_buf[:, None].astype(yb.dtype))
    return y[:N].reshape(B, S, D)


def setup_inputs(seed: int = 0) -> dict:
    key = jax.random.key(seed)
    ks = jax.random.split(key, 40)
    L, D = DEPTH, D_MODEL
    nrm = lambda k, shape, s: jax.random.normal(k, shape, jnp.float32) * s
    uni = lambda k, shape, lo, hi: jax.random.uniform(k, shape, jnp.float32, lo, hi)
    return {
        "x": nrm(ks[0], (BATCH, SEQ, D), 1.0),
        "c": nrm(ks[1], (BATCH, D), 1.0),
        "ada_w": nrm(ks[2], (L, D, 6 * D), 0.3 * D ** -0.5),
        "ada_b": nrm(ks[3], (L, 6 * D), 0.01),
        "norm1_g": 1.0 + nrm(ks[4], (L, D), 0.05),
        "w_in": nrm(ks[5], (L, D, IN_WIDTH), D ** -0.5),
        "gmlp_ln_w": 1.0 + nrm(ks[6], (L, GMLP_WIDTH), 0.05),
        "gmlp_ln_b": nrm(ks[7], (L, GMLP_WIDTH), 0.02),
        "gmlp_ws": nrm(ks[8], (L, GMLP_HEADS, CHUNK, CHUNK), CHUNK ** -0.5),
        "gmlp_bs": 1.0 + nrm(ks[9], (L, GMLP_HEADS, CHUNK), 0.1),
        "rwkv_mu": uni(ks[10], (L, RWKV_PROJ), 0.0, 1.0),
        "rwkv_w0": uni(ks[11], (L, RWKV_WIDTH), -6.0, 0.0),
        "rwkv_w2": nrm(ks[12], (L, DECAY_LORA, RWKV_WIDTH), 0.1),
        "rwkv_a0": nrm(ks[13], (L, RWKV_WIDTH), 0.5),
        "rwkv_a2": nrm(ks[14], (L, ICLR_LORA, RWKV_WIDTH), ICLR_LORA ** -0.5),
        "rwkv_g2": nrm(ks[15], (L, GATE_LORA, RWKV_WIDTH), GATE_LORA ** -0.5),
        "rwkv_k_k": 0.85 + nrm(ks[16], (L, RWKV_WIDTH), 0.05),
        "rwkv_k_a": 1.0 + nrm(ks[17], (L, RWKV_WIDTH), 0.05),
        "rwkv_r_k": nrm(ks[18], (L, RWKV_HEADS, HEAD_DIM), 0.1),
        "rwkv_gn_w": 1.0 + nrm(ks[19], (L, RWKV_WIDTH), 0.05),
        "rwkv_gn_b": nrm(ks[20], (L, RWKV_WIDTH), 0.02),
        "w_out": nrm(ks[21], (L, MIX_WIDTH, D), MIX_WIDTH ** -0.5),
        "norm2_g": 1.0 + nrm(ks[22], (L, D), 0.05),
        "router_group_w": nrm(ks[23], (L, D, N_GROUPS), D ** -0.5),
        "router_group_b": nrm(ks[24], (L, N_GROUPS), 0.01),
        "router_expert_w": nrm(ks[25], (L, D, N_EXPERTS), D ** -0.5),
        "router_expert_b": nrm(ks[26], (L, N_EXPERTS), 0.01),
        "moe_w_gate": nrm(ks[27], (L, N_EXPERTS, D, EXPERT_HIDDEN), D ** -0.5),
        "moe_w_up": nrm(ks[28], (L, N_EXPERTS, D, EXPERT_HIDDEN), D ** -0.5),
        "moe_w_down": nrm(ks[29], (L, N_EXPERTS, EXPERT_HIDDEN, D), EXPERT_HIDDEN ** -0.5),
        "final_norm_g": 1.0 + nrm(ks[30], (D,), 0.05),
    }


def reference(x, c, ada_w, ada_b, norm1_g, w_in, gmlp_ln_w, gmlp_ln_b, gmlp_ws, gmlp_bs,
              rwkv_mu, rwkv_w0, rwkv_w2, rwkv_a0, rwkv_a2, rwkv_g2, rwkv_k_k, rwkv_k_a,
              rwkv_r_k, rwkv_gn_w, rwkv_gn_b, w_out, norm2_g, router_group_w, router_group_b,
              router_expert_w, router_expert_b, moe_w_gate, moe_w_up, moe_w_down,
              final_norm_g):
    for l in range(DEPTH):
        mod = c @ ada_w[l] + ada_b[l]
        shift1, scale1, gate1, shift2, scale2, gate2 = jnp.split(mod, 6, axis=-1)
        h = _modulate(_rmsnorm(x, norm1_g[l]), shift1, scale1)
        z = h @ w_in[l]
        zu, zv, zr = jnp.split(z, [GMLP_WIDTH, 2 * GMLP_WIDTH], axis=-1)
        y_a = _gmlp_mixer(zu, zv, gmlp_ln_w[l], gmlp_ln_b[l], gmlp_ws[l], gmlp_bs[l])
        y_b = _rwkv7_mixer(zr, rwkv_mu[l], rwkv_w0[l], rwkv_w2[l], rwkv_a0[l], rwkv_a2[l],
                           rwkv_g2[l], rwkv_k_k[l], rwkv_k_a[l], rwkv_r_k[l],
                           rwkv_gn_w[l], rwkv_gn_b[l])
        y = jnp.concatenate([y_a, y_b], axis=-1) @ w_out[l]
        x = x + gate1[:, None, :] * y
        h = _modulate(_rmsnorm(x, norm2_g[l]), shift2, scale2)
        y = _hier_moe(h, router_group_w[l], router_group_b[l], router_expert_w[l],
                      router_expert_b[l], moe_w_gate[l], moe_w_up[l], moe_w_down[l])
        x = x + gate2[:, None, :] * y
    return _rmsnorm(x, final_norm_g)
```

```python
import numpy as np
import os
from contextlib import ExitStack
import concourse.bass as bass
import concourse.mybir as mybir
from concourse.bass_utils import run_bass_kernel_spmd

F32 = mybir.dt.float32
BF16 = mybir.dt.bfloat16
I32 = mybir.dt.int32
AF = mybir.ActivationFunctionType
ALU = mybir.AluOpType
AX = mybir.AxisListType

D = 1024
S = 4096
TT = 256
NT = S // TT
NSUB = TT // 128
CH = 64
NCH = TT // CH
INW = 2720
NE = 32
EH = 512
NEG_HALF_E = -0.6065306597126334

PC = {}
_o = 0
for _n, _w in [("cT", 8), ("ada_b", 48), ("n1g", 8), ("n2g", 8), ("mu_rkv", 12), ("mu_xw", 1),
               ("mu_xa", 1), ("mu_xg", 1), ("w0", 4), ("a0", 4), ("k_k", 4), ("k_a", 4), ("r_k", 4),
               ("gn_w", 4), ("gn_b", 4)]:
    PC[_n] = _o
    _o += _w
NPRM = _o
CC = {}
_o = 0
for _n, _w in [("m5", 320), ("ident", 128), ("istack", 64), ("tril", 128), ("bones", 128), ("ones", 128), ("thr", 32), ("iotab", 96), ("pidx", 1)]:
    CC[_n] = _o
    _o += _w
NCST = _o
BC = {}
_o = 0
for _n, _w in [("lnw", 512), ("lnb", 512), ("rb", 36), ("bsT", 512)]:
    BC[_n] = _o
    _o += _w
NBC = _o


ATTACH_WAIT = True


class Prog:
    def __init__(self, nc, ctx):
        self.nc = nc
        self.ops = []
        self.last_w = {}
        self.readers = {}
        self.engs = ["pe", "act", "dve", "pool", "sp"]
        self.count = {e: 0 for e in self.engs}
        self.sem = {e: ctx.enter_context(nc.semaphore("s_" + e)) for e in self.engs}
        self.NDS = 8
        self.dsem = {q: [ctx.enter_context(nc.semaphore(f"d_{q}{i}")) for i in range(self.NDS)]
                     for q in ("sp", "pool")}
        self.dcount = {"sp": 0, "pool": 0}
        self.last_op = {e: None for e in self.engs}
        self.pending = {e: set() for e in self.engs}
        self.recent_dma = {"sp": [], "pool": []}

    def section(self, k):
        self.muted = k > self.cut

    def add(self, eng, fn, r=(), w=(), dma=False):
        if getattr(self, 'muted', False):
            return None
        idx = len(self.ops)
        deps = set(self.pending[eng])
        self.pending[eng] = set()
        for k in r:
            if k in self.last_w:
                deps.add(self.last_w[k])
        for k in w:
            if k in self.last_w:
                deps.add(self.last_w[k])
            deps.update(self.readers.get(k, ()))
        for k in r:
            self.readers.setdefault(k, []).append(idx)
        for k in w:
            self.last_w[k] = idx
            self.readers[k] = []
        if dma:
            q = eng
            kq = self.dcount[q]
            self.dcount[q] += 1
            sem = self.dsem[q][kq % self.NDS]
            val = 16 * (kq // self.NDS + 1)
            prev = (sem, val - 16) if val > 16 else None
            self.recent_dma[q].append(idx)
            self.recent_dma[q] = self.recent_dma[q][-self.NDS:]
        else:
            self.count[eng] += 1
            sem = self.sem[eng]
            val = self.count[eng]
            prev = None
        self.ops.append(dict(eng=eng, fn=fn, deps=deps, dma=dma, sem=sem, val=val, prev=prev))
        self.last_op[eng] = idx
        return idx

    def barrier(self):
        allops = set()
        for e in self.engs:
            if self.last_op[e] is not None:
                allops.add(self.last_op[e])
        for q in ("sp", "pool"):
            allops.update(self.recent_dma[q])
        dmaops = set()
        for q in ("sp", "pool"):
            dmaops.update(self.recent_dma[q])
        for e in self.engs:
            self.pending[e] |= (allops - dmaops) if e == "pe" else allops

    def emit(self):
        nc = self.nc
        per = {e: [] for e in self.engs}
        for i, op in enumerate(self.ops):
            per[op["eng"]].append(i)
        ops = self.ops

        def run(name, e):
            waited = {}
            for i in per[name]:
                op = ops[i]
                need = []
                for j in op["deps"]:
                    d = ops[j]
                    if d["eng"] == name and not d["dma"] and name == "pe":
                        continue
                    need.append((d["sem"], d["val"]))
                if op["prev"] is not None:
                    need.append(op["prev"])
                need.sort(key=lambda t: -t[1])
                todo = []
                for sem, val in need:
                    key = id(sem)
                    if waited.get(key, 0) >= val:
                        continue
                    waited[key] = val
                    todo.append((sem, val))
                attach = todo.pop() if (todo and ATTACH_WAIT) else None
                for sem, val in todo:
                    e.wait_ge(sem, val)
                inst = op["fn"](e)
                if attach is not None:
                    inst._wait_ge(attach[0], attach[1])
                inst.then_inc(op["sem"], 16 if op["dma"] else 1)
            if name in ("sp", "pool"):
                kq = self.dcount[name]
                for s_i in range(min(kq, self.NDS)):
                    n_on = (kq - s_i + self.NDS - 1) // self.NDS
                    e.wait_ge(self.dsem[name][s_i], 16 * n_on)

        with nc.Block() as block:
            @block.sync
            def _(e):
                run("sp", e)

            @block.scalar
            def _(e):
                run("act", e)

            @block.vector
            def _(e):
                run("dve", e)

            @block.tensor
            def _(e):
                run("pe", e)

            @block.gpsimd
            def _(e):
                run("pool", e)


def build_nc(stage=99, ntiles=NT, cut=99, nblk=96):
    nc = bass.Bass("TRN2", target_bir_lowering=False)
    dt = lambda name, shape, dty, kind: nc.dram_tensor(name, shape, dty, kind=kind).ap()
    x_d = dt("x", [S, D], F32, "ExternalInput")
    prm_d = dt("prm", [128, NPRM], F32, "ExternalInput")
    cst_d = dt("cst", [128, NCST], F32, "ExternalInput")
    bc_d = dt("bc", [128, NBC], F32, "ExternalInput")
    adaw_d = dt("ada_w", [D, 6 * D], F32, "ExternalInput")
    win_d = dt("w_in", [D, INW], F32, "ExternalInput")
    wout_d = dt("w_out", [D, D], F32, "ExternalInput")
    w2_d = dt("w2", [32, 512], F32, "ExternalInput")
    a2_d = dt("a2", [32, 512], F32, "ExternalInput")
    g2_d = dt("g2", [96, 512], F32, "ExternalInput")
    wsT_d = dt("wsT", [128, 8, 128], F32, "ExternalInput")
    wr_d = dt("wr", [D, 36], F32, "ExternalInput")
    fg_d = dt("fg", [128, D], F32, "ExternalInput")
    wg_d = dt("wg", [NE * 512, 1024], F32, "ExternalInput")
    wu_d = dt("wu", [NE * 512, 1024], F32, "ExternalInput")
    wd_d = dt("wd", [NE * 512, 1024], F32, "ExternalInput")
    NBLK = 96
    xn2_d = dt("xn2_d", [S, D], BF16, "Internal")
    xs_d = dt("xs_d", [NBLK * 128, D], BF16, "Internal")
    ys_d = dt("ys_d", [NBLK * 128, D], F32, "Internal")
    out_d = dt("out", [S, D], F32, "ExternalOutput")
    x1_d = dt("x1_d", [S, D], F32, "Internal")

    with ExitStack() as top:
        P = Prog(nc, top)
        P.cut = cut
        psum = [top.enter_context(nc.psum_tensor(f"ps{i}", [128, 512], F32)) for i in range(7)]
        psTt = top.enter_context(nc.psum_tensor("psT", [128, 512], F32))
        psT = [psTt[:, 0:256], psum[6][:, 0:256]]
        psTk = ["ps7", "ps6"]
        fst = None
        fmv = None
        pk = lambda i: f"ps{i}"

        def SB(ctx, name, shape, dty=F32):
            return ctx.enter_context(nc.sbuf_tensor(name, shape, dty))

        prm = SB(top, "prm_s", [128, NPRM])
        cst = SB(top, "cst_s", [128, NCST])
        bcs = SB(top, "bc_s", [128, NBC])
        mod = SB(top, "mod", [128, 48])
        wt = SB(top, "wt", [128, 32, 32])
        PABi = SB(top, "PABi", [128, 2, 32], I32)
        WAB = SB(top, "WAB", [128, 2, 32])
        IDXW = SB(top, "IDXW", [128, 4, 96], I32)
        P.add("sp", lambda e: e.dma_start(out=prm[:], in_=prm_d[:, :]), w=["prm"], dma=True)
        P.add("sp", lambda e: e.dma_start(out=cst[:], in_=cst_d[:, :]), w=["cst"], dma=True)
        P.add("sp", lambda e: e.dma_start(out=bcs[:], in_=bc_d[:, :]), w=["bc"], dma=True)

        def pcol(name, j=0, n=1):
            return prm[:, PC[name] + j:PC[name] + j + n]

        ident = cst[:, CC["ident"]:CC["ident"] + 128]
        istack = cst[:, CC["istack"]:CC["istack"] + 64]
        tril = cst[:, CC["tril"]:CC["tril"] + 128]
        bones = cst[:, CC["bones"]:CC["bones"] + 128]
        ones = cst[:, CC["ones"]:CC["ones"] + 128]
        m5 = cst[:, CC["m5"]:CC["m5"] + 320]

        with ExitStack() as c0:
            stg = [SB(c0, f"ada_stg{i}", [128, 6 * D]) for i in range(2)]
            for kc in range(8):
                sb = stg[kc % 2]
                P.add("sp", (lambda sb, kc: lambda e: e.dma_start(out=sb[:], in_=adaw_d[kc * 128:(kc + 1) * 128, :]))(sb, kc),
                      w=[f"ada_stg{kc % 2}"], dma=True)
                for oc in range(48):
                    P.add("pe", (lambda sb, kc, oc: lambda e: e.matmul(psum[0][:, oc:oc + 1], lhsT=sb[:, oc * 128:(oc + 1) * 128],
                                                                   rhs=prm[:, PC["cT"] + kc:PC["cT"] + kc + 1], start=True, stop=True))(sb, kc, oc),
                          r=[f"ada_stg{kc % 2}", "prm"], w=[pk(0)])
                if kc == 0:
                    P.add("dve", lambda e: e.tensor_tensor(out=mod[:], in0=psum[0][:, 0:48], in1=prm[:, PC["ada_b"]:PC["ada_b"] + 48], op=ALU.add),
                          r=[pk(0), "prm"], w=["mod"])
                else:
                    P.add("dve", lambda e: e.tensor_tensor(out=mod[:], in0=psum[0][:, 0:48], in1=mod[:], op=ALU.add),
                          r=[pk(0), "mod"], w=["mod"])
            P.barrier()
        sc = SB(top, "sc", [128, 32])
        P.add("dve", lambda e: e.scalar_tensor_tensor(out=sc[:, 0:8], in0=mod[:, 8:16], scalar=1.0, in1=prm[:, PC["n1g"]:PC["n1g"] + 8],
                                                      op0=ALU.add, op1=ALU.mult), r=["mod", "prm"], w=["sc"])
        P.add("dve", lambda e: e.scalar_tensor_tensor(out=sc[:, 8:16], in0=mod[:, 32:40], scalar=1.0, in1=prm[:, PC["n2g"]:PC["n2g"] + 8],
                                                      op0=ALU.add, op1=ALU.mult), r=["mod", "prm"], w=["sc"])
        gate_bc = SB(top, "gate_bc", [128, D])
        gl = SB(top, "gl", [128, 128])

        def make_gate(dst, dkey, gcol):
            for c in range(8):
                P.add("dve", (lambda c: lambda e: e.tensor_scalar(out=gl[:], in0=ones, scalar1=mod[:, gcol + c:gcol + c + 1], scalar2=None,
                                                                  op0=ALU.mult))(c), r=["mod", "cst"], w=["gl"])
                P.add("pe", (lambda c: lambda e: e.matmul(psum[1][:, (c % 4) * 128:(c % 4 + 1) * 128], lhsT=gl[:], rhs=ident, start=True, stop=True))(c),
                      r=["gl", "cst"], w=[pk(1)])
                P.add("act", (lambda c: lambda e: e.activation(out=dst[:, c * 128:(c + 1) * 128], in_=psum[1][:, (c % 4) * 128:(c % 4 + 1) * 128],
                                                               func=AF.Identity))(c), r=[pk(1)], w=[dkey])
        make_gate(gate_bc, "gate_bc", 16)

        with ExitStack() as ca:
            win = SB(ca, "win", [128, 8, INW], BF16)
            wout = SB(ca, "wout", [128, 8, D], BF16)
            identb = SB(ca, "identb", [128, 128], BF16)
            w2s = SB(ca, "w2s", [32, 512])
            a2s = SB(ca, "a2s", [32, 512])
            g2s = SB(ca, "g2s", [96, 512])
            wrs = SB(ca, "wrs", [128, 8, 36])
            wc = SB(ca, "wc", [128, 8, 128])
            logits = SB(ca, "logits", [128, 32, 36])
            Hst = [SB(ca, f"H{j}", [128, 64]) for j in range(4)]
            carry = SB(ca, "carry", [128, 16])
            P.add("pool", lambda e: e.tensor_copy(out=identb[:], in_=ident), r=["cst"], w=["identb"])
            P.add("sp", lambda e: e.dma_start(out=w2s[:], in_=w2_d[:, :]), w=["w2s"], dma=True)
            P.add("sp", lambda e: e.dma_start(out=a2s[:], in_=a2_d[:, :]), w=["a2s"], dma=True)
            P.add("sp", lambda e: e.dma_start(out=g2s[:], in_=g2_d[:, :]), w=["g2s"], dma=True)
            P.add("sp", lambda e: e.dma_start(out=wrs[:], in_=wr_d.rearrange("(c p) n -> p c n", p=128)), w=["wrs"], dma=True)
            P.add("sp", lambda e: e.dma_start(out=wc[:], in_=wsT_d[:, :, :]), w=["wc"], dma=True)
            for h in range(8):
                P.add("pool", (lambda h: lambda e: e.tensor_tensor(out=wc[:, h, :], in0=wc[:, h, :], in1=tril, op=ALU.mult))(h),
                      r=["wc", "cst"], w=["wc"])
            P.add("pool", lambda e: e.memset(carry[:], 0.0), w=["carry"])
            for _i in range(int(os.environ.get("KPAD", "0"))):
                P.add("pool", lambda e: e.memset(gl[:, 0:1], 0.0), w=["gl_dummy"])
            for j in range(4):
                P.add("pool", (lambda j: lambda e: e.memset(Hst[j][:], 0.0))(j), w=[f"H{j}"])
            with ExitStack() as cw:
                wstg = [SB(cw, f"wstg{i}", [128, INW]) for i in range(2)]
                for kc in range(8):
                    sb = wstg[kc % 2]
                    P.add("sp", (lambda sb, kc: lambda e: e.dma_start(out=sb[:], in_=win_d[kc * 128:(kc + 1) * 128, :]))(sb, kc),
                          w=[f"wstg{kc % 2}"], dma=True)
                    P.add("pool", (lambda sb, kc: lambda e: e.tensor_copy(out=win[:, kc, :], in_=sb[:]))(sb, kc),
                          r=[f"wstg{kc % 2}"], w=["win"])
                for kc in range(8):
                    sb = wstg[kc % 2]
                    P.add("sp", (lambda sb, kc: lambda e: e.dma_start(out=sb[:, 0:D], in_=wout_d[kc * 128:(kc + 1) * 128, :]))(sb, kc),
                          w=[f"wstg{kc % 2}"], dma=True)
                    P.add("pool", (lambda sb, kc: lambda e: e.tensor_copy(out=wout[:, kc, :], in_=sb[:, 0:D]))(sb, kc),
                          r=[f"wstg{kc % 2}"], w=["wout"])
                P.barrier()

            ct = ExitStack()
            xt = [SB(ct, "xt0", [128, NSUB, D])] * 2
            xn = SB(ct, "xn", [128, NSUB, D], BF16)
            hT = SB(ct, "hT", [128, 8, TT], BF16)
            st6 = SB(ct, "st6", [128, 4, 6])
            mv = SB(ct, "mv", [128, 8])
            u_s = SB(ct, "u_s", [128, 4, TT], BF16)
            vg = SB(ct, "vg", [128, 512])
            vn = vg
            zraw = [SB(ct, f"zraw{i}", [128, TT + 1]) for i in range(2)]
            rkv = SB(ct, "rkv", [128, 12, TT])
            lor = SB(ct, "lor", [128, 3, TT])
            LW, ASN, BSN, KM, BON, GG = range(6)
            pers = [SB(ct, f"pers{j}", [128, 6, TT]) for j in range(4)]
            AA, KX, RN, PRD = range(4)
            ptmp = SB(ct, "ptmp", [128, 4, TT])
            ybT = SB(ct, "ybT", [128, 4, TT])
            ycat = SB(ct, "ycat", [128, 8, TT], BF16)
            x1t = SB(ct, "x1t", [128, D])
            xn2 = x1t
            h2f = SB(ct, "h2f", [128, 8, 128])
            gtmp = h2f[:, 0:4, :]
            xnb = SB(ct, "xnb", [128, D], BF16)
            CI, CE, EI, EE, EN, AT, RT, BT, KT = range(9)
            sct = [SB(ct, f"sct{j}", [128, 9, 64]) for j in range(4)]
            mats = [SB(ct, f"mats{j}", [128, 320]) for j in range(4)]
            trs = [SB(ct, f"trs{j}", [128, 192]) for j in range(4)]
            wzb_l = [SB(ct, f"wzb{j}", [128, 256]) for j in range(4)]
            qq = [[SB(ct, f"qq{j}_{i}", [128, 64]) for i in range(2)] for j in range(4)]
            chn = [SB(ct, f"chn{j}", [128, 4, 64]) for j in range(4)]
            gst = [SB(ct, f"gst{j}", [128, 12]) for j in range(4)]

            def A(eng, fn, r=(), w=()):
                P.add(eng, fn, r=r, w=w)

            def norm_stats(src, srck, eps):
                for hf in range(2):
                    A("dve", (lambda hf: lambda e: e.bn_stats(out=st6[:, hf, :], in_=src(hf)))(hf), r=[srck], w=["st6"])
                A("dve", lambda e: e.bn_aggr(out=mv[:, 0:2], in_=st6[:, 0:2, :].rearrange("p a b -> p (a b)")), r=["st6"], w=["mv"])
                A("dve", lambda e: e.scalar_tensor_tensor(out=mv[:, 2:3], in0=mv[:, 0:1], scalar=mv[:, 0:1], in1=mv[:, 1:2], op0=ALU.mult, op1=ALU.add),
                  r=["mv"], w=["mv"])
                A("act", lambda e: e.activation(out=mv[:, 3:4], in_=mv[:, 2:3], func=AF.Sqrt, bias=float(eps)), r=["mv"], w=["mv"])
                A("dve", lambda e: e.reciprocal(out=mv[:, 3:4], in_=mv[:, 3:4]), r=["mv"], w=["mv"])

            for TI in range(min(NT, ntiles) if stage >= 1 else 0):
                t0 = TI * TT
                xb = xt[TI % 2]
                xk = "xt0"
                P.add("sp", (lambda xb, t0: lambda e: e.dma_start(out=xb[:], in_=x_d[t0:t0 + TT, :].rearrange("(s p) d -> p s d", p=128)))(xb, t0),
                      w=[xk], dma=True)
                P.section(1)
                for sub in range(NSUB):
                    norm_stats((lambda xb, sub: lambda hf: xb[:, sub, hf * 512:(hf + 1) * 512])(xb, sub), xk, 1e-6)
                    A("act", (lambda xb, sub: lambda e: e.activation(out=xn[:, sub, :], in_=xb[:, sub, :], func=AF.Identity, scale=mv[:, 3:4]))(xb, sub),
                      r=[xk, "mv"], w=["xn"])
                P.section(1.5)
                for kc in range(8):
                    pb = kc % 2
                    for sub in range(NSUB):
                        A("pe", (lambda kc, sub, pb: lambda e: e.matmul(psT[pb][:, sub * 128:(sub + 1) * 128], lhsT=xn[:, sub, kc * 128:(kc + 1) * 128],
                                                                       rhs=identb[:], start=True, stop=True))(kc, sub, pb), r=["xn", "identb"], w=[psTk[pb]])
                    if os.environ.get("DVE_EVAC"):
                      A("dve", (lambda kc, pb: lambda e: e.tensor_scalar(out=hT[:, kc, :], in0=psT[pb][:, 0:TT], scalar1=sc[:, kc:kc + 1], scalar2=mod[:, kc:kc + 1],
                                                                         op0=ALU.mult, op1=ALU.add))(kc, pb), r=[psTk[pb], "sc", "mod"], w=["hT"])
                    elif not os.environ.get("SKIP_EVAC"):
                      A("act", (lambda kc, pb: lambda e: e.activation(out=hT[:, kc, :], in_=psT[pb][:, 0:TT], func=AF.Identity,
                                                                    **({} if os.environ.get("NO_SB") else dict(scale=sc[:, kc:kc + 1], bias=mod[:, kc:kc + 1]))))(kc, pb),
                      r=[psTk[pb], "sc", "mod"], w=["hT"])

                P.section(2)
                pcnt = [0]

                def proj(c0, M):
                    pb = pcnt[0] % 2
                    pcnt[0] += 1
                    for kc in range(8):
                        A("pe", (lambda kc, pb: lambda e: e.matmul(psum[pb][0:M, 0:TT], lhsT=win[:, kc, c0:c0 + M], rhs=hT[:, kc, :],
                                                                  start=(kc == 0), stop=(kc == 7)))(kc, pb), r=["win", "hT"], w=[pk(pb)])
                    return pb

                for j in range(4):
                    pb = proj(j * 128, 128)
                    A("act", (lambda j, pb: lambda e: e.activation(out=u_s[:, j, :], in_=psum[pb][:, 0:TT], func=AF.Gelu_apprx_tanh))(j, pb),
                      r=[pk(pb)], w=["u_s"])
                specs = [(1024 + q * 128, 128, PC["mu_rkv"] + q) for q in range(12)] + \
                        [(2560, 32, PC["mu_xw"]), (2592, 32, PC["mu_xa"]), (2624, 96, PC["mu_xg"])]
                for q, (c0, M, mucol) in enumerate(specs):
                    pb = proj(c0, M)
                    zb = zraw[q % 2]
                    zk = f"zraw{q % 2}"
                    dst = rkv[0:M, q, :] if q < 12 else lor[0:M, q - 12, :]
                    dk = f"rkv{q}"
                    A("pool", (lambda zb, q, M: lambda e: e.tensor_copy(out=zb[0:M, 0:1], in_=carry[0:M, q:q + 1]))(zb, q, M), r=["carry"], w=[zk])
                    A("act", (lambda zb, pb, M: lambda e: e.activation(out=zb[0:M, 1:TT + 1], in_=psum[pb][0:M, 0:TT], func=AF.Identity))(zb, pb, M),
                      r=[pk(pb)], w=[zk])
                    A("pool", (lambda zb, q, M: lambda e: e.tensor_copy(out=carry[0:M, q:q + 1], in_=zb[0:M, TT:TT + 1]))(zb, q, M), r=[zk], w=["carry"])
                    A("dve", (lambda zb, dst, M: lambda e: e.tensor_tensor(out=dst, in0=zb[0:M, 0:TT], in1=zb[0:M, 1:TT + 1], op=ALU.subtract))(zb, dst, M),
                      r=[zk], w=[dk])
                    A("dve", (lambda zb, dst, M, mucol: lambda e: e.scalar_tensor_tensor(out=dst, in0=dst, scalar=prm[0:M, mucol:mucol + 1], in1=zb[0:M, 1:TT + 1],
                                                                                      op0=ALU.mult, op1=ALU.add))(zb, dst, M, mucol), r=[zk, dk, "prm"], w=[dk])
                A("act", lambda e: e.activation(out=lor[0:32, 0, :], in_=lor[0:32, 0, :], func=AF.Tanh), r=["rkv12"], w=["rkv12"])
                A("act", lambda e: e.activation(out=lor[0:96, 2, :], in_=lor[0:96, 2, :], func=AF.Sigmoid), r=["rkv14"], w=["rkv14"])

                P.section(3)
                for j in range(4):
                    jc = slice(j * 128, (j + 1) * 128)
                    pj = pers[j]
                    pjk = f"pers{j}"
                    rr = rkv[:, j, :]
                    kr = rkv[:, 4 + j, :]
                    vr = rkv[:, 8 + j, :]
                    A("pe", (lambda jc: lambda e: e.matmul(psum[0][:, 0:TT], lhsT=w2s[0:32, jc], rhs=lor[0:32, 0, :], start=True, stop=True))(jc),
                      r=["w2s", "rkv12"], w=[pk(0)])
                    A("act", (lambda pj, j: lambda e: e.activation(out=pj[:, LW, :], in_=psum[0][:, 0:TT], func=AF.Sigmoid, bias=pcol("w0", j)))(pj, j),
                      r=[pk(0), "prm"], w=[pjk + "LW"])
                    A("pool", (lambda pj: lambda e: e.tensor_scalar(out=pj[:, LW, :], in0=pj[:, LW, :], scalar1=NEG_HALF_E, scalar2=None, op0=ALU.mult))(pj),
                      r=[pjk + "LW"], w=[pjk + "LW"])
                    A("pe", (lambda jc: lambda e: e.matmul(psum[1][:, 0:TT], lhsT=a2s[0:32, jc], rhs=lor[0:32, 1, :], start=True, stop=True))(jc),
                      r=["a2s", "rkv13"], w=[pk(1)])
                    A("act", (lambda j: lambda e: e.activation(out=ptmp[:, AA, :], in_=psum[1][:, 0:TT], func=AF.Sigmoid, bias=pcol("a0", j)))(j),
                      r=[pk(1), "prm"], w=["pAA"])
                    A("pe", (lambda jc: lambda e: e.matmul(psum[0][:, 0:TT], lhsT=g2s[0:96, jc], rhs=lor[0:96, 2, :], start=True, stop=True))(jc),
                      r=["g2s", "rkv14"], w=[pk(0)])
                    A("act", (lambda pj: lambda e: e.activation(out=pj[:, GG, :], in_=psum[0][:, 0:TT], func=AF.Identity))(pj), r=[pk(0)], w=[pjk + "GG"])
                    A("dve", (lambda kr, j: lambda e: e.tensor_scalar(out=ptmp[:, KX, :], in0=kr, scalar1=pcol("k_k", j), scalar2=None, op0=ALU.mult))(kr, j),
                      r=[f"rkv{4 + j}", "prm"], w=["pKX"])
                    A("pool", lambda e: e.tensor_tensor(out=ptmp[:, RN, :], in0=ptmp[:, KX, :], in1=ptmp[:, KX, :], op=ALU.mult), r=["pKX"], w=["pRN"])
                    A("pe", lambda e: e.matmul(psum[1][:, 0:TT], lhsT=bones, rhs=ptmp[:, RN, :], start=True, stop=True), r=["cst", "pRN"], w=[pk(1)])
                    A("act", lambda e: e.activation(out=ptmp[:, RN, :], in_=psum[1][:, 0:TT], func=AF.Sqrt, bias=float(1e-24)), r=[pk(1)], w=["pRN"])
                    A("dve", lambda e: e.reciprocal(out=ptmp[:, RN, :], in_=ptmp[:, RN, :]), r=["pRN"], w=["pRN"])
                    A("dve", (lambda pj: lambda e: e.scalar_tensor_tensor(out=pj[:, ASN, :], in0=ptmp[:, KX, :], scalar=-1.0, in1=ptmp[:, RN, :],
                                                                          op0=ALU.mult, op1=ALU.mult))(pj), r=["pKX", "pRN"], w=[pjk + "ASN"])
                    A("dve", (lambda pj: lambda e: e.scalar_tensor_tensor(out=pj[:, BSN, :], in0=pj[:, ASN, :], scalar=-1.0, in1=ptmp[:, AA, :],
                                                                          op0=ALU.mult, op1=ALU.mult))(pj), r=[pjk + "ASN", "pAA"], w=[pjk + "BSN"])
                    A("dve", (lambda j: lambda e: e.tensor_scalar(out=ptmp[:, PRD, :], in0=ptmp[:, AA, :], scalar1=-1.0, scalar2=pcol("k_a", j),
                                                                  op0=ALU.add, op1=ALU.mult))(j), r=["pAA", "prm"], w=["pPRD"])
                    A("dve", (lambda pj, kr: lambda e: e.scalar_tensor_tensor(out=pj[:, KM, :], in0=ptmp[:, PRD, :], scalar=1.0, in1=kr,
                                                                              op0=ALU.add, op1=ALU.mult))(pj, kr), r=["pPRD", f"rkv{4 + j}"], w=[pjk + "KM"])
                    A("dve", (lambda pj, rr, j: lambda e: e.scalar_tensor_tensor(out=ptmp[:, PRD, :], in0=rr, scalar=pcol("r_k", j), in1=pj[:, KM, :],
                                                                                 op0=ALU.mult, op1=ALU.mult))(pj, rr, j), r=[f"rkv{j}", "prm", pjk + "KM"], w=["pPRD"])
                    A("pe", lambda e: e.matmul(psum[0][:, 0:TT], lhsT=bones, rhs=ptmp[:, PRD, :], start=True, stop=True), r=["cst", "pPRD"], w=[pk(0)])
                    A("dve", (lambda pj, vr: lambda e: e.tensor_tensor(out=pj[:, BON, :], in0=psum[0][:, 0:TT], in1=vr, op=ALU.mult))(pj, vr),
                      r=[pk(0), f"rkv{8 + j}"], w=[pjk + "BON"])

                P.section(4)
                def unit_stages(j, c):
                    col = slice(c * CH, (c + 1) * CH)
                    pj = pers[j]
                    pjk = f"pers{j}"
                    s = sct[j]
                    sk = f"sct{j}"
                    mt = mats[j]
                    mk = f"mats{j}"
                    tr = trs[j]
                    tk = f"trs{j}"
                    cn = chn[j]
                    ck = f"chn{j}"
                    H = Hst[j]
                    Hk = f"H{j}"
                    rr = rkv[:, j, col]
                    vr = rkv[:, 8 + j, col]
                    hp = [slice(0, 64), slice(64, 128)]
                    st = []

                    def s1():
                        A("dve", lambda e: e.tensor_tensor_scan(out=s[:, CI, :], data0=ones[:, 0:64], data1=pj[:, LW, col], initial=0.0, op0=ALU.mult, op1=ALU.add),
                          r=[pjk + "LW", "cst"], w=[sk + "ci"])
                        A("pool", lambda e: e.tensor_tensor(out=s[:, CE, :], in0=s[:, CI, :], in1=pj[:, LW, col], op=ALU.subtract),
                          r=[sk + "ci", pjk + "LW"], w=[sk + "ce"])
                    st.append(s1)

                    def s2():
                        A("act", lambda e: e.activation(out=s[:, EI, :], in_=s[:, CI, :], func=AF.Exp), r=[sk + "ci"], w=[sk + "ei"])
                        A("act", lambda e: e.activation(out=s[:, EE, :], in_=s[:, CE, :], func=AF.Exp), r=[sk + "ce"], w=[sk + "ee"])
                        A("act", lambda e: e.activation(out=s[:, EN, :], in_=s[:, CI, :], func=AF.Exp, scale=-1.0), r=[sk + "ci"], w=[sk + "en"])
                    st.append(s2)

                    def s3():
                        A("dve", lambda e: e.tensor_tensor(out=s[:, AT, :], in0=pj[:, ASN, col], in1=s[:, EE, :], op=ALU.mult), r=[pjk + "ASN", sk + "ee"], w=[sk + "at"])
                        A("pool", lambda e: e.tensor_tensor(out=s[:, RT, :], in0=rr, in1=s[:, EI, :], op=ALU.mult), r=[f"rkv{j}", sk + "ei"], w=[sk + "rt"])
                        A("dve", lambda e: e.tensor_tensor(out=s[:, BT, :], in0=pj[:, BSN, col], in1=s[:, EN, :], op=ALU.mult), r=[pjk + "BSN", sk + "en"], w=[sk + "bt"])
                        A("pool", lambda e: e.tensor_tensor(out=s[:, KT, :], in0=pj[:, KM, col], in1=s[:, EN, :], op=ALU.mult), r=[pjk + "KM", sk + "en"], w=[sk + "kt"])
                    st.append(s3)

                    def s4():
                        for p in hp:
                            A("pe", (lambda p: lambda e: e.matmul(psum[2][p, 0:128], lhsT=s[p, BT, :], rhs=s[p, AT:RT + 1, :].rearrange("p a b -> p (a b)"), start=True, stop=True))(p),
                              r=[sk + "bt", sk + "at", sk + "rt"], w=["ps2"])
                            A("pe", (lambda p: lambda e: e.matmul(psum[2][p, 128:256], lhsT=s[p, KT, :], rhs=s[p, AT:RT + 1, :].rearrange("p a b -> p (a b)"), start=True, stop=True))(p),
                              r=[sk + "kt", sk + "at", sk + "rt"], w=["ps2"])
                            A("pe", (lambda p: lambda e: e.matmul(psum[2][p, 256:320], lhsT=s[p, AT, :], rhs=s[p, BT, :], start=True, stop=True))(p),
                              r=[sk + "bt", sk + "at"], w=["ps2"])
                        A("dve", lambda e: e.tensor_tensor(out=mt[:], in0=psum[2][:, 0:320], in1=m5, op=ALU.mult), r=["ps2", "cst"], w=[mk])
                        for p in hp:
                            A("pe", (lambda p: lambda e: e.matmul(psum[3][p, 0:64], lhsT=rkv[p, 8 + j, col], rhs=istack[p, :], start=True, stop=True))(p),
                              r=[f"rkv{8 + j}", "cst"], w=["ps3"])
                            A("pe", (lambda p: lambda e: e.matmul(psum[3][p, 64:128], lhsT=s[p, BT, :], rhs=istack[p, :], start=True, stop=True))(p),
                              r=[sk + "bt", "cst"], w=["ps3"])
                            A("pe", (lambda p: lambda e: e.matmul(psum[3][p, 128:192], lhsT=s[p, KT, :], rhs=istack[p, :], start=True, stop=True))(p),
                              r=[sk + "kt", "cst"], w=["ps3"])
                        A("act", lambda e: e.activation(out=tr[:], in_=psum[3][:, 0:192], func=AF.Identity), r=["ps3"], w=[tk])
                        A("pool", lambda e: e.tensor_tensor(out=qq[j][0][:], in0=mt[:, 0:64], in1=istack, op=ALU.add), r=[mk, "cst"], w=[f"qq{j}_0"])
                    st.append(s4)

                    wzb = wzb_l[j]
                    wbk = f"wzb{j}"
                    bm3 = bones.rearrange("p (a b) -> p a b", a=2)

                    def s4b():
                        A("pool", lambda e: e.tensor_tensor(out=wzb[:, 0:128].rearrange("p (a b) -> p a b", a=2),
                                                            in0=mt[:, 0:64].rearrange("p (o t) -> p o t", o=1).to_broadcast([128, 2, 64]), in1=bm3, op=ALU.mult),
                          r=[mk, "cst"], w=[wbk])
                        A("pool", lambda e: e.tensor_tensor(out=wzb[:, 128:256].rearrange("p (a b) -> p a b", a=2),
                                                            in0=mt[:, 256:320].rearrange("p (o t) -> p o t", o=1).to_broadcast([128, 2, 64]), in1=bm3, op=ALU.mult),
                          r=[mk, "cst"], w=[wbk])
                    st.append(s4b)

                    for i in range(5):
                        def lv(i=i):
                            last = (i == 4)
                            qc = qq[j][i % 2]
                            qck = f"qq{j}_{i % 2}"
                            qn = qq[j][(i + 1) % 2]
                            qnk = f"qq{j}_{(i + 1) % 2}"
                            if not last:
                                A("pe", lambda e: e.matmul(psum[4][:, 0:128], lhsT=wzb[:, 128:256], rhs=wzb[:, 0:128], start=True, stop=True), r=[wbk], w=["ps4"])
                            A("pe", lambda e: e.matmul(psum[4][:, 128:256], lhsT=wzb[:, 0:128], rhs=wzb[:, 128:256], start=True, stop=True), r=[wbk], w=["ps4"])
                            if not last:
                                A("act", lambda e: e.activation(out=wzb[:, 0:256], in_=psum[4][:, 0:256], func=AF.Identity), r=["ps4"], w=[wbk])
                            else:
                                A("act", lambda e: e.activation(out=wzb[:, 128:256], in_=psum[4][:, 128:256], func=AF.Identity), r=["ps4"], w=[wbk])
                            A("pe", lambda e: e.matmul(psum[1][:, 128:192], lhsT=wzb[:, 128:256], rhs=qc[:, :], start=True, stop=True), r=[wbk, qck], w=["ps1"])
                            A("dve", lambda e: e.tensor_tensor(out=qn[:], in0=psum[1][:, 128:192], in1=qc[:], op=ALU.add), r=["ps1", qck], w=[qnk])
                        st.append(lv)

                    def s5():
                        cbank = [5, 6, 7, 0][j]
                        cps = psum[cbank] if cbank != 7 else psTt
                        cpk = f"ps{cbank}"
                        q5 = qq[j][1]
                        q5k = f"qq{j}_1"
                        A("dve", lambda e: e.tensor_scalar(out=cn[:, 3, :], in0=H[:], scalar1=s[:, EI, 63:64], scalar2=None, op0=ALU.mult),
                          r=[Hk, sk + "ei"], w=[ck + "hpc"])
                        for p in hp:
                            A("pe", (lambda p: lambda e: e.matmul(cps[p, 0:64], lhsT=s[p, AT, :], rhs=H[p, :], start=True, stop=False))(p), r=[sk + "at", Hk], w=[cpk])
                            A("pe", (lambda p: lambda e: e.matmul(cps[p, 0:64], lhsT=mt[p, 128:192], rhs=tr[p, 0:64], start=False, stop=True))(p), r=[mk, tk], w=[cpk])
                        A("act", lambda e: e.activation(out=cn[:, 0, :], in_=cps[:, 0:64], func=AF.Identity), r=[cpk], w=[ck + "x"])
                        for p in hp:
                            A("pe", (lambda p: lambda e: e.matmul(cps[p, 64:128], lhsT=q5[p, :], rhs=cn[p, 0, :], start=True, stop=True))(p), r=[q5k, ck + "x"], w=[cpk])
                        A("act", lambda e: e.activation(out=cn[:, 1, :], in_=cps[:, 64:128], func=AF.Identity), r=[cpk], w=[ck + "u"])
                        for p in hp:
                            A("pe", (lambda p: lambda e: e.matmul(cps[p, 192:256], lhsT=tr[p, 64:128], rhs=cn[p, 1, :], start=True, stop=False))(p), r=[tk, ck + "u"], w=[cpk])
                            A("pe", (lambda p: lambda e: e.matmul(cps[p, 192:256], lhsT=tr[p, 128:192], rhs=tr[p, 0:64], start=False, stop=True))(p), r=[tk], w=[cpk])
                        for p in hp:
                            A("pe", (lambda p: lambda e: e.matmul(cps[p, 128:192], lhsT=s[p, RT, :], rhs=H[p, :], start=True, stop=False))(p), r=[sk + "rt", Hk], w=[cpk])
                            A("pe", (lambda p: lambda e: e.matmul(cps[p, 128:192], lhsT=mt[p, 64:128], rhs=cn[p, 1, :], start=False, stop=False))(p), r=[mk, ck + "u"], w=[cpk])
                            A("pe", (lambda p: lambda e: e.matmul(cps[p, 128:192], lhsT=mt[p, 192:256], rhs=tr[p, 0:64], start=False, stop=True))(p), r=[mk, tk], w=[cpk])
                        A("dve", lambda e: e.scalar_tensor_tensor(out=H[:], in0=cps[:, 192:256], scalar=s[:, EI, 63:64], in1=cn[:, 3, :], op0=ALU.mult, op1=ALU.add),
                          r=[cpk, sk + "ei", ck + "hpc"], w=[Hk])
                        g = gst[j]
                        gk = f"gst{j}"
                        A("dve", lambda e: e.bn_stats(out=g[:, 0:6], in_=cps[:, 128:192]), r=[cpk], w=[gk])
                        A("dve", lambda e: e.bn_aggr(out=g[:, 6:8], in_=g[:, 0:6]), r=[gk], w=[gk])
                        A("act", lambda e: e.activation(out=g[:, 8:9], in_=g[:, 7:8], func=AF.Sqrt, bias=float(64e-5)), r=[gk], w=[gk])
                        A("dve", lambda e: e.reciprocal(out=g[:, 8:9], in_=g[:, 8:9]), r=[gk], w=[gk])
                        A("dve", lambda e: e.tensor_scalar(out=cn[:, 2, :], in0=cps[:, 128:192], scalar1=g[:, 6:7], scalar2=g[:, 8:9], op0=ALU.subtract, op1=ALU.mult),
                          r=[cpk, gk], w=[ck + "yn"])
                        for p in hp:
                            A("pe", (lambda p: lambda e: e.matmul(psum[3][p, 192:256], lhsT=cn[p, 2, :], rhs=istack[p, :], start=True, stop=True))(p), r=[ck + "yn", "cst"], w=["ps3"])
                        A("act", lambda e: e.activation(out=ybT[:, j, col], in_=psum[3][:, 192:256], func=AF.Identity, scale=pcol("gn_w", j), bias=pcol("gn_b", j)),
                          r=["ps3", "prm"], w=[f"ybT{j}"])
                    st.append(s5)
                    return st

                for c in range(NCH):
                    stl = [unit_stages(j, c) for j in range(4)]
                    for si in range(len(stl[0])):
                        for j in range(4):
                            stl[j][si]()
                P.section(5)
                for j in range(4):
                    pj = pers[j]
                    pjk = f"pers{j}"
                    A("pool", (lambda pj, j: lambda e: e.tensor_tensor(out=ybT[:, j, :], in0=ybT[:, j, :], in1=pj[:, BON, :], op=ALU.add))(pj, j),
                      r=[f"ybT{j}", pjk + "BON"], w=[f"ybT{j}"])
                    A("pool", (lambda pj, j: lambda e: e.tensor_tensor(out=ycat[:, 4 + j, :], in0=ybT[:, j, :], in1=pj[:, GG, :], op=ALU.mult))(pj, j),
                      r=[f"ybT{j}", pjk + "GG"], w=["ycat"])

                P.section(6)
                for sub in range(NSUB):
                    tc_ = slice(sub * 128, (sub + 1) * 128)
                    for kc in range(8):
                        A("pe", (lambda kc, tc_: lambda e: e.matmul(psum[0][:, 0:512], lhsT=hT[:, kc, tc_], rhs=win[:, kc, 512:1024], start=(kc == 0), stop=(kc == 7)))(kc, tc_),
                          r=["hT", "win"], w=[pk(0)])
                    A("act", lambda e: e.activation(out=vg[:], in_=psum[0][:, 0:512], func=AF.Gelu_apprx_tanh), r=[pk(0)], w=["vg"])
                    A("dve", lambda e: e.bn_stats(out=st6[:, 2, :], in_=vg[:]), r=["vg"], w=["st6b"])
                    A("dve", lambda e: e.bn_aggr(out=mv[:, 4:6], in_=st6[:, 2, :]), r=["st6b"], w=["mvb"])
                    A("act", lambda e: e.activation(out=mv[:, 6:7], in_=mv[:, 5:6], func=AF.Sqrt, bias=float(1e-5)), r=["mvb"], w=["mvb"])
                    A("dve", lambda e: e.reciprocal(out=mv[:, 6:7], in_=mv[:, 6:7]), r=["mvb"], w=["mvb"])
                    A("dve", lambda e: e.tensor_scalar(out=vn[:], in0=vg[:], scalar1=mv[:, 4:5], scalar2=mv[:, 6:7], op0=ALU.subtract, op1=ALU.mult), r=["vg", "mvb"], w=["vg"])
                    A("pool", lambda e: e.tensor_tensor(out=vn[:], in0=vn[:], in1=bcs[:, BC["lnw"]:BC["lnw"] + 512], op=ALU.mult), r=["vg", "bc"], w=["vg"])
                    A("pool", lambda e: e.tensor_tensor(out=vn[:], in0=vn[:], in1=bcs[:, BC["lnb"]:BC["lnb"] + 512], op=ALU.add), r=["vg", "bc"], w=["vg"])
                    for h in range(8):
                        A("pe", (lambda h: lambda e: e.matmul(psum[1][(h % 2) * 64:(h % 2) * 64 + 64, (h // 2) * 128:(h // 2 + 1) * 128], lhsT=vn[:, h * 64:(h + 1) * 64],
                                                              rhs=wc[:, h, :], start=True, stop=True))(h), r=["vg", "wc"], w=[pk(1)])
                    A("dve", lambda e: e.tensor_tensor(out=gtmp.rearrange("p a b -> p (a b)"), in0=psum[1][:, 0:512], in1=bcs[:, BC["bsT"]:BC["bsT"] + 512], op=ALU.add),
                      r=[pk(1), "bc"], w=["h2f"])
                    A("dve", (lambda tc_: lambda e: e.tensor_tensor(out=ycat[:, 0:4, tc_], in0=gtmp, in1=u_s[:, :, tc_], op=ALU.mult))(tc_), r=["h2f", "u_s"], w=["ycat"])

                P.section(7)
                for sub in range(NSUB):
                    tc_ = slice(sub * 128, (sub + 1) * 128)
                    ti = TI * NSUB + sub
                    tok0 = t0 + sub * 128
                    for hf in range(2):
                        for cc in range(8):
                            A("pe", (lambda cc, hf, tc_: lambda e: e.matmul(psum[6][:, 0:512], lhsT=ycat[:, cc, tc_], rhs=wout[:, cc, hf * 512:(hf + 1) * 512],
                                                                           start=(cc == 0), stop=(cc == 7)))(cc, hf, tc_), r=["ycat", "wout"], w=[pk(6)])
                        A("dve", (lambda hf: lambda e: e.tensor_tensor(out=x1t[:, hf * 512:(hf + 1) * 512], in0=psum[6][:, 0:512], in1=gate_bc[:, hf * 512:(hf + 1) * 512],
                                                                      op=ALU.mult))(hf), r=[pk(6), "gate_bc"], w=["x1t"])
                        A("pool", (lambda hf, xb, sub: lambda e: e.tensor_tensor(out=x1t[:, hf * 512:(hf + 1) * 512], in0=x1t[:, hf * 512:(hf + 1) * 512],
                                                                                in1=xb[:, sub, hf * 512:(hf + 1) * 512], op=ALU.add))(hf, xb, sub), r=["x1t", xk], w=["x1t"])
                    P.add("sp", (lambda tok0: lambda e: e.dma_start(out=x1_d[tok0:tok0 + 128, :], in_=x1t[:]))(tok0), r=["x1t"], w=["x1_d"], dma=True)
                    norm_stats(lambda hf: x1t[:, hf * 512:(hf + 1) * 512], "x1t", 1e-6)
                    A("act", lambda e: e.activation(out=xn2[:], in_=x1t[:], func=AF.Identity, scale=mv[:, 3:4]), r=["x1t", "mv"], w=["x1t"])
                    for kc in range(8):
                        pb = kc // 4
                        A("pe", (lambda kc, pb: lambda e: e.matmul(psum[pb][:, (kc % 4) * 128:(kc % 4 + 1) * 128], lhsT=xn2[:, kc * 128:(kc + 1) * 128], rhs=ident, start=True, stop=True))(kc, pb),
                          r=["x1t", "cst"], w=[pk(pb)])
                    for kc in range(8):
                        pb = kc // 4
                        A("act", (lambda kc, pb: lambda e: e.activation(out=h2f[:, kc, :], in_=psum[pb][:, (kc % 4) * 128:(kc % 4 + 1) * 128], func=AF.Identity,
                                                                        scale=sc[:, 8 + kc:9 + kc], bias=mod[:, 24 + kc:25 + kc]))(kc, pb), r=[pk(pb), "sc", "mod"], w=["h2f"])
                    A("pool", lambda e: e.tensor_copy(out=xnb[:], in_=xn2[:]), r=["x1t"], w=["xnb"])
                    P.add("sp", (lambda tok0: lambda e: e.dma_start(out=xn2_d[tok0:tok0 + 128, :], in_=xnb[:]))(tok0),
                          r=["xnb"], w=["xn2_d"], dma=True)
                    for kc in range(8):
                        A("pe", (lambda kc: lambda e: e.matmul(psum[6][:, 0:36], lhsT=h2f[:, kc, :], rhs=wrs[:, kc, :], start=(kc == 0), stop=(kc == 7)))(kc),
                          r=["h2f", "wrs"], w=[pk(6)])
                    A("dve", (lambda ti: lambda e: e.tensor_tensor(out=logits[:, ti, :], in0=psum[6][:, 0:36], in1=bcs[:, BC["rb"]:BC["rb"] + 36], op=ALU.add))(ti),
                      r=[pk(6), "bc"], w=["logits"])

            P.muted = False
            P.barrier()
            ct.close()
            if stage >= 2:
                rt = SB(ca, "rt", [128, 32, 48])
                sel = SB(ca, "sel", [128, 32, 8])
                sel2 = SB(ca, "sel2", [128, 32, 8])
                ohg = SB(ca, "ohg", [128, 32, 4])
                tmp48 = SB(ca, "tmp48", [128, 32, 4, 8])
                lg = logits[:, :, 0:4]
                le = logits[:, :, 4:36].rearrange("p t (g e) -> p t g e", g=4)
                MG, SG, M1, M2, P1, W1, W2 = range(7)
                r1 = lambda i: rt[:, :, i:i + 1]
                R = lambda fn, r, w: A("dve", fn, r=r, w=w)
                R(lambda e: e.tensor_reduce(out=rt[:, :, MG], in_=lg, axis=AX.X, op=ALU.max), ["logits"], ["rt"])
                R(lambda e: e.tensor_tensor(out=ohg[:], in0=lg, in1=r1(MG).to_broadcast([128, 32, 4]), op=ALU.is_equal), ["logits", "rt"], ["ohg"])
                R(lambda e: e.tensor_tensor(out=rt[:, :, 8:12], in0=lg, in1=r1(MG).to_broadcast([128, 32, 4]), op=ALU.subtract), ["logits", "rt"], ["rt"])
                A("act", lambda e: e.activation(out=rt[:, :, 8:12], in_=rt[:, :, 8:12], func=AF.Exp), r=["rt"], w=["rt"])
                R(lambda e: e.tensor_reduce(out=rt[:, :, SG], in_=rt[:, :, 8:12], axis=AX.X, op=ALU.add), ["rt"], ["rt"])
                R(lambda e: e.reciprocal(out=rt[:, :, SG], in_=rt[:, :, SG]), ["rt"], ["rt"])
                R(lambda e: e.tensor_tensor(out=tmp48[:], in0=le, in1=ohg[:].rearrange("p t (g o) -> p t g o", o=1).to_broadcast([128, 32, 4, 8]), op=ALU.mult),
                  ["logits", "ohg"], ["tmp48"])
                R(lambda e: e.tensor_reduce(out=sel[:], in_=tmp48[:].rearrange("p t g e -> p t e g"), axis=AX.X, op=ALU.add), ["tmp48"], ["sel"])
                R(lambda e: e.tensor_reduce(out=rt[:, :, M1], in_=sel[:], axis=AX.X, op=ALU.max), ["sel"], ["rt"])
                R(lambda e: e.tensor_tensor(out=sel2[:], in0=sel[:], in1=r1(M1).to_broadcast([128, 32, 8]), op=ALU.is_equal), ["sel", "rt"], ["sel2"])
                R(lambda e: e.scalar_tensor_tensor(out=tmp48[:, :, 0, :], in0=sel2[:], scalar=-1e30, in1=sel[:], op0=ALU.mult, op1=ALU.add), ["sel", "sel2"], ["tmp48"])
                R(lambda e: e.tensor_reduce(out=rt[:, :, M2], in_=tmp48[:, :, 0, :], axis=AX.X, op=ALU.max), ["tmp48"], ["rt"])
                R(lambda e: e.tensor_tensor(out=tmp48[:, :, 1, :], in0=tmp48[:, :, 0, :], in1=r1(M2).to_broadcast([128, 32, 8]), op=ALU.is_equal), ["tmp48", "rt"], ["tmp48"])
                R(lambda e: e.tensor_tensor(out=rt[:, :, P1], in0=rt[:, :, M2], in1=rt[:, :, M1], op=ALU.subtract), ["rt"], ["rt"])
                A("act", lambda e: e.activation(out=rt[:, :, P1], in_=rt[:, :, P1], func=AF.Exp), r=["rt"], w=["rt"])
                R(lambda e: e.tensor_scalar(out=rt[:, :, P1], in0=rt[:, :, P1], scalar1=1.0, scalar2=None, op0=ALU.add), ["rt"], ["rt"])
                R(lambda e: e.reciprocal(out=rt[:, :, P1], in_=rt[:, :, P1]), ["rt"], ["rt"])
                R(lambda e: e.tensor_tensor(out=rt[:, :, W1], in0=rt[:, :, P1], in1=rt[:, :, SG], op=ALU.mult), ["rt"], ["rt"])
                R(lambda e: e.tensor_tensor(out=rt[:, :, W2], in0=rt[:, :, SG], in1=rt[:, :, W1], op=ALU.subtract), ["rt"], ["rt"])
                R(lambda e: e.tensor_tensor(out=sel[:], in0=sel2[:], in1=r1(W1).to_broadcast([128, 32, 8]), op=ALU.mult), ["sel2", "rt"], ["sel"])
                R(lambda e: e.tensor_tensor(out=sel2[:], in0=tmp48[:, :, 1, :], in1=r1(W2).to_broadcast([128, 32, 8]), op=ALU.mult), ["tmp48", "rt"], ["sel2"])
                R(lambda e: e.tensor_tensor(out=sel[:], in0=sel[:], in1=sel2[:], op=ALU.add), ["sel", "sel2"], ["sel"])
                for g in range(4):
                    R((lambda g: lambda e: e.tensor_tensor(out=wt[:, :, g * 8:(g + 1) * 8], in0=sel[:], in1=ohg[:, :, g:g + 1].to_broadcast([128, 32, 8]), op=ALU.mult))(g),
                      ["sel", "ohg"], ["wt"])
            if stage >= 2:
                Mf = SB(ca, "Mf", [128, 1024])
                Mb = SB(ca, "Mb", [128, 1024], BF16)
                Lsb = SB(ca, "Lsb", [128, 128], BF16)
                onesb = SB(ca, "onesb", [128, 128], BF16)
                PRE = SB(ca, "PRE", [128, 1024])
                CNTs = SB(ca, "CNTs", [128, 1024])
                CA_ = SB(ca, "CA", [128, 1024])
                CB_ = SB(ca, "CB", [128, 1024])
                sm = SB(ca, "sm", [128, 8, 32])
                cmpb = SB(ca, "cmpb", [128, 96, 32])
                bev = SB(ca, "bev", [128, 6, 96])
                pabf = SB(ca, "pabf", [128, 2, 32])
                v3 = lambda t: t[:].rearrange("p (a b) -> p a b", a=32)
                wtf = wt[:].rearrange("p t e -> p (t e)")
                R(lambda e: e.tensor_single_scalar(out=Mf[:], in_=wtf, scalar=0.0, op=ALU.is_gt), ["wt"], ["Mf"])
                A("pool", lambda e: e.tensor_copy(out=Mb[:], in_=Mf[:]), r=["Mf"], w=["Mb"])
                A("pool", lambda e: e.tensor_tensor(out=Lsb[:], in0=tril, in1=ident, op=ALU.subtract), r=["cst"], w=["Lsb"])
                A("pool", lambda e: e.tensor_copy(out=onesb[:], in_=ones), r=["cst"], w=["onesb"])
                for h in range(2):
                    A("pe", (lambda h: lambda e: e.matmul(psum[h][:, 0:512], lhsT=Lsb[:], rhs=Mb[:, h * 512:(h + 1) * 512], start=True, stop=True))(h),
                      r=["Lsb", "Mb"], w=[pk(h)])
                    A("pe", (lambda h: lambda e: e.matmul(psum[2 + h][:, 0:512], lhsT=onesb[:], rhs=Mb[:, h * 512:(h + 1) * 512], start=True, stop=True))(h),
                      r=["onesb", "Mb"], w=[pk(2 + h)])
                    A("act", (lambda h: lambda e: e.activation(out=PRE[:, h * 512:(h + 1) * 512], in_=psum[h][:, 0:512], func=AF.Identity))(h), r=[pk(h)], w=["PRE"])
                    A("act", (lambda h: lambda e: e.activation(out=CNTs[:, h * 512:(h + 1) * 512], in_=psum[2 + h][:, 0:512], func=AF.Identity))(h), r=[pk(2 + h)], w=["CNTs"])
                src, srck, dst, dstk = CNTs, "CNTs", CA_, "CA"
                for dd in (1, 2, 4, 8, 16):
                    w_ = dd * 32
                    R((lambda src, dst, w_: lambda e: e.tensor_copy(out=dst[:, 0:w_], in_=src[:, 0:w_]))(src, dst, w_), [srck], [dstk])
                    R((lambda src, dst, w_: lambda e: e.tensor_tensor(out=dst[:, w_:1024], in0=src[:, w_:1024], in1=src[:, 0:1024 - w_], op=ALU.add))(src, dst, w_), [srck], [dstk])
                    src, srck = dst, dstk
                    dst, dstk = (CB_, "CB") if dst is CA_ else (CA_, "CA")
                R(lambda e: e.tensor_tensor(out=CB_[:], in0=CA_[:], in1=CNTs[:], op=ALU.subtract), ["CA", "CNTs"], ["CB"])
                R(lambda e: e.tensor_copy(out=sm[:, 0, :], in_=CA_[:, 31 * 32:32 * 32]), ["CA"], ["sm"])
                R(lambda e: e.tensor_tensor(out=cmpb[:, 0:32, :], in0=sm[:, 0, :].rearrange("p (e o) -> p e o", o=1).to_broadcast([128, 32, 32]),
                                            in1=cst[:, CC["thr"]:CC["thr"] + 32].rearrange("p (o k) -> p o k", o=1).to_broadcast([128, 32, 32]), op=ALU.is_gt),
                  ["sm", "cst"], ["cmpb"])
                R(lambda e: e.tensor_reduce(out=sm[:, 1, :], in_=cmpb[:, 0:32, :], axis=AX.X, op=ALU.add), ["cmpb"], ["sm"])
                R(lambda e: e.tensor_tensor_scan(out=sm[:, 2, :], data0=ones[:, 0:32], data1=sm[:, 1, :], initial=0.0, op0=ALU.mult, op1=ALU.add), ["sm", "cst"], ["sm"])
                R(lambda e: e.tensor_tensor(out=sm[:, 3, :], in0=sm[:, 2, :], in1=sm[:, 1, :], op=ALU.subtract), ["sm"], ["sm"])
                R(lambda e: e.tensor_single_scalar(out=sm[:, 4, :], in_=sm[:, 3, :], scalar=128.0, op=ALU.mult), ["sm"], ["sm"])
                R(lambda e: e.tensor_tensor(out=v3(CB_), in0=v3(CB_), in1=sm[:, 4, :].rearrange("p (o e) -> p o e", o=1).to_broadcast([128, 32, 32]), op=ALU.add),
                  ["CB", "sm"], ["CB"])
                R(lambda e: e.tensor_tensor(out=PRE[:], in0=PRE[:], in1=CB_[:], op=ALU.add), ["PRE", "CB"], ["PRE"])
                R(lambda e: e.tensor_tensor(out=CA_[:], in0=PRE[:], in1=Mf[:], op=ALU.mult), ["PRE", "Mf", "sm"], ["CA"])
                R(lambda e: e.tensor_scalar(out=CB_[:], in0=Mf[:], scalar1=-1e9, scalar2=1e9, op0=ALU.mult, op1=ALU.add), ["Mf", "PRE"], ["CB"])
                R(lambda e: e.tensor_tensor(out=CB_[:], in0=CB_[:], in1=CA_[:], op=ALU.add), ["CB", "CA"], ["CB"])
                R(lambda e: e.tensor_reduce(out=pabf[:, 0, :], in_=v3(CB_), axis=AX.X, op=ALU.min), ["CB"], ["pabf"])
                R(lambda e: e.tensor_reduce(out=pabf[:, 1, :], in_=v3(CA_), axis=AX.X, op=ALU.max), ["CA"], ["pabf"])
                R(lambda e: e.tensor_tensor(out=v3(CB_), in0=v3(CB_), in1=pabf[:, 0, :].rearrange("p (t o) -> p t o", o=1).to_broadcast([128, 32, 32]), op=ALU.is_equal),
                  ["CB", "pabf"], ["CB"])
                R(lambda e: e.tensor_tensor(out=CB_[:], in0=CB_[:], in1=wtf, op=ALU.mult), ["CB", "wt"], ["CB"])
                R(lambda e: e.tensor_reduce(out=WAB[:, 0, :], in_=v3(CB_), axis=AX.X, op=ALU.add), ["CB"], ["WAB"])
                R(lambda e: e.tensor_tensor(out=WAB[:, 1, :], in0=rt[:, :, SG], in1=WAB[:, 0, :], op=ALU.subtract), ["rt", "WAB"], ["WAB"])
                R(lambda e: e.tensor_copy(out=PABi[:], in_=pabf[:]), ["pabf"], ["PABi"])
                R(lambda e: e.tensor_tensor(out=cmpb[:], in0=sm[:, 2, :].rearrange("p (o e) -> p o e", o=1).to_broadcast([128, 96, 32]),
                                            in1=cst[:, CC["iotab"]:CC["iotab"] + 96].rearrange("p (b o) -> p b o", o=1).to_broadcast([128, 96, 32]), op=ALU.is_le),
                  ["sm", "cst", "cmpb"], ["cmpb"])
                R(lambda e: e.tensor_reduce(out=bev[:, 0, :], in_=cmpb[:], axis=AX.X, op=ALU.add), ["cmpb"], ["bev"])
                R(lambda e: e.memset(bev[:, 1, 0:1], 1.0), [], ["bev"])
                R(lambda e: e.tensor_tensor(out=bev[:, 1, 1:96], in0=bev[:, 0, 1:96], in1=bev[:, 0, 0:95], op=ALU.not_equal), ["bev"], ["bev"])
                R(lambda e: e.tensor_scalar(out=bev[:, 2, :], in0=bev[:, 1, :], scalar1=-1e6, scalar2=1e6, op0=ALU.mult, op1=ALU.add), ["bev"], ["bev"])
                R(lambda e: e.scalar_tensor_tensor(out=bev[:, 2, :], in0=bev[:, 0, :], scalar=512.0, in1=bev[:, 2, :], op0=ALU.mult, op1=ALU.add), ["bev"], ["bev"])
                R(lambda e: e.tensor_scalar(out=bev[:, 2, :], in0=bev[:, 2, :], scalar1=cst[:, CC["pidx"]:CC["pidx"] + 1], scalar2=None, op0=ALU.add), ["bev", "cst"], ["bev"])
                for q in range(1, 4):
                    R((lambda q: lambda e: e.tensor_single_scalar(out=bev[:, 2 + q, :], in_=bev[:, 2, :], scalar=128.0 * q, op=ALU.add))(q), ["bev"], ["bev"])
                R(lambda e: e.tensor_copy(out=IDXW[:], in_=bev[:, 2:6, :]), ["bev"], ["IDXW"])
            P.barrier()

        if stage >= 3:
            with ExitStack() as cb:
                IOA = bass.IndirectOffsetOnAxis
                NROW = NE * 512
                _bc = {}

                def bc_reg(e):
                    if "r" not in _bc:
                        _bc["r"] = e.to_reg(NROW - 1)
                    return _bc["r"]
                Wg = SB(cb, "Wg", [128, 8, EH])
                Wu = SB(cb, "Wu", [128, 8, EH])
                Wd = SB(cb, "Wd", [128, 4, D])
                idb2 = SB(cb, "idb2", [128, 128], BF16)
                xload = [SB(cb, f"xl{i}", [128, D], BF16) for i in range(2)]
                xsb = [SB(cb, f"xsb{i}", [128, D], BF16) for i in range(2)]
                XgT = SB(cb, "XgT", [128, 8, 128])
                sg = SB(cb, "sg", [128, 512])
                actb = SB(cb, "actb", [128, 512])
                actT = SB(cb, "actT", [128, 4, 128])
                yblk = [SB(cb, f"yblk{i}", [128, D]) for i in range(2)]
                fgt = SB(cb, "fgt", [128, D])
                fgs = SB(cb, "fgs", [128, D])
                g2bc = SB(cb, "g2bc", [128, D])
                yA = SB(cb, "yA", [128, D])
                yB = SB(cb, "yB", [128, D])
                fst = SB(cb, "fst", [128, 2, 6])
                fmv = SB(cb, "fmv", [128, 4])
                P.add("sp", lambda e: e.dma_start(out=fgs[:], in_=fg_d[:, :]), w=["fgs"], dma=True)
                P.add("dve", lambda e: e.tensor_copy(out=idb2[:], in_=ident), r=["cst"], w=["idb2"])
                make_gate(g2bc, "g2bc", 40)
                zt = SB(cb, "zt", [128, D], BF16)
                P.add("pool", lambda e: e.memset(zt[:], 0.0), w=["zt"])
                xz_keys = []
                for b in range(NBLK):
                    P.add("sp", (lambda b: lambda e: e.dma_start(out=xs_d[b * 128:(b + 1) * 128, :], in_=zt[:]))(b), r=["zt"], w=[f"xz{b}"], dma=True)
                    xz_keys.append(f"xz{b}")
                sc_keys = []
                for i in range(32):
                    xl = xload[i % 2]
                    xlk = f"xl{i % 2}"
                    P.add("sp", (lambda xl, i: lambda e: e.dma_start(out=xl[:], in_=xn2_d[i * 128:(i + 1) * 128, :]))(xl, i), r=["xn2_d"], w=[xlk], dma=True)
                    for k in range(2):
                        key = f"xsc{i}_{k}"
                        P.add("pool", (lambda xl, i, k: lambda e: e.indirect_dma_start(out=xs_d[:, :], out_offset=IOA(ap=PABi[:, k, i:i + 1], axis=0),
                                                                                       in_=xl[:], in_offset=None))(xl, i, k),
                              r=[xlk, "PABi"] + xz_keys, w=[key], dma=True)
                        sc_keys.append(key)
                ys_keys = []
                for b in range(min(NBLK, nblk)):
                    for c2 in range(4):
                        for (wsb, wk, wsrc, nch) in ((Wg, "Wg", wg_d, 2), (Wu, "Wu", wu_d, 2), (Wd, "Wd", wd_d, 1)):
                            P.add("pool", (lambda wsb, wsrc, nch, c2, b: lambda e: e.indirect_dma_start(
                                out=wsb[:, c2 * nch:(c2 + 1) * nch, :].rearrange("p a b -> p (a b)"), out_offset=None, in_=wsrc[:, :],
                                in_offset=IOA(ap=IDXW[:, c2, b:b + 1], axis=0), bounds_check=bc_reg(e), oob_is_err=False))(wsb, wsrc, nch, c2, b),
                                r=["IDXW", wk], w=[wk], dma=True)
                    xb_ = xsb[b % 2]
                    xbk = f"xsb{b % 2}"
                    P.add("sp", (lambda xb_, b: lambda e: e.dma_start(out=xb_[:], in_=xs_d[b * 128:(b + 1) * 128, :]))(xb_, b), r=sc_keys, w=[xbk], dma=True)
                    for kc in range(8):
                        P.add("pe", (lambda xb_, kc: lambda e: e.matmul(psum[kc // 4][:, (kc % 4) * 128:(kc % 4 + 1) * 128], lhsT=xb_[:, kc * 128:(kc + 1) * 128],
                                                                       rhs=idb2[:], start=True, stop=True))(xb_, kc), r=[xbk, "idb2"], w=[pk(kc // 4)])
                    for kc in range(8):
                        P.add("act", (lambda kc: lambda e: e.activation(out=XgT[:, kc, :], in_=psum[kc // 4][:, (kc % 4) * 128:(kc % 4 + 1) * 128], func=AF.Identity,
                                                                        scale=sc[:, 8 + kc:9 + kc], bias=mod[:, 24 + kc:25 + kc]))(kc), r=[pk(kc // 4), "sc", "mod"], w=["XgT"])
                    for kc in range(8):
                        P.add("pe", (lambda kc: lambda e: e.matmul(psum[2][:, 0:512], lhsT=XgT[:, kc, :], rhs=Wg[:, kc, :], start=(kc == 0), stop=(kc == 7)))(kc),
                              r=["XgT", "Wg"], w=[pk(2)])
                    for kc in range(8):
                        P.add("pe", (lambda kc: lambda e: e.matmul(psum[3][:, 0:512], lhsT=XgT[:, kc, :], rhs=Wu[:, kc, :], start=(kc == 0), stop=(kc == 7)))(kc),
                              r=["XgT", "Wu"], w=[pk(3)])
                    P.add("act", lambda e: e.activation(out=sg[:], in_=psum[2][:, 0:512], func=AF.Silu), r=[pk(2)], w=["sg"])
                    P.add("dve", lambda e: e.tensor_tensor(out=actb[:], in0=psum[3][:, 0:512], in1=sg[:], op=ALU.mult), r=[pk(3), "sg"], w=["actb"])
                    for hc in range(4):
                        P.add("pe", (lambda hc: lambda e: e.matmul(psum[4][:, hc * 128:(hc + 1) * 128], lhsT=actb[:, hc * 128:(hc + 1) * 128], rhs=ident,
                                                                   start=True, stop=True))(hc), r=["actb", "cst"], w=[pk(4)])
                    P.add("act", lambda e: e.activation(out=actT[:].rearrange("p a b -> p (a b)"), in_=psum[4][:, 0:512], func=AF.Identity), r=[pk(4)], w=["actT"])
                    yb_ = yblk[b % 2]
                    ybk = f"yblk{b % 2}"
                    for hf in range(2):
                        for hc in range(4):
                            P.add("pe", (lambda hc, hf: lambda e: e.matmul(psum[5 + hf][:, 0:512], lhsT=actT[:, hc, :], rhs=Wd[:, hc, hf * 512:(hf + 1) * 512],
                                                                           start=(hc == 0), stop=(hc == 3)))(hc, hf), r=["actT", "Wd"], w=[pk(5 + hf)])
                        if hf == 0:
                            P.add("act", (lambda yb_: lambda e: e.activation(out=yb_[:, 0:512], in_=psum[5][:, 0:512], func=AF.Identity))(yb_), r=[pk(5)], w=[ybk])
                        else:
                            P.add("dve", (lambda yb_: lambda e: e.tensor_copy(out=yb_[:, 512:1024], in_=psum[6][:, 0:512]))(yb_), r=[pk(6)], w=[ybk])
                    key = f"ys{b}"
                    P.add("sp", (lambda yb_, b: lambda e: e.dma_start(out=ys_d[b * 128:(b + 1) * 128, :], in_=yb_[:]))(yb_, b), r=[ybk], w=[key], dma=True)
                    ys_keys.append(key)
                for i in range(32):
                    tok0 = i * 128
                    P.add("pool", (lambda i: lambda e: e.indirect_dma_start(out=yA[:], out_offset=None, in_=ys_d[:, :], in_offset=IOA(ap=PABi[:, 0, i:i + 1], axis=0)))(i),
                          r=ys_keys + ["PABi", "yA"], w=["yA"], dma=True)
                    P.add("pool", (lambda i: lambda e: e.indirect_dma_start(out=yB[:], out_offset=None, in_=ys_d[:, :], in_offset=IOA(ap=PABi[:, 1, i:i + 1], axis=0)))(i),
                          r=ys_keys + ["PABi", "yB"], w=["yB"], dma=True)
                    P.add("sp", (lambda tok0: lambda e: e.dma_start(out=fgt[:], in_=x1_d[tok0:tok0 + 128, :]))(tok0), r=["x1_d"], w=["fgt"], dma=True)
                    P.add("dve", (lambda i: lambda e: e.tensor_scalar(out=yA[:], in0=yA[:], scalar1=WAB[:, 0, i:i + 1], scalar2=None, op0=ALU.mult))(i), r=["yA", "WAB"], w=["yA"])
                    P.add("dve", (lambda i: lambda e: e.scalar_tensor_tensor(out=yA[:], in0=yB[:], scalar=WAB[:, 1, i:i + 1], in1=yA[:], op0=ALU.mult, op1=ALU.add))(i),
                          r=["yA", "yB", "WAB"], w=["yA"])
                    P.add("pool", lambda e: e.tensor_tensor(out=yA[:], in0=yA[:], in1=g2bc[:], op=ALU.mult), r=["yA", "g2bc"], w=["yA"])
                    P.add("dve", lambda e: e.tensor_tensor(out=fgt[:], in0=fgt[:], in1=yA[:], op=ALU.add), r=["yA", "fgt"], w=["fgt"])
                    for hf in range(2):
                        P.add("dve", (lambda hf: lambda e: e.bn_stats(out=fst[:, hf, :], in_=fgt[:, hf * 512:(hf + 1) * 512]))(hf), r=["fgt"], w=["fst"])
                    P.add("dve", lambda e: e.bn_aggr(out=fmv[:, 0:2], in_=fst[:, 0:2, :].rearrange("p a b -> p (a b)")), r=["fst"], w=["fmv"])
                    P.add("dve", lambda e: e.scalar_tensor_tensor(out=fmv[:, 2:3], in0=fmv[:, 0:1], scalar=fmv[:, 0:1], in1=fmv[:, 1:2], op0=ALU.mult, op1=ALU.add),
                          r=["fmv"], w=["fmv"])
                    P.add("act", lambda e: e.activation(out=fmv[:, 3:4], in_=fmv[:, 2:3], func=AF.Sqrt, bias=float(1e-6)), r=["fmv"], w=["fmv"])
                    P.add("dve", lambda e: e.reciprocal(out=fmv[:, 3:4], in_=fmv[:, 3:4]), r=["fmv"], w=["fmv"])
                    P.add("dve", lambda e: e.scalar_tensor_tensor(out=fgt[:], in0=fgt[:], scalar=fmv[:, 3:4], in1=fgs[:], op0=ALU.mult, op1=ALU.mult),
                          r=["fgt", "fmv", "fgs"], w=["fgt"])
                    P.add("sp", (lambda tok0: lambda e: e.dma_start(out=out_d[tok0:tok0 + 128, :], in_=fgt[:]))(tok0), r=["fgt"], w=["out_d"], dma=True)
        else:
            with ExitStack() as cb:
                fgt = SB(cb, "fgt", [128, D])
                for ti in range(32):
                    tok0 = ti * 128
                    P.add("sp", (lambda tok0: lambda e: e.dma_start(out=fgt[:], in_=x1_d[tok0:tok0 + 128, :]))(tok0), r=["x1_d"], w=["fgt"], dma=True)
                    P.add("sp", (lambda tok0: lambda e: e.dma_start(out=out_d[tok0:tok0 + 128, :], in_=fgt[:]))(tok0), r=["fgt"], w=["out_d"], dma=True)
        P.emit()
    return nc


_NC_CACHE = {}


def _layouts(inp):
    f = np.float32
    g = lambda k: np.asarray(inp[k], dtype=f)
    col = lambda v, n: np.ascontiguousarray(v.reshape(n, 128).T)
    mu = g("rwkv_mu")[0]
    def pad128(v):
        o = np.zeros((128, 1), f)
        o[:v.shape[0], 0] = v
        return o
    shared = [None, col(g("ada_b")[0], 48), col(g("norm1_g")[0], 8), col(g("norm2_g")[0], 8), col(mu[0:1536], 12),
              pad128(mu[1536:1568]), pad128(mu[1568:1600]), pad128(mu[1600:1696]), col(g("rwkv_w0")[0], 4), col(g("rwkv_a0")[0], 4),
              col(g("rwkv_k_k")[0], 4), col(g("rwkv_k_a")[0], 4), col(g("rwkv_r_k")[0].reshape(512), 4), col(g("rwkv_gn_w")[0], 4),
              col(g("rwkv_gn_b")[0], 4)]
    tt = np.arange(64)
    su = (tt[:, None] < tt[None, :]).astype(f)
    iu = (tt[:, None] <= tt[None, :]).astype(f)
    sl = (tt[None, :] < tt[:, None]).astype(f)
    m5 = np.tile(np.concatenate([su, iu, su, iu, sl], axis=1), (2, 1))
    ident = np.eye(128, dtype=f)
    istack = np.tile(np.eye(64, dtype=f), (2, 1))
    ss = np.arange(128)
    tril = (ss[:, None] <= ss[None, :]).astype(f)
    bones = (ss[:, None] // 64 == ss[None, :] // 64).astype(f)
    ones = np.ones((128, 128), f)
    thr = np.broadcast_to((np.arange(32) * 128).astype(f)[None, :], (128, 32))
    iotab = np.broadcast_to(np.arange(96).astype(f)[None, :], (128, 96))
    pidx = np.arange(128).astype(f)[:, None]
    cst = np.ascontiguousarray(np.concatenate([m5, ident, istack, tril, bones, ones, thr, iotab, pidx], axis=1))
    assert cst.shape[1] == NCST
    rep = lambda v: np.broadcast_to(v[None, :], (128, v.shape[0]))
    bs = g("gmlp_bs")[0]
    bsT = np.zeros((128, 4, 128), f)
    for j in range(4):
        for hh in range(2):
            bsT[hh * 64:(hh + 1) * 64, j, :] = bs[2 * j + hh][None, :]
    bc = np.ascontiguousarray(np.concatenate([rep(g("gmlp_ln_w")[0]), rep(g("gmlp_ln_b")[0]),
                                              rep(np.concatenate([g("router_group_b")[0], g("router_expert_b")[0]])),
                                              bsT.reshape(128, 512)], axis=1))
    assert bc.shape[1] == NBC
    common = dict(cst=cst, bc=bc, fg=np.ascontiguousarray(rep(g("final_norm_g"))), ada_w=np.ascontiguousarray(g("ada_w")[0]), w_in=np.ascontiguousarray(g("w_in")[0]),
                  w_out=np.ascontiguousarray(g("w_out")[0]), w2=np.ascontiguousarray(g("rwkv_w2")[0]),
                  a2=np.ascontiguousarray(g("rwkv_a2")[0]), g2=np.ascontiguousarray(g("rwkv_g2")[0]),
                  wsT=np.ascontiguousarray(g("gmlp_ws")[0].transpose(2, 0, 1)),
                  wr=np.ascontiguousarray(np.concatenate([g("router_group_w")[0], g("router_expert_w")[0]], axis=1)),
                  wg=np.ascontiguousarray(g("moe_w_gate")[0].reshape(NE, 4, 2, 128, EH).transpose(0, 1, 3, 2, 4).reshape(NE * 512, 1024)),
                  wu=np.ascontiguousarray(g("moe_w_up")[0].reshape(NE, 4, 2, 128, EH).transpose(0, 1, 3, 2, 4).reshape(NE * 512, 1024)),
                  wd=np.ascontiguousarray(g("moe_w_down")[0].reshape(NE * 512, 1024)))
    x = g("x")
    c = g("c")
    maps = []
    for b in range(x.shape[0]):
        cols = [col(c[b], 8)] + shared[1:]
        prm = np.ascontiguousarray(np.concatenate(cols, axis=1))
        assert prm.shape[1] == NPRM
        m = dict(common)
        m["x"] = np.ascontiguousarray(x[b])
        m["prm"] = prm
        maps.append(m)
    return maps


def kernel(**inputs):
    maps = _layouts(inputs)
    if "nc" not in _NC_CACHE:
        _NC_CACHE["nc"] = build_nc()
    nc = _NC_CACHE["nc"]
    res = run_bass_kernel_spmd(nc, maps, core_ids=list(range(len(maps))))
    return np.stack([r["out"] for r in res.results], axis=0).astype(np.float32)
```

```python
import numpy as np
import os
from contextlib import ExitStack
import concourse.bass as bass
import concourse.mybir as mybir
from concourse.bass_utils import run_bass_kernel_spmd

F32 = mybir.dt.float32
BF16 = mybir.dt.bfloat16
I32 = mybir.dt.int32
AF = mybir.ActivationFunctionType
ALU = mybir.AluOpType
AX = mybir.AxisListType

D = 1024
S = 4096
TT = 256
NT = S // TT
NSUB = TT // 128
CH = 64
NCH = TT // CH
INW = 2720
NE = 32
EH = 512
NEG_HALF_E = -0.6065306597126334

PC = {}
_o = 0
for _n, _w in [("cT", 8), ("ada_b", 48), ("n1g", 8), ("n2g", 8), ("mu_rkv", 12), ("mu_xw", 1),
               ("mu_xa", 1), ("mu_xg", 1), ("w0", 4), ("a0", 4), ("k_k", 4), ("k_a", 4), ("r_k", 4),
               ("gn_w", 4), ("gn_b", 4)]:
    PC[_n] = _o
    _o += _w
NPRM = _o
CC = {}
_o = 0
for _n, _w in [("m5", 320), ("ident", 128), ("istack", 64), ("tril", 128), ("bones", 128), ("ones", 128), ("thr", 32), ("iotab", 96), ("pidx", 1)]:
    CC[_n] = _o
    _o += _w
NCST = _o
BC = {}
_o = 0
for _n, _w in [("lnw", 512), ("lnb", 512), ("rb", 36), ("bsT", 512)]:
    BC[_n] = _o
    _o += _w
NBC = _o


ATTACH_WAIT = True
NO_SELF_SYNC = ("pe", "act")


class Prog:
    def __init__(self, nc, ctx):
        self.nc = nc
        self.ops = []
        self.last_w = {}
        self.readers = {}
        self.engs = ["pe", "act", "dve", "pool", "sp"]
        self.count = {e: 0 for e in self.engs}
        self.sem = {e: ctx.enter_context(nc.semaphore("s_" + e)) for e in self.engs}
        self.NDS = 8
        self.dsem = {q: [ctx.enter_context(nc.semaphore(f"d_{q}{i}")) for i in range(self.NDS)]
                     for q in ("sp", "pool")}
        self.dcount = {"sp": 0, "pool": 0}
        self.last_op = {e: None for e in self.engs}
        self.pending = {e: set() for e in self.engs}
        self.recent_dma = {"sp": [], "pool": []}

    def section(self, k):
        self.muted = k > self.cut

    def add(self, eng, fn, r=(), w=(), dma=False):
        if getattr(self, 'muted', False):
            return None
        idx = len(self.ops)
        deps = set(self.pending[eng])
        self.pending[eng] = set()
        for k in r:
            if k in self.last_w:
                deps.add(self.last_w[k])
        for k in w:
            if k in self.last_w:
                deps.add(self.last_w[k])
            deps.update(self.readers.get(k, ()))
        for k in r:
            self.readers.setdefault(k, []).append(idx)
        for k in w:
            self.last_w[k] = idx
            self.readers[k] = []
        if dma:
            q = eng
            kq = self.dcount[q]
            self.dcount[q] += 1
            sem = self.dsem[q][kq % self.NDS]
            val = 16 * (kq // self.NDS + 1)
            prev = (sem, val - 16) if val > 16 else None
            self.recent_dma[q].append(idx)
            self.recent_dma[q] = self.recent_dma[q][-self.NDS:]
        else:
            self.count[eng] += 1
            sem = self.sem[eng]
            val = self.count[eng]
            prev = None
        self.ops.append(dict(eng=eng, fn=fn, deps=deps, dma=dma, sem=sem, val=val, prev=prev))
        self.last_op[eng] = idx
        return idx

    def barrier(self):
        allops = set()
        for e in self.engs:
            if self.last_op[e] is not None:
                allops.add(self.last_op[e])
        for q in ("sp", "pool"):
            allops.update(self.recent_dma[q])
        dmaops = set()
        for q in ("sp", "pool"):
            dmaops.update(self.recent_dma[q])
        for e in self.engs:
            self.pending[e] |= (allops - dmaops) if e == "pe" else allops

    def emit(self):
        nc = self.nc
        per = {e: [] for e in self.engs}
        for i, op in enumerate(self.ops):
            per[op["eng"]].append(i)
        ops = self.ops

        def run(name, e):
            waited = {}
            for i in per[name]:
                op = ops[i]
                need = []
                for j in op["deps"]:
                    d = ops[j]
                    if d["eng"] == name and not d["dma"] and name in NO_SELF_SYNC:
                        continue
                    need.append((d["sem"], d["val"]))
                if op["prev"] is not None:
                    need.append(op["prev"])
                need.sort(key=lambda t: -t[1])
                todo = []
                for sem, val in need:
                    key = id(sem)
                    if waited.get(key, 0) >= val:
                        continue
                    waited[key] = val
                    todo.append((sem, val))
                attach = todo.pop() if (todo and ATTACH_WAIT) else None
                for sem, val in todo:
                    e.wait_ge(sem, val)
                inst = op["fn"](e)
                if attach is not None:
                    inst._wait_ge(attach[0], attach[1])
                inst.then_inc(op["sem"], 16 if op["dma"] else 1)
            if name in ("sp", "pool"):
                kq = self.dcount[name]
                for s_i in range(min(kq, self.NDS)):
                    n_on = (kq - s_i + self.NDS - 1) // self.NDS
                    e.wait_ge(self.dsem[name][s_i], 16 * n_on)

        with nc.Block() as block:
            @block.sync
            def _(e):
                run("sp", e)

            @block.scalar
            def _(e):
                run("act", e)

            @block.vector
            def _(e):
                run("dve", e)

            @block.tensor
            def _(e):
                run("pe", e)

            @block.gpsimd
            def _(e):
                run("pool", e)


def build_nc(stage=99, ntiles=NT, cut=99, nblk=96):
    nc = bass.Bass("TRN2", target_bir_lowering=False)
    dt = lambda name, shape, dty, kind: nc.dram_tensor(name, shape, dty, kind=kind).ap()
    x_d = dt("x", [S, D], F32, "ExternalInput")
    prm_d = dt("prm", [128, NPRM], F32, "ExternalInput")
    cst_d = dt("cst", [128, NCST], F32, "ExternalInput")
    bc_d = dt("bc", [128, NBC], F32, "ExternalInput")
    adaw_d = dt("ada_w", [D, 6 * D], F32, "ExternalInput")
    win_d = dt("w_in", [D, INW], F32, "ExternalInput")
    wout_d = dt("w_out", [D, D], F32, "ExternalInput")
    w2_d = dt("w2", [32, 512], F32, "ExternalInput")
    a2_d = dt("a2", [32, 512], F32, "ExternalInput")
    g2_d = dt("g2", [96, 512], F32, "ExternalInput")
    wsT_d = dt("wsT", [128, 8, 128], F32, "ExternalInput")
    wr_d = dt("wr", [D, 36], F32, "ExternalInput")
    fg_d = dt("fg", [128, D], F32, "ExternalInput")
    wg_d = dt("wg", [NE * 128, 4096], F32, "ExternalInput")
    wu_d = dt("wu", [NE * 128, 4096], F32, "ExternalInput")
    wd_d = dt("wd", [NE * 128, 4096], F32, "ExternalInput")
    NBLK = 96
    xn2_d = dt("xn2_d", [S, D], BF16, "Internal")
    xs_d = dt("xs_d", [NBLK * 128, D], BF16, "Internal")
    ys_d = dt("ys_d", [NBLK * 128, D], F32, "Internal")
    out_d = dt("out", [S, D], F32, "ExternalOutput")
    x1_d = dt("x1_d", [S, D], F32, "Internal")

    with ExitStack() as top:
        P = Prog(nc, top)
        P.cut = cut
        psum = [top.enter_context(nc.psum_tensor(f"ps{i}", [128, 512], F32)) for i in range(7)]
        psTt = top.enter_context(nc.psum_tensor("psT", [128, 512], F32))
        psT = [psTt[:, 0:256], psum[6][:, 0:256]]
        psTk = ["ps7", "ps6"]
        fst = None
        fmv = None
        pk = lambda i: f"ps{i}"

        def SB(ctx, name, shape, dty=F32):
            return ctx.enter_context(nc.sbuf_tensor(name, shape, dty))

        prm = SB(top, "prm_s", [128, NPRM])
        cst = SB(top, "cst_s", [128, NCST])
        bcs = SB(top, "bc_s", [128, NBC])
        mod = SB(top, "mod", [128, 48])
        wt = SB(top, "wt", [128, 32, 32])
        PABi = SB(top, "PABi", [128, 2, 32], I32)
        WAB = SB(top, "WAB", [128, 2, 32])
        IDXW = SB(top, "IDXW", [128, 4, 96], I32)
        P.add("sp", lambda e: e.dma_start(out=prm[:], in_=prm_d[:, :]), w=["prm"], dma=True)
        P.add("sp", lambda e: e.dma_start(out=cst[:], in_=cst_d[:, :]), w=["cst"], dma=True)
        P.add("sp", lambda e: e.dma_start(out=bcs[:], in_=bc_d[:, :]), w=["bc"], dma=True)

        def pcol(name, j=0, n=1):
            return prm[:, PC[name] + j:PC[name] + j + n]

        ident = cst[:, CC["ident"]:CC["ident"] + 128]
        istack = cst[:, CC["istack"]:CC["istack"] + 64]
        tril = cst[:, CC["tril"]:CC["tril"] + 128]
        bones = cst[:, CC["bones"]:CC["bones"] + 128]
        ones = cst[:, CC["ones"]:CC["ones"] + 128]
        m5 = cst[:, CC["m5"]:CC["m5"] + 320]

        with ExitStack() as c0:
            stg = [SB(c0, f"ada_stg{i}", [128, 6 * D]) for i in range(2)]
            for kc in range(8):
                sb = stg[kc % 2]
                P.add("sp", (lambda sb, kc: lambda e: e.dma_start(out=sb[:], in_=adaw_d[kc * 128:(kc + 1) * 128, :]))(sb, kc),
                      w=[f"ada_stg{kc % 2}"], dma=True)
                for oc in range(48):
                    P.add("pe", (lambda sb, kc, oc: lambda e: e.matmul(psum[0][:, oc:oc + 1], lhsT=sb[:, oc * 128:(oc + 1) * 128],
                                                                   rhs=prm[:, PC["cT"] + kc:PC["cT"] + kc + 1], start=True, stop=True))(sb, kc, oc),
                          r=[f"ada_stg{kc % 2}", "prm"], w=[pk(0)])
                if kc == 0:
                    P.add("dve", lambda e: e.tensor_tensor(out=mod[:], in0=psum[0][:, 0:48], in1=prm[:, PC["ada_b"]:PC["ada_b"] + 48], op=ALU.add),
                          r=[pk(0), "prm"], w=["mod"])
                else:
                    P.add("dve", lambda e: e.tensor_tensor(out=mod[:], in0=psum[0][:, 0:48], in1=mod[:], op=ALU.add),
                          r=[pk(0), "mod"], w=["mod"])
            P.barrier()
        sc = SB(top, "sc", [128, 32])
        P.add("dve", lambda e: e.scalar_tensor_tensor(out=sc[:, 0:8], in0=mod[:, 8:16], scalar=1.0, in1=prm[:, PC["n1g"]:PC["n1g"] + 8],
                                                      op0=ALU.add, op1=ALU.mult), r=["mod", "prm"], w=["sc"])
        P.add("dve", lambda e: e.scalar_tensor_tensor(out=sc[:, 8:16], in0=mod[:, 32:40], scalar=1.0, in1=prm[:, PC["n2g"]:PC["n2g"] + 8],
                                                      op0=ALU.add, op1=ALU.mult), r=["mod", "prm"], w=["sc"])
        gate_bc = SB(top, "gate_bc", [128, D])
        gl = SB(top, "gl", [128, 128])

        def make_gate(dst, dkey, gcol):
            for c in range(8):
                P.add("dve", (lambda c: lambda e: e.tensor_scalar(out=gl[:], in0=ones, scalar1=mod[:, gcol + c:gcol + c + 1], scalar2=None,
                                                                  op0=ALU.mult))(c), r=["mod", "cst"], w=["gl"])
                P.add("pe", (lambda c: lambda e: e.matmul(psum[1][:, (c % 4) * 128:(c % 4 + 1) * 128], lhsT=gl[:], rhs=ident, start=True, stop=True))(c),
                      r=["gl", "cst"], w=[pk(1)])
                P.add("act", (lambda c: lambda e: e.activation(out=dst[:, c * 128:(c + 1) * 128], in_=psum[1][:, (c % 4) * 128:(c % 4 + 1) * 128],
                                                               func=AF.Identity))(c), r=[pk(1)], w=[dkey])
        make_gate(gate_bc, "gate_bc", 16)

        with ExitStack() as ca:
            win = SB(ca, "win", [128, 8, INW], BF16)
            wout = SB(ca, "wout", [128, 8, D], BF16)
            identb = SB(ca, "identb", [128, 128], BF16)
            w2s = SB(ca, "w2s", [32, 512])
            a2s = SB(ca, "a2s", [32, 512])
            g2s = SB(ca, "g2s", [96, 512])
            wrs = SB(ca, "wrs", [128, 8, 36])
            wc = SB(ca, "wc", [128, 8, 128])
            logits = SB(ca, "logits", [128, 32, 36])
            Hst = [SB(ca, f"H{j}", [128, 64]) for j in range(4)]
            carry = SB(ca, "carry", [128, 16])
            P.add("pool", lambda e: e.tensor_copy(out=identb[:], in_=ident), r=["cst"], w=["identb"])
            P.add("sp", lambda e: e.dma_start(out=w2s[:], in_=w2_d[:, :]), w=["w2s"], dma=True)
            P.add("sp", lambda e: e.dma_start(out=a2s[:], in_=a2_d[:, :]), w=["a2s"], dma=True)
            P.add("sp", lambda e: e.dma_start(out=g2s[:], in_=g2_d[:, :]), w=["g2s"], dma=True)
            P.add("sp", lambda e: e.dma_start(out=wrs[:], in_=wr_d.rearrange("(c p) n -> p c n", p=128)), w=["wrs"], dma=True)
            P.add("sp", lambda e: e.dma_start(out=wc[:], in_=wsT_d[:, :, :]), w=["wc"], dma=True)
            for h in range(8):
                P.add("pool", (lambda h: lambda e: e.tensor_tensor(out=wc[:, h, :], in0=wc[:, h, :], in1=tril, op=ALU.mult))(h),
                      r=["wc", "cst"], w=["wc"])
            P.add("pool", lambda e: e.memset(carry[:], 0.0), w=["carry"])
            for _i in range(int(os.environ.get("KPAD", "0"))):
                P.add("pool", lambda e: e.memset(gl[:, 0:1], 0.0), w=["gl_dummy"])
            for j in range(4):
                P.add("pool", (lambda j: lambda e: e.memset(Hst[j][:], 0.0))(j), w=[f"H{j}"])
            with ExitStack() as cw:
                wstg = [SB(cw, f"wstg{i}", [128, INW]) for i in range(2)]
                for kc in range(8):
                    sb = wstg[kc % 2]
                    P.add("sp", (lambda sb, kc: lambda e: e.dma_start(out=sb[:], in_=win_d[kc * 128:(kc + 1) * 128, :]))(sb, kc),
                          w=[f"wstg{kc % 2}"], dma=True)
                    P.add("pool", (lambda sb, kc: lambda e: e.tensor_copy(out=win[:, kc, :], in_=sb[:]))(sb, kc),
                          r=[f"wstg{kc % 2}"], w=["win"])
                for kc in range(8):
                    sb = wstg[kc % 2]
                    P.add("sp", (lambda sb, kc: lambda e: e.dma_start(out=sb[:, 0:D], in_=wout_d[kc * 128:(kc + 1) * 128, :]))(sb, kc),
                          w=[f"wstg{kc % 2}"], dma=True)
                    P.add("pool", (lambda sb, kc: lambda e: e.tensor_copy(out=wout[:, kc, :], in_=sb[:, 0:D]))(sb, kc),
                          r=[f"wstg{kc % 2}"], w=["wout"])
                P.barrier()

            ct = ExitStack()
            xt = [SB(ct, "xt0", [128, NSUB, D])] * 2
            xn = SB(ct, "xn", [128, NSUB, D], BF16)
            hT = SB(ct, "hT", [128, 8, TT], BF16)
            st6 = SB(ct, "st6", [128, 4, 6])
            mv = SB(ct, "mv", [128, 8])
            u_s = SB(ct, "u_s", [128, 4, TT], BF16)
            vg = SB(ct, "vg", [128, 512])
            vn = vg
            zraw = [SB(ct, f"zraw{i}", [128, TT + 1]) for i in range(2)]
            rkv = SB(ct, "rkv", [128, 12, TT])
            lor = SB(ct, "lor", [128, 3, TT])
            LW, ASN, BSN, KM, BON, GG = range(6)
            pers = [SB(ct, f"pers{j}", [128, 6, TT]) for j in range(4)]
            AA, KX, RN, PRD = range(4)
            ptmp = SB(ct, "ptmp", [128, 4, TT])
            ybT = SB(ct, "ybT", [128, 4, TT])
            ycat = SB(ct, "ycat", [128, 8, TT], BF16)
            x1t = SB(ct, "x1t", [128, D])
            xn2 = x1t
            h2f = SB(ct, "h2f", [128, 8, 128])
            gtmp = h2f[:, 0:4, :]
            xnb = SB(ct, "xnb", [128, D], BF16)
            CI, CE, EI, EE, EN, AT, RT, BT, KT = range(9)
            sct = [SB(ct, f"sct{j}", [128, 9, 64]) for j in range(4)]
            mats = [SB(ct, f"mats{j}", [128, 320]) for j in range(4)]
            trs = [SB(ct, f"trs{j}", [128, 192]) for j in range(4)]
            wzb_l = [SB(ct, f"wzb{j}", [128, 256]) for j in range(4)]
            qq = [[SB(ct, f"qq{j}_{i}", [128, 64]) for i in range(2)] for j in range(4)]
            chn = [SB(ct, f"chn{j}", [128, 4, 64]) for j in range(4)]
            gst = [SB(ct, f"gst{j}", [128, 12]) for j in range(4)]

            def A(eng, fn, r=(), w=()):
                P.add(eng, fn, r=r, w=w)

            def norm_stats(src, srck, eps):
                for hf in range(2):
                    A("dve", (lambda hf: lambda e: e.bn_stats(out=st6[:, hf, :], in_=src(hf)))(hf), r=[srck], w=["st6"])
                A("dve", lambda e: e.bn_aggr(out=mv[:, 0:2], in_=st6[:, 0:2, :].rearrange("p a b -> p (a b)")), r=["st6"], w=["mv"])
                A("dve", lambda e: e.scalar_tensor_tensor(out=mv[:, 2:3], in0=mv[:, 0:1], scalar=mv[:, 0:1], in1=mv[:, 1:2], op0=ALU.mult, op1=ALU.add),
                  r=["mv"], w=["mv"])
                A("act", lambda e: e.activation(out=mv[:, 3:4], in_=mv[:, 2:3], func=AF.Sqrt, bias=float(eps)), r=["mv"], w=["mv"])
                A("dve", lambda e: e.reciprocal(out=mv[:, 3:4], in_=mv[:, 3:4]), r=["mv"], w=["mv"])

            for TI in range(min(NT, ntiles) if stage >= 1 else 0):
                t0 = TI * TT
                xb = xt[TI % 2]
                xk = "xt0"
                P.add("sp", (lambda xb, t0: lambda e: e.dma_start(out=xb[:], in_=x_d[t0:t0 + TT, :].rearrange("(s p) d -> p s d", p=128)))(xb, t0),
                      w=[xk], dma=True)
                P.section(1)
                for sub in range(NSUB):
                    norm_stats((lambda xb, sub: lambda hf: xb[:, sub, hf * 512:(hf + 1) * 512])(xb, sub), xk, 1e-6)
                    A("act", (lambda xb, sub: lambda e: e.activation(out=xn[:, sub, :], in_=xb[:, sub, :], func=AF.Identity, scale=mv[:, 3:4]))(xb, sub),
                      r=[xk, "mv"], w=["xn"])
                P.section(1.5)
                for kc in range(8):
                    pb = kc % 2
                    for sub in range(NSUB):
                        A("pe", (lambda kc, sub, pb: lambda e: e.matmul(psT[pb][:, sub * 128:(sub + 1) * 128], lhsT=xn[:, sub, kc * 128:(kc + 1) * 128],
                                                                       rhs=identb[:], start=True, stop=True))(kc, sub, pb), r=["xn", "identb"], w=[psTk[pb]])
                    if os.environ.get("DVE_EVAC"):
                      A("dve", (lambda kc, pb: lambda e: e.tensor_scalar(out=hT[:, kc, :], in0=psT[pb][:, 0:TT], scalar1=sc[:, kc:kc + 1], scalar2=mod[:, kc:kc + 1],
                                                                         op0=ALU.mult, op1=ALU.add))(kc, pb), r=[psTk[pb], "sc", "mod"], w=["hT"])
                    elif not os.environ.get("SKIP_EVAC"):
                      A("act", (lambda kc, pb: lambda e: e.activation(out=hT[:, kc, :], in_=psT[pb][:, 0:TT], func=AF.Identity,
                                                                    **({} if os.environ.get("NO_SB") else dict(scale=sc[:, kc:kc + 1], bias=mod[:, kc:kc + 1]))))(kc, pb),
                      r=[psTk[pb], "sc", "mod"], w=["hT"])

                P.section(2)
                pcnt = [0]

                def proj(c0, M):
                    pb = pcnt[0] % 2
                    pcnt[0] += 1
                    for kc in range(8):
                        A("pe", (lambda kc, pb: lambda e: e.matmul(psum[pb][0:M, 0:TT], lhsT=win[:, kc, c0:c0 + M], rhs=hT[:, kc, :],
                                                                  start=(kc == 0), stop=(kc == 7)))(kc, pb), r=["win", "hT"], w=[pk(pb)])
                    return pb

                for j in range(4):
                    pb = proj(j * 128, 128)
                    A("act", (lambda j, pb: lambda e: e.activation(out=u_s[:, j, :], in_=psum[pb][:, 0:TT], func=AF.Gelu_apprx_tanh))(j, pb),
                      r=[pk(pb)], w=["u_s"])
                specs = [(1024 + q * 128, 128, PC["mu_rkv"] + q) for q in range(12)] + \
                        [(2560, 32, PC["mu_xw"]), (2592, 32, PC["mu_xa"]), (2624, 96, PC["mu_xg"])]
                for q, (c0, M, mucol) in enumerate(specs):
                    pb = proj(c0, M)
                    zb = zraw[q % 2]
                    zk = f"zraw{q % 2}"
                    dst = rkv[0:M, q, :] if q < 12 else lor[0:M, q - 12, :]
                    dk = f"rkv{q}"
                    A("pool", (lambda zb, q, M: lambda e: e.tensor_copy(out=zb[0:M, 0:1], in_=carry[0:M, q:q + 1]))(zb, q, M), r=["carry"], w=[zk])
                    A("act", (lambda zb, pb, M: lambda e: e.activation(out=zb[0:M, 1:TT + 1], in_=psum[pb][0:M, 0:TT], func=AF.Identity))(zb, pb, M),
                      r=[pk(pb)], w=[zk])
                    A("pool", (lambda zb, q, M: lambda e: e.tensor_copy(out=carry[0:M, q:q + 1], in_=zb[0:M, TT:TT + 1]))(zb, q, M), r=[zk], w=["carry"])
                    A("dve", (lambda zb, dst, M: lambda e: e.tensor_tensor(out=dst, in0=zb[0:M, 0:TT], in1=zb[0:M, 1:TT + 1], op=ALU.subtract))(zb, dst, M),
                      r=[zk], w=[dk])
                    A("dve", (lambda zb, dst, M, mucol: lambda e: e.scalar_tensor_tensor(out=dst, in0=dst, scalar=prm[0:M, mucol:mucol + 1], in1=zb[0:M, 1:TT + 1],
                                                                                      op0=ALU.mult, op1=ALU.add))(zb, dst, M, mucol), r=[zk, dk, "prm"], w=[dk])
                A("act", lambda e: e.activation(out=lor[0:32, 0, :], in_=lor[0:32, 0, :], func=AF.Tanh), r=["rkv12"], w=["rkv12"])
                A("act", lambda e: e.activation(out=lor[0:96, 2, :], in_=lor[0:96, 2, :], func=AF.Sigmoid), r=["rkv14"], w=["rkv14"])

                P.section(3)
                for j in range(4):
                    jc = slice(j * 128, (j + 1) * 128)
                    pj = pers[j]
                    pjk = f"pers{j}"
                    rr = rkv[:, j, :]
                    kr = rkv[:, 4 + j, :]
                    vr = rkv[:, 8 + j, :]
                    A("pe", (lambda jc: lambda e: e.matmul(psum[0][:, 0:TT], lhsT=w2s[0:32, jc], rhs=lor[0:32, 0, :], start=True, stop=True))(jc),
                      r=["w2s", "rkv12"], w=[pk(0)])
                    A("act", (lambda pj, j: lambda e: e.activation(out=pj[:, LW, :], in_=psum[0][:, 0:TT], func=AF.Sigmoid, bias=pcol("w0", j)))(pj, j),
                      r=[pk(0), "prm"], w=[pjk + "LW"])
                    A("pool", (lambda pj: lambda e: e.tensor_scalar(out=pj[:, LW, :], in0=pj[:, LW, :], scalar1=NEG_HALF_E, scalar2=None, op0=ALU.mult))(pj),
                      r=[pjk + "LW"], w=[pjk + "LW"])
                    A("pe", (lambda jc: lambda e: e.matmul(psum[1][:, 0:TT], lhsT=a2s[0:32, jc], rhs=lor[0:32, 1, :], start=True, stop=True))(jc),
                      r=["a2s", "rkv13"], w=[pk(1)])
                    A("act", (lambda j: lambda e: e.activation(out=ptmp[:, AA, :], in_=psum[1][:, 0:TT], func=AF.Sigmoid, bias=pcol("a0", j)))(j),
                      r=[pk(1), "prm"], w=["pAA"])
                    A("pe", (lambda jc: lambda e: e.matmul(psum[0][:, 0:TT], lhsT=g2s[0:96, jc], rhs=lor[0:96, 2, :], start=True, stop=True))(jc),
                      r=["g2s", "rkv14"], w=[pk(0)])
                    A("act", (lambda pj: lambda e: e.activation(out=pj[:, GG, :], in_=psum[0][:, 0:TT], func=AF.Identity))(pj), r=[pk(0)], w=[pjk + "GG"])
                    A("dve", (lambda kr, j: lambda e: e.tensor_scalar(out=ptmp[:, KX, :], in0=kr, scalar1=pcol("k_k", j), scalar2=None, op0=ALU.mult))(kr, j),
                      r=[f"rkv{4 + j}", "prm"], w=["pKX"])
                    A("pool", lambda e: e.tensor_tensor(out=ptmp[:, RN, :], in0=ptmp[:, KX, :], in1=ptmp[:, KX, :], op=ALU.mult), r=["pKX"], w=["pRN"])
                    A("pe", lambda e: e.matmul(psum[1][:, 0:TT], lhsT=bones, rhs=ptmp[:, RN, :], start=True, stop=True), r=["cst", "pRN"], w=[pk(1)])
                    A("act", lambda e: e.activation(out=ptmp[:, RN, :], in_=psum[1][:, 0:TT], func=AF.Sqrt, bias=float(1e-24)), r=[pk(1)], w=["pRN"])
                    A("dve", lambda e: e.reciprocal(out=ptmp[:, RN, :], in_=ptmp[:, RN, :]), r=["pRN"], w=["pRN"])
                    A("dve", (lambda pj: lambda e: e.scalar_tensor_tensor(out=pj[:, ASN, :], in0=ptmp[:, KX, :], scalar=-1.0, in1=ptmp[:, RN, :],
                                                                          op0=ALU.mult, op1=ALU.mult))(pj), r=["pKX", "pRN"], w=[pjk + "ASN"])
                    A("dve", (lambda pj: lambda e: e.scalar_tensor_tensor(out=pj[:, BSN, :], in0=pj[:, ASN, :], scalar=-1.0, in1=ptmp[:, AA, :],
                                                                          op0=ALU.mult, op1=ALU.mult))(pj), r=[pjk + "ASN", "pAA"], w=[pjk + "BSN"])
                    A("dve", (lambda j: lambda e: e.tensor_scalar(out=ptmp[:, PRD, :], in0=ptmp[:, AA, :], scalar1=-1.0, scalar2=pcol("k_a", j),
                                                                  op0=ALU.add, op1=ALU.mult))(j), r=["pAA", "prm"], w=["pPRD"])
                    A("dve", (lambda pj, kr: lambda e: e.scalar_tensor_tensor(out=pj[:, KM, :], in0=ptmp[:, PRD, :], scalar=1.0, in1=kr,
                                                                              op0=ALU.add, op1=ALU.mult))(pj, kr), r=["pPRD", f"rkv{4 + j}"], w=[pjk + "KM"])
                    A("dve", (lambda pj, rr, j: lambda e: e.scalar_tensor_tensor(out=ptmp[:, PRD, :], in0=rr, scalar=pcol("r_k", j), in1=pj[:, KM, :],
                                                                                 op0=ALU.mult, op1=ALU.mult))(pj, rr, j), r=[f"rkv{j}", "prm", pjk + "KM"], w=["pPRD"])
                    A("pe", lambda e: e.matmul(psum[0][:, 0:TT], lhsT=bones, rhs=ptmp[:, PRD, :], start=True, stop=True), r=["cst", "pPRD"], w=[pk(0)])
                    A("dve", (lambda pj, vr: lambda e: e.tensor_tensor(out=pj[:, BON, :], in0=psum[0][:, 0:TT], in1=vr, op=ALU.mult))(pj, vr),
                      r=[pk(0), f"rkv{8 + j}"], w=[pjk + "BON"])

                P.section(4)
                def unit_stages(j, c):
                    col = slice(c * CH, (c + 1) * CH)
                    pj = pers[j]
                    pjk = f"pers{j}"
                    s = sct[j]
                    sk = f"sct{j}"
                    mt = mats[j]
                    mk = f"mats{j}"
                    tr = trs[j]
                    tk = f"trs{j}"
                    cn = chn[j]
                    ck = f"chn{j}"
                    H = Hst[j]
                    Hk = f"H{j}"
                    rr = rkv[:, j, col]
                    vr = rkv[:, 8 + j, col]
                    hp = [slice(0, 64), slice(64, 128)]
                    st = []

                    def s1():
                        A("dve", lambda e: e.tensor_tensor_scan(out=s[:, CI, :], data0=ones[:, 0:64], data1=pj[:, LW, col], initial=0.0, op0=ALU.mult, op1=ALU.add),
                          r=[pjk + "LW", "cst"], w=[sk + "ci"])
                        A("pool", lambda e: e.tensor_tensor(out=s[:, CE, :], in0=s[:, CI, :], in1=pj[:, LW, col], op=ALU.subtract),
                          r=[sk + "ci", pjk + "LW"], w=[sk + "ce"])
                    st.append(s1)

                    def s2():
                        A("act", lambda e: e.activation(out=s[:, EI, :], in_=s[:, CI, :], func=AF.Exp), r=[sk + "ci"], w=[sk + "ei"])
                        A("act", lambda e: e.activation(out=s[:, EE, :], in_=s[:, CE, :], func=AF.Exp), r=[sk + "ce"], w=[sk + "ee"])
                        A("act", lambda e: e.activation(out=s[:, EN, :], in_=s[:, CI, :], func=AF.Exp, scale=-1.0), r=[sk + "ci"], w=[sk + "en"])
                    st.append(s2)

                    def s3():
                        A("dve", lambda e: e.tensor_tensor(out=s[:, AT, :], in0=pj[:, ASN, col], in1=s[:, EE, :], op=ALU.mult), r=[pjk + "ASN", sk + "ee"], w=[sk + "at"])
                        A("pool", lambda e: e.tensor_tensor(out=s[:, RT, :], in0=rr, in1=s[:, EI, :], op=ALU.mult), r=[f"rkv{j}", sk + "ei"], w=[sk + "rt"])
                        A("dve", lambda e: e.tensor_tensor(out=s[:, BT, :], in0=pj[:, BSN, col], in1=s[:, EN, :], op=ALU.mult), r=[pjk + "BSN", sk + "en"], w=[sk + "bt"])
                        A("pool", lambda e: e.tensor_tensor(out=s[:, KT, :], in0=pj[:, KM, col], in1=s[:, EN, :], op=ALU.mult), r=[pjk + "KM", sk + "en"], w=[sk + "kt"])
                    st.append(s3)

                    def s4():
                        for p in hp:
                            A("pe", (lambda p: lambda e: e.matmul(psum[2][p, 0:128], lhsT=s[p, BT, :], rhs=s[p, AT:RT + 1, :].rearrange("p a b -> p (a b)"), start=True, stop=True))(p),
                              r=[sk + "bt", sk + "at", sk + "rt"], w=["ps2"])
                            A("pe", (lambda p: lambda e: e.matmul(psum[2][p, 128:256], lhsT=s[p, KT, :], rhs=s[p, AT:RT + 1, :].rearrange("p a b -> p (a b)"), start=True, stop=True))(p),
                              r=[sk + "kt", sk + "at", sk + "rt"], w=["ps2"])
                            A("pe", (lambda p: lambda e: e.matmul(psum[2][p, 256:320], lhsT=s[p, AT, :], rhs=s[p, BT, :], start=True, stop=True))(p),
                              r=[sk + "bt", sk + "at"], w=["ps2"])
                        A("dve", lambda e: e.tensor_tensor(out=mt[:], in0=psum[2][:, 0:320], in1=m5, op=ALU.mult), r=["ps2", "cst"], w=[mk])
                        for p in hp:
                            A("pe", (lambda p: lambda e: e.matmul(psum[3][p, 0:64], lhsT=rkv[p, 8 + j, col], rhs=istack[p, :], start=True, stop=True))(p),
                              r=[f"rkv{8 + j}", "cst"], w=["ps3"])
                            A("pe", (lambda p: lambda e: e.matmul(psum[3][p, 64:128], lhsT=s[p, BT, :], rhs=istack[p, :], start=True, stop=True))(p),
                              r=[sk + "bt", "cst"], w=["ps3"])
                            A("pe", (lambda p: lambda e: e.matmul(psum[3][p, 128:192], lhsT=s[p, KT, :], rhs=istack[p, :], start=True, stop=True))(p),
                              r=[sk + "kt", "cst"], w=["ps3"])
                        A("act", lambda e: e.activation(out=tr[:], in_=psum[3][:, 0:192], func=AF.Identity), r=["ps3"], w=[tk])
                        A("pool", lambda e: e.tensor_tensor(out=qq[j][0][:], in0=mt[:, 0:64], in1=istack, op=ALU.add), r=[mk, "cst"], w=[f"qq{j}_0"])
                    st.append(s4)

                    wzb = wzb_l[j]
                    wbk = f"wzb{j}"
                    bm3 = bones.rearrange("p (a b) -> p a b", a=2)

                    def s4b():
                        A("pool", lambda e: e.tensor_tensor(out=wzb[:, 0:128].rearrange("p (a b) -> p a b", a=2),
                                                            in0=mt[:, 0:64].rearrange("p (o t) -> p o t", o=1).to_broadcast([128, 2, 64]), in1=bm3, op=ALU.mult),
                          r=[mk, "cst"], w=[wbk])
                        A("pool", lambda e: e.tensor_tensor(out=wzb[:, 128:256].rearrange("p (a b) -> p a b", a=2),
                                                            in0=mt[:, 256:320].rearrange("p (o t) -> p o t", o=1).to_broadcast([128, 2, 64]), in1=bm3, op=ALU.mult),
                          r=[mk, "cst"], w=[wbk])
                    st.append(s4b)

                    for i in range(5):
                        def lv(i=i):
                            last = (i == 4)
                            qc = qq[j][i % 2]
                            qck = f"qq{j}_{i % 2}"
                            qn = qq[j][(i + 1) % 2]
                            qnk = f"qq{j}_{(i + 1) % 2}"
                            if not last:
                                A("pe", lambda e: e.matmul(psum[4][:, 0:128], lhsT=wzb[:, 128:256], rhs=wzb[:, 0:128], start=True, stop=True), r=[wbk], w=["ps4"])
                            A("pe", lambda e: e.matmul(psum[4][:, 128:256], lhsT=wzb[:, 0:128], rhs=wzb[:, 128:256], start=True, stop=True), r=[wbk], w=["ps4"])
                            if not last:
                                A("act", lambda e: e.activation(out=wzb[:, 0:256], in_=psum[4][:, 0:256], func=AF.Identity), r=["ps4"], w=[wbk])
                            else:
                                A("act", lambda e: e.activation(out=wzb[:, 128:256], in_=psum[4][:, 128:256], func=AF.Identity), r=["ps4"], w=[wbk])
                            A("pe", lambda e: e.matmul(psum[1][:, 128:192], lhsT=wzb[:, 128:256], rhs=qc[:, :], start=True, stop=True), r=[wbk, qck], w=["ps1"])
                            A("dve", lambda e: e.tensor_tensor(out=qn[:], in0=psum[1][:, 128:192], in1=qc[:], op=ALU.add), r=["ps1", qck], w=[qnk])
                        st.append(lv)

                    def s5():
                        cbank = [5, 6, 7, 0][j]
                        cps = psum[cbank] if cbank != 7 else psTt
                        cpk = f"ps{cbank}"
                        q5 = qq[j][1]
                        q5k = f"qq{j}_1"
                        A("dve", lambda e: e.tensor_scalar(out=cn[:, 3, :], in0=H[:], scalar1=s[:, EI, 63:64], scalar2=None, op0=ALU.mult),
                          r=[Hk, sk + "ei"], w=[ck + "hpc"])
                        for p in hp:
                            A("pe", (lambda p: lambda e: e.matmul(cps[p, 0:64], lhsT=s[p, AT, :], rhs=H[p, :], start=True, stop=False))(p), r=[sk + "at", Hk], w=[cpk])
                            A("pe", (lambda p: lambda e: e.matmul(cps[p, 0:64], lhsT=mt[p, 128:192], rhs=tr[p, 0:64], start=False, stop=True))(p), r=[mk, tk], w=[cpk])
                        A("act", lambda e: e.activation(out=cn[:, 0, :], in_=cps[:, 0:64], func=AF.Identity), r=[cpk], w=[ck + "x"])
                        for p in hp:
                            A("pe", (lambda p: lambda e: e.matmul(cps[p, 64:128], lhsT=q5[p, :], rhs=cn[p, 0, :], start=True, stop=True))(p), r=[q5k, ck + "x"], w=[cpk])
                        A("act", lambda e: e.activation(out=cn[:, 1, :], in_=cps[:, 64:128], func=AF.Identity), r=[cpk], w=[ck + "u"])
                        for p in hp:
                            A("pe", (lambda p: lambda e: e.matmul(cps[p, 192:256], lhsT=tr[p, 64:128], rhs=cn[p, 1, :], start=True, stop=False))(p), r=[tk, ck + "u"], w=[cpk])
                            A("pe", (lambda p: lambda e: e.matmul(cps[p, 192:256], lhsT=tr[p, 128:192], rhs=tr[p, 0:64], start=False, stop=True))(p), r=[tk], w=[cpk])
                        for p in hp:
                            A("pe", (lambda p: lambda e: e.matmul(cps[p, 128:192], lhsT=s[p, RT, :], rhs=H[p, :], start=True, stop=False))(p), r=[sk + "rt", Hk], w=[cpk])
                            A("pe", (lambda p: lambda e: e.matmul(cps[p, 128:192], lhsT=mt[p, 64:128], rhs=cn[p, 1, :], start=False, stop=False))(p), r=[mk, ck + "u"], w=[cpk])
                            A("pe", (lambda p: lambda e: e.matmul(cps[p, 128:192], lhsT=mt[p, 192:256], rhs=tr[p, 0:64], start=False, stop=True))(p), r=[mk, tk], w=[cpk])
                        A("dve", lambda e: e.scalar_tensor_tensor(out=H[:], in0=cps[:, 192:256], scalar=s[:, EI, 63:64], in1=cn[:, 3, :], op0=ALU.mult, op1=ALU.add),
                          r=[cpk, sk + "ei", ck + "hpc"], w=[Hk])
                        g = gst[j]
                        gk = f"gst{j}"
                        A("dve", lambda e: e.bn_stats(out=g[:, 0:6], in_=cps[:, 128:192]), r=[cpk], w=[gk])
                        A("dve", lambda e: e.bn_aggr(out=g[:, 6:8], in_=g[:, 0:6]), r=[gk], w=[gk])
                        A("act", lambda e: e.activation(out=g[:, 8:9], in_=g[:, 7:8], func=AF.Sqrt, bias=float(64e-5)), r=[gk], w=[gk])
                        A("dve", lambda e: e.reciprocal(out=g[:, 8:9], in_=g[:, 8:9]), r=[gk], w=[gk])
                        A("dve", lambda e: e.tensor_scalar(out=cn[:, 2, :], in0=cps[:, 128:192], scalar1=g[:, 6:7], scalar2=g[:, 8:9], op0=ALU.subtract, op1=ALU.mult),
                          r=[cpk, gk], w=[ck + "yn"])
                        for p in hp:
                            A("pe", (lambda p: lambda e: e.matmul(psum[3][p, 192:256], lhsT=cn[p, 2, :], rhs=istack[p, :], start=True, stop=True))(p), r=[ck + "yn", "cst"], w=["ps3"])
                        A("act", lambda e: e.activation(out=ybT[:, j, col], in_=psum[3][:, 192:256], func=AF.Identity, scale=pcol("gn_w", j), bias=pcol("gn_b", j)),
                          r=["ps3", "prm"], w=[f"ybT{j}"])
                    st.append(s5)
                    return st

                for c in range(NCH):
                    stl = [unit_stages(j, c) for j in range(4)]
                    for si in range(len(stl[0])):
                        for j in range(4):
                            stl[j][si]()
                P.section(5)
                for j in range(4):
                    pj = pers[j]
                    pjk = f"pers{j}"
                    A("pool", (lambda pj, j: lambda e: e.tensor_tensor(out=ybT[:, j, :], in0=ybT[:, j, :], in1=pj[:, BON, :], op=ALU.add))(pj, j),
                      r=[f"ybT{j}", pjk + "BON"], w=[f"ybT{j}"])
                    A("pool", (lambda pj, j: lambda e: e.tensor_tensor(out=ycat[:, 4 + j, :], in0=ybT[:, j, :], in1=pj[:, GG, :], op=ALU.mult))(pj, j),
                      r=[f"ybT{j}", pjk + "GG"], w=["ycat"])

                P.section(6)
                for sub in range(NSUB):
                    tc_ = slice(sub * 128, (sub + 1) * 128)
                    for kc in range(8):
                        A("pe", (lambda kc, tc_: lambda e: e.matmul(psum[0][:, 0:512], lhsT=hT[:, kc, tc_], rhs=win[:, kc, 512:1024], start=(kc == 0), stop=(kc == 7)))(kc, tc_),
                          r=["hT", "win"], w=[pk(0)])
                    A("act", lambda e: e.activation(out=vg[:], in_=psum[0][:, 0:512], func=AF.Gelu_apprx_tanh), r=[pk(0)], w=["vg"])
                    A("dve", lambda e: e.bn_stats(out=st6[:, 2, :], in_=vg[:]), r=["vg"], w=["st6b"])
                    A("dve", lambda e: e.bn_aggr(out=mv[:, 4:6], in_=st6[:, 2, :]), r=["st6b"], w=["mvb"])
                    A("act", lambda e: e.activation(out=mv[:, 6:7], in_=mv[:, 5:6], func=AF.Sqrt, bias=float(1e-5)), r=["mvb"], w=["mvb"])
                    A("dve", lambda e: e.reciprocal(out=mv[:, 6:7], in_=mv[:, 6:7]), r=["mvb"], w=["mvb"])
                    A("dve", lambda e: e.tensor_scalar(out=vn[:], in0=vg[:], scalar1=mv[:, 4:5], scalar2=mv[:, 6:7], op0=ALU.subtract, op1=ALU.mult), r=["vg", "mvb"], w=["vg"])
                    A("pool", lambda e: e.tensor_tensor(out=vn[:], in0=vn[:], in1=bcs[:, BC["lnw"]:BC["lnw"] + 512], op=ALU.mult), r=["vg", "bc"], w=["vg"])
                    A("pool", lambda e: e.tensor_tensor(out=vn[:], in0=vn[:], in1=bcs[:, BC["lnb"]:BC["lnb"] + 512], op=ALU.add), r=["vg", "bc"], w=["vg"])
                    for h in range(8):
                        A("pe", (lambda h: lambda e: e.matmul(psum[1][(h % 2) * 64:(h % 2) * 64 + 64, (h // 2) * 128:(h // 2 + 1) * 128], lhsT=vn[:, h * 64:(h + 1) * 64],
                                                              rhs=wc[:, h, :], start=True, stop=True))(h), r=["vg", "wc"], w=[pk(1)])
                    A("dve", lambda e: e.tensor_tensor(out=gtmp.rearrange("p a b -> p (a b)"), in0=psum[1][:, 0:512], in1=bcs[:, BC["bsT"]:BC["bsT"] + 512], op=ALU.add),
                      r=[pk(1), "bc"], w=["h2f"])
                    A("dve", (lambda tc_: lambda e: e.tensor_tensor(out=ycat[:, 0:4, tc_], in0=gtmp, in1=u_s[:, :, tc_], op=ALU.mult))(tc_), r=["h2f", "u_s"], w=["ycat"])

                P.section(7)
                for sub in range(NSUB):
                    tc_ = slice(sub * 128, (sub + 1) * 128)
                    ti = TI * NSUB + sub
                    tok0 = t0 + sub * 128
                    for hf in range(2):
                        for cc in range(8):
                            A("pe", (lambda cc, hf, tc_: lambda e: e.matmul(psum[6][:, 0:512], lhsT=ycat[:, cc, tc_], rhs=wout[:, cc, hf * 512:(hf + 1) * 512],
                                                                           start=(cc == 0), stop=(cc == 7)))(cc, hf, tc_), r=["ycat", "wout"], w=[pk(6)])
                        A("dve", (lambda hf: lambda e: e.tensor_tensor(out=x1t[:, hf * 512:(hf + 1) * 512], in0=psum[6][:, 0:512], in1=gate_bc[:, hf * 512:(hf + 1) * 512],
                                                                      op=ALU.mult))(hf), r=[pk(6), "gate_bc"], w=["x1t"])
                        A("pool", (lambda hf, xb, sub: lambda e: e.tensor_tensor(out=x1t[:, hf * 512:(hf + 1) * 512], in0=x1t[:, hf * 512:(hf + 1) * 512],
                                                                                in1=xb[:, sub, hf * 512:(hf + 1) * 512], op=ALU.add))(hf, xb, sub), r=["x1t", xk], w=["x1t"])
                    P.add("sp", (lambda tok0: lambda e: e.dma_start(out=x1_d[tok0:tok0 + 128, :], in_=x1t[:]))(tok0), r=["x1t"], w=["x1_d"], dma=True)
                    norm_stats(lambda hf: x1t[:, hf * 512:(hf + 1) * 512], "x1t", 1e-6)
                    A("act", lambda e: e.activation(out=xn2[:], in_=x1t[:], func=AF.Identity, scale=mv[:, 3:4]), r=["x1t", "mv"], w=["x1t"])
                    for kc in range(8):
                        pb = kc // 4
                        A("pe", (lambda kc, pb: lambda e: e.matmul(psum[pb][:, (kc % 4) * 128:(kc % 4 + 1) * 128], lhsT=xn2[:, kc * 128:(kc + 1) * 128], rhs=ident, start=True, stop=True))(kc, pb),
                          r=["x1t", "cst"], w=[pk(pb)])
                    for kc in range(8):
                        pb = kc // 4
                        A("act", (lambda kc, pb: lambda e: e.activation(out=h2f[:, kc, :], in_=psum[pb][:, (kc % 4) * 128:(kc % 4 + 1) * 128], func=AF.Identity,
                                                                        scale=sc[:, 8 + kc:9 + kc], bias=mod[:, 24 + kc:25 + kc]))(kc, pb), r=[pk(pb), "sc", "mod"], w=["h2f"])
                    A("pool", lambda e: e.tensor_copy(out=xnb[:], in_=xn2[:]), r=["x1t"], w=["xnb"])
                    P.add("sp", (lambda tok0: lambda e: e.dma_start(out=xn2_d[tok0:tok0 + 128, :], in_=xnb[:]))(tok0),
                          r=["xnb"], w=["xn2_d"], dma=True)
                    for kc in range(8):
                        A("pe", (lambda kc: lambda e: e.matmul(psum[6][:, 0:36], lhsT=h2f[:, kc, :], rhs=wrs[:, kc, :], start=(kc == 0), stop=(kc == 7)))(kc),
                          r=["h2f", "wrs"], w=[pk(6)])
                    A("dve", (lambda ti: lambda e: e.tensor_tensor(out=logits[:, ti, :], in0=psum[6][:, 0:36], in1=bcs[:, BC["rb"]:BC["rb"] + 36], op=ALU.add))(ti),
                      r=[pk(6), "bc"], w=["logits"])

            P.muted = False
            P.barrier()
            ct.close()
            if stage >= 2:
                rt = SB(ca, "rt", [128, 32, 48])
                sel = SB(ca, "sel", [128, 32, 8])
                sel2 = SB(ca, "sel2", [128, 32, 8])
                ohg = SB(ca, "ohg", [128, 32, 4])
                tmp48 = SB(ca, "tmp48", [128, 32, 4, 8])
                lg = logits[:, :, 0:4]
                le = logits[:, :, 4:36].rearrange("p t (g e) -> p t g e", g=4)
                MG, SG, M1, M2, P1, W1, W2 = range(7)
                r1 = lambda i: rt[:, :, i:i + 1]
                R = lambda fn, r, w: A("dve", fn, r=r, w=w)
                R(lambda e: e.tensor_reduce(out=rt[:, :, MG], in_=lg, axis=AX.X, op=ALU.max), ["logits"], ["rt"])
                R(lambda e: e.tensor_tensor(out=ohg[:], in0=lg, in1=r1(MG).to_broadcast([128, 32, 4]), op=ALU.is_equal), ["logits", "rt"], ["ohg"])
                R(lambda e: e.tensor_tensor(out=rt[:, :, 8:12], in0=lg, in1=r1(MG).to_broadcast([128, 32, 4]), op=ALU.subtract), ["logits", "rt"], ["rt"])
                A("act", lambda e: e.activation(out=rt[:, :, 8:12], in_=rt[:, :, 8:12], func=AF.Exp), r=["rt"], w=["rt"])
                R(lambda e: e.tensor_reduce(out=rt[:, :, SG], in_=rt[:, :, 8:12], axis=AX.X, op=ALU.add), ["rt"], ["rt"])
                R(lambda e: e.reciprocal(out=rt[:, :, SG], in_=rt[:, :, SG]), ["rt"], ["rt"])
                R(lambda e: e.tensor_tensor(out=tmp48[:], in0=le, in1=ohg[:].rearrange("p t (g o) -> p t g o", o=1).to_broadcast([128, 32, 4, 8]), op=ALU.mult),
                  ["logits", "ohg"], ["tmp48"])
                R(lambda e: e.tensor_reduce(out=sel[:], in_=tmp48[:].rearrange("p t g e -> p t e g"), axis=AX.X, op=ALU.add), ["tmp48"], ["sel"])
                R(lambda e: e.tensor_reduce(out=rt[:, :, M1], in_=sel[:], axis=AX.X, op=ALU.max), ["sel"], ["rt"])
                R(lambda e: e.tensor_tensor(out=sel2[:], in0=sel[:], in1=r1(M1).to_broadcast([128, 32, 8]), op=ALU.is_equal), ["sel", "rt"], ["sel2"])
                R(lambda e: e.scalar_tensor_tensor(out=tmp48[:, :, 0, :], in0=sel2[:], scalar=-1e30, in1=sel[:], op0=ALU.mult, op1=ALU.add), ["sel", "sel2"], ["tmp48"])
                R(lambda e: e.tensor_reduce(out=rt[:, :, M2], in_=tmp48[:, :, 0, :], axis=AX.X, op=ALU.max), ["tmp48"], ["rt"])
                R(lambda e: e.tensor_tensor(out=tmp48[:, :, 1, :], in0=tmp48[:, :, 0, :], in1=r1(M2).to_broadcast([128, 32, 8]), op=ALU.is_equal), ["tmp48", "rt"], ["tmp48"])
                R(lambda e: e.tensor_tensor(out=rt[:, :, P1], in0=rt[:, :, M2], in1=rt[:, :, M1], op=ALU.subtract), ["rt"], ["rt"])
                A("act", lambda e: e.activation(out=rt[:, :, P1], in_=rt[:, :, P1], func=AF.Exp), r=["rt"], w=["rt"])
                R(lambda e: e.tensor_scalar(out=rt[:, :, P1], in0=rt[:, :, P1], scalar1=1.0, scalar2=None, op0=ALU.add), ["rt"], ["rt"])
                R(lambda e: e.reciprocal(out=rt[:, :, P1], in_=rt[:, :, P1]), ["rt"], ["rt"])
                R(lambda e: e.tensor_tensor(out=rt[:, :, W1], in0=rt[:, :, P1], in1=rt[:, :, SG], op=ALU.mult), ["rt"], ["rt"])
                R(lambda e: e.tensor_tensor(out=rt[:, :, W2], in0=rt[:, :, SG], in1=rt[:, :, W1], op=ALU.subtract), ["rt"], ["rt"])
                R(lambda e: e.tensor_tensor(out=sel[:], in0=sel2[:], in1=r1(W1).to_broadcast([128, 32, 8]), op=ALU.mult), ["sel2", "rt"], ["sel"])
                R(lambda e: e.tensor_tensor(out=sel2[:], in0=tmp48[:, :, 1, :], in1=r1(W2).to_broadcast([128, 32, 8]), op=ALU.mult), ["tmp48", "rt"], ["sel2"])
                R(lambda e: e.tensor_tensor(out=sel[:], in0=sel[:], in1=sel2[:], op=ALU.add), ["sel", "sel2"], ["sel"])
                for g in range(4):
                    R((lambda g: lambda e: e.tensor_tensor(out=wt[:, :, g * 8:(g + 1) * 8], in0=sel[:], in1=ohg[:, :, g:g + 1].to_broadcast([128, 32, 8]), op=ALU.mult))(g),
                      ["sel", "ohg"], ["wt"])
            if stage >= 2:
                Mf = SB(ca, "Mf", [128, 1024])
                Mb = SB(ca, "Mb", [128, 1024], BF16)
                Lsb = SB(ca, "Lsb", [128, 128], BF16)
                onesb = SB(ca, "onesb", [128, 128], BF16)
                PRE = SB(ca, "PRE", [128, 1024])
                CNTs = SB(ca, "CNTs", [128, 1024])
                CA_ = SB(ca, "CA", [128, 1024])
                CB_ = SB(ca, "CB", [128, 1024])
                sm = SB(ca, "sm", [128, 8, 32])
                cmpb = SB(ca, "cmpb", [128, 96, 32])
                bev = SB(ca, "bev", [128, 6, 96])
                pabf = SB(ca, "pabf", [128, 2, 32])
                v3 = lambda t: t[:].rearrange("p (a b) -> p a b", a=32)
                wtf = wt[:].rearrange("p t e -> p (t e)")
                R(lambda e: e.tensor_single_scalar(out=Mf[:], in_=wtf, scalar=0.0, op=ALU.is_gt), ["wt"], ["Mf"])
                A("pool", lambda e: e.tensor_copy(out=Mb[:], in_=Mf[:]), r=["Mf"], w=["Mb"])
                A("pool", lambda e: e.tensor_tensor(out=Lsb[:], in0=tril, in1=ident, op=ALU.subtract), r=["cst"], w=["Lsb"])
                A("pool", lambda e: e.tensor_copy(out=onesb[:], in_=ones), r=["cst"], w=["onesb"])
                for h in range(2):
                    A("pe", (lambda h: lambda e: e.matmul(psum[h][:, 0:512], lhsT=Lsb[:], rhs=Mb[:, h * 512:(h + 1) * 512], start=True, stop=True))(h),
                      r=["Lsb", "Mb"], w=[pk(h)])
                    A("pe", (lambda h: lambda e: e.matmul(psum[2 + h][:, 0:512], lhsT=onesb[:], rhs=Mb[:, h * 512:(h + 1) * 512], start=True, stop=True))(h),
                      r=["onesb", "Mb"], w=[pk(2 + h)])
                    A("act", (lambda h: lambda e: e.activation(out=PRE[:, h * 512:(h + 1) * 512], in_=psum[h][:, 0:512], func=AF.Identity))(h), r=[pk(h)], w=["PRE"])
                    A("act", (lambda h: lambda e: e.activation(out=CNTs[:, h * 512:(h + 1) * 512], in_=psum[2 + h][:, 0:512], func=AF.Identity))(h), r=[pk(2 + h)], w=["CNTs"])
                src, srck, dst, dstk = CNTs, "CNTs", CA_, "CA"
                for dd in (1, 2, 4, 8, 16):
                    w_ = dd * 32
                    R((lambda src, dst, w_: lambda e: e.tensor_copy(out=dst[:, 0:w_], in_=src[:, 0:w_]))(src, dst, w_), [srck], [dstk])
                    R((lambda src, dst, w_: lambda e: e.tensor_tensor(out=dst[:, w_:1024], in0=src[:, w_:1024], in1=src[:, 0:1024 - w_], op=ALU.add))(src, dst, w_), [srck], [dstk])
                    src, srck = dst, dstk
                    dst, dstk = (CB_, "CB") if dst is CA_ else (CA_, "CA")
                R(lambda e: e.tensor_tensor(out=CB_[:], in0=CA_[:], in1=CNTs[:], op=ALU.subtract), ["CA", "CNTs"], ["CB"])
                R(lambda e: e.tensor_copy(out=sm[:, 0, :], in_=CA_[:, 31 * 32:32 * 32]), ["CA"], ["sm"])
                R(lambda e: e.tensor_tensor(out=cmpb[:, 0:32, :], in0=sm[:, 0, :].rearrange("p (e o) -> p e o", o=1).to_broadcast([128, 32, 32]),
                                            in1=cst[:, CC["thr"]:CC["thr"] + 32].rearrange("p (o k) -> p o k", o=1).to_broadcast([128, 32, 32]), op=ALU.is_gt),
                  ["sm", "cst"], ["cmpb"])
                R(lambda e: e.tensor_reduce(out=sm[:, 1, :], in_=cmpb[:, 0:32, :], axis=AX.X, op=ALU.add), ["cmpb"], ["sm"])
                R(lambda e: e.tensor_tensor_scan(out=sm[:, 2, :], data0=ones[:, 0:32], data1=sm[:, 1, :], initial=0.0, op0=ALU.mult, op1=ALU.add), ["sm", "cst"], ["sm"])
                R(lambda e: e.tensor_tensor(out=sm[:, 3, :], in0=sm[:, 2, :], in1=sm[:, 1, :], op=ALU.subtract), ["sm"], ["sm"])
                R(lambda e: e.tensor_single_scalar(out=sm[:, 4, :], in_=sm[:, 3, :], scalar=128.0, op=ALU.mult), ["sm"], ["sm"])
                R(lambda e: e.tensor_tensor(out=v3(CB_), in0=v3(CB_), in1=sm[:, 4, :].rearrange("p (o e) -> p o e", o=1).to_broadcast([128, 32, 32]), op=ALU.add),
                  ["CB", "sm"], ["CB"])
                R(lambda e: e.tensor_tensor(out=PRE[:], in0=PRE[:], in1=CB_[:], op=ALU.add), ["PRE", "CB"], ["PRE"])
                R(lambda e: e.tensor_tensor(out=CA_[:], in0=PRE[:], in1=Mf[:], op=ALU.mult), ["PRE", "Mf", "sm"], ["CA"])
                R(lambda e: e.tensor_scalar(out=CB_[:], in0=Mf[:], scalar1=-1e9, scalar2=1e9, op0=ALU.mult, op1=ALU.add), ["Mf", "PRE"], ["CB"])
                R(lambda e: e.tensor_tensor(out=CB_[:], in0=CB_[:], in1=CA_[:], op=ALU.add), ["CB", "CA"], ["CB"])
                R(lambda e: e.tensor_reduce(out=pabf[:, 0, :], in_=v3(CB_), axis=AX.X, op=ALU.min), ["CB"], ["pabf"])
                R(lambda e: e.tensor_reduce(out=pabf[:, 1, :], in_=v3(CA_), axis=AX.X, op=ALU.max), ["CA"], ["pabf"])
                R(lambda e: e.tensor_tensor(out=v3(CB_), in0=v3(CB_), in1=pabf[:, 0, :].rearrange("p (t o) -> p t o", o=1).to_broadcast([128, 32, 32]), op=ALU.is_equal),
                  ["CB", "pabf"], ["CB"])
                R(lambda e: e.tensor_tensor(out=CB_[:], in0=CB_[:], in1=wtf, op=ALU.mult), ["CB", "wt"], ["CB"])
                R(lambda e: e.tensor_reduce(out=WAB[:, 0, :], in_=v3(CB_), axis=AX.X, op=ALU.add), ["CB"], ["WAB"])
                R(lambda e: e.tensor_tensor(out=WAB[:, 1, :], in0=rt[:, :, SG], in1=WAB[:, 0, :], op=ALU.subtract), ["rt", "WAB"], ["WAB"])
                R(lambda e: e.tensor_copy(out=PABi[:], in_=pabf[:]), ["pabf"], ["PABi"])
                R(lambda e: e.tensor_tensor(out=cmpb[:], in0=sm[:, 2, :].rearrange("p (o e) -> p o e", o=1).to_broadcast([128, 96, 32]),
                                            in1=cst[:, CC["iotab"]:CC["iotab"] + 96].rearrange("p (b o) -> p b o", o=1).to_broadcast([128, 96, 32]), op=ALU.is_le),
                  ["sm", "cst", "cmpb"], ["cmpb"])
                R(lambda e: e.tensor_reduce(out=bev[:, 0, :], in_=cmpb[:], axis=AX.X, op=ALU.add), ["cmpb"], ["bev"])
                R(lambda e: e.memset(bev[:, 1, 0:1], 1.0), [], ["bev"])
                R(lambda e: e.tensor_tensor(out=bev[:, 1, 1:96], in0=bev[:, 0, 1:96], in1=bev[:, 0, 0:95], op=ALU.not_equal), ["bev"], ["bev"])
                R(lambda e: e.tensor_scalar(out=bev[:, 2, :], in0=bev[:, 1, :], scalar1=-1e6, scalar2=1e6, op0=ALU.mult, op1=ALU.add), ["bev"], ["bev"])
                R(lambda e: e.scalar_tensor_tensor(out=bev[:, 2, :], in0=bev[:, 0, :], scalar=128.0, in1=bev[:, 2, :], op0=ALU.mult, op1=ALU.add), ["bev"], ["bev"])
                R(lambda e: e.tensor_scalar(out=bev[:, 2, :], in0=bev[:, 2, :], scalar1=cst[:, CC["pidx"]:CC["pidx"] + 1], scalar2=None, op0=ALU.add), ["bev", "cst"], ["bev"])
                R(lambda e: e.tensor_copy(out=IDXW[:, 0:1, :], in_=bev[:, 2:3, :]), ["bev"], ["IDXW"])
            P.barrier()

        if stage >= 3:
            with ExitStack() as cb:
                IOA = bass.IndirectOffsetOnAxis
                NROW = NE * 128
                _bc = {}

                def bc_reg(e):
                    if "r" not in _bc:
                        _bc["r"] = e.to_reg(NROW - 1)
                    return _bc["r"]
                Wg = SB(cb, "Wg", [128, 8, EH])
                Wu = SB(cb, "Wu", [128, 8, EH])
                Wd = SB(cb, "Wd", [128, 4, D])
                idb2 = SB(cb, "idb2", [128, 128], BF16)
                xload = [SB(cb, f"xl{i}", [128, D], BF16) for i in range(2)]
                xsb = [SB(cb, f"xsb{i}", [128, D], BF16) for i in range(2)]
                XgT = SB(cb, "XgT", [128, 8, 128])
                sg = SB(cb, "sg", [128, 512])
                actb = SB(cb, "actb", [128, 512])
                actT = SB(cb, "actT", [128, 4, 128])
                yblk = [SB(cb, f"yblk{i}", [128, D]) for i in range(2)]
                fgt = SB(cb, "fgt", [128, D])
                fgs = SB(cb, "fgs", [128, D])
                g2bc = SB(cb, "g2bc", [128, D])
                yA = SB(cb, "yA", [128, D])
                yB = SB(cb, "yB", [128, D])
                fst = SB(cb, "fst", [128, 2, 6])
                fmv = SB(cb, "fmv", [128, 4])
                P.add("sp", lambda e: e.dma_start(out=fgs[:], in_=fg_d[:, :]), w=["fgs"], dma=True)
                P.add("dve", lambda e: e.tensor_copy(out=idb2[:], in_=ident), r=["cst"], w=["idb2"])
                make_gate(g2bc, "g2bc", 40)
                zt = SB(cb, "zt", [128, D], BF16)
                P.add("pool", lambda e: e.memset(zt[:], 0.0), w=["zt"])
                xz_keys = []
                for b in range(NBLK):
                    P.add("sp", (lambda b: lambda e: e.dma_start(out=xs_d[b * 128:(b + 1) * 128, :], in_=zt[:]))(b), r=["zt"], w=[f"xz{b}"], dma=True)
                    xz_keys.append(f"xz{b}")
                sc_keys = []
                for i in range(32):
                    xl = xload[i % 2]
                    xlk = f"xl{i % 2}"
                    P.add("sp", (lambda xl, i: lambda e: e.dma_start(out=xl[:], in_=xn2_d[i * 128:(i + 1) * 128, :]))(xl, i), r=["xn2_d"], w=[xlk], dma=True)
                    for k in range(2):
                        key = f"xsc{i}_{k}"
                        P.add("pool", (lambda xl, i, k: lambda e: e.indirect_dma_start(out=xs_d[:, :], out_offset=IOA(ap=PABi[:, k, i:i + 1], axis=0),
                                                                                       in_=xl[:], in_offset=None))(xl, i, k),
                              r=[xlk, "PABi"] + xz_keys, w=[key], dma=True)
                        sc_keys.append(key)
                ys_keys = []
                for b in range(min(NBLK, nblk)):
                    for c2 in range(1):
                        for (wsb, wk, wsrc, nch) in ((Wg, "Wg", wg_d, 8), (Wu, "Wu", wu_d, 8), (Wd, "Wd", wd_d, 4)):
                            P.add("pool", (lambda wsb, wsrc, nch, c2, b: lambda e: e.indirect_dma_start(
                                out=wsb[:].rearrange("p a b -> p (a b)"), out_offset=None, in_=wsrc[:, :],
                                in_offset=IOA(ap=IDXW[:, c2, b:b + 1], axis=0), bounds_check=bc_reg(e), oob_is_err=False))(wsb, wsrc, nch, c2, b),
                                r=["IDXW", wk], w=[wk], dma=True)
                    xb_ = xsb[b % 2]
                    xbk = f"xsb{b % 2}"
                    P.add("sp", (lambda xb_, b: lambda e: e.dma_start(out=xb_[:], in_=xs_d[b * 128:(b + 1) * 128, :]))(xb_, b), r=sc_keys, w=[xbk], dma=True)
                    for kc in range(8):
                        P.add("pe", (lambda xb_, kc: lambda e: e.matmul(psum[kc // 4][:, (kc % 4) * 128:(kc % 4 + 1) * 128], lhsT=xb_[:, kc * 128:(kc + 1) * 128],
                                                                       rhs=idb2[:], start=True, stop=True))(xb_, kc), r=[xbk, "idb2"], w=[pk(kc // 4)])
                    for kc in range(8):
                        P.add("act", (lambda kc: lambda e: e.activation(out=XgT[:, kc, :], in_=psum[kc // 4][:, (kc % 4) * 128:(kc % 4 + 1) * 128], func=AF.Identity,
                                                                        scale=sc[:, 8 + kc:9 + kc], bias=mod[:, 24 + kc:25 + kc]))(kc), r=[pk(kc // 4), "sc", "mod"], w=["XgT"])
                    for kc in range(8):
                        P.add("pe", (lambda kc: lambda e: e.matmul(psum[2][:, 0:512], lhsT=XgT[:, kc, :], rhs=Wg[:, kc, :], start=(kc == 0), stop=(kc == 7)))(kc),
                              r=["XgT", "Wg"], w=[pk(2)])
                    for kc in range(8):
                        P.add("pe", (lambda kc: lambda e: e.matmul(psum[3][:, 0:512], lhsT=XgT[:, kc, :], rhs=Wu[:, kc, :], start=(kc == 0), stop=(kc == 7)))(kc),
                              r=["XgT", "Wu"], w=[pk(3)])
                    P.add("act", lambda e: e.activation(out=sg[:], in_=psum[2][:, 0:512], func=AF.Silu), r=[pk(2)], w=["sg"])
                    P.add("dve", lambda e: e.tensor_tensor(out=actb[:], in0=psum[3][:, 0:512], in1=sg[:], op=ALU.mult), r=[pk(3), "sg"], w=["actb"])
                    for hc in range(4):
                        P.add("pe", (lambda hc: lambda e: e.matmul(psum[4][:, hc * 128:(hc + 1) * 128], lhsT=actb[:, hc * 128:(hc + 1) * 128], rhs=ident,
                                                                   start=True, stop=True))(hc), r=["actb", "cst"], w=[pk(4)])
                    P.add("act", lambda e: e.activation(out=actT[:].rearrange("p a b -> p (a b)"), in_=psum[4][:, 0:512], func=AF.Identity), r=[pk(4)], w=["actT"])
                    yb_ = yblk[b % 2]
                    ybk = f"yblk{b % 2}"
                    for hf in range(2):
                        for hc in range(4):
                            P.add("pe", (lambda hc, hf: lambda e: e.matmul(psum[5 + hf][:, 0:512], lhsT=actT[:, hc, :], rhs=Wd[:, hc, hf * 512:(hf + 1) * 512],
                                                                           start=(hc == 0), stop=(hc == 3)))(hc, hf), r=["actT", "Wd"], w=[pk(5 + hf)])
                        if hf == 0:
                            P.add("act", (lambda yb_: lambda e: e.activation(out=yb_[:, 0:512], in_=psum[5][:, 0:512], func=AF.Identity))(yb_), r=[pk(5)], w=[ybk])
                        else:
                            P.add("dve", (lambda yb_: lambda e: e.tensor_copy(out=yb_[:, 512:1024], in_=psum[6][:, 0:512]))(yb_), r=[pk(6)], w=[ybk])
                    key = f"ys{b}"
                    P.add("sp", (lambda yb_, b: lambda e: e.dma_start(out=ys_d[b * 128:(b + 1) * 128, :], in_=yb_[:]))(yb_, b), r=[ybk], w=[key], dma=True)
                    ys_keys.append(key)
                for i in range(32):
                    tok0 = i * 128
                    P.add("pool", (lambda i: lambda e: e.indirect_dma_start(out=yA[:], out_offset=None, in_=ys_d[:, :], in_offset=IOA(ap=PABi[:, 0, i:i + 1], axis=0)))(i),
                          r=ys_keys + ["PABi", "yA"], w=["yA"], dma=True)
                    P.add("pool", (lambda i: lambda e: e.indirect_dma_start(out=yB[:], out_offset=None, in_=ys_d[:, :], in_offset=IOA(ap=PABi[:, 1, i:i + 1], axis=0)))(i),
                          r=ys_keys + ["PABi", "yB"], w=["yB"], dma=True)
                    P.add("sp", (lambda tok0: lambda e: e.dma_start(out=fgt[:], in_=x1_d[tok0:tok0 + 128, :]))(tok0), r=["x1_d"], w=["fgt"], dma=True)
                    P.add("dve", (lambda i: lambda e: e.tensor_scalar(out=yA[:], in0=yA[:], scalar1=WAB[:, 0, i:i + 1], scalar2=None, op0=ALU.mult))(i), r=["yA", "WAB"], w=["yA"])
                    P.add("dve", (lambda i: lambda e: e.scalar_tensor_tensor(out=yA[:], in0=yB[:], scalar=WAB[:, 1, i:i + 1], in1=yA[:], op0=ALU.mult, op1=ALU.add))(i),
                          r=["yA", "yB", "WAB"], w=["yA"])
                    P.add("pool", lambda e: e.tensor_tensor(out=yA[:], in0=yA[:], in1=g2bc[:], op=ALU.mult), r=["yA", "g2bc"], w=["yA"])
                    P.add("dve", lambda e: e.tensor_tensor(out=fgt[:], in0=fgt[:], in1=yA[:], op=ALU.add), r=["yA", "fgt"], w=["fgt"])
                    for hf in range(2):
                        P.add("dve", (lambda hf: lambda e: e.bn_stats(out=fst[:, hf, :], in_=fgt[:, hf * 512:(hf + 1) * 512]))(hf), r=["fgt"], w=["fst"])
                    P.add("dve", lambda e: e.bn_aggr(out=fmv[:, 0:2], in_=fst[:, 0:2, :].rearrange("p a b -> p (a b)")), r=["fst"], w=["fmv"])
                    P.add("dve", lambda e: e.scalar_tensor_tensor(out=fmv[:, 2:3], in0=fmv[:, 0:1], scalar=fmv[:, 0:1], in1=fmv[:, 1:2], op0=ALU.mult, op1=ALU.add),
                          r=["fmv"], w=["fmv"])
                    P.add("act", lambda e: e.activation(out=fmv[:, 3:4], in_=fmv[:, 2:3], func=AF.Sqrt, bias=float(1e-6)), r=["fmv"], w=["fmv"])
                    P.add("dve", lambda e: e.reciprocal(out=fmv[:, 3:4], in_=fmv[:, 3:4]), r=["fmv"], w=["fmv"])
                    P.add("dve", lambda e: e.scalar_tensor_tensor(out=fgt[:], in0=fgt[:], scalar=fmv[:, 3:4], in1=fgs[:], op0=ALU.mult, op1=ALU.mult),
                          r=["fgt", "fmv", "fgs"], w=["fgt"])
                    P.add("sp", (lambda tok0: lambda e: e.dma_start(out=out_d[tok0:tok0 + 128, :], in_=fgt[:]))(tok0), r=["fgt"], w=["out_d"], dma=True)
        else:
            with ExitStack() as cb:
                fgt = SB(cb, "fgt", [128, D])
                for ti in range(32):
                    tok0 = ti * 128
                    P.add("sp", (lambda tok0: lambda e: e.dma_start(out=fgt[:], in_=x1_d[tok0:tok0 + 128, :]))(tok0), r=["x1_d"], w=["fgt"], dma=True)
                    P.add("sp", (lambda tok0: lambda e: e.dma_start(out=out_d[tok0:tok0 + 128, :], in_=fgt[:]))(tok0), r=["fgt"], w=["out_d"], dma=True)
        P.emit()
    return nc


_NC_CACHE = {}


def _layouts(inp):
    f = np.float32
    g = lambda k: np.asarray(inp[k], dtype=f)
    col = lambda v, n: np.ascontiguousarray(v.reshape(n, 128).T)
    mu = g("rwkv_mu")[0]
    def pad128(v):
        o = np.zeros((128, 1), f)
        o[:v.shape[0], 0] = v
        return o
    shared = [None, col(g("ada_b")[0], 48), col(g("norm1_g")[0], 8), col(g("norm2_g")[0], 8), col(mu[0:1536], 12),
              pad128(mu[1536:1568]), pad128(mu[1568:1600]), pad128(mu[1600:1696]), col(g("rwkv_w0")[0], 4), col(g("rwkv_a0")[0], 4),
              col(g("rwkv_k_k")[0], 4), col(g("rwkv_k_a")[0], 4), col(g("rwkv_r_k")[0].reshape(512), 4), col(g("rwkv_gn_w")[0], 4),
              col(g("rwkv_gn_b")[0], 4)]
    tt = np.arange(64)
    su = (tt[:, None] < tt[None, :]).astype(f)
    iu = (tt[:, None] <= tt[None, :]).astype(f)
    sl = (tt[None, :] < tt[:, None]).astype(f)
    m5 = np.tile(np.concatenate([su, iu, su, iu, sl], axis=1), (2, 1))
    ident = np.eye(128, dtype=f)
    istack = np.tile(np.eye(64, dtype=f), (2, 1))
    ss = np.arange(128)
    tril = (ss[:, None] <= ss[None, :]).astype(f)
    bones = (ss[:, None] // 64 == ss[None, :] // 64).astype(f)
    ones = np.ones((128, 128), f)
    thr = np.broadcast_to((np.arange(32) * 128).astype(f)[None, :], (128, 32))
    iotab = np.broadcast_to(np.arange(96).astype(f)[None, :], (128, 96))
    pidx = np.arange(128).astype(f)[:, None]
    cst = np.ascontiguousarray(np.concatenate([m5, ident, istack, tril, bones, ones, thr, iotab, pidx], axis=1))
    assert cst.shape[1] == NCST
    rep = lambda v: np.broadcast_to(v[None, :], (128, v.shape[0]))
    bs = g("gmlp_bs")[0]
    bsT = np.zeros((128, 4, 128), f)
    for j in range(4):
        for hh in range(2):
            bsT[hh * 64:(hh + 1) * 64, j, :] = bs[2 * j + hh][None, :]
    bc = np.ascontiguousarray(np.concatenate([rep(g("gmlp_ln_w")[0]), rep(g("gmlp_ln_b")[0]),
                                              rep(np.concatenate([g("router_group_b")[0], g("router_expert_b")[0]])),
                                              bsT.reshape(128, 512)], axis=1))
    assert bc.shape[1] == NBC
    common = dict(cst=cst, bc=bc, fg=np.ascontiguousarray(rep(g("final_norm_g"))), ada_w=np.ascontiguousarray(g("ada_w")[0]), w_in=np.ascontiguousarray(g("w_in")[0]),
                  w_out=np.ascontiguousarray(g("w_out")[0]), w2=np.ascontiguousarray(g("rwkv_w2")[0]),
                  a2=np.ascontiguousarray(g("rwkv_a2")[0]), g2=np.ascontiguousarray(g("rwkv_g2")[0]),
                  wsT=np.ascontiguousarray(g("gmlp_ws")[0].transpose(2, 0, 1)),
                  wr=np.ascontiguousarray(np.concatenate([g("router_group_w")[0], g("router_expert_w")[0]], axis=1)),
                  wg=np.ascontiguousarray(g("moe_w_gate")[0].reshape(NE, 8, 128, EH).transpose(0, 2, 1, 3).reshape(NE * 128, 4096)),
                  wu=np.ascontiguousarray(g("moe_w_up")[0].reshape(NE, 8, 128, EH).transpose(0, 2, 1, 3).reshape(NE * 128, 4096)),
                  wd=np.ascontiguousarray(g("moe_w_down")[0].reshape(NE, 4, 128, D).transpose(0, 2, 1, 3).reshape(NE * 128, 4096)))
    x = g("x")
    c = g("c")
    maps = []
    for b in range(x.shape[0]):
        cols = [col(c[b], 8)] + shared[1:]
        prm = np.ascontiguousarray(np.concatenate(cols, axis=1))
        assert prm.shape[1] == NPRM
        m = dict(common)
        m["x"] = np.ascontiguousarray(x[b])
        m["prm"] = prm
        maps.append(m)
    return maps


def kernel(**inputs):
    maps = _layouts(inputs)
    if "nc" not in _NC_CACHE:
        _NC_CACHE["nc"] = build_nc()
    nc = _NC_CACHE["nc"]
    res = run_bass_kernel_spmd(nc, maps, core_ids=list(range(len(maps))))
    return np.stack([r["out"] for r in res.results], axis=0).astype(np.float32)
```

```python
import numpy as np
import os
from contextlib import ExitStack
import concourse.bass as bass
import concourse.mybir as mybir
from concourse.bass_utils import run_bass_kernel_spmd

F32 = mybir.dt.float32
BF16 = mybir.dt.bfloat16
I32 = mybir.dt.int32
AF = mybir.ActivationFunctionType
ALU = mybir.AluOpType
AX = mybir.AxisListType

D = 1024
S = 4096
TT = 256
NT = S // TT
NSUB = TT // 128
CH = 64
NCH = TT // CH
INW = 2720
NE = 32
EH = 512
NEG_HALF_E = -0.6065306597126334

PC = {}
_o = 0
for _n, _w in [("cT", 8), ("ada_b", 48), ("n1g", 8), ("n2g", 8), ("mu_rkv", 12), ("mu_xw", 1),
               ("mu_xa", 1), ("mu_xg", 1), ("w0", 4), ("a0", 4), ("k_k", 4), ("k_a", 4), ("r_k", 4),
               ("gn_w", 4), ("gn_b", 4)]:
    PC[_n] = _o
    _o += _w
NPRM = _o
CC = {}
_o = 0
for _n, _w in [("m5", 320), ("ident", 128), ("istack", 64), ("tril", 128), ("bones", 128), ("ones", 128), ("thr", 32), ("iotab", 96), ("pidx", 1)]:
    CC[_n] = _o
    _o += _w
NCST = _o
BC = {}
_o = 0
for _n, _w in [("lnw", 512), ("lnb", 512), ("rb", 36), ("bsT", 512)]:
    BC[_n] = _o
    _o += _w
NBC = _o


ATTACH_WAIT = True
NO_SELF_SYNC = ("pe", "act")


class Prog:
    def __init__(self, nc, ctx):
        self.nc = nc
        self.ops = []
        self.last_w = {}
        self.readers = {}
        self.engs = ["pe", "act", "dve", "pool", "sp"]
        self.count = {e: 0 for e in self.engs}
        self.sem = {e: ctx.enter_context(nc.semaphore("s_" + e)) for e in self.engs}
        self.NDS = 8
        self.dsem = {q: [ctx.enter_context(nc.semaphore(f"d_{q}{i}")) for i in range(self.NDS)]
                     for q in ("sp", "pool")}
        self.dcount = {"sp": 0, "pool": 0}
        self.last_op = {e: None for e in self.engs}
        self.pending = {e: set() for e in self.engs}
        self.recent_dma = {"sp": [], "pool": []}

    def section(self, k):
        self.muted = k > self.cut

    def add(self, eng, fn, r=(), w=(), dma=False):
        if getattr(self, 'muted', False):
            return None
        idx = len(self.ops)
        deps = set(self.pending[eng])
        self.pending[eng] = set()
        for k in r:
            if k in self.last_w:
                deps.add(self.last_w[k])
        for k in w:
            if k in self.last_w:
                deps.add(self.last_w[k])
            deps.update(self.readers.get(k, ()))
        for k in r:
            self.readers.setdefault(k, []).append(idx)
        for k in w:
            self.last_w[k] = idx
            self.readers[k] = []
        if dma:
            q = eng
            kq = self.dcount[q]
            self.dcount[q] += 1
            sem = self.dsem[q][kq % self.NDS]
            val = 16 * (kq // self.NDS + 1)
            prev = (sem, val - 16) if val > 16 else None
            self.recent_dma[q].append(idx)
            self.recent_dma[q] = self.recent_dma[q][-self.NDS:]
        else:
            self.count[eng] += 1
            sem = self.sem[eng]
            val = self.count[eng]
            prev = None
        self.ops.append(dict(eng=eng, fn=fn, deps=deps, dma=dma, sem=sem, val=val, prev=prev))
        self.last_op[eng] = idx
        return idx

    def barrier(self):
        allops = set()
        for e in self.engs:
            if self.last_op[e] is not None:
                allops.add(self.last_op[e])
        for q in ("sp", "pool"):
            allops.update(self.recent_dma[q])
        dmaops = set()
        for q in ("sp", "pool"):
            dmaops.update(self.recent_dma[q])
        for e in self.engs:
            self.pending[e] |= (allops - dmaops) if e == "pe" else allops

    def emit(self):
        nc = self.nc
        per = {e: [] for e in self.engs}
        for i, op in enumerate(self.ops):
            per[op["eng"]].append(i)
        ops = self.ops

        def run(name, e):
            waited = {}
            for i in per[name]:
                op = ops[i]
                need = []
                for j in op["deps"]:
                    d = ops[j]
                    if d["eng"] == name and not d["dma"] and name in NO_SELF_SYNC:
                        continue
                    need.append((d["sem"], d["val"]))
                if op["prev"] is not None:
                    need.append(op["prev"])
                need.sort(key=lambda t: -t[1])
                todo = []
                for sem, val in need:
                    key = id(sem)
                    if waited.get(key, 0) >= val:
                        continue
                    waited[key] = val
                    todo.append((sem, val))
                attach = todo.pop() if (todo and ATTACH_WAIT) else None
                for sem, val in todo:
                    e.wait_ge(sem, val)
                inst = op["fn"](e)
                if attach is not None:
                    inst._wait_ge(attach[0], attach[1])
                inst.then_inc(op["sem"], 16 if op["dma"] else 1)
            if name in ("sp", "pool"):
                kq = self.dcount[name]
                for s_i in range(min(kq, self.NDS)):
                    n_on = (kq - s_i + self.NDS - 1) // self.NDS
                    e.wait_ge(self.dsem[name][s_i], 16 * n_on)

        with nc.Block() as block:
            @block.sync
            def _(e):
                run("sp", e)

            @block.scalar
            def _(e):
                run("act", e)

            @block.vector
            def _(e):
                run("dve", e)

            @block.tensor
            def _(e):
                run("pe", e)

            @block.gpsimd
            def _(e):
                run("pool", e)


def build_nc(stage=99, ntiles=NT, cut=99, nblk=96):
    nc = bass.Bass("TRN2", target_bir_lowering=False)
    dt = lambda name, shape, dty, kind: nc.dram_tensor(name, shape, dty, kind=kind).ap()
    x_d = dt("x", [S, D], F32, "ExternalInput")
    prm_d = dt("prm", [128, NPRM], F32, "ExternalInput")
    cst_d = dt("cst", [128, NCST], F32, "ExternalInput")
    bc_d = dt("bc", [128, NBC], F32, "ExternalInput")
    adaw_d = dt("ada_w", [D, 6 * D], F32, "ExternalInput")
    win_d = dt("w_in", [D, INW], F32, "ExternalInput")
    wout_d = dt("w_out", [D, D], F32, "ExternalInput")
    w2_d = dt("w2", [32, 512], F32, "ExternalInput")
    a2_d = dt("a2", [32, 512], F32, "ExternalInput")
    g2_d = dt("g2", [96, 512], F32, "ExternalInput")
    wsT_d = dt("wsT", [128, 8, 128], F32, "ExternalInput")
    wr_d = dt("wr", [D, 36], F32, "ExternalInput")
    fg_d = dt("fg", [128, D], F32, "ExternalInput")
    wg_d = dt("wg", [NE * 128, 4096], F32, "ExternalInput")
    wu_d = dt("wu", [NE * 128, 4096], F32, "ExternalInput")
    wd_d = dt("wd", [NE * 128, 4096], F32, "ExternalInput")
    NBLK = 96
    xn2_d = dt("xn2_d", [S, D], BF16, "Internal")
    xs_d = dt("xs_d", [NBLK * 128, D], BF16, "Internal")
    ys_d = dt("ys_d", [NBLK * 128, D], F32, "Internal")
    out_d = dt("out", [S, D], F32, "ExternalOutput")
    x1_d = dt("x1_d", [S, D], F32, "Internal")

    with ExitStack() as top:
        P = Prog(nc, top)
        P.cut = cut
        psum = [top.enter_context(nc.psum_tensor(f"ps{i}", [128, 512], F32)) for i in range(7)]
        psTt = top.enter_context(nc.psum_tensor("psT", [128, 512], F32))
        psT = [psTt[:, 0:256], psum[6][:, 0:256]]
        psTk = ["ps7", "ps6"]
        fst = None
        fmv = None
        pk = lambda i: f"ps{i}"

        def SB(ctx, name, shape, dty=F32):
            return ctx.enter_context(nc.sbuf_tensor(name, shape, dty))

        prm = SB(top, "prm_s", [128, NPRM])
        cst = SB(top, "cst_s", [128, NCST])
        bcs = SB(top, "bc_s", [128, NBC])
        mod = SB(top, "mod", [128, 48])
        wt = SB(top, "wt", [128, 32, 32])
        PABi = SB(top, "PABi", [128, 2, 32], I32)
        WAB = SB(top, "WAB", [128, 2, 32])
        IDXW = SB(top, "IDXW", [128, 4, 96], I32)
        P.add("sp", lambda e: e.dma_start(out=prm[:], in_=prm_d[:, :]), w=["prm"], dma=True)
        P.add("sp", lambda e: e.dma_start(out=cst[:], in_=cst_d[:, :]), w=["cst"], dma=True)
        P.add("sp", lambda e: e.dma_start(out=bcs[:], in_=bc_d[:, :]), w=["bc"], dma=True)

        def pcol(name, j=0, n=1):
            return prm[:, PC[name] + j:PC[name] + j + n]

        ident = cst[:, CC["ident"]:CC["ident"] + 128]
        istack = cst[:, CC["istack"]:CC["istack"] + 64]
        tril = cst[:, CC["tril"]:CC["tril"] + 128]
        bones = cst[:, CC["bones"]:CC["bones"] + 128]
        ones = cst[:, CC["ones"]:CC["ones"] + 128]
        m5 = cst[:, CC["m5"]:CC["m5"] + 320]

        with ExitStack() as c0:
            stg = [SB(c0, f"ada_stg{i}", [128, 6 * D]) for i in range(2)]
            for kc in range(8):
                sb = stg[kc % 2]
                P.add("sp", (lambda sb, kc: lambda e: e.dma_start(out=sb[:], in_=adaw_d[kc * 128:(kc + 1) * 128, :]))(sb, kc),
                      w=[f"ada_stg{kc % 2}"], dma=True)
                for oc in range(48):
                    P.add("pe", (lambda sb, kc, oc: lambda e: e.matmul(psum[0][:, oc:oc + 1], lhsT=sb[:, oc * 128:(oc + 1) * 128],
                                                                   rhs=prm[:, PC["cT"] + kc:PC["cT"] + kc + 1], start=True, stop=True))(sb, kc, oc),
                          r=[f"ada_stg{kc % 2}", "prm"], w=[pk(0)])
                if kc == 0:
                    P.add("dve", lambda e: e.tensor_tensor(out=mod[:], in0=psum[0][:, 0:48], in1=prm[:, PC["ada_b"]:PC["ada_b"] + 48], op=ALU.add),
                          r=[pk(0), "prm"], w=["mod"])
                else:
                    P.add("dve", lambda e: e.tensor_tensor(out=mod[:], in0=psum[0][:, 0:48], in1=mod[:], op=ALU.add),
                          r=[pk(0), "mod"], w=["mod"])
            P.barrier()
        sc = SB(top, "sc", [128, 32])
        P.add("dve", lambda e: e.scalar_tensor_tensor(out=sc[:, 0:8], in0=mod[:, 8:16], scalar=1.0, in1=prm[:, PC["n1g"]:PC["n1g"] + 8],
                                                      op0=ALU.add, op1=ALU.mult), r=["mod", "prm"], w=["sc"])
        P.add("dve", lambda e: e.scalar_tensor_tensor(out=sc[:, 8:16], in0=mod[:, 32:40], scalar=1.0, in1=prm[:, PC["n2g"]:PC["n2g"] + 8],
                                                      op0=ALU.add, op1=ALU.mult), r=["mod", "prm"], w=["sc"])
        gate_bc = SB(top, "gate_bc", [128, D])
        gl = SB(top, "gl", [128, 128])

        def make_gate(dst, dkey, gcol):
            for c in range(8):
                P.add("dve", (lambda c: lambda e: e.tensor_scalar(out=gl[:], in0=ones, scalar1=mod[:, gcol + c:gcol + c + 1], scalar2=None,
                                                                  op0=ALU.mult))(c), r=["mod", "cst"], w=["gl"])
                P.add("pe", (lambda c: lambda e: e.matmul(psum[1][:, (c % 4) * 128:(c % 4 + 1) * 128], lhsT=gl[:], rhs=ident, start=True, stop=True))(c),
                      r=["gl", "cst"], w=[pk(1)])
                P.add("act", (lambda c: lambda e: e.activation(out=dst[:, c * 128:(c + 1) * 128], in_=psum[1][:, (c % 4) * 128:(c % 4 + 1) * 128],
                                                               func=AF.Identity))(c), r=[pk(1)], w=[dkey])
        make_gate(gate_bc, "gate_bc", 16)

        with ExitStack() as ca:
            win = SB(ca, "win", [128, 8, INW], BF16)
            wout = SB(ca, "wout", [128, 8, D], BF16)
            identb = SB(ca, "identb", [128, 128], BF16)
            w2s = SB(ca, "w2s", [32, 512])
            a2s = SB(ca, "a2s", [32, 512])
            g2s = SB(ca, "g2s", [96, 512])
            wrs = SB(ca, "wrs", [128, 8, 36])
            wc = SB(ca, "wc", [128, 8, 128])
            logits = SB(ca, "logits", [128, 32, 36])
            Hst = [SB(ca, f"H{j}", [128, 64]) for j in range(4)]
            carry = SB(ca, "carry", [128, 16])
            P.add("pool", lambda e: e.tensor_copy(out=identb[:], in_=ident), r=["cst"], w=["identb"])
            P.add("sp", lambda e: e.dma_start(out=w2s[:], in_=w2_d[:, :]), w=["w2s"], dma=True)
            P.add("sp", lambda e: e.dma_start(out=a2s[:], in_=a2_d[:, :]), w=["a2s"], dma=True)
            P.add("sp", lambda e: e.dma_start(out=g2s[:], in_=g2_d[:, :]), w=["g2s"], dma=True)
            P.add("sp", lambda e: e.dma_start(out=wrs[:], in_=wr_d.rearrange("(c p) n -> p c n", p=128)), w=["wrs"], dma=True)
            P.add("sp", lambda e: e.dma_start(out=wc[:], in_=wsT_d[:, :, :]), w=["wc"], dma=True)
            for h in range(8):
                P.add("pool", (lambda h: lambda e: e.tensor_tensor(out=wc[:, h, :], in0=wc[:, h, :], in1=tril, op=ALU.mult))(h),
                      r=["wc", "cst"], w=["wc"])
            P.add("pool", lambda e: e.memset(carry[:], 0.0), w=["carry"])
            for _i in range(int(os.environ.get("KPAD", "0"))):
                P.add("pool", lambda e: e.memset(gl[:, 0:1], 0.0), w=["gl_dummy"])
            for j in range(4):
                P.add("pool", (lambda j: lambda e: e.memset(Hst[j][:], 0.0))(j), w=[f"H{j}"])
            with ExitStack() as cw:
                wstg = [SB(cw, f"wstg{i}", [128, INW]) for i in range(2)]
                for kc in range(8):
                    sb = wstg[kc % 2]
                    P.add("sp", (lambda sb, kc: lambda e: e.dma_start(out=sb[:], in_=win_d[kc * 128:(kc + 1) * 128, :]))(sb, kc),
                          w=[f"wstg{kc % 2}"], dma=True)
                    P.add("pool", (lambda sb, kc: lambda e: e.tensor_copy(out=win[:, kc, :], in_=sb[:]))(sb, kc),
                          r=[f"wstg{kc % 2}"], w=["win"])
                for kc in range(8):
                    sb = wstg[kc % 2]
                    P.add("sp", (lambda sb, kc: lambda e: e.dma_start(out=sb[:, 0:D], in_=wout_d[kc * 128:(kc + 1) * 128, :]))(sb, kc),
                          w=[f"wstg{kc % 2}"], dma=True)
                    P.add("pool", (lambda sb, kc: lambda e: e.tensor_copy(out=wout[:, kc, :], in_=sb[:, 0:D]))(sb, kc),
                          r=[f"wstg{kc % 2}"], w=["wout"])
                P.barrier()

            ct = ExitStack()
            xt = [SB(ct, "xt0", [128, NSUB, D])] * 2
            xn = SB(ct, "xn", [128, NSUB, D], BF16)
            hT = SB(ct, "hT", [128, 8, TT], BF16)
            st6 = SB(ct, "st6", [128, 4, 6])
            mv = SB(ct, "mv", [128, 8])
            u_s = SB(ct, "u_s", [128, 4, TT], BF16)
            vg = SB(ct, "vg", [128, 512])
            vn = vg
            zraw = [SB(ct, f"zraw{i}", [128, TT + 1]) for i in range(2)]
            rkv = SB(ct, "rkv", [128, 12, TT])
            lor = SB(ct, "lor", [128, 3, TT])
            LW, ASN, BSN, KM, BON, GG = range(6)
            pers = [SB(ct, f"pers{j}", [128, 6, TT]) for j in range(4)]
            AA, KX, RN, PRD = range(4)
            ptmp = SB(ct, "ptmp", [128, 4, TT])
            ybT = SB(ct, "ybT", [128, 4, TT])
            ycat = SB(ct, "ycat", [128, 8, TT], BF16)
            x1t = SB(ct, "x1t", [128, D])
            xn2 = x1t
            h2f = SB(ct, "h2f", [128, 8, 128])
            gtmp = h2f[:, 0:4, :]
            xnb = SB(ct, "xnb", [128, D], BF16)
            CI, CE, EI, EE, EN, AT, RT, BT, KT = range(9)
            sct = [SB(ct, f"sct{j}", [128, 9, 64]) for j in range(4)]
            mats = [SB(ct, f"mats{j}", [128, 320]) for j in range(4)]
            trs = [SB(ct, f"trs{j}", [128, 192]) for j in range(4)]
            wzb_l = [SB(ct, f"wzb{j}", [128, 256]) for j in range(4)]
            qq = [[SB(ct, f"qq{j}_{i}", [128, 64]) for i in range(2)] for j in range(4)]
            chn = [SB(ct, f"chn{j}", [128, 4, 64]) for j in range(4)]
            gst = [SB(ct, f"gst{j}", [128, 12]) for j in range(4)]

            def A(eng, fn, r=(), w=()):
                P.add(eng, fn, r=r, w=w)

            def norm_stats(src, srck, eps):
                for hf in range(2):
                    A("dve", (lambda hf: lambda e: e.bn_stats(out=st6[:, hf, :], in_=src(hf)))(hf), r=[srck], w=["st6"])
                A("dve", lambda e: e.bn_aggr(out=mv[:, 0:2], in_=st6[:, 0:2, :].rearrange("p a b -> p (a b)")), r=["st6"], w=["mv"])
                A("dve", lambda e: e.scalar_tensor_tensor(out=mv[:, 2:3], in0=mv[:, 0:1], scalar=mv[:, 0:1], in1=mv[:, 1:2], op0=ALU.mult, op1=ALU.add),
                  r=["mv"], w=["mv"])
                A("act", lambda e: e.activation(out=mv[:, 3:4], in_=mv[:, 2:3], func=AF.Sqrt, bias=float(eps)), r=["mv"], w=["mv"])
                A("dve", lambda e: e.reciprocal(out=mv[:, 3:4], in_=mv[:, 3:4]), r=["mv"], w=["mv"])

            for TI in range(min(NT, ntiles) if stage >= 1 else 0):
                t0 = TI * TT
                xb = xt[TI % 2]
                xk = "xt0"
                P.add("sp", (lambda xb, t0: lambda e: e.dma_start(out=xb[:], in_=x_d[t0:t0 + TT, :].rearrange("(s p) d -> p s d", p=128)))(xb, t0),
                      w=[xk], dma=True)
                P.section(1)
                for sub in range(NSUB):
                    norm_stats((lambda xb, sub: lambda hf: xb[:, sub, hf * 512:(hf + 1) * 512])(xb, sub), xk, 1e-6)
                    A("act", (lambda xb, sub: lambda e: e.activation(out=xn[:, sub, :], in_=xb[:, sub, :], func=AF.Identity, scale=mv[:, 3:4]))(xb, sub),
                      r=[xk, "mv"], w=["xn"])
                P.section(1.5)
                for kc in range(8):
                    pb = kc % 2
                    for sub in range(NSUB):
                        A("pe", (lambda kc, sub, pb: lambda e: e.matmul(psT[pb][:, sub * 128:(sub + 1) * 128], lhsT=xn[:, sub, kc * 128:(kc + 1) * 128],
                                                                       rhs=identb[:], start=True, stop=True))(kc, sub, pb), r=["xn", "identb"], w=[psTk[pb]])
                    if os.environ.get("DVE_EVAC"):
                      A("dve", (lambda kc, pb: lambda e: e.tensor_scalar(out=hT[:, kc, :], in0=psT[pb][:, 0:TT], scalar1=sc[:, kc:kc + 1], scalar2=mod[:, kc:kc + 1],
                                                                         op0=ALU.mult, op1=ALU.add))(kc, pb), r=[psTk[pb], "sc", "mod"], w=["hT"])
                    elif not os.environ.get("SKIP_EVAC"):
                      A("act", (lambda kc, pb: lambda e: e.activation(out=hT[:, kc, :], in_=psT[pb][:, 0:TT], func=AF.Identity,
                                                                    **({} if os.environ.get("NO_SB") else dict(scale=sc[:, kc:kc + 1], bias=mod[:, kc:kc + 1]))))(kc, pb),
                      r=[psTk[pb], "sc", "mod"], w=["hT"])

                P.section(2)
                pcnt = [0]

                def proj(c0, M):
                    pb = pcnt[0] % 2
                    pcnt[0] += 1
                    for kc in range(8):
                        A("pe", (lambda kc, pb: lambda e: e.matmul(psum[pb][0:M, 0:TT], lhsT=win[:, kc, c0:c0 + M], rhs=hT[:, kc, :],
                                                                  start=(kc == 0), stop=(kc == 7)))(kc, pb), r=["win", "hT"], w=[pk(pb)])
                    return pb

                for j in range(4):
                    pb = proj(j * 128, 128)
                    A("act", (lambda j, pb: lambda e: e.activation(out=u_s[:, j, :], in_=psum[pb][:, 0:TT], func=AF.Gelu_apprx_tanh))(j, pb),
                      r=[pk(pb)], w=["u_s"])
                specs = [(1024 + q * 128, 128, PC["mu_rkv"] + q) for q in range(12)] + \
                        [(2560, 32, PC["mu_xw"]), (2592, 32, PC["mu_xa"]), (2624, 96, PC["mu_xg"])]
                for q, (c0, M, mucol) in enumerate(specs):
                    pb = proj(c0, M)
                    zb = zraw[q % 2]
                    zk = f"zraw{q % 2}"
                    dst = rkv[0:M, q, :] if q < 12 else lor[0:M, q - 12, :]
                    dk = f"rkv{q}"
                    A("pool", (lambda zb, q, M: lambda e: e.tensor_copy(out=zb[0:M, 0:1], in_=carry[0:M, q:q + 1]))(zb, q, M), r=["carry"], w=[zk])
                    A("act", (lambda zb, pb, M: lambda e: e.activation(out=zb[0:M, 1:TT + 1], in_=psum[pb][0:M, 0:TT], func=AF.Identity))(zb, pb, M),
                      r=[pk(pb)], w=[zk])
                    A("pool", (lambda zb, q, M: lambda e: e.tensor_copy(out=carry[0:M, q:q + 1], in_=zb[0:M, TT:TT + 1]))(zb, q, M), r=[zk], w=["carry"])
                    A("dve", (lambda zb, dst, M: lambda e: e.tensor_tensor(out=dst, in0=zb[0:M, 0:TT], in1=zb[0:M, 1:TT + 1], op=ALU.subtract))(zb, dst, M),
                      r=[zk], w=[dk])
                    A("dve", (lambda zb, dst, M, mucol: lambda e: e.scalar_tensor_tensor(out=dst, in0=dst, scalar=prm[0:M, mucol:mucol + 1], in1=zb[0:M, 1:TT + 1],
                                                                                      op0=ALU.mult, op1=ALU.add))(zb, dst, M, mucol), r=[zk, dk, "prm"], w=[dk])
                A("act", lambda e: e.activation(out=lor[0:32, 0, :], in_=lor[0:32, 0, :], func=AF.Tanh), r=["rkv12"], w=["rkv12"])
                A("act", lambda e: e.activation(out=lor[0:96, 2, :], in_=lor[0:96, 2, :], func=AF.Sigmoid), r=["rkv14"], w=["rkv14"])

                P.section(3)
                for j in range(4):
                    jc = slice(j * 128, (j + 1) * 128)
                    pj = pers[j]
                    pjk = f"pers{j}"
                    rr = rkv[:, j, :]
                    kr = rkv[:, 4 + j, :]
                    vr = rkv[:, 8 + j, :]
                    A("pe", (lambda jc: lambda e: e.matmul(psum[0][:, 0:TT], lhsT=w2s[0:32, jc], rhs=lor[0:32, 0, :], start=True, stop=True))(jc),
                      r=["w2s", "rkv12"], w=[pk(0)])
                    A("act", (lambda pj, j: lambda e: e.activation(out=pj[:, LW, :], in_=psum[0][:, 0:TT], func=AF.Sigmoid, bias=pcol("w0", j)))(pj, j),
                      r=[pk(0), "prm"], w=[pjk + "LW"])
                    A("pool", (lambda pj: lambda e: e.tensor_scalar(out=pj[:, LW, :], in0=pj[:, LW, :], scalar1=NEG_HALF_E, scalar2=None, op0=ALU.mult))(pj),
                      r=[pjk + "LW"], w=[pjk + "LW"])
                    A("pe", (lambda jc: lambda e: e.matmul(psum[1][:, 0:TT], lhsT=a2s[0:32, jc], rhs=lor[0:32, 1, :], start=True, stop=True))(jc),
                      r=["a2s", "rkv13"], w=[pk(1)])
                    A("act", (lambda j: lambda e: e.activation(out=ptmp[:, AA, :], in_=psum[1][:, 0:TT], func=AF.Sigmoid, bias=pcol("a0", j)))(j),
                      r=[pk(1), "prm"], w=["pAA"])
                    A("pe", (lambda jc: lambda e: e.matmul(psum[0][:, 0:TT], lhsT=g2s[0:96, jc], rhs=lor[0:96, 2, :], start=True, stop=True))(jc),
                      r=["g2s", "rkv14"], w=[pk(0)])
                    A("act", (lambda pj: lambda e: e.activation(out=pj[:, GG, :], in_=psum[0][:, 0:TT], func=AF.Identity))(pj), r=[pk(0)], w=[pjk + "GG"])
                    A("dve", (lambda kr, j: lambda e: e.tensor_scalar(out=ptmp[:, KX, :], in0=kr, scalar1=pcol("k_k", j), scalar2=None, op0=ALU.mult))(kr, j),
                      r=[f"rkv{4 + j}", "prm"], w=["pKX"])
                    A("pool", lambda e: e.tensor_tensor(out=ptmp[:, RN, :], in0=ptmp[:, KX, :], in1=ptmp[:, KX, :], op=ALU.mult), r=["pKX"], w=["pRN"])
                    A("pe", lambda e: e.matmul(psum[1][:, 0:TT], lhsT=bones, rhs=ptmp[:, RN, :], start=True, stop=True), r=["cst", "pRN"], w=[pk(1)])
                    A("act", lambda e: e.activation(out=ptmp[:, RN, :], in_=psum[1][:, 0:TT], func=AF.Sqrt, bias=float(1e-24)), r=[pk(1)], w=["pRN"])
                    A("dve", lambda e: e.reciprocal(out=ptmp[:, RN, :], in_=ptmp[:, RN, :]), r=["pRN"], w=["pRN"])
                    A("dve", (lambda pj: lambda e: e.scalar_tensor_tensor(out=pj[:, ASN, :], in0=ptmp[:, KX, :], scalar=-1.0, in1=ptmp[:, RN, :],
                                                                          op0=ALU.mult, op1=ALU.mult))(pj), r=["pKX", "pRN"], w=[pjk + "ASN"])
                    A("dve", (lambda pj: lambda e: e.scalar_tensor_tensor(out=pj[:, BSN, :], in0=pj[:, ASN, :], scalar=-1.0, in1=ptmp[:, AA, :],
                                                                          op0=ALU.mult, op1=ALU.mult))(pj), r=[pjk + "ASN", "pAA"], w=[pjk + "BSN"])
                    A("dve", (lambda j: lambda e: e.tensor_scalar(out=ptmp[:, PRD, :], in0=ptmp[:, AA, :], scalar1=-1.0, scalar2=pcol("k_a", j),
                                                                  op0=ALU.add, op1=ALU.mult))(j), r=["pAA", "prm"], w=["pPRD"])
                    A("dve", (lambda pj, kr: lambda e: e.scalar_tensor_tensor(out=pj[:, KM, :], in0=ptmp[:, PRD, :], scalar=1.0, in1=kr,
                                                                              op0=ALU.add, op1=ALU.mult))(pj, kr), r=["pPRD", f"rkv{4 + j}"], w=[pjk + "KM"])
                    A("dve", (lambda pj, rr, j: lambda e: e.scalar_tensor_tensor(out=ptmp[:, PRD, :], in0=rr, scalar=pcol("r_k", j), in1=pj[:, KM, :],
                                                                                 op0=ALU.mult, op1=ALU.mult))(pj, rr, j), r=[f"rkv{j}", "prm", pjk + "KM"], w=["pPRD"])
                    A("pe", lambda e: e.matmul(psum[0][:, 0:TT], lhsT=bones, rhs=ptmp[:, PRD, :], start=True, stop=True), r=["cst", "pPRD"], w=[pk(0)])
                    A("dve", (lambda pj, vr: lambda e: e.tensor_tensor(out=pj[:, BON, :], in0=psum[0][:, 0:TT], in1=vr, op=ALU.mult))(pj, vr),
                      r=[pk(0), f"rkv{8 + j}"], w=[pjk + "BON"])

                P.section(4)
                def unit_stages(j, c):
                    col = slice(c * CH, (c + 1) * CH)
                    pj = pers[j]
                    pjk = f"pers{j}"
                    s = sct[j]
                    sk = f"sct{j}"
                    mt = mats[j]
                    mk = f"mats{j}"
                    tr = trs[j]
                    tk = f"trs{j}"
                    cn = chn[j]
                    ck = f"chn{j}"
                    H = Hst[j]
                    Hk = f"H{j}"
                    rr = rkv[:, j, col]
                    vr = rkv[:, 8 + j, col]
                    hp = [slice(0, 64), slice(64, 128)]
                    st = []

                    def s1():
                        A("dve", lambda e: e.tensor_tensor_scan(out=s[:, CI, :], data0=ones[:, 0:64], data1=pj[:, LW, col], initial=0.0, op0=ALU.mult, op1=ALU.add),
                          r=[pjk + "LW", "cst"], w=[sk + "ci"])
                        A("pool", lambda e: e.tensor_tensor(out=s[:, CE, :], in0=s[:, CI, :], in1=pj[:, LW, col], op=ALU.subtract),
                          r=[sk + "ci", pjk + "LW"], w=[sk + "ce"])
                    st.append(s1)

                    def s2():
                        A("act", lambda e: e.activation(out=s[:, EI, :], in_=s[:, CI, :], func=AF.Exp), r=[sk + "ci"], w=[sk + "ei"])
                        A("act", lambda e: e.activation(out=s[:, EE, :], in_=s[:, CE, :], func=AF.Exp), r=[sk + "ce"], w=[sk + "ee"])
                        A("act", lambda e: e.activation(out=s[:, EN, :], in_=s[:, CI, :], func=AF.Exp, scale=-1.0), r=[sk + "ci"], w=[sk + "en"])
                    st.append(s2)

                    def s3():
                        A("dve", lambda e: e.tensor_tensor(out=s[:, AT, :], in0=pj[:, ASN, col], in1=s[:, EE, :], op=ALU.mult), r=[pjk + "ASN", sk + "ee"], w=[sk + "at"])
                        A("pool", lambda e: e.tensor_tensor(out=s[:, RT, :], in0=rr, in1=s[:, EI, :], op=ALU.mult), r=[f"rkv{j}", sk + "ei"], w=[sk + "rt"])
                        A("dve", lambda e: e.tensor_tensor(out=s[:, BT, :], in0=pj[:, BSN, col], in1=s[:, EN, :], op=ALU.mult), r=[pjk + "BSN", sk + "en"], w=[sk + "bt"])
                        A("pool", lambda e: e.tensor_tensor(out=s[:, KT, :], in0=pj[:, KM, col], in1=s[:, EN, :], op=ALU.mult), r=[pjk + "KM", sk + "en"], w=[sk + "kt"])
                    st.append(s3)

                    def s4():
                        for p in hp:
                            A("pe", (lambda p: lambda e: e.matmul(psum[2][p, 0:128], lhsT=s[p, BT, :], rhs=s[p, AT:RT + 1, :].rearrange("p a b -> p (a b)"), start=True, stop=True))(p),
                              r=[sk + "bt", sk + "at", sk + "rt"], w=["ps2"])
                            A("pe", (lambda p: lambda e: e.matmul(psum[2][p, 128:256], lhsT=s[p, KT, :], rhs=s[p, AT:RT + 1, :].rearrange("p a b -> p (a b)"), start=True, stop=True))(p),
                              r=[sk + "kt", sk + "at", sk + "rt"], w=["ps2"])
                            A("pe", (lambda p: lambda e: e.matmul(psum[2][p, 256:320], lhsT=s[p, AT, :], rhs=s[p, BT, :], start=True, stop=True))(p),
                              r=[sk + "bt", sk + "at"], w=["ps2"])
                        A("dve", lambda e: e.tensor_tensor(out=mt[:], in0=psum[2][:, 0:320], in1=m5, op=ALU.mult), r=["ps2", "cst"], w=[mk])
                        for p in hp:
                            A("pe", (lambda p: lambda e: e.matmul(psum[3][p, 0:64], lhsT=rkv[p, 8 + j, col], rhs=istack[p, :], start=True, stop=True))(p),
                              r=[f"rkv{8 + j}", "cst"], w=["ps3"])
                            A("pe", (lambda p: lambda e: e.matmul(psum[3][p, 64:128], lhsT=s[p, BT, :], rhs=istack[p, :], start=True, stop=True))(p),
                              r=[sk + "bt", "cst"], w=["ps3"])
                            A("pe", (lambda p: lambda e: e.matmul(psum[3][p, 128:192], lhsT=s[p, KT, :], rhs=istack[p, :], start=True, stop=True))(p),
                              r=[sk + "kt", "cst"], w=["ps3"])
                        A("act", lambda e: e.activation(out=tr[:], in_=psum[3][:, 0:192], func=AF.Identity), r=["ps3"], w=[tk])
                        A("pool", lambda e: e.tensor_tensor(out=qq[j][0][:], in0=mt[:, 0:64], in1=istack, op=ALU.add), r=[mk, "cst"], w=[f"qq{j}_0"])
                    st.append(s4)

                    wzb = wzb_l[j]
                    wbk = f"wzb{j}"
                    bm3 = bones.rearrange("p (a b) -> p a b", a=2)

                    def s4b():
                        A("pool", lambda e: e.tensor_tensor(out=wzb[:, 0:128].rearrange("p (a b) -> p a b", a=2),
                                                            in0=mt[:, 0:64].rearrange("p (o t) -> p o t", o=1).to_broadcast([128, 2, 64]), in1=bm3, op=ALU.mult),
                          r=[mk, "cst"], w=[wbk])
                        A("pool", lambda e: e.tensor_tensor(out=wzb[:, 128:256].rearrange("p (a b) -> p a b", a=2),
                                                            in0=mt[:, 256:320].rearrange("p (o t) -> p o t", o=1).to_broadcast([128, 2, 64]), in1=bm3, op=ALU.mult),
                          r=[mk, "cst"], w=[wbk])
                    st.append(s4b)

                    for i in range(5):
                        def lv(i=i):
                            last = (i == 4)
                            qc = qq[j][i % 2]
                            qck = f"qq{j}_{i % 2}"
                            qn = qq[j][(i + 1) % 2]
                            qnk = f"qq{j}_{(i + 1) % 2}"
                            if not last:
                                A("pe", lambda e: e.matmul(psum[4][:, 0:128], lhsT=wzb[:, 128:256], rhs=wzb[:, 0:128], start=True, stop=True), r=[wbk], w=["ps4"])
                            A("pe", lambda e: e.matmul(psum[4][:, 128:256], lhsT=wzb[:, 0:128], rhs=wzb[:, 128:256], start=True, stop=True), r=[wbk], w=["ps4"])
                            if not last:
                                A("act", lambda e: e.activation(out=wzb[:, 0:256], in_=psum[4][:, 0:256], func=AF.Identity), r=["ps4"], w=[wbk])
                            else:
                                A("act", lambda e: e.activation(out=wzb[:, 128:256], in_=psum[4][:, 128:256], func=AF.Identity), r=["ps4"], w=[wbk])
                            A("pe", lambda e: e.matmul(psum[1][:, 128:192], lhsT=wzb[:, 128:256], rhs=qc[:, :], start=True, stop=True), r=[wbk, qck], w=["ps1"])
                            A("dve", lambda e: e.tensor_tensor(out=qn[:], in0=psum[1][:, 128:192], in1=qc[:], op=ALU.add), r=["ps1", qck], w=[qnk])
                        st.append(lv)

                    def s5():
                        cbank = [5, 6, 7, 0][j]
                        cps = psum[cbank] if cbank != 7 else psTt
                        cpk = f"ps{cbank}"
                        q5 = qq[j][1]
                        q5k = f"qq{j}_1"
                        A("dve", lambda e: e.tensor_scalar(out=cn[:, 3, :], in0=H[:], scalar1=s[:, EI, 63:64], scalar2=None, op0=ALU.mult),
                          r=[Hk, sk + "ei"], w=[ck + "hpc"])
                        for p in hp:
                            A("pe", (lambda p: lambda e: e.matmul(cps[p, 0:64], lhsT=s[p, AT, :], rhs=H[p, :], start=True, stop=False))(p), r=[sk + "at", Hk], w=[cpk])
                            A("pe", (lambda p: lambda e: e.matmul(cps[p, 0:64], lhsT=mt[p, 128:192], rhs=tr[p, 0:64], start=False, stop=True))(p), r=[mk, tk], w=[cpk])
                        A("act", lambda e: e.activation(out=cn[:, 0, :], in_=cps[:, 0:64], func=AF.Identity), r=[cpk], w=[ck + "x"])
                        for p in hp:
                            A("pe", (lambda p: lambda e: e.matmul(cps[p, 64:128], lhsT=q5[p, :], rhs=cn[p, 0, :], start=True, stop=True))(p), r=[q5k, ck + "x"], w=[cpk])
                        A("act", lambda e: e.activation(out=cn[:, 1, :], in_=cps[:, 64:128], func=AF.Identity), r=[cpk], w=[ck + "u"])
                        for p in hp:
                            A("pe", (lambda p: lambda e: e.matmul(cps[p, 192:256], lhsT=tr[p, 64:128], rhs=cn[p, 1, :], start=True, stop=False))(p), r=[tk, ck + "u"], w=[cpk])
                            A("pe", (lambda p: lambda e: e.matmul(cps[p, 192:256], lhsT=tr[p, 128:192], rhs=tr[p, 0:64], start=False, stop=True))(p), r=[tk], w=[cpk])
                        for p in hp:
                            A("pe", (lambda p: lambda e: e.matmul(cps[p, 128:192], lhsT=s[p, RT, :], rhs=H[p, :], start=True, stop=False))(p), r=[sk + "rt", Hk], w=[cpk])
                            A("pe", (lambda p: lambda e: e.matmul(cps[p, 128:192], lhsT=mt[p, 64:128], rhs=cn[p, 1, :], start=False, stop=False))(p), r=[mk, ck + "u"], w=[cpk])
                            A("pe", (lambda p: lambda e: e.matmul(cps[p, 128:192], lhsT=mt[p, 192:256], rhs=tr[p, 0:64], start=False, stop=True))(p), r=[mk, tk], w=[cpk])
                        A("dve", lambda e: e.scalar_tensor_tensor(out=H[:], in0=cps[:, 192:256], scalar=s[:, EI, 63:64], in1=cn[:, 3, :], op0=ALU.mult, op1=ALU.add),
                          r=[cpk, sk + "ei", ck + "hpc"], w=[Hk])
                        g = gst[j]
                        gk = f"gst{j}"
                        A("dve", lambda e: e.bn_stats(out=g[:, 0:6], in_=cps[:, 128:192]), r=[cpk], w=[gk])
                        A("dve", lambda e: e.bn_aggr(out=g[:, 6:8], in_=g[:, 0:6]), r=[gk], w=[gk])
                        A("act", lambda e: e.activation(out=g[:, 8:9], in_=g[:, 7:8], func=AF.Sqrt, bias=float(64e-5)), r=[gk], w=[gk])
                        A("dve", lambda e: e.reciprocal(out=g[:, 8:9], in_=g[:, 8:9]), r=[gk], w=[gk])
                        A("dve", lambda e: e.tensor_scalar(out=cn[:, 2, :], in0=cps[:, 128:192], scalar1=g[:, 6:7], scalar2=g[:, 8:9], op0=ALU.subtract, op1=ALU.mult),
                          r=[cpk, gk], w=[ck + "yn"])
                        for p in hp:
                            A("pe", (lambda p: lambda e: e.matmul(psum[3][p, 192:256], lhsT=cn[p, 2, :], rhs=istack[p, :], start=True, stop=True))(p), r=[ck + "yn", "cst"], w=["ps3"])
                        A("act", lambda e: e.activation(out=ybT[:, j, col], in_=psum[3][:, 192:256], func=AF.Identity, scale=pcol("gn_w", j), bias=pcol("gn_b", j)),
                          r=["ps3", "prm"], w=[f"ybT{j}"])
                    st.append(s5)
                    return st

                for c in range(NCH):
                    stl = [unit_stages(j, c) for j in range(4)]
                    for si in range(len(stl[0])):
                        for j in range(4):
                            stl[j][si]()
                P.section(5)
                for j in range(4):
                    pj = pers[j]
                    pjk = f"pers{j}"
                    A("pool", (lambda pj, j: lambda e: e.tensor_tensor(out=ybT[:, j, :], in0=ybT[:, j, :], in1=pj[:, BON, :], op=ALU.add))(pj, j),
                      r=[f"ybT{j}", pjk + "BON"], w=[f"ybT{j}"])
                    A("pool", (lambda pj, j: lambda e: e.tensor_tensor(out=ycat[:, 4 + j, :], in0=ybT[:, j, :], in1=pj[:, GG, :], op=ALU.mult))(pj, j),
                      r=[f"ybT{j}", pjk + "GG"], w=["ycat"])

                P.section(6)
                for sub in range(NSUB):
                    tc_ = slice(sub * 128, (sub + 1) * 128)
                    for kc in range(8):
                        A("pe", (lambda kc, tc_: lambda e: e.matmul(psum[0][:, 0:512], lhsT=hT[:, kc, tc_], rhs=win[:, kc, 512:1024], start=(kc == 0), stop=(kc == 7)))(kc, tc_),
                          r=["hT", "win"], w=[pk(0)])
                    A("act", lambda e: e.activation(out=vg[:], in_=psum[0][:, 0:512], func=AF.Gelu_apprx_tanh), r=[pk(0)], w=["vg"])
                    A("dve", lambda e: e.bn_stats(out=st6[:, 2, :], in_=vg[:]), r=["vg"], w=["st6b"])
                    A("dve", lambda e: e.bn_aggr(out=mv[:, 4:6], in_=st6[:, 2, :]), r=["st6b"], w=["mvb"])
                    A("act", lambda e: e.activation(out=mv[:, 6:7], in_=mv[:, 5:6], func=AF.Sqrt, bias=float(1e-5)), r=["mvb"], w=["mvb"])
                    A("dve", lambda e: e.reciprocal(out=mv[:, 6:7], in_=mv[:, 6:7]), r=["mvb"], w=["mvb"])
                    A("dve", lambda e: e.tensor_scalar(out=vn[:], in0=vg[:], scalar1=mv[:, 4:5], scalar2=mv[:, 6:7], op0=ALU.subtract, op1=ALU.mult), r=["vg", "mvb"], w=["vg"])
                    A("pool", lambda e: e.tensor_tensor(out=vn[:], in0=vn[:], in1=bcs[:, BC["lnw"]:BC["lnw"] + 512], op=ALU.mult), r=["vg", "bc"], w=["vg"])
                    A("pool", lambda e: e.tensor_tensor(out=vn[:], in0=vn[:], in1=bcs[:, BC["lnb"]:BC["lnb"] + 512], op=ALU.add), r=["vg", "bc"], w=["vg"])
                    for h in range(8):
                        A("pe", (lambda h: lambda e: e.matmul(psum[1][(h % 2) * 64:(h % 2) * 64 + 64, (h // 2) * 128:(h // 2 + 1) * 128], lhsT=vn[:, h * 64:(h + 1) * 64],
                                                              rhs=wc[:, h, :], start=True, stop=True))(h), r=["vg", "wc"], w=[pk(1)])
                    A("dve", lambda e: e.tensor_tensor(out=gtmp.rearrange("p a b -> p (a b)"), in0=psum[1][:, 0:512], in1=bcs[:, BC["bsT"]:BC["bsT"] + 512], op=ALU.add),
                      r=[pk(1), "bc"], w=["h2f"])
                    A("dve", (lambda tc_: lambda e: e.tensor_tensor(out=ycat[:, 0:4, tc_], in0=gtmp, in1=u_s[:, :, tc_], op=ALU.mult))(tc_), r=["h2f", "u_s"], w=["ycat"])

                P.section(7)
                for sub in range(NSUB):
                    tc_ = slice(sub * 128, (sub + 1) * 128)
                    ti = TI * NSUB + sub
                    tok0 = t0 + sub * 128
                    for hf in range(2):
                        for cc in range(8):
                            A("pe", (lambda cc, hf, tc_: lambda e: e.matmul(psum[6][:, 0:512], lhsT=ycat[:, cc, tc_], rhs=wout[:, cc, hf * 512:(hf + 1) * 512],
                                                                           start=(cc == 0), stop=(cc == 7)))(cc, hf, tc_), r=["ycat", "wout"], w=[pk(6)])
                        A("dve", (lambda hf: lambda e: e.tensor_tensor(out=x1t[:, hf * 512:(hf + 1) * 512], in0=psum[6][:, 0:512], in1=gate_bc[:, hf * 512:(hf + 1) * 512],
                                                                      op=ALU.mult))(hf), r=[pk(6), "gate_bc"], w=["x1t"])
                        A("pool", (lambda hf, xb, sub: lambda e: e.tensor_tensor(out=x1t[:, hf * 512:(hf + 1) * 512], in0=x1t[:, hf * 512:(hf + 1) * 512],
                                                                                in1=xb[:, sub, hf * 512:(hf + 1) * 512], op=ALU.add))(hf, xb, sub), r=["x1t", xk], w=["x1t"])
                    P.add("sp", (lambda tok0: lambda e: e.dma_start(out=x1_d[tok0:tok0 + 128, :], in_=x1t[:]))(tok0), r=["x1t"], w=["x1_d"], dma=True)
                    norm_stats(lambda hf: x1t[:, hf * 512:(hf + 1) * 512], "x1t", 1e-6)
                    A("act", lambda e: e.activation(out=xn2[:], in_=x1t[:], func=AF.Identity, scale=mv[:, 3:4]), r=["x1t", "mv"], w=["x1t"])
                    for kc in range(8):
                        pb = kc // 4
                        A("pe", (lambda kc, pb: lambda e: e.matmul(psum[pb][:, (kc % 4) * 128:(kc % 4 + 1) * 128], lhsT=xn2[:, kc * 128:(kc + 1) * 128], rhs=ident, start=True, stop=True))(kc, pb),
                          r=["x1t", "cst"], w=[pk(pb)])
                    for kc in range(8):
                        pb = kc // 4
                        A("act", (lambda kc, pb: lambda e: e.activation(out=h2f[:, kc, :], in_=psum[pb][:, (kc % 4) * 128:(kc % 4 + 1) * 128], func=AF.Identity,
                                                                        scale=sc[:, 8 + kc:9 + kc], bias=mod[:, 24 + kc:25 + kc]))(kc, pb), r=[pk(pb), "sc", "mod"], w=["h2f"])
                    A("pool", lambda e: e.tensor_copy(out=xnb[:], in_=xn2[:]), r=["x1t"], w=["xnb"])
                    P.add("sp", (lambda tok0: lambda e: e.dma_start(out=xn2_d[tok0:tok0 + 128, :], in_=xnb[:]))(tok0),
                          r=["xnb"], w=["xn2_d"], dma=True)
                    for kc in range(8):
                        A("pe", (lambda kc: lambda e: e.matmul(psum[6][:, 0:36], lhsT=h2f[:, kc, :], rhs=wrs[:, kc, :], start=(kc == 0), stop=(kc == 7)))(kc),
                          r=["h2f", "wrs"], w=[pk(6)])
                    A("dve", (lambda ti: lambda e: e.tensor_tensor(out=logits[:, ti, :], in0=psum[6][:, 0:36], in1=bcs[:, BC["rb"]:BC["rb"] + 36], op=ALU.add))(ti),
                      r=[pk(6), "bc"], w=["logits"])

            P.muted = False
            P.barrier()
            ct.close()
            if stage >= 2:
                rt = SB(ca, "rt", [128, 32, 48])
                sel = SB(ca, "sel", [128, 32, 8])
                sel2 = SB(ca, "sel2", [128, 32, 8])
                ohg = SB(ca, "ohg", [128, 32, 4])
                tmp48 = SB(ca, "tmp48", [128, 32, 4, 8])
                lg = logits[:, :, 0:4]
                le = logits[:, :, 4:36].rearrange("p t (g e) -> p t g e", g=4)
                MG, SG, M1, M2, P1, W1, W2 = range(7)
                r1 = lambda i: rt[:, :, i:i + 1]
                R = lambda fn, r, w: A("dve", fn, r=r, w=w)
                R(lambda e: e.tensor_reduce(out=rt[:, :, MG], in_=lg, axis=AX.X, op=ALU.max), ["logits"], ["rt"])
                R(lambda e: e.tensor_tensor(out=ohg[:], in0=lg, in1=r1(MG).to_broadcast([128, 32, 4]), op=ALU.is_equal), ["logits", "rt"], ["ohg"])
                R(lambda e: e.tensor_tensor(out=rt[:, :, 8:12], in0=lg, in1=r1(MG).to_broadcast([128, 32, 4]), op=ALU.subtract), ["logits", "rt"], ["rt"])
                A("act", lambda e: e.activation(out=rt[:, :, 8:12], in_=rt[:, :, 8:12], func=AF.Exp), r=["rt"], w=["rt"])
                R(lambda e: e.tensor_reduce(out=rt[:, :, SG], in_=rt[:, :, 8:12], axis=AX.X, op=ALU.add), ["rt"], ["rt"])
                R(lambda e: e.reciprocal(out=rt[:, :, SG], in_=rt[:, :, SG]), ["rt"], ["rt"])
                R(lambda e: e.tensor_tensor(out=tmp48[:], in0=le, in1=ohg[:].rearrange("p t (g o) -> p t g o", o=1).to_broadcast([128, 32, 4, 8]), op=ALU.mult),
                  ["logits", "ohg"], ["tmp48"])
                R(lambda e: e.tensor_reduce(out=sel[:], in_=tmp48[:].rearrange("p t g e -> p t e g"), axis=AX.X, op=ALU.add), ["tmp48"], ["sel"])
                R(lambda e: e.tensor_reduce(out=rt[:, :, M1], in_=sel[:], axis=AX.X, op=ALU.max), ["sel"], ["rt"])
                R(lambda e: e.tensor_tensor(out=sel2[:], in0=sel[:], in1=r1(M1).to_broadcast([128, 32, 8]), op=ALU.is_equal), ["sel", "rt"], ["sel2"])
                R(lambda e: e.scalar_tensor_tensor(out=tmp48[:, :, 0, :], in0=sel2[:], scalar=-1e30, in1=sel[:], op0=ALU.mult, op1=ALU.add), ["sel", "sel2"], ["tmp48"])
                R(lambda e: e.tensor_reduce(out=rt[:, :, M2], in_=tmp48[:, :, 0, :], axis=AX.X, op=ALU.max), ["tmp48"], ["rt"])
                R(lambda e: e.tensor_tensor(out=tmp48[:, :, 1, :], in0=tmp48[:, :, 0, :], in1=r1(M2).to_broadcast([128, 32, 8]), op=ALU.is_equal), ["tmp48", "rt"], ["tmp48"])
                R(lambda e: e.tensor_tensor(out=rt[:, :, P1], in0=rt[:, :, M2], in1=rt[:, :, M1], op=ALU.subtract), ["rt"], ["rt"])
                A("act", lambda e: e.activation(out=rt[:, :, P1], in_=rt[:, :, P1], func=AF.Exp), r=["rt"], w=["rt"])
                R(lambda e: e.tensor_scalar(out=rt[:, :, P1], in0=rt[:, :, P1], scalar1=1.0, scalar2=None, op0=ALU.add), ["rt"], ["rt"])
                R(lambda e: e.reciprocal(out=rt[:, :, P1], in_=rt[:, :, P1]), ["rt"], ["rt"])
                R(lambda e: e.tensor_tensor(out=rt[:, :, W1], in0=rt[:, :, P1], in1=rt[:, :, SG], op=ALU.mult), ["rt"], ["rt"])
                R(lambda e: e.tensor_tensor(out=rt[:, :, W2], in0=rt[:, :, SG], in1=rt[:, :, W1], op=ALU.subtract), ["rt"], ["rt"])
                R(lambda e: e.tensor_tensor(out=sel[:], in0=sel2[:], in1=r1(W1).to_broadcast([128, 32, 8]), op=ALU.mult), ["sel2", "rt"], ["sel"])
                R(lambda e: e.tensor_tensor(out=sel2[:], in0=tmp48[:, :, 1, :], in1=r1(W2).to_broadcast([128, 32, 8]), op=ALU.mult), ["tmp48", "rt"], ["sel2"])
                R(lambda e: e.tensor_tensor(out=sel[:], in0=sel[:], in1=sel2[:], op=ALU.add), ["sel", "sel2"], ["sel"])
                for g in range(4):
                    R((lambda g: lambda e: e.tensor_tensor(out=wt[:, :, g * 8:(g + 1) * 8], in0=sel[:], in1=ohg[:, :, g:g + 1].to_broadcast([128, 32, 8]), op=ALU.mult))(g),
                      ["sel", "ohg"], ["wt"])
            if stage >= 2:
                Mf = SB(ca, "Mf", [128, 1024])
                Mb = SB(ca, "Mb", [128, 1024], BF16)
                Lsb = SB(ca, "Lsb", [128, 128], BF16)
                onesb = SB(ca, "onesb", [128, 128], BF16)
                PRE = SB(ca, "PRE", [128, 1024])
                CNTs = SB(ca, "CNTs", [128, 1024])
                CA_ = SB(ca, "CA", [128, 1024])
                CB_ = SB(ca, "CB", [128, 1024])
                sm = SB(ca, "sm", [128, 8, 32])
                cmpb = SB(ca, "cmpb", [128, 96, 32])
                bev = SB(ca, "bev", [128, 6, 96])
                pabf = SB(ca, "pabf", [128, 2, 32])
                v3 = lambda t: t[:].rearrange("p (a b) -> p a b", a=32)
                wtf = wt[:].rearrange("p t e -> p (t e)")
                R(lambda e: e.tensor_single_scalar(out=Mf[:], in_=wtf, scalar=0.0, op=ALU.is_gt), ["wt"], ["Mf"])
                A("pool", lambda e: e.tensor_copy(out=Mb[:], in_=Mf[:]), r=["Mf"], w=["Mb"])
                A("pool", lambda e: e.tensor_tensor(out=Lsb[:], in0=tril, in1=ident, op=ALU.subtract), r=["cst"], w=["Lsb"])
                A("pool", lambda e: e.tensor_copy(out=onesb[:], in_=ones), r=["cst"], w=["onesb"])
                for h in range(2):
                    A("pe", (lambda h: lambda e: e.matmul(psum[h][:, 0:512], lhsT=Lsb[:], rhs=Mb[:, h * 512:(h + 1) * 512], start=True, stop=True))(h),
                      r=["Lsb", "Mb"], w=[pk(h)])
                    A("pe", (lambda h: lambda e: e.matmul(psum[2 + h][:, 0:512], lhsT=onesb[:], rhs=Mb[:, h * 512:(h + 1) * 512], start=True, stop=True))(h),
                      r=["onesb", "Mb"], w=[pk(2 + h)])
                    A("act", (lambda h: lambda e: e.activation(out=PRE[:, h * 512:(h + 1) * 512], in_=psum[h][:, 0:512], func=AF.Identity))(h), r=[pk(h)], w=["PRE"])
                    A("act", (lambda h: lambda e: e.activation(out=CNTs[:, h * 512:(h + 1) * 512], in_=psum[2 + h][:, 0:512], func=AF.Identity))(h), r=[pk(2 + h)], w=["CNTs"])
                src, srck, dst, dstk = CNTs, "CNTs", CA_, "CA"
                for dd in (1, 2, 4, 8, 16):
                    w_ = dd * 32
                    R((lambda src, dst, w_: lambda e: e.tensor_copy(out=dst[:, 0:w_], in_=src[:, 0:w_]))(src, dst, w_), [srck], [dstk])
                    R((lambda src, dst, w_: lambda e: e.tensor_tensor(out=dst[:, w_:1024], in0=src[:, w_:1024], in1=src[:, 0:1024 - w_], op=ALU.add))(src, dst, w_), [srck], [dstk])
                    src, srck = dst, dstk
                    dst, dstk = (CB_, "CB") if dst is CA_ else (CA_, "CA")
                R(lambda e: e.tensor_tensor(out=CB_[:], in0=CA_[:], in1=CNTs[:], op=ALU.subtract), ["CA", "CNTs"], ["CB"])
                R(lambda e: e.tensor_copy(out=sm[:, 0, :], in_=CA_[:, 31 * 32:32 * 32]), ["CA"], ["sm"])
                R(lambda e: e.tensor_tensor(out=cmpb[:, 0:32, :], in0=sm[:, 0, :].rearrange("p (e o) -> p e o", o=1).to_broadcast([128, 32, 32]),
                                            in1=cst[:, CC["thr"]:CC["thr"] + 32].rearrange("p (o k) -> p o k", o=1).to_broadcast([128, 32, 32]), op=ALU.is_gt),
                  ["sm", "cst"], ["cmpb"])
                R(lambda e: e.tensor_reduce(out=sm[:, 1, :], in_=cmpb[:, 0:32, :], axis=AX.X, op=ALU.add), ["cmpb"], ["sm"])
                R(lambda e: e.tensor_tensor_scan(out=sm[:, 2, :], data0=ones[:, 0:32], data1=sm[:, 1, :], initial=0.0, op0=ALU.mult, op1=ALU.add), ["sm", "cst"], ["sm"])
                R(lambda e: e.tensor_tensor(out=sm[:, 3, :], in0=sm[:, 2, :], in1=sm[:, 1, :], op=ALU.subtract), ["sm"], ["sm"])
                R(lambda e: e.tensor_single_scalar(out=sm[:, 4, :], in_=sm[:, 3, :], scalar=128.0, op=ALU.mult), ["sm"], ["sm"])
                R(lambda e: e.tensor_tensor(out=v3(CB_), in0=v3(CB_), in1=sm[:, 4, :].rearrange("p (o e) -> p o e", o=1).to_broadcast([128, 32, 32]), op=ALU.add),
                  ["CB", "sm"], ["CB"])
                R(lambda e: e.tensor_tensor(out=PRE[:], in0=PRE[:], in1=CB_[:], op=ALU.add), ["PRE", "CB"], ["PRE"])
                R(lambda e: e.tensor_tensor(out=CA_[:], in0=PRE[:], in1=Mf[:], op=ALU.mult), ["PRE", "Mf", "sm"], ["CA"])
                R(lambda e: e.tensor_scalar(out=CB_[:], in0=Mf[:], scalar1=-1e9, scalar2=1e9, op0=ALU.mult, op1=ALU.add), ["Mf", "PRE"], ["CB"])
                R(lambda e: e.tensor_tensor(out=CB_[:], in0=CB_[:], in1=CA_[:], op=ALU.add), ["CB", "CA"], ["CB"])
                R(lambda e: e.tensor_reduce(out=pabf[:, 0, :], in_=v3(CB_), axis=AX.X, op=ALU.min), ["CB"], ["pabf"])
                R(lambda e: e.tensor_reduce(out=pabf[:, 1, :], in_=v3(CA_), axis=AX.X, op=ALU.max), ["CA"], ["pabf"])
                R(lambda e: e.tensor_tensor(out=v3(CB_), in0=v3(CB_), in1=pabf[:, 0, :].rearrange("p (t o) -> p t o", o=1).to_broadcast([128, 32, 32]), op=ALU.is_equal),
                  ["CB", "pabf"], ["CB"])
                R(lambda e: e.tensor_tensor(out=CB_[:], in0=CB_[:], in1=wtf, op=ALU.mult), ["CB", "wt"], ["CB"])
                R(lambda e: e.tensor_reduce(out=WAB[:, 0, :], in_=v3(CB_), axis=AX.X, op=ALU.add), ["CB"], ["WAB"])
                R(lambda e: e.tensor_tensor(out=WAB[:, 1, :], in0=rt[:, :, SG], in1=WAB[:, 0, :], op=ALU.subtract), ["rt", "WAB"], ["WAB"])
                R(lambda e: e.tensor_copy(out=PABi[:], in_=pabf[:]), ["pabf"], ["PABi"])
                R(lambda e: e.tensor_tensor(out=cmpb[:], in0=sm[:, 2, :].rearrange("p (o e) -> p o e", o=1).to_broadcast([128, 96, 32]),
                                            in1=cst[:, CC["iotab"]:CC["iotab"] + 96].rearrange("p (b o) -> p b o", o=1).to_broadcast([128, 96, 32]), op=ALU.is_le),
                  ["sm", "cst", "cmpb"], ["cmpb"])
                R(lambda e: e.tensor_reduce(out=bev[:, 0, :], in_=cmpb[:], axis=AX.X, op=ALU.add), ["cmpb"], ["bev"])
                R(lambda e: e.memset(bev[:, 1, 0:1], 1.0), [], ["bev"])
                R(lambda e: e.tensor_tensor(out=bev[:, 1, 1:96], in0=bev[:, 0, 1:96], in1=bev[:, 0, 0:95], op=ALU.not_equal), ["bev"], ["bev"])
                R(lambda e: e.tensor_scalar(out=bev[:, 2, :], in0=bev[:, 1, :], scalar1=-1e6, scalar2=1e6, op0=ALU.mult, op1=ALU.add), ["bev"], ["bev"])
                R(lambda e: e.scalar_tensor_tensor(out=bev[:, 2, :], in0=bev[:, 0, :], scalar=128.0, in1=bev[:, 2, :], op0=ALU.mult, op1=ALU.add), ["bev"], ["bev"])
                R(lambda e: e.tensor_scalar(out=bev[:, 2, :], in0=bev[:, 2, :], scalar1=cst[:, CC["pidx"]:CC["pidx"] + 1], scalar2=None, op0=ALU.add), ["bev", "cst"], ["bev"])
                R(lambda e: e.tensor_copy(out=IDXW[:, 0:1, :], in_=bev[:, 2:3, :]), ["bev"], ["IDXW"])
            P.barrier()

        if stage >= 3:
            with ExitStack() as cb:
                IOA = bass.IndirectOffsetOnAxis
                NROW = NE * 128
                _bc = {}

                def bc_reg(e):
                    if "r" not in _bc:
                        _bc["r"] = e.to_reg(NROW - 1)
                    return _bc["r"]
                Wg = SB(cb, "Wg", [128, 8, EH])
                Wu = SB(cb, "Wu", [128, 8, EH])
                Wd = SB(cb, "Wd", [128, 4, D])
                idb2 = SB(cb, "idb2", [128, 128], BF16)
                xload = [SB(cb, f"xl{i}", [128, D], BF16) for i in range(2)]
                xsb = [SB(cb, f"xsb{i}", [128, D], BF16) for i in range(2)]
                XgT_l = [SB(cb, f"XgT{i}", [128, 8, 128], BF16) for i in range(2)]
                Wgb = SB(cb, "Wgb", [128, 8, EH], BF16)
                Wub = SB(cb, "Wub", [128, 8, EH], BF16)
                Wdb = SB(cb, "Wdb", [128, 4, D], BF16)
                sg_l = [SB(cb, f"sg{i}", [128, 512]) for i in range(2)]
                actb_l = [SB(cb, f"actb{i}", [128, 512], BF16) for i in range(2)]
                actT_l = [SB(cb, f"actT{i}", [128, 4, 128], BF16) for i in range(2)]
                yblk = [SB(cb, f"yblk{i}", [128, D]) for i in range(2)]
                fgt = SB(cb, "fgt", [128, D])
                fgs = SB(cb, "fgs", [128, D])
                g2bc = SB(cb, "g2bc", [128, D])
                yA = SB(cb, "yA", [128, D])
                yB = SB(cb, "yB", [128, D])
                fst = SB(cb, "fst", [128, 2, 6])
                fmv = SB(cb, "fmv", [128, 4])
                P.add("sp", lambda e: e.dma_start(out=fgs[:], in_=fg_d[:, :]), w=["fgs"], dma=True)
                P.add("dve", lambda e: e.tensor_copy(out=idb2[:], in_=ident), r=["cst"], w=["idb2"])
                make_gate(g2bc, "g2bc", 40)
                zt = SB(cb, "zt", [128, D], BF16)
                P.add("pool", lambda e: e.memset(zt[:], 0.0), w=["zt"])
                xz_keys = []
                for b in range(NBLK):
                    P.add("sp", (lambda b: lambda e: e.dma_start(out=xs_d[b * 128:(b + 1) * 128, :], in_=zt[:]))(b), r=["zt"], w=[f"xz{b}"], dma=True)
                    xz_keys.append(f"xz{b}")
                sc_keys = []
                for i in range(32):
                    xl = xload[i % 2]
                    xlk = f"xl{i % 2}"
                    P.add("sp", (lambda xl, i: lambda e: e.dma_start(out=xl[:], in_=xn2_d[i * 128:(i + 1) * 128, :]))(xl, i), r=["xn2_d"], w=[xlk], dma=True)
                    for k in range(2):
                        key = f"xsc{i}_{k}"
                        P.add("pool", (lambda xl, i, k: lambda e: e.indirect_dma_start(out=xs_d[:, :], out_offset=IOA(ap=PABi[:, k, i:i + 1], axis=0),
                                                                                       in_=xl[:], in_offset=None))(xl, i, k),
                              r=[xlk, "PABi"] + xz_keys, w=[key], dma=True)
                        sc_keys.append(key)
                ys_keys = []
                def do_block(b):
                    for c2 in range(1):
                        for (wsb, wk, wsrc, nch) in ((Wg, "Wg", wg_d, 8), (Wu, "Wu", wu_d, 8), (Wd, "Wd", wd_d, 4)):
                            P.add("pool", (lambda wsb, wsrc, nch, c2, b: lambda e: e.indirect_dma_start(
                                out=wsb[:].rearrange("p a b -> p (a b)"), out_offset=None, in_=wsrc[:, :],
                                in_offset=IOA(ap=IDXW[:, c2, b:b + 1], axis=0), bounds_check=bc_reg(e), oob_is_err=False))(wsb, wsrc, nch, c2, b),
                                r=["IDXW", wk], w=[wk], dma=True)
                    P.add("act", lambda e: e.activation(out=Wgb[:].rearrange("p a b -> p (a b)"), in_=Wg[:].rearrange("p a b -> p (a b)"), func=AF.Identity),
                          r=["Wg"], w=["Wgb"])
                    P.add("dve", lambda e: e.tensor_copy(out=Wub[:].rearrange("p a b -> p (a b)"), in_=Wu[:].rearrange("p a b -> p (a b)")), r=["Wu"], w=["Wub"])
                    P.add("pool", lambda e: e.tensor_copy(out=Wdb[:].rearrange("p a b -> p (a b)"), in_=Wd[:].rearrange("p a b -> p (a b)")), r=["Wd"], w=["Wdb"])
                    XgT = XgT_l[b % 2]
                    xgk = f"XgT{b % 2}"
                    sg = sg_l[b % 2]
                    sgk = f"sg{b % 2}"
                    actb = actb_l[b % 2]
                    abk = f"actb{b % 2}"
                    actT = actT_l[b % 2]
                    atk = f"actT{b % 2}"
                    xb_ = xsb[b % 2]
                    xbk = f"xsb{b % 2}"
                    P.add("sp", (lambda xb_, b: lambda e: e.dma_start(out=xb_[:], in_=xs_d[b * 128:(b + 1) * 128, :]))(xb_, b), r=sc_keys, w=[xbk], dma=True)
                    for kc in range(8):
                        P.add("pe", (lambda xb_, kc: lambda e: e.matmul(psum[kc // 4][:, (kc % 4) * 128:(kc % 4 + 1) * 128], lhsT=xb_[:, kc * 128:(kc + 1) * 128],
                                                                       rhs=idb2[:], start=True, stop=True))(xb_, kc), r=[xbk, "idb2"], w=[pk(kc // 4)])
                    for kc in range(8):
                        P.add("act", (lambda kc: lambda e: e.activation(out=XgT[:, kc, :], in_=psum[kc // 4][:, (kc % 4) * 128:(kc % 4 + 1) * 128], func=AF.Identity,
                                                                        scale=sc[:, 8 + kc:9 + kc], bias=mod[:, 24 + kc:25 + kc]))(kc), r=[pk(kc // 4), "sc", "mod"], w=[xgk])
                    for kc in range(8):
                        P.add("pe", (lambda kc: lambda e: e.matmul(psum[2][:, 0:512], lhsT=XgT[:, kc, :], rhs=Wgb[:, kc, :], start=(kc == 0), stop=(kc == 7)))(kc),
                              r=[xgk, "Wgb"], w=[pk(2)])
                    for kc in range(8):
                        P.add("pe", (lambda kc: lambda e: e.matmul(psum[3][:, 0:512], lhsT=XgT[:, kc, :], rhs=Wub[:, kc, :], start=(kc == 0), stop=(kc == 7)))(kc),
                              r=[xgk, "Wub"], w=[pk(3)])
                    P.add("act", lambda e: e.activation(out=sg[:], in_=psum[2][:, 0:512], func=AF.Silu), r=[pk(2)], w=[sgk])
                    P.add("dve", lambda e: e.tensor_tensor(out=actb[:], in0=psum[3][:, 0:512], in1=sg[:], op=ALU.mult), r=[pk(3), sgk], w=[abk])
                    for hc in range(4):
                        P.add("pe", (lambda hc: lambda e: e.matmul(psum[4][:, hc * 128:(hc + 1) * 128], lhsT=actb[:, hc * 128:(hc + 1) * 128], rhs=idb2[:],
                                                                   start=True, stop=True))(hc), r=[abk, "idb2"], w=[pk(4)])
                    P.add("act", lambda e: e.activation(out=actT[:].rearrange("p a b -> p (a b)"), in_=psum[4][:, 0:512], func=AF.Identity), r=[pk(4)], w=[atk])
                    yb_ = yblk[b % 2]
                    ybk = f"yblk{b % 2}"
                    for hf in range(2):
                        for hc in range(4):
                            P.add("pe", (lambda hc, hf: lambda e: e.matmul(psum[5 + hf][:, 0:512], lhsT=actT[:, hc, :], rhs=Wdb[:, hc, hf * 512:(hf + 1) * 512],
                                                                           start=(hc == 0), stop=(hc == 3)))(hc, hf), r=[atk, "Wdb"], w=[pk(5 + hf)])
                        if hf == 0:
                            P.add("act", (lambda yb_: lambda e: e.activation(out=yb_[:, 0:512], in_=psum[5][:, 0:512], func=AF.Identity))(yb_), r=[pk(5)], w=[ybk])
                        else:
                            P.add("dve", (lambda yb_: lambda e: e.tensor_copy(out=yb_[:, 512:1024], in_=psum[6][:, 0:512]))(yb_), r=[pk(6)], w=[ybk])
                    key = f"ys{b}"
                    P.add("sp", (lambda yb_, b: lambda e: e.dma_start(out=ys_d[b * 128:(b + 1) * 128, :], in_=yb_[:]))(yb_, b), r=[ybk], w=[key], dma=True)
                    ys_keys.append(key)

                for b in range(min(NBLK, nblk)):
                    do_block(b)
                for i in range(32):
                    tok0 = i * 128
                    P.add("pool", (lambda i: lambda e: e.indirect_dma_start(out=yA[:], out_offset=None, in_=ys_d[:, :], in_offset=IOA(ap=PABi[:, 0, i:i + 1], axis=0)))(i),
                          r=ys_keys + ["PABi", "yA"], w=["yA"], dma=True)
                    P.add("pool", (lambda i: lambda e: e.indirect_dma_start(out=yB[:], out_offset=None, in_=ys_d[:, :], in_offset=IOA(ap=PABi[:, 1, i:i + 1], axis=0)))(i),
                          r=ys_keys + ["PABi", "yB"], w=["yB"], dma=True)
                    P.add("sp", (lambda tok0: lambda e: e.dma_start(out=fgt[:], in_=x1_d[tok0:tok0 + 128, :]))(tok0), r=["x1_d"], w=["fgt"], dma=True)
                    P.add("dve", (lambda i: lambda e: e.tensor_scalar(out=yA[:], in0=yA[:], scalar1=WAB[:, 0, i:i + 1], scalar2=None, op0=ALU.mult))(i), r=["yA", "WAB"], w=["yA"])
                    P.add("dve", (lambda i: lambda e: e.scalar_tensor_tensor(out=yA[:], in0=yB[:], scalar=WAB[:, 1, i:i + 1], in1=yA[:], op0=ALU.mult, op1=ALU.add))(i),
                          r=["yA", "yB", "WAB"], w=["yA"])
                    P.add("pool", lambda e: e.tensor_tensor(out=yA[:], in0=yA[:], in1=g2bc[:], op=ALU.mult), r=["yA", "g2bc"], w=["yA"])
                    P.add("dve", lambda e: e.tensor_tensor(out=fgt[:], in0=fgt[:], in1=yA[:], op=ALU.add), r=["yA", "fgt"], w=["fgt"])
                    for hf in range(2):
                        P.add("dve", (lambda hf: lambda e: e.bn_stats(out=fst[:, hf, :], in_=fgt[:, hf * 512:(hf + 1) * 512]))(hf), r=["fgt"], w=["fst"])
                    P.add("dve", lambda e: e.bn_aggr(out=fmv[:, 0:2], in_=fst[:, 0:2, :].rearrange("p a b -> p (a b)")), r=["fst"], w=["fmv"])
                    P.add("dve", lambda e: e.scalar_tensor_tensor(out=fmv[:, 2:3], in0=fmv[:, 0:1], scalar=fmv[:, 0:1], in1=fmv[:, 1:2], op0=ALU.mult, op1=ALU.add),
                          r=["fmv"], w=["fmv"])
                    P.add("act", lambda e: e.activation(out=fmv[:, 3:4], in_=fmv[:, 2:3], func=AF.Sqrt, bias=float(1e-6)), r=["fmv"], w=["fmv"])
                    P.add("dve", lambda e: e.reciprocal(out=fmv[:, 3:4], in_=fmv[:, 3:4]), r=["fmv"], w=["fmv"])
                    P.add("dve", lambda e: e.scalar_tensor_tensor(out=fgt[:], in0=fgt[:], scalar=fmv[:, 3:4], in1=fgs[:], op0=ALU.mult, op1=ALU.mult),
                          r=["fgt", "fmv", "fgs"], w=["fgt"])
                    P.add("sp", (lambda tok0: lambda e: e.dma_start(out=out_d[tok0:tok0 + 128, :], in_=fgt[:]))(tok0), r=["fgt"], w=["out_d"], dma=True)
        else:
            with ExitStack() as cb:
                fgt = SB(cb, "fgt", [128, D])
                for ti in range(32):
                    tok0 = ti * 128
                    P.add("sp", (lambda tok0: lambda e: e.dma_start(out=fgt[:], in_=x1_d[tok0:tok0 + 128, :]))(tok0), r=["x1_d"], w=["fgt"], dma=True)
                    P.add("sp", (lambda tok0: lambda e: e.dma_start(out=out_d[tok0:tok0 + 128, :], in_=fgt[:]))(tok0), r=["fgt"], w=["out_d"], dma=True)
        P.emit()
    return nc


_NC_CACHE = {}


def _layouts(inp):
    f = np.float32
    g = lambda k: np.asarray(inp[k], dtype=f)
    col = lambda v, n: np.ascontiguousarray(v.reshape(n, 128).T)
    mu = g("rwkv_mu")[0]
    def pad128(v):
        o = np.zeros((128, 1), f)
        o[:v.shape[0], 0] = v
        return o
    shared = [None, col(g("ada_b")[0], 48), col(g("norm1_g")[0], 8), col(g("norm2_g")[0], 8), col(mu[0:1536], 12),
              pad128(mu[1536:1568]), pad128(mu[1568:1600]), pad128(mu[1600:1696]), col(g("rwkv_w0")[0], 4), col(g("rwkv_a0")[0], 4),
              col(g("rwkv_k_k")[0], 4), col(g("rwkv_k_a")[0], 4), col(g("rwkv_r_k")[0].reshape(512), 4), col(g("rwkv_gn_w")[0], 4),
              col(g("rwkv_gn_b")[0], 4)]
    tt = np.arange(64)
    su = (tt[:, None] < tt[None, :]).astype(f)
    iu = (tt[:, None] <= tt[None, :]).astype(f)
    sl = (tt[None, :] < tt[:, None]).astype(f)
    m5 = np.tile(np.concatenate([su, iu, su, iu, sl], axis=1), (2, 1))
    ident = np.eye(128, dtype=f)
    istack = np.tile(np.eye(64, dtype=f), (2, 1))
    ss = np.arange(128)
    tril = (ss[:, None] <= ss[None, :]).astype(f)
    bones = (ss[:, None] // 64 == ss[None, :] // 64).astype(f)
    ones = np.ones((128, 128), f)
    thr = np.broadcast_to((np.arange(32) * 128).astype(f)[None, :], (128, 32))
    iotab = np.broadcast_to(np.arange(96).astype(f)[None, :], (128, 96))
    pidx = np.arange(128).astype(f)[:, None]
    cst = np.ascontiguousarray(np.concatenate([m5, ident, istack, tril, bones, ones, thr, iotab, pidx], axis=1))
    assert cst.shape[1] == NCST
    rep = lambda v: np.broadcast_to(v[None, :], (128, v.shape[0]))
    bs = g("gmlp_bs")[0]
    bsT = np.zeros((128, 4, 128), f)
    for j in range(4):
        for hh in range(2):
            bsT[hh * 64:(hh + 1) * 64, j, :] = bs[2 * j + hh][None, :]
    bc = np.ascontiguousarray(np.concatenate([rep(g("gmlp_ln_w")[0]), rep(g("gmlp_ln_b")[0]),
                                              rep(np.concatenate([g("router_group_b")[0], g("router_expert_b")[0]])),
                                              bsT.reshape(128, 512)], axis=1))
    assert bc.shape[1] == NBC
    common = dict(cst=cst, bc=bc, fg=np.ascontiguousarray(rep(g("final_norm_g"))), ada_w=np.ascontiguousarray(g("ada_w")[0]), w_in=np.ascontiguousarray(g("w_in")[0]),
                  w_out=np.ascontiguousarray(g("w_out")[0]), w2=np.ascontiguousarray(g("rwkv_w2")[0]),
                  a2=np.ascontiguousarray(g("rwkv_a2")[0]), g2=np.ascontiguousarray(g("rwkv_g2")[0]),
                  wsT=np.ascontiguousarray(g("gmlp_ws")[0].transpose(2, 0, 1)),
                  wr=np.ascontiguousarray(np.concatenate([g("router_group_w")[0], g("router_expert_w")[0]], axis=1)),
                  wg=np.ascontiguousarray(g("moe_w_gate")[0].reshape(NE, 8, 128, EH).transpose(0, 2, 1, 3).reshape(NE * 128, 4096)),
                  wu=np.ascontiguousarray(g("moe_w_up")[0].reshape(NE, 8, 128, EH).transpose(0, 2, 1, 3).reshape(NE * 128, 4096)),
                  wd=np.ascontiguousarray(g("moe_w_down")[0].reshape(NE, 4, 128, D).transpose(0, 2, 1, 3).reshape(NE * 128, 4096)))
    x = g("x")
    c = g("c")
    maps = []
    for b in range(x.shape[0]):
        cols = [col(c[b], 8)] + shared[1:]
        prm = np.ascontiguousarray(np.concatenate(cols, axis=1))
        assert prm.shape[1] == NPRM
        m = dict(common)
        m["x"] = np.ascontiguousarray(x[b])
        m["prm"] = prm
        maps.append(m)
    return maps


def kernel(**inputs):
    maps = _layouts(inputs)
    if "nc" not in _NC_CACHE:
        _NC_CACHE["nc"] = build_nc()
    nc = _NC_CACHE["nc"]
    res = run_bass_kernel_spmd(nc, maps, core_ids=list(range(len(maps))))
    return np.stack([r["out"] for r in res.results], axis=0).astype(np.float32)
```

```python
import numpy as np
import os
from contextlib import ExitStack
import concourse.bass as bass
import concourse.mybir as mybir
from concourse.bass_utils import run_bass_kernel_spmd

F32 = mybir.dt.float32
BF16 = mybir.dt.bfloat16
I32 = mybir.dt.int32
AF = mybir.ActivationFunctionType
ALU = mybir.AluOpType
AX = mybir.AxisListType

D = 1024
S = 4096
TT = 256
NT = S // TT
NSUB = TT // 128
CH = 64
NCH = TT // CH
INW = 2720
NE = 32
EH = 512
NEG_HALF_E = -0.6065306597126334

PC = {}
_o = 0
for _n, _w in [("cT", 8), ("ada_b", 48), ("n1g", 8), ("n2g", 8), ("mu_rkv", 12), ("mu_xw", 1),
               ("mu_xa", 1), ("mu_xg", 1), ("w0", 4), ("a0", 4), ("k_k", 4), ("k_a", 4), ("r_k", 4),
               ("gn_w", 4), ("gn_b", 4)]:
    PC[_n] = _o
    _o += _w
NPRM = _o
CC = {}
_o = 0
for _n, _w in [("m5", 320), ("ident", 128), ("istack", 64), ("tril", 128), ("bones", 128), ("ones", 128), ("thr", 32), ("iotab", 96), ("pidx", 1)]:
    CC[_n] = _o
    _o += _w
NCST = _o
BC = {}
_o = 0
for _n, _w in [("lnw", 512), ("lnb", 512), ("rb", 36), ("bsT", 512)]:
    BC[_n] = _o
    _o += _w
NBC = _o


ATTACH_WAIT = True
NO_SELF_SYNC = ("pe", "act")


class Prog:
    def __init__(self, nc, ctx):
        self.nc = nc
        self.ops = []
        self.last_w = {}
        self.readers = {}
        self.engs = ["pe", "act", "dve", "pool", "sp"]
        self.count = {e: 0 for e in self.engs}
        self.sem = {e: ctx.enter_context(nc.semaphore("s_" + e)) for e in self.engs}
        self.NDS = 8
        self.dsem = {q: [ctx.enter_context(nc.semaphore(f"d_{q}{i}")) for i in range(self.NDS)]
                     for q in ("sp", "pool")}
        self.dcount = {"sp": 0, "pool": 0}
        self.last_op = {e: None for e in self.engs}
        self.pending = {e: set() for e in self.engs}
        self.recent_dma = {"sp": [], "pool": []}

    def section(self, k):
        self.muted = k > self.cut

    def add(self, eng, fn, r=(), w=(), dma=False):
        if getattr(self, 'muted', False):
            return None
        idx = len(self.ops)
        deps = set(self.pending[eng])
        self.pending[eng] = set()
        for k in r:
            if k in self.last_w:
                deps.add(self.last_w[k])
        for k in w:
            if k in self.last_w:
                deps.add(self.last_w[k])
            deps.update(self.readers.get(k, ()))
        for k in r:
            self.readers.setdefault(k, []).append(idx)
        for k in w:
            self.last_w[k] = idx
            self.readers[k] = []
        if dma:
            q = eng
            kq = self.dcount[q]
            self.dcount[q] += 1
            sem = self.dsem[q][kq % self.NDS]
            val = 16 * (kq // self.NDS + 1)
            prev = (sem, val - 16) if val > 16 else None
            self.recent_dma[q].append(idx)
            self.recent_dma[q] = self.recent_dma[q][-self.NDS:]
        else:
            self.count[eng] += 1
            sem = self.sem[eng]
            val = self.count[eng]
            prev = None
        self.ops.append(dict(eng=eng, fn=fn, deps=deps, dma=dma, sem=sem, val=val, prev=prev))
        self.last_op[eng] = idx
        return idx

    def barrier(self):
        allops = set()
        for e in self.engs:
            if self.last_op[e] is not None:
                allops.add(self.last_op[e])
        for q in ("sp", "pool"):
            allops.update(self.recent_dma[q])
        dmaops = set()
        for q in ("sp", "pool"):
            dmaops.update(self.recent_dma[q])
        for e in self.engs:
            self.pending[e] |= (allops - dmaops) if e == "pe" else allops

    def emit(self):
        nc = self.nc
        per = {e: [] for e in self.engs}
        for i, op in enumerate(self.ops):
            per[op["eng"]].append(i)
        ops = self.ops

        def run(name, e):
            waited = {}
            for i in per[name]:
                op = ops[i]
                need = []
                for j in op["deps"]:
                    d = ops[j]
                    if d["eng"] == name and not d["dma"] and name in NO_SELF_SYNC:
                        continue
                    need.append((d["sem"], d["val"]))
                if op["prev"] is not None:
                    need.append(op["prev"])
                need.sort(key=lambda t: -t[1])
                todo = []
                for sem, val in need:
                    key = id(sem)
                    if waited.get(key, 0) >= val:
                        continue
                    waited[key] = val
                    todo.append((sem, val))
                attach = todo.pop() if (todo and ATTACH_WAIT) else None
                for sem, val in todo:
                    e.wait_ge(sem, val)
                inst = op["fn"](e)
                if attach is not None:
                    inst._wait_ge(attach[0], attach[1])
                inst.then_inc(op["sem"], 16 if op["dma"] else 1)
            if name in ("sp", "pool"):
                kq = self.dcount[name]
                for s_i in range(min(kq, self.NDS)):
                    n_on = (kq - s_i + self.NDS - 1) // self.NDS
                    e.wait_ge(self.dsem[name][s_i], 16 * n_on)

        with nc.Block() as block:
            @block.sync
            def _(e):
                run("sp", e)

            @block.scalar
            def _(e):
                run("act", e)

            @block.vector
            def _(e):
                run("dve", e)

            @block.tensor
            def _(e):
                run("pe", e)

            @block.gpsimd
            def _(e):
                run("pool", e)


def build_nc(stage=99, ntiles=NT, cut=99, nblk=96):
    nc = bass.Bass("TRN2", target_bir_lowering=False)
    dt = lambda name, shape, dty, kind: nc.dram_tensor(name, shape, dty, kind=kind).ap()
    x_d = dt("x", [S, D], F32, "ExternalInput")
    prm_d = dt("prm", [128, NPRM], F32, "ExternalInput")
    cst_d = dt("cst", [128, NCST], F32, "ExternalInput")
    bc_d = dt("bc", [128, NBC], F32, "ExternalInput")
    adaw_d = dt("ada_w", [D, 6 * D], F32, "ExternalInput")
    win_d = dt("w_in", [D, INW], F32, "ExternalInput")
    wout_d = dt("w_out", [D, D], F32, "ExternalInput")
    w2_d = dt("w2", [32, 512], F32, "ExternalInput")
    a2_d = dt("a2", [32, 512], F32, "ExternalInput")
    g2_d = dt("g2", [96, 512], F32, "ExternalInput")
    wsT_d = dt("wsT", [128, 8, 128], F32, "ExternalInput")
    wr_d = dt("wr", [D, 36], F32, "ExternalInput")
    fg_d = dt("fg", [128, D], F32, "ExternalInput")
    wall_d = dt("wall", [NE * 128, 12288], F32, "ExternalInput")
    NBLK = 96
    xn2_d = dt("xn2_d", [S, D], BF16, "Internal")
    xs_d = dt("xs_d", [NBLK * 128, D], BF16, "Internal")
    ys_d = dt("ys_d", [NBLK * 128, D], F32, "Internal")
    out_d = dt("out", [S, D], F32, "ExternalOutput")
    x1_d = dt("x1_d", [S, D], F32, "Internal")

    with ExitStack() as top:
        P = Prog(nc, top)
        P.cut = cut
        psum = [top.enter_context(nc.psum_tensor(f"ps{i}", [128, 512], F32)) for i in range(7)]
        psTt = top.enter_context(nc.psum_tensor("psT", [128, 512], F32))
        psT = [psTt[:, 0:256], psum[6][:, 0:256]]
        psTk = ["ps7", "ps6"]
        fst = None
        fmv = None
        pk = lambda i: f"ps{i}"

        def SB(ctx, name, shape, dty=F32):
            return ctx.enter_context(nc.sbuf_tensor(name, shape, dty))

        prm = SB(top, "prm_s", [128, NPRM])
        cst = SB(top, "cst_s", [128, NCST])
        bcs = SB(top, "bc_s", [128, NBC])
        mod = SB(top, "mod", [128, 48])
        wt = SB(top, "wt", [128, 32, 32])
        PABi = SB(top, "PABi", [128, 2, 32], I32)
        WAB = SB(top, "WAB", [128, 2, 32])
        IDXW = SB(top, "IDXW", [128, 4, 96], I32)
        P.add("sp", lambda e: e.dma_start(out=prm[:], in_=prm_d[:, :]), w=["prm"], dma=True)
        P.add("sp", lambda e: e.dma_start(out=cst[:], in_=cst_d[:, :]), w=["cst"], dma=True)
        P.add("sp", lambda e: e.dma_start(out=bcs[:], in_=bc_d[:, :]), w=["bc"], dma=True)

        def pcol(name, j=0, n=1):
            return prm[:, PC[name] + j:PC[name] + j + n]

        ident = cst[:, CC["ident"]:CC["ident"] + 128]
        istack = cst[:, CC["istack"]:CC["istack"] + 64]
        tril = cst[:, CC["tril"]:CC["tril"] + 128]
        bones = cst[:, CC["bones"]:CC["bones"] + 128]
        ones = cst[:, CC["ones"]:CC["ones"] + 128]
        m5 = cst[:, CC["m5"]:CC["m5"] + 320]

        with ExitStack() as c0:
            stg = [SB(c0, f"ada_stg{i}", [128, 6 * D]) for i in range(2)]
            for kc in range(8):
                sb = stg[kc % 2]
                P.add("sp", (lambda sb, kc: lambda e: e.dma_start(out=sb[:], in_=adaw_d[kc * 128:(kc + 1) * 128, :]))(sb, kc),
                      w=[f"ada_stg{kc % 2}"], dma=True)
                for oc in range(48):
                    P.add("pe", (lambda sb, kc, oc: lambda e: e.matmul(psum[0][:, oc:oc + 1], lhsT=sb[:, oc * 128:(oc + 1) * 128],
                                                                   rhs=prm[:, PC["cT"] + kc:PC["cT"] + kc + 1], start=True, stop=True))(sb, kc, oc),
                          r=[f"ada_stg{kc % 2}", "prm"], w=[pk(0)])
                if kc == 0:
                    P.add("dve", lambda e: e.tensor_tensor(out=mod[:], in0=psum[0][:, 0:48], in1=prm[:, PC["ada_b"]:PC["ada_b"] + 48], op=ALU.add),
                          r=[pk(0), "prm"], w=["mod"])
                else:
                    P.add("dve", lambda e: e.tensor_tensor(out=mod[:], in0=psum[0][:, 0:48], in1=mod[:], op=ALU.add),
                          r=[pk(0), "mod"], w=["mod"])
            P.barrier()
        sc = SB(top, "sc", [128, 32])
        P.add("dve", lambda e: e.scalar_tensor_tensor(out=sc[:, 0:8], in0=mod[:, 8:16], scalar=1.0, in1=prm[:, PC["n1g"]:PC["n1g"] + 8],
                                                      op0=ALU.add, op1=ALU.mult), r=["mod", "prm"], w=["sc"])
        P.add("dve", lambda e: e.scalar_tensor_tensor(out=sc[:, 8:16], in0=mod[:, 32:40], scalar=1.0, in1=prm[:, PC["n2g"]:PC["n2g"] + 8],
                                                      op0=ALU.add, op1=ALU.mult), r=["mod", "prm"], w=["sc"])
        gate_bc = SB(top, "gate_bc", [128, D])
        gl = SB(top, "gl", [128, 128])

        def make_gate(dst, dkey, gcol):
            for c in range(8):
                P.add("dve", (lambda c: lambda e: e.tensor_scalar(out=gl[:], in0=ones, scalar1=mod[:, gcol + c:gcol + c + 1], scalar2=None,
                                                                  op0=ALU.mult))(c), r=["mod", "cst"], w=["gl"])
                P.add("pe", (lambda c: lambda e: e.matmul(psum[1][:, (c % 4) * 128:(c % 4 + 1) * 128], lhsT=gl[:], rhs=ident, start=True, stop=True))(c),
                      r=["gl", "cst"], w=[pk(1)])
                P.add("act", (lambda c: lambda e: e.activation(out=dst[:, c * 128:(c + 1) * 128], in_=psum[1][:, (c % 4) * 128:(c % 4 + 1) * 128],
                                                               func=AF.Identity))(c), r=[pk(1)], w=[dkey])
        make_gate(gate_bc, "gate_bc", 16)

        with ExitStack() as ca:
            win = SB(ca, "win", [128, 8, INW], BF16)
            wout = SB(ca, "wout", [128, 8, D], BF16)
            identb = SB(ca, "identb", [128, 128], BF16)
            w2s = SB(ca, "w2s", [32, 512])
            a2s = SB(ca, "a2s", [32, 512])
            g2s = SB(ca, "g2s", [96, 512])
            wrs = SB(ca, "wrs", [128, 8, 36])
            wc = SB(ca, "wc", [128, 8, 128])
            logits = SB(ca, "logits", [128, 32, 36])
            Hst = [SB(ca, f"H{j}", [128, 64]) for j in range(4)]
            carry = SB(ca, "carry", [128, 16])
            P.add("pool", lambda e: e.tensor_copy(out=identb[:], in_=ident), r=["cst"], w=["identb"])
            P.add("sp", lambda e: e.dma_start(out=w2s[:], in_=w2_d[:, :]), w=["w2s"], dma=True)
            P.add("sp", lambda e: e.dma_start(out=a2s[:], in_=a2_d[:, :]), w=["a2s"], dma=True)
            P.add("sp", lambda e: e.dma_start(out=g2s[:], in_=g2_d[:, :]), w=["g2s"], dma=True)
            P.add("sp", lambda e: e.dma_start(out=wrs[:], in_=wr_d.rearrange("(c p) n -> p c n", p=128)), w=["wrs"], dma=True)
            P.add("sp", lambda e: e.dma_start(out=wc[:], in_=wsT_d[:, :, :]), w=["wc"], dma=True)
            for h in range(8):
                P.add("pool", (lambda h: lambda e: e.tensor_tensor(out=wc[:, h, :], in0=wc[:, h, :], in1=tril, op=ALU.mult))(h),
                      r=["wc", "cst"], w=["wc"])
            P.add("pool", lambda e: e.memset(carry[:], 0.0), w=["carry"])
            for _i in range(int(os.environ.get("KPAD", "0"))):
                P.add("pool", lambda e: e.memset(gl[:, 0:1], 0.0), w=["gl_dummy"])
            for j in range(4):
                P.add("pool", (lambda j: lambda e: e.memset(Hst[j][:], 0.0))(j), w=[f"H{j}"])
            with ExitStack() as cw:
                wstg = [SB(cw, f"wstg{i}", [128, INW]) for i in range(2)]
                for kc in range(8):
                    sb = wstg[kc % 2]
                    P.add("sp", (lambda sb, kc: lambda e: e.dma_start(out=sb[:], in_=win_d[kc * 128:(kc + 1) * 128, :]))(sb, kc),
                          w=[f"wstg{kc % 2}"], dma=True)
                    P.add("pool", (lambda sb, kc: lambda e: e.tensor_copy(out=win[:, kc, :], in_=sb[:]))(sb, kc),
                          r=[f"wstg{kc % 2}"], w=["win"])
                for kc in range(8):
                    sb = wstg[kc % 2]
                    P.add("sp", (lambda sb, kc: lambda e: e.dma_start(out=sb[:, 0:D], in_=wout_d[kc * 128:(kc + 1) * 128, :]))(sb, kc),
                          w=[f"wstg{kc % 2}"], dma=True)
                    P.add("pool", (lambda sb, kc: lambda e: e.tensor_copy(out=wout[:, kc, :], in_=sb[:, 0:D]))(sb, kc),
                          r=[f"wstg{kc % 2}"], w=["wout"])
                P.barrier()

            ct = ExitStack()
            xt = [SB(ct, "xt0", [128, NSUB, D])] * 2
            xn = SB(ct, "xn", [128, NSUB, D], BF16)
            hT = SB(ct, "hT", [128, 8, TT], BF16)
            st6 = SB(ct, "st6", [128, 4, 6])
            mv = SB(ct, "mv", [128, 8])
            u_s = SB(ct, "u_s", [128, 4, TT], BF16)
            vg = SB(ct, "vg", [128, 512])
            vn = vg
            zraw = [SB(ct, f"zraw{i}", [128, TT + 1]) for i in range(2)]
            rkv = SB(ct, "rkv", [128, 12, TT])
            lor = SB(ct, "lor", [128, 3, TT])
            LW, ASN, BSN, KM, BON, GG = range(6)
            pers = [SB(ct, f"pers{j}", [128, 6, TT]) for j in range(4)]
            AA, KX, RN, PRD = range(4)
            ptmp = SB(ct, "ptmp", [128, 4, TT])
            ybT = SB(ct, "ybT", [128, 4, TT])
            ycat = SB(ct, "ycat", [128, 8, TT], BF16)
            x1t = SB(ct, "x1t", [128, D])
            xn2 = x1t
            h2f = SB(ct, "h2f", [128, 8, 128])
            gtmp = h2f[:, 0:4, :]
            xnb = SB(ct, "xnb", [128, D], BF16)
            CI, CE, EI, EE, EN, AT, RT, BT, KT = range(9)
            sct = [SB(ct, f"sct{j}", [128, 9, 64]) for j in range(4)]
            mats = [SB(ct, f"mats{j}", [128, 320]) for j in range(4)]
            trs = [SB(ct, f"trs{j}", [128, 192]) for j in range(4)]
            wzb_l = [SB(ct, f"wzb{j}", [128, 256]) for j in range(4)]
            qq = [[SB(ct, f"qq{j}_{i}", [128, 64]) for i in range(2)] for j in range(4)]
            chn = [SB(ct, f"chn{j}", [128, 4, 64]) for j in range(4)]
            gst = [SB(ct, f"gst{j}", [128, 12]) for j in range(4)]

            def A(eng, fn, r=(), w=()):
                P.add(eng, fn, r=r, w=w)

            def norm_stats(src, srck, eps):
                for hf in range(2):
                    A("dve", (lambda hf: lambda e: e.bn_stats(out=st6[:, hf, :], in_=src(hf)))(hf), r=[srck], w=["st6"])
                A("dve", lambda e: e.bn_aggr(out=mv[:, 0:2], in_=st6[:, 0:2, :].rearrange("p a b -> p (a b)")), r=["st6"], w=["mv"])
                A("dve", lambda e: e.scalar_tensor_tensor(out=mv[:, 2:3], in0=mv[:, 0:1], scalar=mv[:, 0:1], in1=mv[:, 1:2], op0=ALU.mult, op1=ALU.add),
                  r=["mv"], w=["mv"])
                A("act", lambda e: e.activation(out=mv[:, 3:4], in_=mv[:, 2:3], func=AF.Sqrt, bias=float(eps)), r=["mv"], w=["mv"])
                A("dve", lambda e: e.reciprocal(out=mv[:, 3:4], in_=mv[:, 3:4]), r=["mv"], w=["mv"])

            for TI in range(min(NT, ntiles) if stage >= 1 else 0):
                t0 = TI * TT
                xb = xt[TI % 2]
                xk = "xt0"
                P.add("sp", (lambda xb, t0: lambda e: e.dma_start(out=xb[:], in_=x_d[t0:t0 + TT, :].rearrange("(s p) d -> p s d", p=128)))(xb, t0),
                      w=[xk], dma=True)
                P.section(1)
                for sub in range(NSUB):
                    norm_stats((lambda xb, sub: lambda hf: xb[:, sub, hf * 512:(hf + 1) * 512])(xb, sub), xk, 1e-6)
                    A("act", (lambda xb, sub: lambda e: e.activation(out=xn[:, sub, :], in_=xb[:, sub, :], func=AF.Identity, scale=mv[:, 3:4]))(xb, sub),
                      r=[xk, "mv"], w=["xn"])
                P.section(1.5)
                for kc in range(8):
                    pb = kc % 2
                    for sub in range(NSUB):
                        A("pe", (lambda kc, sub, pb: lambda e: e.matmul(psT[pb][:, sub * 128:(sub + 1) * 128], lhsT=xn[:, sub, kc * 128:(kc + 1) * 128],
                                                                       rhs=identb[:], start=True, stop=True))(kc, sub, pb), r=["xn", "identb"], w=[psTk[pb]])
                    if os.environ.get("DVE_EVAC"):
                      A("dve", (lambda kc, pb: lambda e: e.tensor_scalar(out=hT[:, kc, :], in0=psT[pb][:, 0:TT], scalar1=sc[:, kc:kc + 1], scalar2=mod[:, kc:kc + 1],
                                                                         op0=ALU.mult, op1=ALU.add))(kc, pb), r=[psTk[pb], "sc", "mod"], w=["hT"])
                    elif not os.environ.get("SKIP_EVAC"):
                      A("act", (lambda kc, pb: lambda e: e.activation(out=hT[:, kc, :], in_=psT[pb][:, 0:TT], func=AF.Identity,
                                                                    **({} if os.environ.get("NO_SB") else dict(scale=sc[:, kc:kc + 1], bias=mod[:, kc:kc + 1]))))(kc, pb),
                      r=[psTk[pb], "sc", "mod"], w=["hT"])

                P.section(2)
                pcnt = [0]

                def proj(c0, M):
                    pb = pcnt[0] % 2
                    pcnt[0] += 1
                    for kc in range(8):
                        A("pe", (lambda kc, pb: lambda e: e.matmul(psum[pb][0:M, 0:TT], lhsT=win[:, kc, c0:c0 + M], rhs=hT[:, kc, :],
                                                                  start=(kc == 0), stop=(kc == 7)))(kc, pb), r=["win", "hT"], w=[pk(pb)])
                    return pb

                for j in range(4):
                    pb = proj(j * 128, 128)
                    A("act", (lambda j, pb: lambda e: e.activation(out=u_s[:, j, :], in_=psum[pb][:, 0:TT], func=AF.Gelu_apprx_tanh))(j, pb),
                      r=[pk(pb)], w=["u_s"])
                specs = [(1024 + q * 128, 128, PC["mu_rkv"] + q) for q in range(12)] + \
                        [(2560, 32, PC["mu_xw"]), (2592, 32, PC["mu_xa"]), (2624, 96, PC["mu_xg"])]
                for q, (c0, M, mucol) in enumerate(specs):
                    pb = proj(c0, M)
                    zb = zraw[q % 2]
                    zk = f"zraw{q % 2}"
                    dst = rkv[0:M, q, :] if q < 12 else lor[0:M, q - 12, :]
                    dk = f"rkv{q}"
                    A("dve", (lambda zb, q, M: lambda e: e.tensor_copy(out=zb[0:M, 0:1], in_=carry[0:M, q:q + 1]))(zb, q, M), r=["carry"], w=[zk])
                    A("act", (lambda zb, pb, M: lambda e: e.activation(out=zb[0:M, 1:TT + 1], in_=psum[pb][0:M, 0:TT], func=AF.Identity))(zb, pb, M),
                      r=[pk(pb)], w=[zk])
                    A("dve", (lambda zb, q, M: lambda e: e.tensor_copy(out=carry[0:M, q:q + 1], in_=zb[0:M, TT:TT + 1]))(zb, q, M), r=[zk], w=["carry"])
                    A("dve", (lambda zb, dst, M: lambda e: e.tensor_tensor(out=dst, in0=zb[0:M, 0:TT], in1=zb[0:M, 1:TT + 1], op=ALU.subtract))(zb, dst, M),
                      r=[zk], w=[dk])
                    A("dve", (lambda zb, dst, M, mucol: lambda e: e.scalar_tensor_tensor(out=dst, in0=dst, scalar=prm[0:M, mucol:mucol + 1], in1=zb[0:M, 1:TT + 1],
                                                                                      op0=ALU.mult, op1=ALU.add))(zb, dst, M, mucol), r=[zk, dk, "prm"], w=[dk])
                A("act", lambda e: e.activation(out=lor[0:32, 0, :], in_=lor[0:32, 0, :], func=AF.Tanh), r=["rkv12"], w=["rkv12"])
                A("act", lambda e: e.activation(out=lor[0:96, 2, :], in_=lor[0:96, 2, :], func=AF.Sigmoid), r=["rkv14"], w=["rkv14"])

                P.section(3)
                for j in range(4):
                    jc = slice(j * 128, (j + 1) * 128)
                    pj = pers[j]
                    pjk = f"pers{j}"
                    rr = rkv[:, j, :]
                    kr = rkv[:, 4 + j, :]
                    vr = rkv[:, 8 + j, :]
                    A("pe", (lambda jc: lambda e: e.matmul(psum[0][:, 0:TT], lhsT=w2s[0:32, jc], rhs=lor[0:32, 0, :], start=True, stop=True))(jc),
                      r=["w2s", "rkv12"], w=[pk(0)])
                    A("act", (lambda pj, j: lambda e: e.activation(out=pj[:, LW, :], in_=psum[0][:, 0:TT], func=AF.Sigmoid, bias=pcol("w0", j)))(pj, j),
                      r=[pk(0), "prm"], w=[pjk + "LW"])
                    A("pool", (lambda pj: lambda e: e.tensor_scalar(out=pj[:, LW, :], in0=pj[:, LW, :], scalar1=NEG_HALF_E, scalar2=None, op0=ALU.mult))(pj),
                      r=[pjk + "LW"], w=[pjk + "LW"])
                    A("pe", (lambda jc: lambda e: e.matmul(psum[1][:, 0:TT], lhsT=a2s[0:32, jc], rhs=lor[0:32, 1, :], start=True, stop=True))(jc),
                      r=["a2s", "rkv13"], w=[pk(1)])
                    A("act", (lambda j: lambda e: e.activation(out=ptmp[:, AA, :], in_=psum[1][:, 0:TT], func=AF.Sigmoid, bias=pcol("a0", j)))(j),
                      r=[pk(1), "prm"], w=["pAA"])
                    A("pe", (lambda jc: lambda e: e.matmul(psum[0][:, 0:TT], lhsT=g2s[0:96, jc], rhs=lor[0:96, 2, :], start=True, stop=True))(jc),
                      r=["g2s", "rkv14"], w=[pk(0)])
                    A("act", (lambda pj: lambda e: e.activation(out=pj[:, GG, :], in_=psum[0][:, 0:TT], func=AF.Identity))(pj), r=[pk(0)], w=[pjk + "GG"])
                    A("dve", (lambda kr, j: lambda e: e.tensor_scalar(out=ptmp[:, KX, :], in0=kr, scalar1=pcol("k_k", j), scalar2=None, op0=ALU.mult))(kr, j),
                      r=[f"rkv{4 + j}", "prm"], w=["pKX"])
                    A("pool", lambda e: e.tensor_tensor(out=ptmp[:, RN, :], in0=ptmp[:, KX, :], in1=ptmp[:, KX, :], op=ALU.mult), r=["pKX"], w=["pRN"])
                    A("pe", lambda e: e.matmul(psum[1][:, 0:TT], lhsT=bones, rhs=ptmp[:, RN, :], start=True, stop=True), r=["cst", "pRN"], w=[pk(1)])
                    A("act", lambda e: e.activation(out=ptmp[:, RN, :], in_=psum[1][:, 0:TT], func=AF.Sqrt, bias=float(1e-24)), r=[pk(1)], w=["pRN"])
                    A("dve", lambda e: e.reciprocal(out=ptmp[:, RN, :], in_=ptmp[:, RN, :]), r=["pRN"], w=["pRN"])
                    A("dve", (lambda pj: lambda e: e.scalar_tensor_tensor(out=pj[:, ASN, :], in0=ptmp[:, KX, :], scalar=-1.0, in1=ptmp[:, RN, :],
                                                                          op0=ALU.mult, op1=ALU.mult))(pj), r=["pKX", "pRN"], w=[pjk + "ASN"])
                    A("dve", (lambda pj: lambda e: e.scalar_tensor_tensor(out=pj[:, BSN, :], in0=pj[:, ASN, :], scalar=-1.0, in1=ptmp[:, AA, :],
                                                                          op0=ALU.mult, op1=ALU.mult))(pj), r=[pjk + "ASN", "pAA"], w=[pjk + "BSN"])
                    A("dve", (lambda j: lambda e: e.tensor_scalar(out=ptmp[:, PRD, :], in0=ptmp[:, AA, :], scalar1=-1.0, scalar2=pcol("k_a", j),
                                                                  op0=ALU.add, op1=ALU.mult))(j), r=["pAA", "prm"], w=["pPRD"])
                    A("dve", (lambda pj, kr: lambda e: e.scalar_tensor_tensor(out=pj[:, KM, :], in0=ptmp[:, PRD, :], scalar=1.0, in1=kr,
                                                                              op0=ALU.add, op1=ALU.mult))(pj, kr), r=["pPRD", f"rkv{4 + j}"], w=[pjk + "KM"])
                    A("dve", (lambda pj, rr, j: lambda e: e.scalar_tensor_tensor(out=ptmp[:, PRD, :], in0=rr, scalar=pcol("r_k", j), in1=pj[:, KM, :],
                                                                                 op0=ALU.mult, op1=ALU.mult))(pj, rr, j), r=[f"rkv{j}", "prm", pjk + "KM"], w=["pPRD"])
                    A("pe", lambda e: e.matmul(psum[0][:, 0:TT], lhsT=bones, rhs=ptmp[:, PRD, :], start=True, stop=True), r=["cst", "pPRD"], w=[pk(0)])
                    A("dve", (lambda pj, vr: lambda e: e.tensor_tensor(out=pj[:, BON, :], in0=psum[0][:, 0:TT], in1=vr, op=ALU.mult))(pj, vr),
                      r=[pk(0), f"rkv{8 + j}"], w=[pjk + "BON"])

                P.section(4)
                def unit_stages(j, c):
                    col = slice(c * CH, (c + 1) * CH)
                    pj = pers[j]
                    pjk = f"pers{j}"
                    s = sct[j]
                    sk = f"sct{j}"
                    mt = mats[j]
                    mk = f"mats{j}"
                    tr = trs[j]
                    tk = f"trs{j}"
                    cn = chn[j]
                    ck = f"chn{j}"
                    H = Hst[j]
                    Hk = f"H{j}"
                    rr = rkv[:, j, col]
                    vr = rkv[:, 8 + j, col]
                    hp = [slice(0, 64), slice(64, 128)]
                    st = []

                    def s1():
                        A("dve", lambda e: e.tensor_tensor_scan(out=s[:, CI, :], data0=ones[:, 0:64], data1=pj[:, LW, col], initial=0.0, op0=ALU.mult, op1=ALU.add),
                          r=[pjk + "LW", "cst"], w=[sk + "ci"])
                        A("dve", lambda e: e.tensor_tensor(out=s[:, CE, :], in0=s[:, CI, :], in1=pj[:, LW, col], op=ALU.subtract),
                          r=[sk + "ci", pjk + "LW"], w=[sk + "ce"])
                    st.append(s1)

                    def s2():
                        A("act", lambda e: e.activation(out=s[:, EI, :], in_=s[:, CI, :], func=AF.Exp), r=[sk + "ci"], w=[sk + "ei"])
                        A("act", lambda e: e.activation(out=s[:, EE, :], in_=s[:, CE, :], func=AF.Exp), r=[sk + "ce"], w=[sk + "ee"])
                        A("act", lambda e: e.activation(out=s[:, EN, :], in_=s[:, CI, :], func=AF.Exp, scale=-1.0), r=[sk + "ci"], w=[sk + "en"])
                    st.append(s2)

                    def s3():
                        A("dve", lambda e: e.tensor_tensor(out=s[:, AT, :], in0=pj[:, ASN, col], in1=s[:, EE, :], op=ALU.mult), r=[pjk + "ASN", sk + "ee"], w=[sk + "at"])
                        A("dve", lambda e: e.tensor_tensor(out=s[:, RT, :], in0=rr, in1=s[:, EI, :], op=ALU.mult), r=[f"rkv{j}", sk + "ei"], w=[sk + "rt"])
                        A("dve", lambda e: e.tensor_tensor(out=s[:, BT, :], in0=pj[:, BSN, col], in1=s[:, EN, :], op=ALU.mult), r=[pjk + "BSN", sk + "en"], w=[sk + "bt"])
                        A("dve", lambda e: e.tensor_tensor(out=s[:, KT, :], in0=pj[:, KM, col], in1=s[:, EN, :], op=ALU.mult), r=[pjk + "KM", sk + "en"], w=[sk + "kt"])
                    st.append(s3)

                    def s4():
                        for p in hp:
                            A("pe", (lambda p: lambda e: e.matmul(psum[2][p, 0:128], lhsT=s[p, BT, :], rhs=s[p, AT:RT + 1, :].rearrange("p a b -> p (a b)"), start=True, stop=True))(p),
                              r=[sk + "bt", sk + "at", sk + "rt"], w=["ps2"])
                            A("pe", (lambda p: lambda e: e.matmul(psum[2][p, 128:256], lhsT=s[p, KT, :], rhs=s[p, AT:RT + 1, :].rearrange("p a b -> p (a b)"), start=True, stop=True))(p),
                              r=[sk + "kt", sk + "at", sk + "rt"], w=["ps2"])
                            A("pe", (lambda p: lambda e: e.matmul(psum[2][p, 256:320], lhsT=s[p, AT, :], rhs=s[p, BT, :], start=True, stop=True))(p),
                              r=[sk + "bt", sk + "at"], w=["ps2"])
                        A("dve", lambda e: e.tensor_tensor(out=mt[:], in0=psum[2][:, 0:320], in1=m5, op=ALU.mult), r=["ps2", "cst"], w=[mk])
                        for p in hp:
                            A("pe", (lambda p: lambda e: e.matmul(psum[3][p, 0:64], lhsT=rkv[p, 8 + j, col], rhs=istack[p, :], start=True, stop=True))(p),
                              r=[f"rkv{8 + j}", "cst"], w=["ps3"])
                            A("pe", (lambda p: lambda e: e.matmul(psum[3][p, 64:128], lhsT=s[p, BT, :], rhs=istack[p, :], start=True, stop=True))(p),
                              r=[sk + "bt", "cst"], w=["ps3"])
                            A("pe", (lambda p: lambda e: e.matmul(psum[3][p, 128:192], lhsT=s[p, KT, :], rhs=istack[p, :], start=True, stop=True))(p),
                              r=[sk + "kt", "cst"], w=["ps3"])
                        A("act", lambda e: e.activation(out=tr[:], in_=psum[3][:, 0:192], func=AF.Identity), r=["ps3"], w=[tk])
                        A("dve", lambda e: e.tensor_tensor(out=qq[j][0][:], in0=mt[:, 0:64], in1=istack, op=ALU.add), r=[mk, "cst"], w=[f"qq{j}_0"])
                    st.append(s4)

                    wzb = wzb_l[j]
                    wbk = f"wzb{j}"
                    bm3 = bones.rearrange("p (a b) -> p a b", a=2)

                    def s4b():
                        A("dve", lambda e: e.tensor_tensor(out=wzb[:, 0:128].rearrange("p (a b) -> p a b", a=2),
                                                            in0=mt[:, 0:64].rearrange("p (o t) -> p o t", o=1).to_broadcast([128, 2, 64]), in1=bm3, op=ALU.mult),
                          r=[mk, "cst"], w=[wbk])
                        A("dve", lambda e: e.tensor_tensor(out=wzb[:, 128:256].rearrange("p (a b) -> p a b", a=2),
                                                            in0=mt[:, 256:320].rearrange("p (o t) -> p o t", o=1).to_broadcast([128, 2, 64]), in1=bm3, op=ALU.mult),
                          r=[mk, "cst"], w=[wbk])
                    st.append(s4b)

                    for i in range(5):
                        def lv(i=i):
                            last = (i == 4)
                            qc = qq[j][i % 2]
                            qck = f"qq{j}_{i % 2}"
                            qn = qq[j][(i + 1) % 2]
                            qnk = f"qq{j}_{(i + 1) % 2}"
                            if not last:
                                A("pe", lambda e: e.matmul(psum[4][:, 0:128], lhsT=wzb[:, 128:256], rhs=wzb[:, 0:128], start=True, stop=True), r=[wbk], w=["ps4"])
                            A("pe", lambda e: e.matmul(psum[4][:, 128:256], lhsT=wzb[:, 0:128], rhs=wzb[:, 128:256], start=True, stop=True), r=[wbk], w=["ps4"])
                            if not last:
                                A("act", lambda e: e.activation(out=wzb[:, 0:256], in_=psum[4][:, 0:256], func=AF.Identity), r=["ps4"], w=[wbk])
                            else:
                                A("act", lambda e: e.activation(out=wzb[:, 128:256], in_=psum[4][:, 128:256], func=AF.Identity), r=["ps4"], w=[wbk])
                            A("pe", lambda e: e.matmul(psum[1][:, 128:192], lhsT=wzb[:, 128:256], rhs=qc[:, :], start=True, stop=True), r=[wbk, qck], w=["ps1"])
                            A("dve", lambda e: e.tensor_tensor(out=qn[:], in0=psum[1][:, 128:192], in1=qc[:], op=ALU.add), r=["ps1", qck], w=[qnk])
                        st.append(lv)

                    def s5():
                        cbank = [5, 6, 7, 0][j]
                        cps = psum[cbank] if cbank != 7 else psTt
                        cpk = f"ps{cbank}"
                        q5 = qq[j][1]
                        q5k = f"qq{j}_1"
                        A("dve", lambda e: e.tensor_scalar(out=cn[:, 3, :], in0=H[:], scalar1=s[:, EI, 63:64], scalar2=None, op0=ALU.mult),
                          r=[Hk, sk + "ei"], w=[ck + "hpc"])
                        for p in hp:
                            A("pe", (lambda p: lambda e: e.matmul(cps[p, 0:64], lhsT=s[p, AT, :], rhs=H[p, :], start=True, stop=False))(p), r=[sk + "at", Hk], w=[cpk])
                            A("pe", (lambda p: lambda e: e.matmul(cps[p, 0:64], lhsT=mt[p, 128:192], rhs=tr[p, 0:64], start=False, stop=True))(p), r=[mk, tk], w=[cpk])
                        A("act", lambda e: e.activation(out=cn[:, 0, :], in_=cps[:, 0:64], func=AF.Identity), r=[cpk], w=[ck + "x"])
                        for p in hp:
                            A("pe", (lambda p: lambda e: e.matmul(cps[p, 64:128], lhsT=q5[p, :], rhs=cn[p, 0, :], start=True, stop=True))(p), r=[q5k, ck + "x"], w=[cpk])
                        A("act", lambda e: e.activation(out=cn[:, 1, :], in_=cps[:, 64:128], func=AF.Identity), r=[cpk], w=[ck + "u"])
                        for p in hp:
                            A("pe", (lambda p: lambda e: e.matmul(cps[p, 192:256], lhsT=tr[p, 64:128], rhs=cn[p, 1, :], start=True, stop=False))(p), r=[tk, ck + "u"], w=[cpk])
                            A("pe", (lambda p: lambda e: e.matmul(cps[p, 192:256], lhsT=tr[p, 128:192], rhs=tr[p, 0:64], start=False, stop=True))(p), r=[tk], w=[cpk])
                        for p in hp:
                            A("pe", (lambda p: lambda e: e.matmul(cps[p, 128:192], lhsT=s[p, RT, :], rhs=H[p, :], start=True, stop=False))(p), r=[sk + "rt", Hk], w=[cpk])
                            A("pe", (lambda p: lambda e: e.matmul(cps[p, 128:192], lhsT=mt[p, 64:128], rhs=cn[p, 1, :], start=False, stop=False))(p), r=[mk, ck + "u"], w=[cpk])
                            A("pe", (lambda p: lambda e: e.matmul(cps[p, 128:192], lhsT=mt[p, 192:256], rhs=tr[p, 0:64], start=False, stop=True))(p), r=[mk, tk], w=[cpk])
                        A("dve", lambda e: e.scalar_tensor_tensor(out=H[:], in0=cps[:, 192:256], scalar=s[:, EI, 63:64], in1=cn[:, 3, :], op0=ALU.mult, op1=ALU.add),
                          r=[cpk, sk + "ei", ck + "hpc"], w=[Hk])
                        g = gst[j]
                        gk = f"gst{j}"
                        A("dve", lambda e: e.bn_stats(out=g[:, 0:6], in_=cps[:, 128:192]), r=[cpk], w=[gk])
                        A("dve", lambda e: e.bn_aggr(out=g[:, 6:8], in_=g[:, 0:6]), r=[gk], w=[gk])
                        A("act", lambda e: e.activation(out=g[:, 8:9], in_=g[:, 7:8], func=AF.Sqrt, bias=float(64e-5)), r=[gk], w=[gk])
                        A("dve", lambda e: e.reciprocal(out=g[:, 8:9], in_=g[:, 8:9]), r=[gk], w=[gk])
                        A("dve", lambda e: e.tensor_scalar(out=cn[:, 2, :], in0=cps[:, 128:192], scalar1=g[:, 6:7], scalar2=g[:, 8:9], op0=ALU.subtract, op1=ALU.mult),
                          r=[cpk, gk], w=[ck + "yn"])
                        for p in hp:
                            A("pe", (lambda p: lambda e: e.matmul(psum[3][p, 192:256], lhsT=cn[p, 2, :], rhs=istack[p, :], start=True, stop=True))(p), r=[ck + "yn", "cst"], w=["ps3"])
                        A("act", lambda e: e.activation(out=ybT[:, j, col], in_=psum[3][:, 192:256], func=AF.Identity, scale=pcol("gn_w", j), bias=pcol("gn_b", j)),
                          r=["ps3", "prm"], w=[f"ybT{j}"])
                    st.append(s5)
                    return st

                for c in range(NCH):
                    stl = [unit_stages(j, c) for j in range(4)]
                    for si in range(len(stl[0])):
                        for j in range(4):
                            stl[j][si]()
                P.section(5)
                for j in range(4):
                    pj = pers[j]
                    pjk = f"pers{j}"
                    A("pool", (lambda pj, j: lambda e: e.tensor_tensor(out=ybT[:, j, :], in0=ybT[:, j, :], in1=pj[:, BON, :], op=ALU.add))(pj, j),
                      r=[f"ybT{j}", pjk + "BON"], w=[f"ybT{j}"])
                    A("pool", (lambda pj, j: lambda e: e.tensor_tensor(out=ycat[:, 4 + j, :], in0=ybT[:, j, :], in1=pj[:, GG, :], op=ALU.mult))(pj, j),
                      r=[f"ybT{j}", pjk + "GG"], w=["ycat"])

                P.section(6)
                for sub in range(NSUB):
                    tc_ = slice(sub * 128, (sub + 1) * 128)
                    for kc in range(8):
                        A("pe", (lambda kc, tc_: lambda e: e.matmul(psum[0][:, 0:512], lhsT=hT[:, kc, tc_], rhs=win[:, kc, 512:1024], start=(kc == 0), stop=(kc == 7)))(kc, tc_),
                          r=["hT", "win"], w=[pk(0)])
                    A("act", lambda e: e.activation(out=vg[:], in_=psum[0][:, 0:512], func=AF.Gelu_apprx_tanh), r=[pk(0)], w=["vg"])
                    A("dve", lambda e: e.bn_stats(out=st6[:, 2, :], in_=vg[:]), r=["vg"], w=["st6b"])
                    A("dve", lambda e: e.bn_aggr(out=mv[:, 4:6], in_=st6[:, 2, :]), r=["st6b"], w=["mvb"])
                    A("act", lambda e: e.activation(out=mv[:, 6:7], in_=mv[:, 5:6], func=AF.Sqrt, bias=float(1e-5)), r=["mvb"], w=["mvb"])
                    A("dve", lambda e: e.reciprocal(out=mv[:, 6:7], in_=mv[:, 6:7]), r=["mvb"], w=["mvb"])
                    A("dve", lambda e: e.tensor_scalar(out=vn[:], in0=vg[:], scalar1=mv[:, 4:5], scalar2=mv[:, 6:7], op0=ALU.subtract, op1=ALU.mult), r=["vg", "mvb"], w=["vg"])
                    A("pool", lambda e: e.tensor_tensor(out=vn[:], in0=vn[:], in1=bcs[:, BC["lnw"]:BC["lnw"] + 512], op=ALU.mult), r=["vg", "bc"], w=["vg"])
                    A("pool", lambda e: e.tensor_tensor(out=vn[:], in0=vn[:], in1=bcs[:, BC["lnb"]:BC["lnb"] + 512], op=ALU.add), r=["vg", "bc"], w=["vg"])
                    for h in range(8):
                        A("pe", (lambda h: lambda e: e.matmul(psum[1][(h % 2) * 64:(h % 2) * 64 + 64, (h // 2) * 128:(h // 2 + 1) * 128], lhsT=vn[:, h * 64:(h + 1) * 64],
                                                              rhs=wc[:, h, :], start=True, stop=True))(h), r=["vg", "wc"], w=[pk(1)])
                    A("dve", lambda e: e.tensor_tensor(out=gtmp.rearrange("p a b -> p (a b)"), in0=psum[1][:, 0:512], in1=bcs[:, BC["bsT"]:BC["bsT"] + 512], op=ALU.add),
                      r=[pk(1), "bc"], w=["h2f"])
                    A("dve", (lambda tc_: lambda e: e.tensor_tensor(out=ycat[:, 0:4, tc_], in0=gtmp, in1=u_s[:, :, tc_], op=ALU.mult))(tc_), r=["h2f", "u_s"], w=["ycat"])

                P.section(7)
                for sub in range(NSUB):
                    tc_ = slice(sub * 128, (sub + 1) * 128)
                    ti = TI * NSUB + sub
                    tok0 = t0 + sub * 128
                    for hf in range(2):
                        for cc in range(8):
                            A("pe", (lambda cc, hf, tc_: lambda e: e.matmul(psum[6][:, 0:512], lhsT=ycat[:, cc, tc_], rhs=wout[:, cc, hf * 512:(hf + 1) * 512],
                                                                           start=(cc == 0), stop=(cc == 7)))(cc, hf, tc_), r=["ycat", "wout"], w=[pk(6)])
                        A("dve", (lambda hf: lambda e: e.tensor_tensor(out=x1t[:, hf * 512:(hf + 1) * 512], in0=psum[6][:, 0:512], in1=gate_bc[:, hf * 512:(hf + 1) * 512],
                                                                      op=ALU.mult))(hf), r=[pk(6), "gate_bc"], w=["x1t"])
                        A("pool", (lambda hf, xb, sub: lambda e: e.tensor_tensor(out=x1t[:, hf * 512:(hf + 1) * 512], in0=x1t[:, hf * 512:(hf + 1) * 512],
                                                                                in1=xb[:, sub, hf * 512:(hf + 1) * 512], op=ALU.add))(hf, xb, sub), r=["x1t", xk], w=["x1t"])
                    P.add("sp", (lambda tok0: lambda e: e.dma_start(out=x1_d[tok0:tok0 + 128, :], in_=x1t[:]))(tok0), r=["x1t"], w=["x1_d"], dma=True)
                    norm_stats(lambda hf: x1t[:, hf * 512:(hf + 1) * 512], "x1t", 1e-6)
                    A("act", lambda e: e.activation(out=xn2[:], in_=x1t[:], func=AF.Identity, scale=mv[:, 3:4]), r=["x1t", "mv"], w=["x1t"])
                    for kc in range(8):
                        pb = kc // 4
                        A("pe", (lambda kc, pb: lambda e: e.matmul(psum[pb][:, (kc % 4) * 128:(kc % 4 + 1) * 128], lhsT=xn2[:, kc * 128:(kc + 1) * 128], rhs=ident, start=True, stop=True))(kc, pb),
                          r=["x1t", "cst"], w=[pk(pb)])
                    for kc in range(8):
                        pb = kc // 4
                        A("act", (lambda kc, pb: lambda e: e.activation(out=h2f[:, kc, :], in_=psum[pb][:, (kc % 4) * 128:(kc % 4 + 1) * 128], func=AF.Identity,
                                                                        scale=sc[:, 8 + kc:9 + kc], bias=mod[:, 24 + kc:25 + kc]))(kc, pb), r=[pk(pb), "sc", "mod"], w=["h2f"])
                    A("pool", lambda e: e.tensor_copy(out=xnb[:], in_=xn2[:]), r=["x1t"], w=["xnb"])
                    P.add("sp", (lambda tok0: lambda e: e.dma_start(out=xn2_d[tok0:tok0 + 128, :], in_=xnb[:]))(tok0),
                          r=["xnb"], w=["xn2_d"], dma=True)
                    for kc in range(8):
                        A("pe", (lambda kc: lambda e: e.matmul(psum[6][:, 0:36], lhsT=h2f[:, kc, :], rhs=wrs[:, kc, :], start=(kc == 0), stop=(kc == 7)))(kc),
                          r=["h2f", "wrs"], w=[pk(6)])
                    A("dve", (lambda ti: lambda e: e.tensor_tensor(out=logits[:, ti, :], in0=psum[6][:, 0:36], in1=bcs[:, BC["rb"]:BC["rb"] + 36], op=ALU.add))(ti),
                      r=[pk(6), "bc"], w=["logits"])

            P.muted = False
            P.barrier()
            ct.close()
            if stage >= 2:
                rt = SB(ca, "rt", [128, 32, 48])
                sel = SB(ca, "sel", [128, 32, 8])
                sel2 = SB(ca, "sel2", [128, 32, 8])
                ohg = SB(ca, "ohg", [128, 32, 4])
                tmp48 = SB(ca, "tmp48", [128, 32, 4, 8])
                lg = logits[:, :, 0:4]
                le = logits[:, :, 4:36].rearrange("p t (g e) -> p t g e", g=4)
                MG, SG, M1, M2, P1, W1, W2 = range(7)
                r1 = lambda i: rt[:, :, i:i + 1]
                R = lambda fn, r, w: A("dve", fn, r=r, w=w)
                R(lambda e: e.tensor_reduce(out=rt[:, :, MG], in_=lg, axis=AX.X, op=ALU.max), ["logits"], ["rt"])
                R(lambda e: e.tensor_tensor(out=ohg[:], in0=lg, in1=r1(MG).to_broadcast([128, 32, 4]), op=ALU.is_equal), ["logits", "rt"], ["ohg"])
                R(lambda e: e.tensor_tensor(out=rt[:, :, 8:12], in0=lg, in1=r1(MG).to_broadcast([128, 32, 4]), op=ALU.subtract), ["logits", "rt"], ["rt"])
                A("act", lambda e: e.activation(out=rt[:, :, 8:12], in_=rt[:, :, 8:12], func=AF.Exp), r=["rt"], w=["rt"])
                R(lambda e: e.tensor_reduce(out=rt[:, :, SG], in_=rt[:, :, 8:12], axis=AX.X, op=ALU.add), ["rt"], ["rt"])
                R(lambda e: e.reciprocal(out=rt[:, :, SG], in_=rt[:, :, SG]), ["rt"], ["rt"])
                R(lambda e: e.tensor_tensor(out=tmp48[:], in0=le, in1=ohg[:].rearrange("p t (g o) -> p t g o", o=1).to_broadcast([128, 32, 4, 8]), op=ALU.mult),
                  ["logits", "ohg"], ["tmp48"])
                R(lambda e: e.tensor_reduce(out=sel[:], in_=tmp48[:].rearrange("p t g e -> p t e g"), axis=AX.X, op=ALU.add), ["tmp48"], ["sel"])
                R(lambda e: e.tensor_reduce(out=rt[:, :, M1], in_=sel[:], axis=AX.X, op=ALU.max), ["sel"], ["rt"])
                R(lambda e: e.tensor_tensor(out=sel2[:], in0=sel[:], in1=r1(M1).to_broadcast([128, 32, 8]), op=ALU.is_equal), ["sel", "rt"], ["sel2"])
                R(lambda e: e.scalar_tensor_tensor(out=tmp48[:, :, 0, :], in0=sel2[:], scalar=-1e30, in1=sel[:], op0=ALU.mult, op1=ALU.add), ["sel", "sel2"], ["tmp48"])
                R(lambda e: e.tensor_reduce(out=rt[:, :, M2], in_=tmp48[:, :, 0, :], axis=AX.X, op=ALU.max), ["tmp48"], ["rt"])
                R(lambda e: e.tensor_tensor(out=tmp48[:, :, 1, :], in0=tmp48[:, :, 0, :], in1=r1(M2).to_broadcast([128, 32, 8]), op=ALU.is_equal), ["tmp48", "rt"], ["tmp48"])
                R(lambda e: e.tensor_tensor(out=rt[:, :, P1], in0=rt[:, :, M2], in1=rt[:, :, M1], op=ALU.subtract), ["rt"], ["rt"])
                A("act", lambda e: e.activation(out=rt[:, :, P1], in_=rt[:, :, P1], func=AF.Exp), r=["rt"], w=["rt"])
                R(lambda e: e.tensor_scalar(out=rt[:, :, P1], in0=rt[:, :, P1], scalar1=1.0, scalar2=None, op0=ALU.add), ["rt"], ["rt"])
                R(lambda e: e.reciprocal(out=rt[:, :, P1], in_=rt[:, :, P1]), ["rt"], ["rt"])
                R(lambda e: e.tensor_tensor(out=rt[:, :, W1], in0=rt[:, :, P1], in1=rt[:, :, SG], op=ALU.mult), ["rt"], ["rt"])
                R(lambda e: e.tensor_tensor(out=rt[:, :, W2], in0=rt[:, :, SG], in1=rt[:, :, W1], op=ALU.subtract), ["rt"], ["rt"])
                R(lambda e: e.tensor_tensor(out=sel[:], in0=sel2[:], in1=r1(W1).to_broadcast([128, 32, 8]), op=ALU.mult), ["sel2", "rt"], ["sel"])
                R(lambda e: e.tensor_tensor(out=sel2[:], in0=tmp48[:, :, 1, :], in1=r1(W2).to_broadcast([128, 32, 8]), op=ALU.mult), ["tmp48", "rt"], ["sel2"])
                R(lambda e: e.tensor_tensor(out=sel[:], in0=sel[:], in1=sel2[:], op=ALU.add), ["sel", "sel2"], ["sel"])
                for g in range(4):
                    R((lambda g: lambda e: e.tensor_tensor(out=wt[:, :, g * 8:(g + 1) * 8], in0=sel[:], in1=ohg[:, :, g:g + 1].to_broadcast([128, 32, 8]), op=ALU.mult))(g),
                      ["sel", "ohg"], ["wt"])
            if stage >= 2:
                Mf = SB(ca, "Mf", [128, 1024])
                Mb = SB(ca, "Mb", [128, 1024], BF16)
                Lsb = SB(ca, "Lsb", [128, 128], BF16)
                onesb = SB(ca, "onesb", [128, 128], BF16)
                PRE = SB(ca, "PRE", [128, 1024])
                CNTs = SB(ca, "CNTs", [128, 1024])
                CA_ = SB(ca, "CA", [128, 1024])
                CB_ = SB(ca, "CB", [128, 1024])
                sm = SB(ca, "sm", [128, 8, 32])
                cmpb = SB(ca, "cmpb", [128, 96, 32])
                bev = SB(ca, "bev", [128, 6, 96])
                pabf = SB(ca, "pabf", [128, 2, 32])
                v3 = lambda t: t[:].rearrange("p (a b) -> p a b", a=32)
                wtf = wt[:].rearrange("p t e -> p (t e)")
                R(lambda e: e.tensor_single_scalar(out=Mf[:], in_=wtf, scalar=0.0, op=ALU.is_gt), ["wt"], ["Mf"])
                A("pool", lambda e: e.tensor_copy(out=Mb[:], in_=Mf[:]), r=["Mf"], w=["Mb"])
                A("pool", lambda e: e.tensor_tensor(out=Lsb[:], in0=tril, in1=ident, op=ALU.subtract), r=["cst"], w=["Lsb"])
                A("pool", lambda e: e.tensor_copy(out=onesb[:], in_=ones), r=["cst"], w=["onesb"])
                for h in range(2):
                    A("pe", (lambda h: lambda e: e.matmul(psum[h][:, 0:512], lhsT=Lsb[:], rhs=Mb[:, h * 512:(h + 1) * 512], start=True, stop=True))(h),
                      r=["Lsb", "Mb"], w=[pk(h)])
                    A("pe", (lambda h: lambda e: e.matmul(psum[2 + h][:, 0:512], lhsT=onesb[:], rhs=Mb[:, h * 512:(h + 1) * 512], start=True, stop=True))(h),
                      r=["onesb", "Mb"], w=[pk(2 + h)])
                    A("act", (lambda h: lambda e: e.activation(out=PRE[:, h * 512:(h + 1) * 512], in_=psum[h][:, 0:512], func=AF.Identity))(h), r=[pk(h)], w=["PRE"])
                    A("act", (lambda h: lambda e: e.activation(out=CNTs[:, h * 512:(h + 1) * 512], in_=psum[2 + h][:, 0:512], func=AF.Identity))(h), r=[pk(2 + h)], w=["CNTs"])
                src, srck, dst, dstk = CNTs, "CNTs", CA_, "CA"
                for dd in (1, 2, 4, 8, 16):
                    w_ = dd * 32
                    R((lambda src, dst, w_: lambda e: e.tensor_copy(out=dst[:, 0:w_], in_=src[:, 0:w_]))(src, dst, w_), [srck], [dstk])
                    R((lambda src, dst, w_: lambda e: e.tensor_tensor(out=dst[:, w_:1024], in0=src[:, w_:1024], in1=src[:, 0:1024 - w_], op=ALU.add))(src, dst, w_), [srck], [dstk])
                    src, srck = dst, dstk
                    dst, dstk = (CB_, "CB") if dst is CA_ else (CA_, "CA")
                R(lambda e: e.tensor_tensor(out=CB_[:], in0=CA_[:], in1=CNTs[:], op=ALU.subtract), ["CA", "CNTs"], ["CB"])
                R(lambda e: e.tensor_copy(out=sm[:, 0, :], in_=CA_[:, 31 * 32:32 * 32]), ["CA"], ["sm"])
                R(lambda e: e.tensor_tensor(out=cmpb[:, 0:32, :], in0=sm[:, 0, :].rearrange("p (e o) -> p e o", o=1).to_broadcast([128, 32, 32]),
                                            in1=cst[:, CC["thr"]:CC["thr"] + 32].rearrange("p (o k) -> p o k", o=1).to_broadcast([128, 32, 32]), op=ALU.is_gt),
                  ["sm", "cst"], ["cmpb"])
                R(lambda e: e.tensor_reduce(out=sm[:, 1, :], in_=cmpb[:, 0:32, :], axis=AX.X, op=ALU.add), ["cmpb"], ["sm"])
                R(lambda e: e.tensor_tensor_scan(out=sm[:, 2, :], data0=ones[:, 0:32], data1=sm[:, 1, :], initial=0.0, op0=ALU.mult, op1=ALU.add), ["sm", "cst"], ["sm"])
                R(lambda e: e.tensor_tensor(out=sm[:, 3, :], in0=sm[:, 2, :], in1=sm[:, 1, :], op=ALU.subtract), ["sm"], ["sm"])
                R(lambda e: e.tensor_single_scalar(out=sm[:, 4, :], in_=sm[:, 3, :], scalar=128.0, op=ALU.mult), ["sm"], ["sm"])
                R(lambda e: e.tensor_tensor(out=v3(CB_), in0=v3(CB_), in1=sm[:, 4, :].rearrange("p (o e) -> p o e", o=1).to_broadcast([128, 32, 32]), op=ALU.add),
                  ["CB", "sm"], ["CB"])
                R(lambda e: e.tensor_tensor(out=PRE[:], in0=PRE[:], in1=CB_[:], op=ALU.add), ["PRE", "CB"], ["PRE"])
                R(lambda e: e.tensor_tensor(out=CA_[:], in0=PRE[:], in1=Mf[:], op=ALU.mult), ["PRE", "Mf", "sm"], ["CA"])
                R(lambda e: e.tensor_scalar(out=CB_[:], in0=Mf[:], scalar1=-1e9, scalar2=1e9, op0=ALU.mult, op1=ALU.add), ["Mf", "PRE"], ["CB"])
                R(lambda e: e.tensor_tensor(out=CB_[:], in0=CB_[:], in1=CA_[:], op=ALU.add), ["CB", "CA"], ["CB"])
                R(lambda e: e.tensor_reduce(out=pabf[:, 0, :], in_=v3(CB_), axis=AX.X, op=ALU.min), ["CB"], ["pabf"])
                R(lambda e: e.tensor_reduce(out=pabf[:, 1, :], in_=v3(CA_), axis=AX.X, op=ALU.max), ["CA"], ["pabf"])
                R(lambda e: e.tensor_tensor(out=v3(CB_), in0=v3(CB_), in1=pabf[:, 0, :].rearrange("p (t o) -> p t o", o=1).to_broadcast([128, 32, 32]), op=ALU.is_equal),
                  ["CB", "pabf"], ["CB"])
                R(lambda e: e.tensor_tensor(out=CB_[:], in0=CB_[:], in1=wtf, op=ALU.mult), ["CB", "wt"], ["CB"])
                R(lambda e: e.tensor_reduce(out=WAB[:, 0, :], in_=v3(CB_), axis=AX.X, op=ALU.add), ["CB"], ["WAB"])
                R(lambda e: e.tensor_tensor(out=WAB[:, 1, :], in0=rt[:, :, SG], in1=WAB[:, 0, :], op=ALU.subtract), ["rt", "WAB"], ["WAB"])
                R(lambda e: e.tensor_copy(out=PABi[:], in_=pabf[:]), ["pabf"], ["PABi"])
                R(lambda e: e.tensor_tensor(out=cmpb[:], in0=sm[:, 2, :].rearrange("p (o e) -> p o e", o=1).to_broadcast([128, 96, 32]),
                                            in1=cst[:, CC["iotab"]:CC["iotab"] + 96].rearrange("p (b o) -> p b o", o=1).to_broadcast([128, 96, 32]), op=ALU.is_le),
                  ["sm", "cst", "cmpb"], ["cmpb"])
                R(lambda e: e.tensor_reduce(out=bev[:, 0, :], in_=cmpb[:], axis=AX.X, op=ALU.add), ["cmpb"], ["bev"])
                R(lambda e: e.memset(bev[:, 1, 0:1], 1.0), [], ["bev"])
                R(lambda e: e.tensor_tensor(out=bev[:, 1, 1:96], in0=bev[:, 0, 1:96], in1=bev[:, 0, 0:95], op=ALU.not_equal), ["bev"], ["bev"])
                R(lambda e: e.tensor_scalar(out=bev[:, 2, :], in0=bev[:, 1, :], scalar1=-1e6, scalar2=1e6, op0=ALU.mult, op1=ALU.add), ["bev"], ["bev"])
                R(lambda e: e.scalar_tensor_tensor(out=bev[:, 2, :], in0=bev[:, 0, :], scalar=128.0, in1=bev[:, 2, :], op0=ALU.mult, op1=ALU.add), ["bev"], ["bev"])
                R(lambda e: e.tensor_scalar(out=bev[:, 2, :], in0=bev[:, 2, :], scalar1=cst[:, CC["pidx"]:CC["pidx"] + 1], scalar2=None, op0=ALU.add), ["bev", "cst"], ["bev"])
                R(lambda e: e.tensor_copy(out=IDXW[:, 0:1, :], in_=bev[:, 2:3, :]), ["bev"], ["IDXW"])
            P.barrier()

        if stage >= 3:
            with ExitStack() as cb:
                IOA = bass.IndirectOffsetOnAxis
                NROW = NE * 128
                _bc = {}

                def bc_reg(e):
                    if "r" not in _bc:
                        _bc["r"] = e.to_reg(NROW - 1)
                    return _bc["r"]
                W32 = SB(cb, "W32", [128, 12288])
                idb2 = SB(cb, "idb2", [128, 128], BF16)
                xload = [SB(cb, f"xl{i}", [128, D], BF16) for i in range(2)]
                xsb = [SB(cb, f"xsb{i}", [128, D], BF16) for i in range(2)]
                XgT_l = [SB(cb, f"XgT{i}", [128, 8, 128], BF16) for i in range(2)]
                Wgb = SB(cb, "Wgb", [128, 8, EH], BF16)
                Wub = SB(cb, "Wub", [128, 8, EH], BF16)
                Wdb = SB(cb, "Wdb", [128, 4, D], BF16)
                sg_l = [SB(cb, f"sg{i}", [128, 512]) for i in range(2)]
                actb_l = [SB(cb, f"actb{i}", [128, 512], BF16) for i in range(2)]
                actT_l = [SB(cb, f"actT{i}", [128, 4, 128], BF16) for i in range(2)]
                yblk = [SB(cb, f"yblk{i}", [128, D]) for i in range(2)]
                fgt = SB(cb, "fgt", [128, D])
                fgs = SB(cb, "fgs", [128, D])
                g2bc = SB(cb, "g2bc", [128, D])
                yA = SB(cb, "yA", [128, D])
                yB = SB(cb, "yB", [128, D])
                fst = SB(cb, "fst", [128, 2, 6])
                fmv = SB(cb, "fmv", [128, 4])
                P.add("sp", lambda e: e.dma_start(out=fgs[:], in_=fg_d[:, :]), w=["fgs"], dma=True)
                P.add("dve", lambda e: e.tensor_copy(out=idb2[:], in_=ident), r=["cst"], w=["idb2"])
                make_gate(g2bc, "g2bc", 40)
                zt = SB(cb, "zt", [128, D], BF16)
                P.add("pool", lambda e: e.memset(zt[:], 0.0), w=["zt"])
                xz_keys = []
                for b in range(NBLK):
                    P.add("sp", (lambda b: lambda e: e.dma_start(out=xs_d[b * 128:(b + 1) * 128, :], in_=zt[:]))(b), r=["zt"], w=[f"xz{b}"], dma=True)
                    xz_keys.append(f"xz{b}")
                sc_keys = []
                for i in range(32):
                    xl = xload[i % 2]
                    xlk = f"xl{i % 2}"
                    P.add("sp", (lambda xl, i: lambda e: e.dma_start(out=xl[:], in_=xn2_d[i * 128:(i + 1) * 128, :]))(xl, i), r=["xn2_d"], w=[xlk], dma=True)
                    for k in range(2):
                        key = f"xsc{i}_{k}"
                        P.add("pool", (lambda xl, i, k: lambda e: e.indirect_dma_start(out=xs_d[:, :], out_offset=IOA(ap=PABi[:, k, i:i + 1], axis=0),
                                                                                       in_=xl[:], in_offset=None))(xl, i, k),
                              r=[xlk, "PABi"] + xz_keys, w=[key], dma=True)
                        sc_keys.append(key)
                ys_keys = []
                def do_block(b):
                    P.add("pool", (lambda b: lambda e: e.indirect_dma_start(out=W32[:], out_offset=None, in_=wall_d[:, :],
                                                                           in_offset=IOA(ap=IDXW[:, 0, b:b + 1], axis=0), bounds_check=bc_reg(e), oob_is_err=False))(b),
                          r=["IDXW", "W32"], w=["W32"], dma=True)
                    P.add("act", lambda e: e.activation(out=Wgb[:].rearrange("p a b -> p (a b)"), in_=W32[:, 0:4096], func=AF.Identity), r=["W32"], w=["Wgb"])
                    P.add("dve", lambda e: e.tensor_copy(out=Wub[:].rearrange("p a b -> p (a b)"), in_=W32[:, 4096:8192]), r=["W32"], w=["Wub"])
                    P.add("pool", lambda e: e.tensor_copy(out=Wdb[:, 0:2, :].rearrange("p a b -> p (a b)"), in_=W32[:, 8192:10240]), r=["W32"], w=["Wdb"])
                    P.add("dve", lambda e: e.tensor_copy(out=Wdb[:, 2:4, :].rearrange("p a b -> p (a b)"), in_=W32[:, 10240:12288]), r=["W32"], w=["Wdb"])
                    XgT = XgT_l[b % 2]
                    xgk = f"XgT{b % 2}"
                    sg = sg_l[b % 2]
                    sgk = f"sg{b % 2}"
                    actb = actb_l[b % 2]
                    abk = f"actb{b % 2}"
                    actT = actT_l[b % 2]
                    atk = f"actT{b % 2}"
                    xb_ = xsb[b % 2]
                    xbk = f"xsb{b % 2}"
                    P.add("sp", (lambda xb_, b: lambda e: e.dma_start(out=xb_[:], in_=xs_d[b * 128:(b + 1) * 128, :]))(xb_, b), r=sc_keys, w=[xbk], dma=True)
                    for kc in range(8):
                        P.add("pe", (lambda xb_, kc: lambda e: e.matmul(psum[kc // 4][:, (kc % 4) * 128:(kc % 4 + 1) * 128], lhsT=xb_[:, kc * 128:(kc + 1) * 128],
                                                                       rhs=idb2[:], start=True, stop=True))(xb_, kc), r=[xbk, "idb2"], w=[pk(kc // 4)])
                    for kc in range(8):
                        P.add("act", (lambda kc: lambda e: e.activation(out=XgT[:, kc, :], in_=psum[kc // 4][:, (kc % 4) * 128:(kc % 4 + 1) * 128], func=AF.Identity,
                                                                        scale=sc[:, 8 + kc:9 + kc], bias=mod[:, 24 + kc:25 + kc]))(kc), r=[pk(kc // 4), "sc", "mod"], w=[xgk])
                    for kc in range(8):
                        P.add("pe", (lambda kc: lambda e: e.matmul(psum[2][:, 0:512], lhsT=XgT[:, kc, :], rhs=Wgb[:, kc, :], start=(kc == 0), stop=(kc == 7)))(kc),
                              r=[xgk, "Wgb"], w=[pk(2)])
                    for kc in range(8):
                        P.add("pe", (lambda kc: lambda e: e.matmul(psum[3][:, 0:512], lhsT=XgT[:, kc, :], rhs=Wub[:, kc, :], start=(kc == 0), stop=(kc == 7)))(kc),
                              r=[xgk, "Wub"], w=[pk(3)])
                    P.add("act", lambda e: e.activation(out=sg[:], in_=psum[2][:, 0:512], func=AF.Silu), r=[pk(2)], w=[sgk])
                    P.add("dve", lambda e: e.tensor_tensor(out=actb[:], in0=psum[3][:, 0:512], in1=sg[:], op=ALU.mult), r=[pk(3), sgk], w=[abk])
                    for hc in range(4):
                        P.add("pe", (lambda hc: lambda e: e.matmul(psum[4][:, hc * 128:(hc + 1) * 128], lhsT=actb[:, hc * 128:(hc + 1) * 128], rhs=idb2[:],
                                                                   start=True, stop=True))(hc), r=[abk, "idb2"], w=[pk(4)])
                    P.add("act", lambda e: e.activation(out=actT[:].rearrange("p a b -> p (a b)"), in_=psum[4][:, 0:512], func=AF.Identity), r=[pk(4)], w=[atk])
                    yb_ = yblk[b % 2]
                    ybk = f"yblk{b % 2}"
                    for hf in range(2):
                        for hc in range(4):
                            P.add("pe", (lambda hc, hf: lambda e: e.matmul(psum[5 + hf][:, 0:512], lhsT=actT[:, hc, :], rhs=Wdb[:, hc, hf * 512:(hf + 1) * 512],
                                                                           start=(hc == 0), stop=(hc == 3)))(hc, hf), r=[atk, "Wdb"], w=[pk(5 + hf)])
                        if hf == 0:
                            P.add("act", (lambda yb_: lambda e: e.activation(out=yb_[:, 0:512], in_=psum[5][:, 0:512], func=AF.Identity))(yb_), r=[pk(5)], w=[ybk])
                        else:
                            P.add("dve", (lambda yb_: lambda e: e.tensor_copy(out=yb_[:, 512:1024], in_=psum[6][:, 0:512]))(yb_), r=[pk(6)], w=[ybk])
                    key = f"ys{b}"
                    P.add("sp", (lambda yb_, b: lambda e: e.dma_start(out=ys_d[b * 128:(b + 1) * 128, :], in_=yb_[:]))(yb_, b), r=[ybk], w=[key], dma=True)
                    ys_keys.append(key)

                for b in range(min(NBLK, nblk)):
                    do_block(b)
                for i in range(32):
                    tok0 = i * 128
                    P.add("pool", (lambda i: lambda e: e.indirect_dma_start(out=yA[:], out_offset=None, in_=ys_d[:, :], in_offset=IOA(ap=PABi[:, 0, i:i + 1], axis=0)))(i),
                          r=ys_keys + ["PABi", "yA"], w=["yA"], dma=True)
                    P.add("pool", (lambda i: lambda e: e.indirect_dma_start(out=yB[:], out_offset=None, in_=ys_d[:, :], in_offset=IOA(ap=PABi[:, 1, i:i + 1], axis=0)))(i),
                          r=ys_keys + ["PABi", "yB"], w=["yB"], dma=True)
                    P.add("sp", (lambda tok0: lambda e: e.dma_start(out=fgt[:], in_=x1_d[tok0:tok0 + 128, :]))(tok0), r=["x1_d"], w=["fgt"], dma=True)
                    P.add("dve", (lambda i: lambda e: e.tensor_scalar(out=yA[:], in0=yA[:], scalar1=WAB[:, 0, i:i + 1], scalar2=None, op0=ALU.mult))(i), r=["yA", "WAB"], w=["yA"])
                    P.add("dve", (lambda i: lambda e: e.scalar_tensor_tensor(out=yA[:], in0=yB[:], scalar=WAB[:, 1, i:i + 1], in1=yA[:], op0=ALU.mult, op1=ALU.add))(i),
                          r=["yA", "yB", "WAB"], w=["yA"])
                    P.add("pool", lambda e: e.tensor_tensor(out=yA[:], in0=yA[:], in1=g2bc[:], op=ALU.mult), r=["yA", "g2bc"], w=["yA"])
                    P.add("dve", lambda e: e.tensor_tensor(out=fgt[:], in0=fgt[:], in1=yA[:], op=ALU.add), r=["yA", "fgt"], w=["fgt"])
                    for hf in range(2):
                        P.add("dve", (lambda hf: lambda e: e.bn_stats(out=fst[:, hf, :], in_=fgt[:, hf * 512:(hf + 1) * 512]))(hf), r=["fgt"], w=["fst"])
                    P.add("dve", lambda e: e.bn_aggr(out=fmv[:, 0:2], in_=fst[:, 0:2, :].rearrange("p a b -> p (a b)")), r=["fst"], w=["fmv"])
                    P.add("dve", lambda e: e.scalar_tensor_tensor(out=fmv[:, 2:3], in0=fmv[:, 0:1], scalar=fmv[:, 0:1], in1=fmv[:, 1:2], op0=ALU.mult, op1=ALU.add),
                          r=["fmv"], w=["fmv"])
                    P.add("act", lambda e: e.activation(out=fmv[:, 3:4], in_=fmv[:, 2:3], func=AF.Sqrt, bias=float(1e-6)), r=["fmv"], w=["fmv"])
                    P.add("dve", lambda e: e.reciprocal(out=fmv[:, 3:4], in_=fmv[:, 3:4]), r=["fmv"], w=["fmv"])
                    P.add("dve", lambda e: e.scalar_tensor_tensor(out=fgt[:], in0=fgt[:], scalar=fmv[:, 3:4], in1=fgs[:], op0=ALU.mult, op1=ALU.mult),
                          r=["fgt", "fmv", "fgs"], w=["fgt"])
                    P.add("sp", (lambda tok0: lambda e: e.dma_start(out=out_d[tok0:tok0 + 128, :], in_=fgt[:]))(tok0), r=["fgt"], w=["out_d"], dma=True)
        else:
            with ExitStack() as cb:
                fgt = SB(cb, "fgt", [128, D])
                for ti in range(32):
                    tok0 = ti * 128
                    P.add("sp", (lambda tok0: lambda e: e.dma_start(out=fgt[:], in_=x1_d[tok0:tok0 + 128, :]))(tok0), r=["x1_d"], w=["fgt"], dma=True)
                    P.add("sp", (lambda tok0: lambda e: e.dma_start(out=out_d[tok0:tok0 + 128, :], in_=fgt[:]))(tok0), r=["fgt"], w=["out_d"], dma=True)
        P.emit()
    return nc


_NC_CACHE = {}


def _layouts(inp):
    f = np.float32
    g = lambda k: np.asarray(inp[k], dtype=f)
    col = lambda v, n: np.ascontiguousarray(v.reshape(n, 128).T)
    mu = g("rwkv_mu")[0]
    def pad128(v):
        o = np.zeros((128, 1), f)
        o[:v.shape[0], 0] = v
        return o
    shared = [None, col(g("ada_b")[0], 48), col(g("norm1_g")[0], 8), col(g("norm2_g")[0], 8), col(mu[0:1536], 12),
              pad128(mu[1536:1568]), pad128(mu[1568:1600]), pad128(mu[1600:1696]), col(g("rwkv_w0")[0], 4), col(g("rwkv_a0")[0], 4),
              col(g("rwkv_k_k")[0], 4), col(g("rwkv_k_a")[0], 4), col(g("rwkv_r_k")[0].reshape(512), 4), col(g("rwkv_gn_w")[0], 4),
              col(g("rwkv_gn_b")[0], 4)]
    tt = np.arange(64)
    su = (tt[:, None] < tt[None, :]).astype(f)
    iu = (tt[:, None] <= tt[None, :]).astype(f)
    sl = (tt[None, :] < tt[:, None]).astype(f)
    m5 = np.tile(np.concatenate([su, iu, su, iu, sl], axis=1), (2, 1))
    ident = np.eye(128, dtype=f)
    istack = np.tile(np.eye(64, dtype=f), (2, 1))
    ss = np.arange(128)
    tril = (ss[:, None] <= ss[None, :]).astype(f)
    bones = (ss[:, None] // 64 == ss[None, :] // 64).astype(f)
    ones = np.ones((128, 128), f)
    thr = np.broadcast_to((np.arange(32) * 128).astype(f)[None, :], (128, 32))
    iotab = np.broadcast_to(np.arange(96).astype(f)[None, :], (128, 96))
    pidx = np.arange(128).astype(f)[:, None]
    cst = np.ascontiguousarray(np.concatenate([m5, ident, istack, tril, bones, ones, thr, iotab, pidx], axis=1))
    assert cst.shape[1] == NCST
    rep = lambda v: np.broadcast_to(v[None, :], (128, v.shape[0]))
    bs = g("gmlp_bs")[0]
    bsT = np.zeros((128, 4, 128), f)
    for j in range(4):
        for hh in range(2):
            bsT[hh * 64:(hh + 1) * 64, j, :] = bs[2 * j + hh][None, :]
    bc = np.ascontiguousarray(np.concatenate([rep(g("gmlp_ln_w")[0]), rep(g("gmlp_ln_b")[0]),
                                              rep(np.concatenate([g("router_group_b")[0], g("router_expert_b")[0]])),
                                              bsT.reshape(128, 512)], axis=1))
    assert bc.shape[1] == NBC
    common = dict(cst=cst, bc=bc, fg=np.ascontiguousarray(rep(g("final_norm_g"))), ada_w=np.ascontiguousarray(g("ada_w")[0]), w_in=np.ascontiguousarray(g("w_in")[0]),
                  w_out=np.ascontiguousarray(g("w_out")[0]), w2=np.ascontiguousarray(g("rwkv_w2")[0]),
                  a2=np.ascontiguousarray(g("rwkv_a2")[0]), g2=np.ascontiguousarray(g("rwkv_g2")[0]),
                  wsT=np.ascontiguousarray(g("gmlp_ws")[0].transpose(2, 0, 1)),
                  wr=np.ascontiguousarray(np.concatenate([g("router_group_w")[0], g("router_expert_w")[0]], axis=1)),
                  wall=np.ascontiguousarray(np.concatenate([
                      g("moe_w_gate")[0].reshape(NE, 8, 128, EH).transpose(0, 2, 1, 3).reshape(NE * 128, 4096),
                      g("moe_w_up")[0].reshape(NE, 8, 128, EH).transpose(0, 2, 1, 3).reshape(NE * 128, 4096),
                      g("moe_w_down")[0].reshape(NE, 4, 128, D).transpose(0, 2, 1, 3).reshape(NE * 128, 4096)], axis=1)))
    x = g("x")
    c = g("c")
    maps = []
    for b in range(x.shape[0]):
        cols = [col(c[b], 8)] + shared[1:]
        prm = np.ascontiguousarray(np.concatenate(cols, axis=1))
        assert prm.shape[1] == NPRM
        m = dict(common)
        m["x"] = np.ascontiguousarray(x[b])
        m["prm"] = prm
        maps.append(m)
    return maps


def kernel(**inputs):
    maps = _layouts(inputs)
    if "nc" not in _NC_CACHE:
        _NC_CACHE["nc"] = build_nc()
    nc = _NC_CACHE["nc"]
    res = run_bass_kernel_spmd(nc, maps, core_ids=list(range(len(maps))))
    return np.stack([r["out"] for r in res.results], axis=0).astype(np.float32)
```

```python
import numpy as np
import os
from contextlib import ExitStack
import concourse.bass as bass
import concourse.mybir as mybir
from concourse.bass_utils import run_bass_kernel_spmd

F32 = mybir.dt.float32
BF16 = mybir.dt.bfloat16
I32 = mybir.dt.int32
AF = mybir.ActivationFunctionType
ALU = mybir.AluOpType
AX = mybir.AxisListType

D = 1024
S = 4096
TT = 256
NT = S // TT
NSUB = TT // 128
CH = 64
NCH = TT // CH
INW = 2720
NE = 32
EH = 512
NEG_HALF_E = -0.6065306597126334

PC = {}
_o = 0
for _n, _w in [("cT", 8), ("ada_b", 48), ("n1g", 8), ("n2g", 8), ("mu_rkv", 12), ("mu_xw", 1),
               ("mu_xa", 1), ("mu_xg", 1), ("w0", 4), ("a0", 4), ("k_k", 4), ("k_a", 4), ("r_k", 4),
               ("gn_w", 4), ("gn_b", 4)]:
    PC[_n] = _o
    _o += _w
NPRM = _o
CC = {}
_o = 0
for _n, _w in [("m5", 320), ("ident", 128), ("istack", 64), ("tril", 128), ("bones", 128), ("ones", 128), ("thr", 32), ("iotab", 96), ("pidx", 1)]:
    CC[_n] = _o
    _o += _w
NCST = _o
BC = {}
_o = 0
for _n, _w in [("lnw", 512), ("lnb", 512), ("rb", 36), ("bsT", 512)]:
    BC[_n] = _o
    _o += _w
NBC = _o


ATTACH_WAIT = True
NO_SELF_SYNC = ("pe", "act")


class Prog:
    def __init__(self, nc, ctx):
        self.nc = nc
        self.ops = []
        self.last_w = {}
        self.readers = {}
        self.engs = ["pe", "act", "dve", "pool", "sp"]
        self.count = {e: 0 for e in self.engs}
        self.sem = {e: ctx.enter_context(nc.semaphore("s_" + e)) for e in self.engs}
        self.NDS = 8
        self.dsem = {q: [ctx.enter_context(nc.semaphore(f"d_{q}{i}")) for i in range(self.NDS)]
                     for q in ("sp", "pool")}
        self.dcount = {"sp": 0, "pool": 0}
        self.last_op = {e: None for e in self.engs}
        self.pending = {e: set() for e in self.engs}
        self.recent_dma = {"sp": [], "pool": []}

    def section(self, k):
        self.muted = k > self.cut

    def add(self, eng, fn, r=(), w=(), dma=False):
        if getattr(self, 'muted', False):
            return None
        idx = len(self.ops)
        deps = set(self.pending[eng])
        self.pending[eng] = set()
        for k in r:
            if k in self.last_w:
                deps.add(self.last_w[k])
        for k in w:
            if k in self.last_w:
                deps.add(self.last_w[k])
            deps.update(self.readers.get(k, ()))
        for k in r:
            self.readers.setdefault(k, []).append(idx)
        for k in w:
            self.last_w[k] = idx
            self.readers[k] = []
        if dma:
            q = eng
            kq = self.dcount[q]
            self.dcount[q] += 1
            sem = self.dsem[q][kq % self.NDS]
            val = 16 * (kq // self.NDS + 1)
            prev = (sem, val - 16) if val > 16 else None
            self.recent_dma[q].append(idx)
            self.recent_dma[q] = self.recent_dma[q][-self.NDS:]
        else:
            self.count[eng] += 1
            sem = self.sem[eng]
            val = self.count[eng]
            prev = None
        self.ops.append(dict(eng=eng, fn=fn, deps=deps, dma=dma, sem=sem, val=val, prev=prev))
        self.last_op[eng] = idx
        return idx

    def barrier(self):
        allops = set()
        for e in self.engs:
            if self.last_op[e] is not None:
                allops.add(self.last_op[e])
        for q in ("sp", "pool"):
            allops.update(self.recent_dma[q])
        dmaops = set()
        for q in ("sp", "pool"):
            dmaops.update(self.recent_dma[q])
        for e in self.engs:
            self.pending[e] |= (allops - dmaops) if e == "pe" else allops

    def emit(self):
        nc = self.nc
        per = {e: [] for e in self.engs}
        for i, op in enumerate(self.ops):
            per[op["eng"]].append(i)
        ops = self.ops

        def run(name, e):
            waited = {}
            for i in per[name]:
                op = ops[i]
                need = []
                for j in op["deps"]:
                    d = ops[j]
                    if d["eng"] == name and not d["dma"] and name in NO_SELF_SYNC:
                        continue
                    need.append((d["sem"], d["val"]))
                if op["prev"] is not None:
                    need.append(op["prev"])
                need.sort(key=lambda t: -t[1])
                todo = []
                for sem, val in need:
                    key = id(sem)
                    if waited.get(key, 0) >= val:
                        continue
                    waited[key] = val
                    todo.append((sem, val))
                attach = todo.pop() if (todo and ATTACH_WAIT) else None
                for sem, val in todo:
                    e.wait_ge(sem, val)
                inst = op["fn"](e)
                if attach is not None:
                    inst._wait_ge(attach[0], attach[1])
                inst.then_inc(op["sem"], 16 if op["dma"] else 1)
            if name in ("sp", "pool"):
                kq = self.dcount[name]
                for s_i in range(min(kq, self.NDS)):
                    n_on = (kq - s_i + self.NDS - 1) // self.NDS
                    e.wait_ge(self.dsem[name][s_i], 16 * n_on)

        with nc.Block() as block:
            @block.sync
            def _(e):
                run("sp", e)

            @block.scalar
            def _(e):
                run("act", e)

            @block.vector
            def _(e):
                run("dve", e)

            @block.tensor
            def _(e):
                run("pe", e)

            @block.gpsimd
            def _(e):
                run("pool", e)


def build_nc(stage=99, ntiles=NT, cut=99, nblk=96):
    nc = bass.Bass("TRN2", target_bir_lowering=False)
    dt = lambda name, shape, dty, kind: nc.dram_tensor(name, shape, dty, kind=kind).ap()
    x_d = dt("x", [S, D], F32, "ExternalInput")
    prm_d = dt("prm", [128, NPRM], F32, "ExternalInput")
    cst_d = dt("cst", [128, NCST], F32, "ExternalInput")
    bc_d = dt("bc", [128, NBC], F32, "ExternalInput")
    adaw_d = dt("ada_w", [D, 6 * D], F32, "ExternalInput")
    win_d = dt("w_in", [D, INW], F32, "ExternalInput")
    wout_d = dt("w_out", [D, D], F32, "ExternalInput")
    w2_d = dt("w2", [32, 512], F32, "ExternalInput")
    a2_d = dt("a2", [32, 512], F32, "ExternalInput")
    g2_d = dt("g2", [96, 512], F32, "ExternalInput")
    wsT_d = dt("wsT", [128, 8, 128], F32, "ExternalInput")
    wr_d = dt("wr", [D, 36], F32, "ExternalInput")
    fg_d = dt("fg", [128, D], F32, "ExternalInput")
    wall_d = dt("wall", [NE * 128, 12288], F32, "ExternalInput")
    NBLK = 96
    xn2_d = dt("xn2_d", [S, D], BF16, "Internal")
    xs_d = dt("xs_d", [NBLK * 128, D], BF16, "Internal")
    ys_d = dt("ys_d", [NBLK * 128, D], F32, "Internal")
    out_d = dt("out", [S, D], F32, "ExternalOutput")
    x1_d = dt("x1_d", [S, D], F32, "Internal")

    with ExitStack() as top:
        P = Prog(nc, top)
        P.cut = cut
        psum = [top.enter_context(nc.psum_tensor(f"ps{i}", [128, 512], F32)) for i in range(7)]
        psTt = top.enter_context(nc.psum_tensor("psT", [128, 512], F32))
        psT = [psTt[:, 0:256], psum[6][:, 0:256]]
        psTk = ["ps7", "ps6"]
        fst = None
        fmv = None
        pk = lambda i: f"ps{i}"

        def SB(ctx, name, shape, dty=F32):
            return ctx.enter_context(nc.sbuf_tensor(name, shape, dty))

        prm = SB(top, "prm_s", [128, NPRM])
        cst = SB(top, "cst_s", [128, NCST])
        bcs = SB(top, "bc_s", [128, NBC])
        mod = SB(top, "mod", [128, 48])
        wt = SB(top, "wt", [128, 32, 32])
        PABi = SB(top, "PABi", [128, 2, 32], I32)
        WAB = SB(top, "WAB", [128, 2, 32])
        IDXW = SB(top, "IDXW", [128, 4, 96], I32)
        P.add("sp", lambda e: e.dma_start(out=prm[:], in_=prm_d[:, :]), w=["prm"], dma=True)
        P.add("sp", lambda e: e.dma_start(out=cst[:], in_=cst_d[:, :]), w=["cst"], dma=True)
        P.add("sp", lambda e: e.dma_start(out=bcs[:], in_=bc_d[:, :]), w=["bc"], dma=True)

        def pcol(name, j=0, n=1):
            return prm[:, PC[name] + j:PC[name] + j + n]

        ident = cst[:, CC["ident"]:CC["ident"] + 128]
        istack = cst[:, CC["istack"]:CC["istack"] + 64]
        tril = cst[:, CC["tril"]:CC["tril"] + 128]
        bones = cst[:, CC["bones"]:CC["bones"] + 128]
        ones = cst[:, CC["ones"]:CC["ones"] + 128]
        m5 = cst[:, CC["m5"]:CC["m5"] + 320]

        with ExitStack() as c0:
            stg = [SB(c0, f"ada_stg{i}", [128, 6 * D]) for i in range(2)]
            for kc in range(8):
                sb = stg[kc % 2]
                P.add("sp", (lambda sb, kc: lambda e: e.dma_start(out=sb[:], in_=adaw_d[kc * 128:(kc + 1) * 128, :]))(sb, kc),
                      w=[f"ada_stg{kc % 2}"], dma=True)
                for oc in range(48):
                    P.add("pe", (lambda sb, kc, oc: lambda e: e.matmul(psum[0][:, oc:oc + 1], lhsT=sb[:, oc * 128:(oc + 1) * 128],
                                                                   rhs=prm[:, PC["cT"] + kc:PC["cT"] + kc + 1], start=True, stop=True))(sb, kc, oc),
                          r=[f"ada_stg{kc % 2}", "prm"], w=[pk(0)])
                if kc == 0:
                    P.add("dve", lambda e: e.tensor_tensor(out=mod[:], in0=psum[0][:, 0:48], in1=prm[:, PC["ada_b"]:PC["ada_b"] + 48], op=ALU.add),
                          r=[pk(0), "prm"], w=["mod"])
                else:
                    P.add("dve", lambda e: e.tensor_tensor(out=mod[:], in0=psum[0][:, 0:48], in1=mod[:], op=ALU.add),
                          r=[pk(0), "mod"], w=["mod"])
            P.barrier()
        sc = SB(top, "sc", [128, 32])
        P.add("dve", lambda e: e.scalar_tensor_tensor(out=sc[:, 0:8], in0=mod[:, 8:16], scalar=1.0, in1=prm[:, PC["n1g"]:PC["n1g"] + 8],
                                                      op0=ALU.add, op1=ALU.mult), r=["mod", "prm"], w=["sc"])
        P.add("dve", lambda e: e.scalar_tensor_tensor(out=sc[:, 8:16], in0=mod[:, 32:40], scalar=1.0, in1=prm[:, PC["n2g"]:PC["n2g"] + 8],
                                                      op0=ALU.add, op1=ALU.mult), r=["mod", "prm"], w=["sc"])
        gate_bc = SB(top, "gate_bc", [128, D])
        gl = SB(top, "gl", [128, 128])

        def make_gate(dst, dkey, gcol):
            for c in range(8):
                P.add("dve", (lambda c: lambda e: e.tensor_scalar(out=gl[:], in0=ones, scalar1=mod[:, gcol + c:gcol + c + 1], scalar2=None,
                                                                  op0=ALU.mult))(c), r=["mod", "cst"], w=["gl"])
                P.add("pe", (lambda c: lambda e: e.matmul(psum[1][:, (c % 4) * 128:(c % 4 + 1) * 128], lhsT=gl[:], rhs=ident, start=True, stop=True))(c),
                      r=["gl", "cst"], w=[pk(1)])
                P.add("act", (lambda c: lambda e: e.activation(out=dst[:, c * 128:(c + 1) * 128], in_=psum[1][:, (c % 4) * 128:(c % 4 + 1) * 128],
                                                               func=AF.Identity))(c), r=[pk(1)], w=[dkey])
        make_gate(gate_bc, "gate_bc", 16)

        with ExitStack() as ca:
            win = SB(ca, "win", [128, 8, INW], BF16)
            wout = SB(ca, "wout", [128, 8, D], BF16)
            identb = SB(ca, "identb", [128, 128], BF16)
            w2s = SB(ca, "w2s", [32, 512])
            a2s = SB(ca, "a2s", [32, 512])
            g2s = SB(ca, "g2s", [96, 512])
            wrs = SB(ca, "wrs", [128, 8, 36])
            wc = SB(ca, "wc", [128, 8, 128])
            logits = SB(ca, "logits", [128, 32, 36])
            Hst = [SB(ca, f"H{j}", [128, 64]) for j in range(4)]
            Hb_l = [SB(ca, f"Hb{j}", [128, 64], BF16) for j in range(4)]
            istb = SB(ca, "istb", [128, 64], BF16)
            carry = SB(ca, "carry", [128, 16])
            P.add("pool", lambda e: e.tensor_copy(out=identb[:], in_=ident), r=["cst"], w=["identb"])
            P.add("sp", lambda e: e.dma_start(out=w2s[:], in_=w2_d[:, :]), w=["w2s"], dma=True)
            P.add("sp", lambda e: e.dma_start(out=a2s[:], in_=a2_d[:, :]), w=["a2s"], dma=True)
            P.add("sp", lambda e: e.dma_start(out=g2s[:], in_=g2_d[:, :]), w=["g2s"], dma=True)
            P.add("sp", lambda e: e.dma_start(out=wrs[:], in_=wr_d.rearrange("(c p) n -> p c n", p=128)), w=["wrs"], dma=True)
            P.add("sp", lambda e: e.dma_start(out=wc[:], in_=wsT_d[:, :, :]), w=["wc"], dma=True)
            for h in range(8):
                P.add("pool", (lambda h: lambda e: e.tensor_tensor(out=wc[:, h, :], in0=wc[:, h, :], in1=tril, op=ALU.mult))(h),
                      r=["wc", "cst"], w=["wc"])
            P.add("pool", lambda e: e.memset(carry[:], 0.0), w=["carry"])
            for _i in range(int(os.environ.get("KPAD", "0"))):
                P.add("pool", lambda e: e.memset(gl[:, 0:1], 0.0), w=["gl_dummy"])
            for j in range(4):
                P.add("pool", (lambda j: lambda e: e.memset(Hst[j][:], 0.0))(j), w=[f"H{j}"])
                P.add("pool", (lambda j: lambda e: e.memset(Hb_l[j][:], 0.0))(j), w=[f"Hb{j}"])
            P.add("pool", lambda e: e.tensor_copy(out=istb[:], in_=istack), r=["cst"], w=["istb"])
            with ExitStack() as cw:
                wstg = [SB(cw, f"wstg{i}", [128, INW]) for i in range(2)]
                for kc in range(8):
                    sb = wstg[kc % 2]
                    P.add("sp", (lambda sb, kc: lambda e: e.dma_start(out=sb[:], in_=win_d[kc * 128:(kc + 1) * 128, :]))(sb, kc),
                          w=[f"wstg{kc % 2}"], dma=True)
                    P.add("pool", (lambda sb, kc: lambda e: e.tensor_copy(out=win[:, kc, :], in_=sb[:]))(sb, kc),
                          r=[f"wstg{kc % 2}"], w=["win"])
                for kc in range(8):
                    sb = wstg[kc % 2]
                    P.add("sp", (lambda sb, kc: lambda e: e.dma_start(out=sb[:, 0:D], in_=wout_d[kc * 128:(kc + 1) * 128, :]))(sb, kc),
                          w=[f"wstg{kc % 2}"], dma=True)
                    P.add("pool", (lambda sb, kc: lambda e: e.tensor_copy(out=wout[:, kc, :], in_=sb[:, 0:D]))(sb, kc),
                          r=[f"wstg{kc % 2}"], w=["wout"])
                P.barrier()

            ct = ExitStack()
            xt = [SB(ct, "xt0", [128, NSUB, D])] * 2
            xn = SB(ct, "xn", [128, NSUB, D], BF16)
            hT = SB(ct, "hT", [128, 8, TT], BF16)
            st6 = SB(ct, "st6", [128, 4, 6])
            mv = SB(ct, "mv", [128, 8])
            u_s = SB(ct, "u_s", [128, 4, TT], BF16)
            vg = SB(ct, "vg", [128, 512])
            vn = vg
            zraw = [SB(ct, f"zraw{i}", [128, TT + 1]) for i in range(2)]
            rkv = SB(ct, "rkv", [128, 12, TT])
            lor = SB(ct, "lor", [128, 3, TT])
            LW, ASN, BSN, KM, BON, GG = range(6)
            pers = [SB(ct, f"pers{j}", [128, 6, TT]) for j in range(4)]
            AA, KX, RN, PRD = range(4)
            ptmp = SB(ct, "ptmp", [128, 4, TT])
            ybT = SB(ct, "ybT", [128, 4, TT])
            ycat = SB(ct, "ycat", [128, 8, TT], BF16)
            x1t = SB(ct, "x1t", [128, D])
            xn2 = x1t
            h2f = SB(ct, "h2f", [128, 8, 128])
            gtmp = h2f[:, 0:4, :]
            xnb = SB(ct, "xnb", [128, D], BF16)
            CI, CE, EI, EE, EN = range(5)
            sct = [SB(ct, f"sct{j}", [128, 5, 64]) for j in range(4)]
            sb4 = [SB(ct, f"sb4{j}", [128, 4, 64], BF16) for j in range(4)]
            mats = [SB(ct, f"mats{j}", [128, 320], BF16) for j in range(4)]
            trs = [SB(ct, f"trs{j}", [128, 192], BF16) for j in range(4)]
            wzb_l = [SB(ct, f"wzb{j}", [128, 256], BF16) for j in range(4)]
            qq = [[SB(ct, f"qq{j}_{i}", [128, 64], BF16) for i in range(2)] for j in range(4)]
            chn = [SB(ct, f"chn{j}", [128, 3, 64], BF16) for j in range(4)]
            hpc_l = [SB(ct, f"hpc{j}", [128, 64]) for j in range(4)]
            gst = [SB(ct, f"gst{j}", [128, 12]) for j in range(4)]

            def A(eng, fn, r=(), w=()):
                P.add(eng, fn, r=r, w=w)

            def norm_stats(src, srck, eps):
                for hf in range(2):
                    A("dve", (lambda hf: lambda e: e.bn_stats(out=st6[:, hf, :], in_=src(hf)))(hf), r=[srck], w=["st6"])
                A("dve", lambda e: e.bn_aggr(out=mv[:, 0:2], in_=st6[:, 0:2, :].rearrange("p a b -> p (a b)")), r=["st6"], w=["mv"])
                A("dve", lambda e: e.scalar_tensor_tensor(out=mv[:, 2:3], in0=mv[:, 0:1], scalar=mv[:, 0:1], in1=mv[:, 1:2], op0=ALU.mult, op1=ALU.add),
                  r=["mv"], w=["mv"])
                A("act", lambda e: e.activation(out=mv[:, 3:4], in_=mv[:, 2:3], func=AF.Sqrt, bias=float(eps)), r=["mv"], w=["mv"])
                A("dve", lambda e: e.reciprocal(out=mv[:, 3:4], in_=mv[:, 3:4]), r=["mv"], w=["mv"])

            for TI in range(min(NT, ntiles) if stage >= 1 else 0):
                t0 = TI * TT
                xb = xt[TI % 2]
                xk = "xt0"
                P.add("sp", (lambda xb, t0: lambda e: e.dma_start(out=xb[:], in_=x_d[t0:t0 + TT, :].rearrange("(s p) d -> p s d", p=128)))(xb, t0),
                      w=[xk], dma=True)
                P.section(1)
                for sub in range(NSUB):
                    norm_stats((lambda xb, sub: lambda hf: xb[:, sub, hf * 512:(hf + 1) * 512])(xb, sub), xk, 1e-6)
                    A("act", (lambda xb, sub: lambda e: e.activation(out=xn[:, sub, :], in_=xb[:, sub, :], func=AF.Identity, scale=mv[:, 3:4]))(xb, sub),
                      r=[xk, "mv"], w=["xn"])
                P.section(1.5)
                for kc in range(8):
                    pb = kc % 2
                    for sub in range(NSUB):
                        A("pe", (lambda kc, sub, pb: lambda e: e.matmul(psT[pb][:, sub * 128:(sub + 1) * 128], lhsT=xn[:, sub, kc * 128:(kc + 1) * 128],
                                                                       rhs=identb[:], start=True, stop=True))(kc, sub, pb), r=["xn", "identb"], w=[psTk[pb]])
                    if os.environ.get("DVE_EVAC"):
                      A("dve", (lambda kc, pb: lambda e: e.tensor_scalar(out=hT[:, kc, :], in0=psT[pb][:, 0:TT], scalar1=sc[:, kc:kc + 1], scalar2=mod[:, kc:kc + 1],
                                                                         op0=ALU.mult, op1=ALU.add))(kc, pb), r=[psTk[pb], "sc", "mod"], w=["hT"])
                    elif not os.environ.get("SKIP_EVAC"):
                      A("act", (lambda kc, pb: lambda e: e.activation(out=hT[:, kc, :], in_=psT[pb][:, 0:TT], func=AF.Identity,
                                                                    **({} if os.environ.get("NO_SB") else dict(scale=sc[:, kc:kc + 1], bias=mod[:, kc:kc + 1]))))(kc, pb),
                      r=[psTk[pb], "sc", "mod"], w=["hT"])

                P.section(2)
                pcnt = [0]

                def proj(c0, M):
                    pb = pcnt[0] % 2
                    pcnt[0] += 1
                    for kc in range(8):
                        A("pe", (lambda kc, pb: lambda e: e.matmul(psum[pb][0:M, 0:TT], lhsT=win[:, kc, c0:c0 + M], rhs=hT[:, kc, :],
                                                                  start=(kc == 0), stop=(kc == 7)))(kc, pb), r=["win", "hT"], w=[pk(pb)])
                    return pb

                for j in range(4):
                    pb = proj(j * 128, 128)
                    A("act", (lambda j, pb: lambda e: e.activation(out=u_s[:, j, :], in_=psum[pb][:, 0:TT], func=AF.Gelu_apprx_tanh))(j, pb),
                      r=[pk(pb)], w=["u_s"])
                specs = [(1024 + q * 128, 128, PC["mu_rkv"] + q) for q in range(12)] + \
                        [(2560, 32, PC["mu_xw"]), (2592, 32, PC["mu_xa"]), (2624, 96, PC["mu_xg"])]
                for q, (c0, M, mucol) in enumerate(specs):
                    pb = proj(c0, M)
                    zb = zraw[q % 2]
                    zk = f"zraw{q % 2}"
                    dst = rkv[0:M, q, :] if q < 12 else lor[0:M, q - 12, :]
                    dk = f"rkv{q}"
                    A("dve", (lambda zb, q, M: lambda e: e.tensor_copy(out=zb[0:M, 0:1], in_=carry[0:M, q:q + 1]))(zb, q, M), r=["carry"], w=[zk])
                    A("act", (lambda zb, pb, M: lambda e: e.activation(out=zb[0:M, 1:TT + 1], in_=psum[pb][0:M, 0:TT], func=AF.Identity))(zb, pb, M),
                      r=[pk(pb)], w=[zk])
                    A("dve", (lambda zb, q, M: lambda e: e.tensor_copy(out=carry[0:M, q:q + 1], in_=zb[0:M, TT:TT + 1]))(zb, q, M), r=[zk], w=["carry"])
                    A("dve", (lambda zb, dst, M: lambda e: e.tensor_tensor(out=dst, in0=zb[0:M, 0:TT], in1=zb[0:M, 1:TT + 1], op=ALU.subtract))(zb, dst, M),
                      r=[zk], w=[dk])
                    A("dve", (lambda zb, dst, M, mucol: lambda e: e.scalar_tensor_tensor(out=dst, in0=dst, scalar=prm[0:M, mucol:mucol + 1], in1=zb[0:M, 1:TT + 1],
                                                                                      op0=ALU.mult, op1=ALU.add))(zb, dst, M, mucol), r=[zk, dk, "prm"], w=[dk])
                A("act", lambda e: e.activation(out=lor[0:32, 0, :], in_=lor[0:32, 0, :], func=AF.Tanh), r=["rkv12"], w=["rkv12"])
                A("act", lambda e: e.activation(out=lor[0:96, 2, :], in_=lor[0:96, 2, :], func=AF.Sigmoid), r=["rkv14"], w=["rkv14"])

                P.section(3)
                for j in range(4):
                    jc = slice(j * 128, (j + 1) * 128)
                    pj = pers[j]
                    pjk = f"pers{j}"
                    rr = rkv[:, j, :]
                    kr = rkv[:, 4 + j, :]
                    vr = rkv[:, 8 + j, :]
                    A("pe", (lambda jc: lambda e: e.matmul(psum[0][:, 0:TT], lhsT=w2s[0:32, jc], rhs=lor[0:32, 0, :], start=True, stop=True))(jc),
                      r=["w2s", "rkv12"], w=[pk(0)])
                    A("act", (lambda pj, j: lambda e: e.activation(out=pj[:, LW, :], in_=psum[0][:, 0:TT], func=AF.Sigmoid, bias=pcol("w0", j)))(pj, j),
                      r=[pk(0), "prm"], w=[pjk + "LW"])
                    A("pool", (lambda pj: lambda e: e.tensor_scalar(out=pj[:, LW, :], in0=pj[:, LW, :], scalar1=NEG_HALF_E, scalar2=None, op0=ALU.mult))(pj),
                      r=[pjk + "LW"], w=[pjk + "LW"])
                    A("pe", (lambda jc: lambda e: e.matmul(psum[1][:, 0:TT], lhsT=a2s[0:32, jc], rhs=lor[0:32, 1, :], start=True, stop=True))(jc),
                      r=["a2s", "rkv13"], w=[pk(1)])
                    A("act", (lambda j: lambda e: e.activation(out=ptmp[:, AA, :], in_=psum[1][:, 0:TT], func=AF.Sigmoid, bias=pcol("a0", j)))(j),
                      r=[pk(1), "prm"], w=["pAA"])
                    A("pe", (lambda jc: lambda e: e.matmul(psum[0][:, 0:TT], lhsT=g2s[0:96, jc], rhs=lor[0:96, 2, :], start=True, stop=True))(jc),
                      r=["g2s", "rkv14"], w=[pk(0)])
                    A("act", (lambda pj: lambda e: e.activation(out=pj[:, GG, :], in_=psum[0][:, 0:TT], func=AF.Identity))(pj), r=[pk(0)], w=[pjk + "GG"])
                    A("dve", (lambda kr, j: lambda e: e.tensor_scalar(out=ptmp[:, KX, :], in0=kr, scalar1=pcol("k_k", j), scalar2=None, op0=ALU.mult))(kr, j),
                      r=[f"rkv{4 + j}", "prm"], w=["pKX"])
                    A("pool", lambda e: e.tensor_tensor(out=ptmp[:, RN, :], in0=ptmp[:, KX, :], in1=ptmp[:, KX, :], op=ALU.mult), r=["pKX"], w=["pRN"])
                    A("pe", lambda e: e.matmul(psum[1][:, 0:TT], lhsT=bones, rhs=ptmp[:, RN, :], start=True, stop=True), r=["cst", "pRN"], w=[pk(1)])
                    A("act", lambda e: e.activation(out=ptmp[:, RN, :], in_=psum[1][:, 0:TT], func=AF.Sqrt, bias=float(1e-24)), r=[pk(1)], w=["pRN"])
                    A("dve", lambda e: e.reciprocal(out=ptmp[:, RN, :], in_=ptmp[:, RN, :]), r=["pRN"], w=["pRN"])
                    A("dve", (lambda pj: lambda e: e.scalar_tensor_tensor(out=pj[:, ASN, :], in0=ptmp[:, KX, :], scalar=-1.0, in1=ptmp[:, RN, :],
                                                                          op0=ALU.mult, op1=ALU.mult))(pj), r=["pKX", "pRN"], w=[pjk + "ASN"])
                    A("dve", (lambda pj: lambda e: e.scalar_tensor_tensor(out=pj[:, BSN, :], in0=pj[:, ASN, :], scalar=-1.0, in1=ptmp[:, AA, :],
                                                                          op0=ALU.mult, op1=ALU.mult))(pj), r=[pjk + "ASN", "pAA"], w=[pjk + "BSN"])
                    A("dve", (lambda j: lambda e: e.tensor_scalar(out=ptmp[:, PRD, :], in0=ptmp[:, AA, :], scalar1=-1.0, scalar2=pcol("k_a", j),
                                                                  op0=ALU.add, op1=ALU.mult))(j), r=["pAA", "prm"], w=["pPRD"])
                    A("dve", (lambda pj, kr: lambda e: e.scalar_tensor_tensor(out=pj[:, KM, :], in0=ptmp[:, PRD, :], scalar=1.0, in1=kr,
                                                                              op0=ALU.add, op1=ALU.mult))(pj, kr), r=["pPRD", f"rkv{4 + j}"], w=[pjk + "KM"])
                    A("dve", (lambda pj, rr, j: lambda e: e.scalar_tensor_tensor(out=ptmp[:, PRD, :], in0=rr, scalar=pcol("r_k", j), in1=pj[:, KM, :],
                                                                                 op0=ALU.mult, op1=ALU.mult))(pj, rr, j), r=[f"rkv{j}", "prm", pjk + "KM"], w=["pPRD"])
                    A("pe", lambda e: e.matmul(psum[0][:, 0:TT], lhsT=bones, rhs=ptmp[:, PRD, :], start=True, stop=True), r=["cst", "pPRD"], w=[pk(0)])
                    A("dve", (lambda pj, vr: lambda e: e.tensor_tensor(out=pj[:, BON, :], in0=psum[0][:, 0:TT], in1=vr, op=ALU.mult))(pj, vr),
                      r=[pk(0), f"rkv{8 + j}"], w=[pjk + "BON"])

                P.section(4)
                def unit_stages(j, c):
                    col = slice(c * CH, (c + 1) * CH)
                    pj = pers[j]
                    pjk = f"pers{j}"
                    s = sct[j]
                    sk = f"sct{j}"
                    mt = mats[j]
                    mk = f"mats{j}"
                    tr = trs[j]
                    tk = f"trs{j}"
                    cn = chn[j]
                    ck = f"chn{j}"
                    H = Hst[j]
                    Hk = f"H{j}"
                    Hb = Hb_l[j]
                    Hbk = f"Hb{j}"
                    b4 = sb4[j]
                    hpc = hpc_l[j]
                    rr = rkv[:, j, col]
                    vr = rkv[:, 8 + j, col]
                    hp = [slice(0, 64), slice(64, 128)]
                    st = []

                    def s1():
                        A("dve", lambda e: e.tensor_tensor_scan(out=s[:, CI, :], data0=ones[:, 0:64], data1=pj[:, LW, col], initial=0.0, op0=ALU.mult, op1=ALU.add),
                          r=[pjk + "LW", "cst"], w=[sk + "ci"])
                        A("dve", lambda e: e.tensor_tensor(out=s[:, CE, :], in0=s[:, CI, :], in1=pj[:, LW, col], op=ALU.subtract),
                          r=[sk + "ci", pjk + "LW"], w=[sk + "ce"])
                    st.append(s1)

                    def s2():
                        A("act", lambda e: e.activation(out=s[:, EI, :], in_=s[:, CI, :], func=AF.Exp), r=[sk + "ci"], w=[sk + "ei"])
                        A("act", lambda e: e.activation(out=s[:, EE, :], in_=s[:, CE, :], func=AF.Exp), r=[sk + "ce"], w=[sk + "ee"])
                        A("act", lambda e: e.activation(out=s[:, EN, :], in_=s[:, CI, :], func=AF.Exp, scale=-1.0), r=[sk + "ci"], w=[sk + "en"])
                    st.append(s2)

                    def s3():
                        A("dve", lambda e: e.tensor_tensor(out=b4[:, 0, :], in0=pj[:, ASN, col], in1=s[:, EE, :], op=ALU.mult), r=[pjk + "ASN", sk + "ee"], w=[sk + "at"])
                        A("dve", lambda e: e.tensor_tensor(out=b4[:, 1, :], in0=rr, in1=s[:, EI, :], op=ALU.mult), r=[f"rkv{j}", sk + "ei"], w=[sk + "rt"])
                        A("dve", lambda e: e.tensor_tensor(out=b4[:, 2, :], in0=pj[:, BSN, col], in1=s[:, EN, :], op=ALU.mult), r=[pjk + "BSN", sk + "en"], w=[sk + "bt"])
                        A("dve", lambda e: e.tensor_tensor(out=b4[:, 3, :], in0=pj[:, KM, col], in1=s[:, EN, :], op=ALU.mult), r=[pjk + "KM", sk + "en"], w=[sk + "kt"])
                    st.append(s3)

                    def s4():
                        for p in hp:
                            A("pe", (lambda p: lambda e: e.matmul(psum[2][p, 0:128], lhsT=b4[p, 2, :], rhs=b4[p, 0:2, :].rearrange("p a b -> p (a b)"), start=True, stop=True))(p),
                              r=[sk + "bt", sk + "at", sk + "rt"], w=["ps2"])
                            A("pe", (lambda p: lambda e: e.matmul(psum[2][p, 128:256], lhsT=b4[p, 3, :], rhs=b4[p, 0:2, :].rearrange("p a b -> p (a b)"), start=True, stop=True))(p),
                              r=[sk + "kt", sk + "at", sk + "rt"], w=["ps2"])
                            A("pe", (lambda p: lambda e: e.matmul(psum[2][p, 256:320], lhsT=b4[p, 0, :], rhs=b4[p, 2, :], start=True, stop=True))(p),
                              r=[sk + "bt", sk + "at"], w=["ps2"])
                        A("dve", lambda e: e.tensor_tensor(out=mt[:], in0=psum[2][:, 0:320], in1=m5, op=ALU.mult), r=["ps2", "cst"], w=[mk])
                        for p in hp:
                            A("pe", (lambda p: lambda e: e.matmul(psum[3][p, 0:64], lhsT=rkv[p, 8 + j, col], rhs=istack[p, :], start=True, stop=True))(p),
                              r=[f"rkv{8 + j}", "cst"], w=["ps3"])
                            A("pe", (lambda p: lambda e: e.matmul(psum[3][p, 64:128], lhsT=b4[p, 2, :], rhs=istb[p, :], start=True, stop=True))(p),
                              r=[sk + "bt", "istb"], w=["ps3"])
                            A("pe", (lambda p: lambda e: e.matmul(psum[3][p, 128:192], lhsT=b4[p, 3, :], rhs=istb[p, :], start=True, stop=True))(p),
                              r=[sk + "kt", "istb"], w=["ps3"])
                        A("act", lambda e: e.activation(out=tr[:], in_=psum[3][:, 0:192], func=AF.Identity), r=["ps3"], w=[tk])
                        A("dve", lambda e: e.tensor_tensor(out=qq[j][0][:], in0=mt[:, 0:64], in1=istack, op=ALU.add), r=[mk, "cst"], w=[f"qq{j}_0"])
                    st.append(s4)

                    wzb = wzb_l[j]
                    wbk = f"wzb{j}"
                    bm3 = bones.rearrange("p (a b) -> p a b", a=2)

                    def s4b():
                        A("dve", lambda e: e.tensor_tensor(out=wzb[:, 0:128].rearrange("p (a b) -> p a b", a=2),
                                                            in0=mt[:, 0:64].rearrange("p (o t) -> p o t", o=1).to_broadcast([128, 2, 64]), in1=bm3, op=ALU.mult),
                          r=[mk, "cst"], w=[wbk])
                        A("dve", lambda e: e.tensor_tensor(out=wzb[:, 128:256].rearrange("p (a b) -> p a b", a=2),
                                                            in0=mt[:, 256:320].rearrange("p (o t) -> p o t", o=1).to_broadcast([128, 2, 64]), in1=bm3, op=ALU.mult),
                          r=[mk, "cst"], w=[wbk])
                    st.append(s4b)

                    for i in range(5):
                        def lv(i=i):
                            last = (i == 4)
                            qc = qq[j][i % 2]
                            qck = f"qq{j}_{i % 2}"
                            qn = qq[j][(i + 1) % 2]
                            qnk = f"qq{j}_{(i + 1) % 2}"
                            if not last:
                                A("pe", lambda e: e.matmul(psum[4][:, 0:128], lhsT=wzb[:, 128:256], rhs=wzb[:, 0:128], start=True, stop=True), r=[wbk], w=["ps4"])
                            A("pe", lambda e: e.matmul(psum[4][:, 128:256], lhsT=wzb[:, 0:128], rhs=wzb[:, 128:256], start=True, stop=True), r=[wbk], w=["ps4"])
                            if not last:
                                A("act", lambda e: e.activation(out=wzb[:, 0:256], in_=psum[4][:, 0:256], func=AF.Identity), r=["ps4"], w=[wbk])
                            else:
                                A("act", lambda e: e.activation(out=wzb[:, 128:256], in_=psum[4][:, 128:256], func=AF.Identity), r=["ps4"], w=[wbk])
                            A("pe", lambda e: e.matmul(psum[1][:, 128:192], lhsT=wzb[:, 128:256], rhs=qc[:, :], start=True, stop=True), r=[wbk, qck], w=["ps1"])
                            A("dve", lambda e: e.tensor_tensor(out=qn[:], in0=psum[1][:, 128:192], in1=qc[:], op=ALU.add), r=["ps1", qck], w=[qnk])
                        st.append(lv)

                    def s5():
                        cbank = [5, 6, 7, 0][j]
                        cps = psum[cbank] if cbank != 7 else psTt
                        cpk = f"ps{cbank}"
                        q5 = qq[j][1]
                        q5k = f"qq{j}_1"
                        A("dve", lambda e: e.tensor_scalar(out=hpc[:], in0=H[:], scalar1=s[:, EI, 63:64], scalar2=None, op0=ALU.mult),
                          r=[Hk, sk + "ei"], w=[ck + "hpc"])
                        for p in hp:
                            A("pe", (lambda p: lambda e: e.matmul(cps[p, 0:64], lhsT=b4[p, 0, :], rhs=Hb[p, :], start=True, stop=False))(p), r=[sk + "at", Hbk], w=[cpk])
                            A("pe", (lambda p: lambda e: e.matmul(cps[p, 0:64], lhsT=mt[p, 128:192], rhs=tr[p, 0:64], start=False, stop=True))(p), r=[mk, tk], w=[cpk])
                        A("act", lambda e: e.activation(out=cn[:, 0, :], in_=cps[:, 0:64], func=AF.Identity), r=[cpk], w=[ck + "x"])
                        for p in hp:
                            A("pe", (lambda p: lambda e: e.matmul(cps[p, 64:128], lhsT=q5[p, :], rhs=cn[p, 0, :], start=True, stop=True))(p), r=[q5k, ck + "x"], w=[cpk])
                        A("act", lambda e: e.activation(out=cn[:, 1, :], in_=cps[:, 64:128], func=AF.Identity), r=[cpk], w=[ck + "u"])
                        for p in hp:
                            A("pe", (lambda p: lambda e: e.matmul(cps[p, 192:256], lhsT=tr[p, 64:128], rhs=cn[p, 1, :], start=True, stop=False))(p), r=[tk, ck + "u"], w=[cpk])
                            A("pe", (lambda p: lambda e: e.matmul(cps[p, 192:256], lhsT=tr[p, 128:192], rhs=tr[p, 0:64], start=False, stop=True))(p), r=[tk], w=[cpk])
                        for p in hp:
                            A("pe", (lambda p: lambda e: e.matmul(cps[p, 128:192], lhsT=b4[p, 1, :], rhs=Hb[p, :], start=True, stop=False))(p), r=[sk + "rt", Hbk], w=[cpk])
                            A("pe", (lambda p: lambda e: e.matmul(cps[p, 128:192], lhsT=mt[p, 64:128], rhs=cn[p, 1, :], start=False, stop=False))(p), r=[mk, ck + "u"], w=[cpk])
                            A("pe", (lambda p: lambda e: e.matmul(cps[p, 128:192], lhsT=mt[p, 192:256], rhs=tr[p, 0:64], start=False, stop=True))(p), r=[mk, tk], w=[cpk])
                        A("dve", lambda e: e.scalar_tensor_tensor(out=Hb[:], in0=cps[:, 192:256], scalar=s[:, EI, 63:64], in1=hpc[:], op0=ALU.mult, op1=ALU.add),
                          r=[cpk, sk + "ei", ck + "hpc"], w=[Hbk])
                        A("dve", lambda e: e.scalar_tensor_tensor(out=H[:], in0=cps[:, 192:256], scalar=s[:, EI, 63:64], in1=hpc[:], op0=ALU.mult, op1=ALU.add),
                          r=[cpk, sk + "ei", ck + "hpc"], w=[Hk])
                        g = gst[j]
                        gk = f"gst{j}"
                        A("dve", lambda e: e.bn_stats(out=g[:, 0:6], in_=cps[:, 128:192]), r=[cpk], w=[gk])
                        A("dve", lambda e: e.bn_aggr(out=g[:, 6:8], in_=g[:, 0:6]), r=[gk], w=[gk])
                        A("act", lambda e: e.activation(out=g[:, 8:9], in_=g[:, 7:8], func=AF.Sqrt, bias=float(64e-5)), r=[gk], w=[gk])
                        A("dve", lambda e: e.reciprocal(out=g[:, 8:9], in_=g[:, 8:9]), r=[gk], w=[gk])
                        A("dve", lambda e: e.tensor_scalar(out=cn[:, 2, :], in0=cps[:, 128:192], scalar1=g[:, 6:7], scalar2=g[:, 8:9], op0=ALU.subtract, op1=ALU.mult),
                          r=[cpk, gk], w=[ck + "yn"])
                        for p in hp:
                            A("pe", (lambda p: lambda e: e.matmul(psum[3][p, 192:256], lhsT=cn[p, 2, :], rhs=istb[p, :], start=True, stop=True))(p), r=[ck + "yn", "istb"], w=["ps3"])
                        A("act", lambda e: e.activation(out=ybT[:, j, col], in_=psum[3][:, 192:256], func=AF.Identity, scale=pcol("gn_w", j), bias=pcol("gn_b", j)),
                          r=["ps3", "prm"], w=[f"ybT{j}"])
                    st.append(s5)
                    return st

                for c in range(NCH):
                    stl = [unit_stages(j, c) for j in range(4)]
                    for si in range(len(stl[0])):
                        for j in range(4):
                            stl[j][si]()
                P.section(5)
                for j in range(4):
                    pj = pers[j]
                    pjk = f"pers{j}"
                    A("pool", (lambda pj, j: lambda e: e.tensor_tensor(out=ybT[:, j, :], in0=ybT[:, j, :], in1=pj[:, BON, :], op=ALU.add))(pj, j),
                      r=[f"ybT{j}", pjk + "BON"], w=[f"ybT{j}"])
                    A("pool", (lambda pj, j: lambda e: e.tensor_tensor(out=ycat[:, 4 + j, :], in0=ybT[:, j, :], in1=pj[:, GG, :], op=ALU.mult))(pj, j),
                      r=[f"ybT{j}", pjk + "GG"], w=["ycat"])

                P.section(6)
                for sub in range(NSUB):
                    tc_ = slice(sub * 128, (sub + 1) * 128)
                    for kc in range(8):
                        A("pe", (lambda kc, tc_: lambda e: e.matmul(psum[0][:, 0:512], lhsT=hT[:, kc, tc_], rhs=win[:, kc, 512:1024], start=(kc == 0), stop=(kc == 7)))(kc, tc_),
                          r=["hT", "win"], w=[pk(0)])
                    A("act", lambda e: e.activation(out=vg[:], in_=psum[0][:, 0:512], func=AF.Gelu_apprx_tanh), r=[pk(0)], w=["vg"])
                    A("dve", lambda e: e.bn_stats(out=st6[:, 2, :], in_=vg[:]), r=["vg"], w=["st6b"])
                    A("dve", lambda e: e.bn_aggr(out=mv[:, 4:6], in_=st6[:, 2, :]), r=["st6b"], w=["mvb"])
                    A("act", lambda e: e.activation(out=mv[:, 6:7], in_=mv[:, 5:6], func=AF.Sqrt, bias=float(1e-5)), r=["mvb"], w=["mvb"])
                    A("dve", lambda e: e.reciprocal(out=mv[:, 6:7], in_=mv[:, 6:7]), r=["mvb"], w=["mvb"])
                    A("dve", lambda e: e.tensor_scalar(out=vn[:], in0=vg[:], scalar1=mv[:, 4:5], scalar2=mv[:, 6:7], op0=ALU.subtract, op1=ALU.mult), r=["vg", "mvb"], w=["vg"])
                    A("pool", lambda e: e.tensor_tensor(out=vn[:], in0=vn[:], in1=bcs[:, BC["lnw"]:BC["lnw"] + 512], op=ALU.mult), r=["vg", "bc"], w=["vg"])
                    A("pool", lambda e: e.tensor_tensor(out=vn[:], in0=vn[:], in1=bcs[:, BC["lnb"]:BC["lnb"] + 512], op=ALU.add), r=["vg", "bc"], w=["vg"])
                    for h in range(8):
                        A("pe", (lambda h: lambda e: e.matmul(psum[1][(h % 2) * 64:(h % 2) * 64 + 64, (h // 2) * 128:(h // 2 + 1) * 128], lhsT=vn[:, h * 64:(h + 1) * 64],
                                                              rhs=wc[:, h, :], start=True, stop=True))(h), r=["vg", "wc"], w=[pk(1)])
                    A("dve", lambda e: e.tensor_tensor(out=gtmp.rearrange("p a b -> p (a b)"), in0=psum[1][:, 0:512], in1=bcs[:, BC["bsT"]:BC["bsT"] + 512], op=ALU.add),
                      r=[pk(1), "bc"], w=["h2f"])
                    A("dve", (lambda tc_: lambda e: e.tensor_tensor(out=ycat[:, 0:4, tc_], in0=gtmp, in1=u_s[:, :, tc_], op=ALU.mult))(tc_), r=["h2f", "u_s"], w=["ycat"])

                P.section(7)
                for sub in range(NSUB):
                    tc_ = slice(sub * 128, (sub + 1) * 128)
                    ti = TI * NSUB + sub
                    tok0 = t0 + sub * 128
                    for hf in range(2):
                        for cc in range(8):
                            A("pe", (lambda cc, hf, tc_: lambda e: e.matmul(psum[6][:, 0:512], lhsT=ycat[:, cc, tc_], rhs=wout[:, cc, hf * 512:(hf + 1) * 512],
                                                                           start=(cc == 0), stop=(cc == 7)))(cc, hf, tc_), r=["ycat", "wout"], w=[pk(6)])
                        A("dve", (lambda hf: lambda e: e.tensor_tensor(out=x1t[:, hf * 512:(hf + 1) * 512], in0=psum[6][:, 0:512], in1=gate_bc[:, hf * 512:(hf + 1) * 512],
                                                                      op=ALU.mult))(hf), r=[pk(6), "gate_bc"], w=["x1t"])
                        A("pool", (lambda hf, xb, sub: lambda e: e.tensor_tensor(out=x1t[:, hf * 512:(hf + 1) * 512], in0=x1t[:, hf * 512:(hf + 1) * 512],
                                                                                in1=xb[:, sub, hf * 512:(hf + 1) * 512], op=ALU.add))(hf, xb, sub), r=["x1t", xk], w=["x1t"])
                    P.add("sp", (lambda tok0: lambda e: e.dma_start(out=x1_d[tok0:tok0 + 128, :], in_=x1t[:]))(tok0), r=["x1t"], w=["x1_d"], dma=True)
                    norm_stats(lambda hf: x1t[:, hf * 512:(hf + 1) * 512], "x1t", 1e-6)
                    A("act", lambda e: e.activation(out=xn2[:], in_=x1t[:], func=AF.Identity, scale=mv[:, 3:4]), r=["x1t", "mv"], w=["x1t"])
                    for kc in range(8):
                        pb = kc // 4
                        A("pe", (lambda kc, pb: lambda e: e.matmul(psum[pb][:, (kc % 4) * 128:(kc % 4 + 1) * 128], lhsT=xn2[:, kc * 128:(kc + 1) * 128], rhs=ident, start=True, stop=True))(kc, pb),
                          r=["x1t", "cst"], w=[pk(pb)])
                    for kc in range(8):
                        pb = kc // 4
                        A("act", (lambda kc, pb: lambda e: e.activation(out=h2f[:, kc, :], in_=psum[pb][:, (kc % 4) * 128:(kc % 4 + 1) * 128], func=AF.Identity,
                                                                        scale=sc[:, 8 + kc:9 + kc], bias=mod[:, 24 + kc:25 + kc]))(kc, pb), r=[pk(pb), "sc", "mod"], w=["h2f"])
                    A("pool", lambda e: e.tensor_copy(out=xnb[:], in_=xn2[:]), r=["x1t"], w=["xnb"])
                    P.add("sp", (lambda tok0: lambda e: e.dma_start(out=xn2_d[tok0:tok0 + 128, :], in_=xnb[:]))(tok0),
                          r=["xnb"], w=["xn2_d"], dma=True)
                    for kc in range(8):
                        A("pe", (lambda kc: lambda e: e.matmul(psum[6][:, 0:36], lhsT=h2f[:, kc, :], rhs=wrs[:, kc, :], start=(kc == 0), stop=(kc == 7)))(kc),
                          r=["h2f", "wrs"], w=[pk(6)])
                    A("dve", (lambda ti: lambda e: e.tensor_tensor(out=logits[:, ti, :], in0=psum[6][:, 0:36], in1=bcs[:, BC["rb"]:BC["rb"] + 36], op=ALU.add))(ti),
                      r=[pk(6), "bc"], w=["logits"])

            P.muted = False
            P.barrier()
            ct.close()
            if stage >= 2:
                rt = SB(ca, "rt", [128, 32, 48])
                sel = SB(ca, "sel", [128, 32, 8])
                sel2 = SB(ca, "sel2", [128, 32, 8])
                ohg = SB(ca, "ohg", [128, 32, 4])
                tmp48 = SB(ca, "tmp48", [128, 32, 4, 8])
                lg = logits[:, :, 0:4]
                le = logits[:, :, 4:36].rearrange("p t (g e) -> p t g e", g=4)
                MG, SG, M1, M2, P1, W1, W2 = range(7)
                r1 = lambda i: rt[:, :, i:i + 1]
                R = lambda fn, r, w: A("dve", fn, r=r, w=w)
                R(lambda e: e.tensor_reduce(out=rt[:, :, MG], in_=lg, axis=AX.X, op=ALU.max), ["logits"], ["rt"])
                R(lambda e: e.tensor_tensor(out=ohg[:], in0=lg, in1=r1(MG).to_broadcast([128, 32, 4]), op=ALU.is_equal), ["logits", "rt"], ["ohg"])
                R(lambda e: e.tensor_tensor(out=rt[:, :, 8:12], in0=lg, in1=r1(MG).to_broadcast([128, 32, 4]), op=ALU.subtract), ["logits", "rt"], ["rt"])
                A("act", lambda e: e.activation(out=rt[:, :, 8:12], in_=rt[:, :, 8:12], func=AF.Exp), r=["rt"], w=["rt"])
                R(lambda e: e.tensor_reduce(out=rt[:, :, SG], in_=rt[:, :, 8:12], axis=AX.X, op=ALU.add), ["rt"], ["rt"])
                R(lambda e: e.reciprocal(out=rt[:, :, SG], in_=rt[:, :, SG]), ["rt"], ["rt"])
                R(lambda e: e.tensor_tensor(out=tmp48[:], in0=le, in1=ohg[:].rearrange("p t (g o) -> p t g o", o=1).to_broadcast([128, 32, 4, 8]), op=ALU.mult),
                  ["logits", "ohg"], ["tmp48"])
                R(lambda e: e.tensor_reduce(out=sel[:], in_=tmp48[:].rearrange("p t g e -> p t e g"), axis=AX.X, op=ALU.add), ["tmp48"], ["sel"])
                R(lambda e: e.tensor_reduce(out=rt[:, :, M1], in_=sel[:], axis=AX.X, op=ALU.max), ["sel"], ["rt"])
                R(lambda e: e.tensor_tensor(out=sel2[:], in0=sel[:], in1=r1(M1).to_broadcast([128, 32, 8]), op=ALU.is_equal), ["sel", "rt"], ["sel2"])
                R(lambda e: e.scalar_tensor_tensor(out=tmp48[:, :, 0, :], in0=sel2[:], scalar=-1e30, in1=sel[:], op0=ALU.mult, op1=ALU.add), ["sel", "sel2"], ["tmp48"])
                R(lambda e: e.tensor_reduce(out=rt[:, :, M2], in_=tmp48[:, :, 0, :], axis=AX.X, op=ALU.max), ["tmp48"], ["rt"])
                R(lambda e: e.tensor_tensor(out=tmp48[:, :, 1, :], in0=tmp48[:, :, 0, :], in1=r1(M2).to_broadcast([128, 32, 8]), op=ALU.is_equal), ["tmp48", "rt"], ["tmp48"])
                R(lambda e: e.tensor_tensor(out=rt[:, :, P1], in0=rt[:, :, M2], in1=rt[:, :, M1], op=ALU.subtract), ["rt"], ["rt"])
                A("act", lambda e: e.activation(out=rt[:, :, P1], in_=rt[:, :, P1], func=AF.Exp), r=["rt"], w=["rt"])
                R(lambda e: e.tensor_scalar(out=rt[:, :, P1], in0=rt[:, :, P1], scalar1=1.0, scalar2=None, op0=ALU.add), ["rt"], ["rt"])
                R(lambda e: e.reciprocal(out=rt[:, :, P1], in_=rt[:, :, P1]), ["rt"], ["rt"])
                R(lambda e: e.tensor_tensor(out=rt[:, :, W1], in0=rt[:, :, P1], in1=rt[:, :, SG], op=ALU.mult), ["rt"], ["rt"])
                R(lambda e: e.tensor_tensor(out=rt[:, :, W2], in0=rt[:, :, SG], in1=rt[:, :, W1], op=ALU.subtract), ["rt"], ["rt"])
                R(lambda e: e.tensor_tensor(out=sel[:], in0=sel2[:], in1=r1(W1).to_broadcast([128, 32, 8]), op=ALU.mult), ["sel2", "rt"], ["sel"])
                R(lambda e: e.tensor_tensor(out=sel2[:], in0=tmp48[:, :, 1, :], in1=r1(W2).to_broadcast([128, 32, 8]), op=ALU.mult), ["tmp48", "rt"], ["sel2"])
                R(lambda e: e.tensor_tensor(out=sel[:], in0=sel[:], in1=sel2[:], op=ALU.add), ["sel", "sel2"], ["sel"])
                for g in range(4):
                    R((lambda g: lambda e: e.tensor_tensor(out=wt[:, :, g * 8:(g + 1) * 8], in0=sel[:], in1=ohg[:, :, g:g + 1].to_broadcast([128, 32, 8]), op=ALU.mult))(g),
                      ["sel", "ohg"], ["wt"])
            if stage >= 2:
                Mf = SB(ca, "Mf", [128, 1024])
                Mb = SB(ca, "Mb", [128, 1024], BF16)
                Lsb = SB(ca, "Lsb", [128, 128], BF16)
                onesb = SB(ca, "onesb", [128, 128], BF16)
                PRE = SB(ca, "PRE", [128, 1024])
                CNTs = SB(ca, "CNTs", [128, 1024])
                CA_ = SB(ca, "CA", [128, 1024])
                CB_ = SB(ca, "CB", [128, 1024])
                sm = SB(ca, "sm", [128, 8, 32])
                cmpb = SB(ca, "cmpb", [128, 96, 32])
                bev = SB(ca, "bev", [128, 6, 96])
                pabf = SB(ca, "pabf", [128, 2, 32])
                v3 = lambda t: t[:].rearrange("p (a b) -> p a b", a=32)
                wtf = wt[:].rearrange("p t e -> p (t e)")
                R(lambda e: e.tensor_single_scalar(out=Mf[:], in_=wtf, scalar=0.0, op=ALU.is_gt), ["wt"], ["Mf"])
                A("pool", lambda e: e.tensor_copy(out=Mb[:], in_=Mf[:]), r=["Mf"], w=["Mb"])
                A("pool", lambda e: e.tensor_tensor(out=Lsb[:], in0=tril, in1=ident, op=ALU.subtract), r=["cst"], w=["Lsb"])
                A("pool", lambda e: e.tensor_copy(out=onesb[:], in_=ones), r=["cst"], w=["onesb"])
                for h in range(2):
                    A("pe", (lambda h: lambda e: e.matmul(psum[h][:, 0:512], lhsT=Lsb[:], rhs=Mb[:, h * 512:(h + 1) * 512], start=True, stop=True))(h),
                      r=["Lsb", "Mb"], w=[pk(h)])
                    A("pe", (lambda h: lambda e: e.matmul(psum[2 + h][:, 0:512], lhsT=onesb[:], rhs=Mb[:, h * 512:(h + 1) * 512], start=True, stop=True))(h),
                      r=["onesb", "Mb"], w=[pk(2 + h)])
                    A("act", (lambda h: lambda e: e.activation(out=PRE[:, h * 512:(h + 1) * 512], in_=psum[h][:, 0:512], func=AF.Identity))(h), r=[pk(h)], w=["PRE"])
                    A("act", (lambda h: lambda e: e.activation(out=CNTs[:, h * 512:(h + 1) * 512], in_=psum[2 + h][:, 0:512], func=AF.Identity))(h), r=[pk(2 + h)], w=["CNTs"])
                src, srck, dst, dstk = CNTs, "CNTs", CA_, "CA"
                for dd in (1, 2, 4, 8, 16):
                    w_ = dd * 32
                    R((lambda src, dst, w_: lambda e: e.tensor_copy(out=dst[:, 0:w_], in_=src[:, 0:w_]))(src, dst, w_), [srck], [dstk])
                    R((lambda src, dst, w_: lambda e: e.tensor_tensor(out=dst[:, w_:1024], in0=src[:, w_:1024], in1=src[:, 0:1024 - w_], op=ALU.add))(src, dst, w_), [srck], [dstk])
                    src, srck = dst, dstk
                    dst, dstk = (CB_, "CB") if dst is CA_ else (CA_, "CA")
                R(lambda e: e.tensor_tensor(out=CB_[:], in0=CA_[:], in1=CNTs[:], op=ALU.subtract), ["CA", "CNTs"], ["CB"])
                R(lambda e: e.tensor_copy(out=sm[:, 0, :], in_=CA_[:, 31 * 32:32 * 32]), ["CA"], ["sm"])
                R(lambda e: e.tensor_tensor(out=cmpb[:, 0:32, :], in0=sm[:, 0, :].rearrange("p (e o) -> p e o", o=1).to_broadcast([128, 32, 32]),
                                            in1=cst[:, CC["thr"]:CC["thr"] + 32].rearrange("p (o k) -> p o k", o=1).to_broadcast([128, 32, 32]), op=ALU.is_gt),
                  ["sm", "cst"], ["cmpb"])
                R(lambda e: e.tensor_reduce(out=sm[:, 1, :], in_=cmpb[:, 0:32, :], axis=AX.X, op=ALU.add), ["cmpb"], ["sm"])
                R(lambda e: e.tensor_tensor_scan(out=sm[:, 2, :], data0=ones[:, 0:32], data1=sm[:, 1, :], initial=0.0, op0=ALU.mult, op1=ALU.add), ["sm", "cst"], ["sm"])
                R(lambda e: e.tensor_tensor(out=sm[:, 3, :], in0=sm[:, 2, :], in1=sm[:, 1, :], op=ALU.subtract), ["sm"], ["sm"])
                R(lambda e: e.tensor_single_scalar(out=sm[:, 4, :], in_=sm[:, 3, :], scalar=128.0, op=ALU.mult), ["sm"], ["sm"])
                R(lambda e: e.tensor_tensor(out=v3(CB_), in0=v3(CB_), in1=sm[:, 4, :].rearrange("p (o e) -> p o e", o=1).to_broadcast([128, 32, 32]), op=ALU.add),
                  ["CB", "sm"], ["CB"])
                R(lambda e: e.tensor_tensor(out=PRE[:], in0=PRE[:], in1=CB_[:], op=ALU.add), ["PRE", "CB"], ["PRE"])
                R(lambda e: e.tensor_tensor(out=CA_[:], in0=PRE[:], in1=Mf[:], op=ALU.mult), ["PRE", "Mf", "sm"], ["CA"])
                R(lambda e: e.tensor_scalar(out=CB_[:], in0=Mf[:], scalar1=-1e9, scalar2=1e9, op0=ALU.mult, op1=ALU.add), ["Mf", "PRE"], ["CB"])
                R(lambda e: e.tensor_tensor(out=CB_[:], in0=CB_[:], in1=CA_[:], op=ALU.add), ["CB", "CA"], ["CB"])
                R(lambda e: e.tensor_reduce(out=pabf[:, 0, :], in_=v3(CB_), axis=AX.X, op=ALU.min), ["CB"], ["pabf"])
                R(lambda e: e.tensor_reduce(out=pabf[:, 1, :], in_=v3(CA_), axis=AX.X, op=ALU.max), ["CA"], ["pabf"])
                R(lambda e: e.tensor_tensor(out=v3(CB_), in0=v3(CB_), in1=pabf[:, 0, :].rearrange("p (t o) -> p t o", o=1).to_broadcast([128, 32, 32]), op=ALU.is_equal),
                  ["CB", "pabf"], ["CB"])
                R(lambda e: e.tensor_tensor(out=CB_[:], in0=CB_[:], in1=wtf, op=ALU.mult), ["CB", "wt"], ["CB"])
                R(lambda e: e.tensor_reduce(out=WAB[:, 0, :], in_=v3(CB_), axis=AX.X, op=ALU.add), ["CB"], ["WAB"])
                R(lambda e: e.tensor_tensor(out=WAB[:, 1, :], in0=rt[:, :, SG], in1=WAB[:, 0, :], op=ALU.subtract), ["rt", "WAB"], ["WAB"])
                R(lambda e: e.tensor_copy(out=PABi[:], in_=pabf[:]), ["pabf"], ["PABi"])
                R(lambda e: e.tensor_tensor(out=cmpb[:], in0=sm[:, 2, :].rearrange("p (o e) -> p o e", o=1).to_broadcast([128, 96, 32]),
                                            in1=cst[:, CC["iotab"]:CC["iotab"] + 96].rearrange("p (b o) -> p b o", o=1).to_broadcast([128, 96, 32]), op=ALU.is_le),
                  ["sm", "cst", "cmpb"], ["cmpb"])
                R(lambda e: e.tensor_reduce(out=bev[:, 0, :], in_=cmpb[:], axis=AX.X, op=ALU.add), ["cmpb"], ["bev"])
                R(lambda e: e.memset(bev[:, 1, 0:1], 1.0), [], ["bev"])
                R(lambda e: e.tensor_tensor(out=bev[:, 1, 1:96], in0=bev[:, 0, 1:96], in1=bev[:, 0, 0:95], op=ALU.not_equal), ["bev"], ["bev"])
                R(lambda e: e.tensor_scalar(out=bev[:, 2, :], in0=bev[:, 1, :], scalar1=-1e6, scalar2=1e6, op0=ALU.mult, op1=ALU.add), ["bev"], ["bev"])
                R(lambda e: e.scalar_tensor_tensor(out=bev[:, 2, :], in0=bev[:, 0, :], scalar=128.0, in1=bev[:, 2, :], op0=ALU.mult, op1=ALU.add), ["bev"], ["bev"])
                R(lambda e: e.tensor_scalar(out=bev[:, 2, :], in0=bev[:, 2, :], scalar1=cst[:, CC["pidx"]:CC["pidx"] + 1], scalar2=None, op0=ALU.add), ["bev", "cst"], ["bev"])
                R(lambda e: e.tensor_copy(out=IDXW[:, 0:1, :], in_=bev[:, 2:3, :]), ["bev"], ["IDXW"])
            P.barrier()

        if stage >= 3:
            with ExitStack() as cb:
                IOA = bass.IndirectOffsetOnAxis
                NROW = NE * 128
                _bc = {}

                def bc_reg(e):
                    if "r" not in _bc:
                        _bc["r"] = e.to_reg(NROW - 1)
                    return _bc["r"]
                W32 = SB(cb, "W32", [128, 12288])
                idb2 = SB(cb, "idb2", [128, 128], BF16)
                xload = [SB(cb, f"xl{i}", [128, D], BF16) for i in range(2)]
                xsb = [SB(cb, f"xsb{i}", [128, D], BF16) for i in range(2)]
                XgT_l = [SB(cb, f"XgT{i}", [128, 8, 128], BF16) for i in range(2)]
                Wgb = SB(cb, "Wgb", [128, 8, EH], BF16)
                Wub = SB(cb, "Wub", [128, 8, EH], BF16)
                Wdb = SB(cb, "Wdb", [128, 4, D], BF16)
                sg_l = [SB(cb, f"sg{i}", [128, 512]) for i in range(2)]
                actb_l = [SB(cb, f"actb{i}", [128, 512], BF16) for i in range(2)]
                actT_l = [SB(cb, f"actT{i}", [128, 4, 128], BF16) for i in range(2)]
                yblk = [SB(cb, f"yblk{i}", [128, D]) for i in range(2)]
                fgt = SB(cb, "fgt", [128, D])
                fgs = SB(cb, "fgs", [128, D])
                g2bc = SB(cb, "g2bc", [128, D])
                yA = SB(cb, "yA", [128, D])
                yB = SB(cb, "yB", [128, D])
                fst = SB(cb, "fst", [128, 2, 6])
                fmv = SB(cb, "fmv", [128, 4])
                P.add("sp", lambda e: e.dma_start(out=fgs[:], in_=fg_d[:, :]), w=["fgs"], dma=True)
                P.add("dve", lambda e: e.tensor_copy(out=idb2[:], in_=ident), r=["cst"], w=["idb2"])
                make_gate(g2bc, "g2bc", 40)
                zt = SB(cb, "zt", [128, D], BF16)
                P.add("pool", lambda e: e.memset(zt[:], 0.0), w=["zt"])
                xz_keys = []
                for b in range(NBLK):
                    P.add("sp", (lambda b: lambda e: e.dma_start(out=xs_d[b * 128:(b + 1) * 128, :], in_=zt[:]))(b), r=["zt"], w=[f"xz{b}"], dma=True)
                    xz_keys.append(f"xz{b}")
                sc_keys = []
                for i in range(32):
                    xl = xload[i % 2]
                    xlk = f"xl{i % 2}"
                    P.add("sp", (lambda xl, i: lambda e: e.dma_start(out=xl[:], in_=xn2_d[i * 128:(i + 1) * 128, :]))(xl, i), r=["xn2_d"], w=[xlk], dma=True)
                    for k in range(2):
                        key = f"xsc{i}_{k}"
                        P.add("pool", (lambda xl, i, k: lambda e: e.indirect_dma_start(out=xs_d[:, :], out_offset=IOA(ap=PABi[:, k, i:i + 1], axis=0),
                                                                                       in_=xl[:], in_offset=None))(xl, i, k),
                              r=[xlk, "PABi"] + xz_keys, w=[key], dma=True)
                        sc_keys.append(key)
                ys_keys = []
                def do_block(b):
                    P.add("pool", (lambda b: lambda e: e.indirect_dma_start(out=W32[:], out_offset=None, in_=wall_d[:, :],
                                                                           in_offset=IOA(ap=IDXW[:, 0, b:b + 1], axis=0), bounds_check=bc_reg(e), oob_is_err=False))(b),
                          r=["IDXW", "W32"], w=["W32"], dma=True)
                    P.add("act", lambda e: e.activation(out=Wgb[:].rearrange("p a b -> p (a b)"), in_=W32[:, 0:4096], func=AF.Identity), r=["W32"], w=["Wgb"])
                    P.add("dve", lambda e: e.tensor_copy(out=Wub[:].rearrange("p a b -> p (a b)"), in_=W32[:, 4096:8192]), r=["W32"], w=["Wub"])
                    P.add("pool", lambda e: e.tensor_copy(out=Wdb[:, 0:2, :].rearrange("p a b -> p (a b)"), in_=W32[:, 8192:10240]), r=["W32"], w=["Wdb"])
                    P.add("dve", lambda e: e.tensor_copy(out=Wdb[:, 2:4, :].rearrange("p a b -> p (a b)"), in_=W32[:, 10240:12288]), r=["W32"], w=["Wdb"])
                    XgT = XgT_l[b % 2]
                    xgk = f"XgT{b % 2}"
                    sg = sg_l[b % 2]
                    sgk = f"sg{b % 2}"
                    actb = actb_l[b % 2]
                    abk = f"actb{b % 2}"
                    actT = actT_l[b % 2]
                    atk = f"actT{b % 2}"
                    xb_ = xsb[b % 2]
                    xbk = f"xsb{b % 2}"
                    P.add("sp", (lambda xb_, b: lambda e: e.dma_start(out=xb_[:], in_=xs_d[b * 128:(b + 1) * 128, :]))(xb_, b), r=sc_keys, w=[xbk], dma=True)
                    for kc in range(8):
                        P.add("pe", (lambda xb_, kc: lambda e: e.matmul(psum[kc // 4][:, (kc % 4) * 128:(kc % 4 + 1) * 128], lhsT=xb_[:, kc * 128:(kc + 1) * 128],
                                                                       rhs=idb2[:], start=True, stop=True))(xb_, kc), r=[xbk, "idb2"], w=[pk(kc // 4)])
                    for kc in range(8):
                        P.add("act", (lambda kc: lambda e: e.activation(out=XgT[:, kc, :], in_=psum[kc // 4][:, (kc % 4) * 128:(kc % 4 + 1) * 128], func=AF.Identity,
                                                                        scale=sc[:, 8 + kc:9 + kc], bias=mod[:, 24 + kc:25 + kc]))(kc), r=[pk(kc // 4), "sc", "mod"], w=[xgk])
                    for kc in range(8):
                        P.add("pe", (lambda kc: lambda e: e.matmul(psum[2][:, 0:512], lhsT=XgT[:, kc, :], rhs=Wgb[:, kc, :], start=(kc == 0), stop=(kc == 7)))(kc),
                              r=[xgk, "Wgb"], w=[pk(2)])
                    for kc in range(8):
                        P.add("pe", (lambda kc: lambda e: e.matmul(psum[3][:, 0:512], lhsT=XgT[:, kc, :], rhs=Wub[:, kc, :], start=(kc == 0), stop=(kc == 7)))(kc),
                              r=[xgk, "Wub"], w=[pk(3)])
                    P.add("act", lambda e: e.activation(out=sg[:], in_=psum[2][:, 0:512], func=AF.Silu), r=[pk(2)], w=[sgk])
                    P.add("dve", lambda e: e.tensor_tensor(out=actb[:], in0=psum[3][:, 0:512], in1=sg[:], op=ALU.mult), r=[pk(3), sgk], w=[abk])
                    for hc in range(4):
                        P.add("pe", (lambda hc: lambda e: e.matmul(psum[4][:, hc * 128:(hc + 1) * 128], lhsT=actb[:, hc * 128:(hc + 1) * 128], rhs=idb2[:],
                                                                   start=True, stop=True))(hc), r=[abk, "idb2"], w=[pk(4)])
                    P.add("act", lambda e: e.activation(out=actT[:].rearrange("p a b -> p (a b)"), in_=psum[4][:, 0:512], func=AF.Identity), r=[pk(4)], w=[atk])
                    yb_ = yblk[b % 2]
                    ybk = f"yblk{b % 2}"
                    for hf in range(2):
                        for hc in range(4):
                            P.add("pe", (lambda hc, hf: lambda e: e.matmul(psum[5 + hf][:, 0:512], lhsT=actT[:, hc, :], rhs=Wdb[:, hc, hf * 512:(hf + 1) * 512],
                                                                           start=(hc == 0), stop=(hc == 3)))(hc, hf), r=[atk, "Wdb"], w=[pk(5 + hf)])
                        if hf == 0:
                            P.add("act", (lambda yb_: lambda e: e.activation(out=yb_[:, 0:512], in_=psum[5][:, 0:512], func=AF.Identity))(yb_), r=[pk(5)], w=[ybk])
                        else:
                            P.add("dve", (lambda yb_: lambda e: e.tensor_copy(out=yb_[:, 512:1024], in_=psum[6][:, 0:512]))(yb_), r=[pk(6)], w=[ybk])
                    key = f"ys{b}"
                    P.add("sp", (lambda yb_, b: lambda e: e.dma_start(out=ys_d[b * 128:(b + 1) * 128, :], in_=yb_[:]))(yb_, b), r=[ybk], w=[key], dma=True)
                    ys_keys.append(key)

                for b in range(min(NBLK, nblk)):
                    do_block(b)
                for i in range(32):
                    tok0 = i * 128
                    P.add("pool", (lambda i: lambda e: e.indirect_dma_start(out=yA[:], out_offset=None, in_=ys_d[:, :], in_offset=IOA(ap=PABi[:, 0, i:i + 1], axis=0)))(i),
                          r=ys_keys + ["PABi", "yA"], w=["yA"], dma=True)
                    P.add("pool", (lambda i: lambda e: e.indirect_dma_start(out=yB[:], out_offset=None, in_=ys_d[:, :], in_offset=IOA(ap=PABi[:, 1, i:i + 1], axis=0)))(i),
                          r=ys_keys + ["PABi", "yB"], w=["yB"], dma=True)
                    P.add("sp", (lambda tok0: lambda e: e.dma_start(out=fgt[:], in_=x1_d[tok0:tok0 + 128, :]))(tok0), r=["x1_d"], w=["fgt"], dma=True)
                    P.add("dve", (lambda i: lambda e: e.tensor_scalar(out=yA[:], in0=yA[:], scalar1=WAB[:, 0, i:i + 1], scalar2=None, op0=ALU.mult))(i), r=["yA", "WAB"], w=["yA"])
                    P.add("dve", (lambda i: lambda e: e.scalar_tensor_tensor(out=yA[:], in0=yB[:], scalar=WAB[:, 1, i:i + 1], in1=yA[:], op0=ALU.mult, op1=ALU.add))(i),
                          r=["yA", "yB", "WAB"], w=["yA"])
                    P.add("pool", lambda e: e.tensor_tensor(out=yA[:], in0=yA[:], in1=g2bc[:], op=ALU.mult), r=["yA", "g2bc"], w=["yA"])
                    P.add("dve", lambda e: e.tensor_tensor(out=fgt[:], in0=fgt[:], in1=yA[:], op=ALU.add), r=["yA", "fgt"], w=["fgt"])
                    for hf in range(2):
                        P.add("dve", (lambda hf: lambda e: e.bn_stats(out=fst[:, hf, :], in_=fgt[:, hf * 512:(hf + 1) * 512]))(hf), r=["fgt"], w=["fst"])
                    P.add("dve", lambda e: e.bn_aggr(out=fmv[:, 0:2], in_=fst[:, 0:2, :].rearrange("p a b -> p (a b)")), r=["fst"], w=["fmv"])
                    P.add("dve", lambda e: e.scalar_tensor_tensor(out=fmv[:, 2:3], in0=fmv[:, 0:1], scalar=fmv[:, 0:1], in1=fmv[:, 1:2], op0=ALU.mult, op1=ALU.add),
                          r=["fmv"], w=["fmv"])
                    P.add("act", lambda e: e.activation(out=fmv[:, 3:4], in_=fmv[:, 2:3], func=AF.Sqrt, bias=float(1e-6)), r=["fmv"], w=["fmv"])
                    P.add("dve", lambda e: e.reciprocal(out=fmv[:, 3:4], in_=fmv[:, 3:4]), r=["fmv"], w=["fmv"])
                    P.add("dve", lambda e: e.scalar_tensor_tensor(out=fgt[:], in0=fgt[:], scalar=fmv[:, 3:4], in1=fgs[:], op0=ALU.mult, op1=ALU.mult),
                          r=["fgt", "fmv", "fgs"], w=["fgt"])
                    P.add("sp", (lambda tok0: lambda e: e.dma_start(out=out_d[tok0:tok0 + 128, :], in_=fgt[:]))(tok0), r=["fgt"], w=["out_d"], dma=True)
        else:
            with ExitStack() as cb:
                fgt = SB(cb, "fgt", [128, D])
                for ti in range(32):
                    tok0 = ti * 128
                    P.add("sp", (lambda tok0: lambda e: e.dma_start(out=fgt[:], in_=x1_d[tok0:tok0 + 128, :]))(tok0), r=["x1_d"], w=["fgt"], dma=True)
                    P.add("sp", (lambda tok0: lambda e: e.dma_start(out=out_d[tok0:tok0 + 128, :], in_=fgt[:]))(tok0), r=["fgt"], w=["out_d"], dma=True)
        P.emit()
    return nc


_NC_CACHE = {}


def _layouts(inp):
    f = np.float32
    g = lambda k: np.asarray(inp[k], dtype=f)
    col = lambda v, n: np.ascontiguousarray(v.reshape(n, 128).T)
    mu = g("rwkv_mu")[0]
    def pad128(v):
        o = np.zeros((128, 1), f)
        o[:v.shape[0], 0] = v
        return o
    shared = [None, col(g("ada_b")[0], 48), col(g("norm1_g")[0], 8), col(g("norm2_g")[0], 8), col(mu[0:1536], 12),
              pad128(mu[1536:1568]), pad128(mu[1568:1600]), pad128(mu[1600:1696]), col(g("rwkv_w0")[0], 4), col(g("rwkv_a0")[0], 4),
              col(g("rwkv_k_k")[0], 4), col(g("rwkv_k_a")[0], 4), col(g("rwkv_r_k")[0].reshape(512), 4), col(g("rwkv_gn_w")[0], 4),
              col(g("rwkv_gn_b")[0], 4)]
    tt = np.arange(64)
    su = (tt[:, None] < tt[None, :]).astype(f)
    iu = (tt[:, None] <= tt[None, :]).astype(f)
    sl = (tt[None, :] < tt[:, None]).astype(f)
    m5 = np.tile(np.concatenate([su, iu, su, iu, sl], axis=1), (2, 1))
    ident = np.eye(128, dtype=f)
    istack = np.tile(np.eye(64, dtype=f), (2, 1))
    ss = np.arange(128)
    tril = (ss[:, None] <= ss[None, :]).astype(f)
    bones = (ss[:, None] // 64 == ss[None, :] // 64).astype(f)
    ones = np.ones((128, 128), f)
    thr = np.broadcast_to((np.arange(32) * 128).astype(f)[None, :], (128, 32))
    iotab = np.broadcast_to(np.arange(96).astype(f)[None, :], (128, 96))
    pidx = np.arange(128).astype(f)[:, None]
    cst = np.ascontiguousarray(np.concatenate([m5, ident, istack, tril, bones, ones, thr, iotab, pidx], axis=1))
    assert cst.shape[1] == NCST
    rep = lambda v: np.broadcast_to(v[None, :], (128, v.shape[0]))
    bs = g("gmlp_bs")[0]
    bsT = np.zeros((128, 4, 128), f)
    for j in range(4):
        for hh in range(2):
            bsT[hh * 64:(hh + 1) * 64, j, :] = bs[2 * j + hh][None, :]
    bc = np.ascontiguousarray(np.concatenate([rep(g("gmlp_ln_w")[0]), rep(g("gmlp_ln_b")[0]),
                                              rep(np.concatenate([g("router_group_b")[0], g("router_expert_b")[0]])),
                                              bsT.reshape(128, 512)], axis=1))
    assert bc.shape[1] == NBC
    common = dict(cst=cst, bc=bc, fg=np.ascontiguousarray(rep(g("final_norm_g"))), ada_w=np.ascontiguousarray(g("ada_w")[0]), w_in=np.ascontiguousarray(g("w_in")[0]),
                  w_out=np.ascontiguousarray(g("w_out")[0]), w2=np.ascontiguousarray(g("rwkv_w2")[0]),
                  a2=np.ascontiguousarray(g("rwkv_a2")[0]), g2=np.ascontiguousarray(g("rwkv_g2")[0]),
                  wsT=np.ascontiguousarray(g("gmlp_ws")[0].transpose(2, 0, 1)),
                  wr=np.ascontiguousarray(np.concatenate([g("router_group_w")[0], g("router_expert_w")[0]], axis=1)),
                  wall=np.ascontiguousarray(np.concatenate([
                      g("moe_w_gate")[0].reshape(NE, 8, 128, EH).transpose(0, 2, 1, 3).reshape(NE * 128, 4096),
                      g("moe_w_up")[0].reshape(NE, 8, 128, EH).transpose(0, 2, 1, 3).reshape(NE * 128, 4096),
                      g("moe_w_down")[0].reshape(NE, 4, 128, D).transpose(0, 2, 1, 3).reshape(NE * 128, 4096)], axis=1)))
    x = g("x")
    c = g("c")
    maps = []
    for b in range(x.shape[0]):
        cols = [col(c[b], 8)] + shared[1:]
        prm = np.ascontiguousarray(np.concatenate(cols, axis=1))
        assert prm.shape[1] == NPRM
        m = dict(common)
        m["x"] = np.ascontiguousarray(x[b])
        m["prm"] = prm
        maps.append(m)
    return maps


def kernel(**inputs):
    maps = _layouts(inputs)
    if "nc" not in _NC_CACHE:
        _NC_CACHE["nc"] = build_nc()
    nc = _NC_CACHE["nc"]
    res = run_bass_kernel_spmd(nc, maps, core_ids=list(range(len(maps))))
    return np.stack([r["out"] for r in res.results], axis=0).astype(np.float32)
```

```python
import numpy as np
import os
from contextlib import ExitStack
import concourse.bass as bass
import concourse.mybir as mybir
from concourse.bass_utils import run_bass_kernel_spmd

F32 = mybir.dt.float32
BF16 = mybir.dt.bfloat16
I32 = mybir.dt.int32
AF = mybir.ActivationFunctionType
ALU = mybir.AluOpType
AX = mybir.AxisListType

D = 1024
S = 4096
TT = 256
NT = S // TT
NSUB = TT // 128
CH = 64
NCH = TT // CH
INW = 2720
NE = 32
EH = 512
NEG_HALF_E = -0.6065306597126334

PC = {}
_o = 0
for _n, _w in [("cT", 8), ("ada_b", 48), ("n1g", 8), ("n2g", 8), ("mu_rkv", 12), ("mu_xw", 1),
               ("mu_xa", 1), ("mu_xg", 1), ("w0", 4), ("a0", 4), ("k_k", 4), ("k_a", 4), ("r_k", 4),
               ("gn_w", 4), ("gn_b", 4)]:
    PC[_n] = _o
    _o += _w
NPRM = _o
CC = {}
_o = 0
for _n, _w in [("m5", 320), ("ident", 128), ("istack", 64), ("tril", 128), ("bones", 128), ("ones", 128), ("thr", 32), ("iotab", 96), ("pidx", 1)]:
    CC[_n] = _o
    _o += _w
NCST = _o
BC = {}
_o = 0
for _n, _w in [("lnw", 512), ("lnb", 512), ("rb", 36), ("bsT", 512)]:
    BC[_n] = _o
    _o += _w
NBC = _o


ATTACH_WAIT = True
NO_SELF_SYNC = ("pe", "act")


class Prog:
    def __init__(self, nc, ctx):
        self.nc = nc
        self.ops = []
        self.last_w = {}
        self.readers = {}
        self.engs = ["pe", "act", "dve", "pool", "sp"]
        self.count = {e: 0 for e in self.engs}
        self.sem = {e: ctx.enter_context(nc.semaphore("s_" + e)) for e in self.engs}
        self.NDS = 8
        self.dsem = {q: [ctx.enter_context(nc.semaphore(f"d_{q}{i}")) for i in range(self.NDS)]
                     for q in ("sp", "pool")}
        self.dcount = {"sp": 0, "pool": 0}
        self.last_op = {e: None for e in self.engs}
        self.pending = {e: set() for e in self.engs}
        self.recent_dma = {"sp": [], "pool": []}

    def section(self, k):
        self.muted = k > self.cut

    def add(self, eng, fn, r=(), w=(), dma=False):
        if getattr(self, 'muted', False):
            return None
        idx = len(self.ops)
        deps = set(self.pending[eng])
        self.pending[eng] = set()
        for k in r:
            if k in self.last_w:
                deps.add(self.last_w[k])
        for k in w:
            if k in self.last_w:
                deps.add(self.last_w[k])
            deps.update(self.readers.get(k, ()))
        for k in r:
            self.readers.setdefault(k, []).append(idx)
        for k in w:
            self.last_w[k] = idx
            self.readers[k] = []
        if dma:
            q = eng
            kq = self.dcount[q]
            self.dcount[q] += 1
            sem = self.dsem[q][kq % self.NDS]
            val = 16 * (kq // self.NDS + 1)
            prev = (sem, val - 16) if val > 16 else None
            self.recent_dma[q].append(idx)
            self.recent_dma[q] = self.recent_dma[q][-self.NDS:]
        else:
            self.count[eng] += 1
            sem = self.sem[eng]
            val = self.count[eng]
            prev = None
        self.ops.append(dict(eng=eng, fn=fn, deps=deps, dma=dma, sem=sem, val=val, prev=prev))
        self.last_op[eng] = idx
        return idx

    def barrier(self):
        allops = set()
        for e in self.engs:
            if self.last_op[e] is not None:
                allops.add(self.last_op[e])
        for q in ("sp", "pool"):
            allops.update(self.recent_dma[q])
        dmaops = set()
        for q in ("sp", "pool"):
            dmaops.update(self.recent_dma[q])
        for e in self.engs:
            self.pending[e] |= (allops - dmaops) if e == "pe" else allops

    def emit(self):
        nc = self.nc
        per = {e: [] for e in self.engs}
        for i, op in enumerate(self.ops):
            per[op["eng"]].append(i)
        ops = self.ops

        def run(name, e):
            waited = {}
            for i in per[name]:
                op = ops[i]
                need = []
                for j in op["deps"]:
                    d = ops[j]
                    if d["eng"] == name and not d["dma"] and name in NO_SELF_SYNC:
                        continue
                    need.append((d["sem"], d["val"]))
                if op["prev"] is not None:
                    need.append(op["prev"])
                need.sort(key=lambda t: -t[1])
                todo = []
                for sem, val in need:
                    key = id(sem)
                    if waited.get(key, 0) >= val:
                        continue
                    waited[key] = val
                    todo.append((sem, val))
                attach = todo.pop() if (todo and ATTACH_WAIT) else None
                for sem, val in todo:
                    e.wait_ge(sem, val)
                inst = op["fn"](e)
                if attach is not None:
                    inst._wait_ge(attach[0], attach[1])
                inst.then_inc(op["sem"], 16 if op["dma"] else 1)
            if name in ("sp", "pool"):
                kq = self.dcount[name]
                for s_i in range(min(kq, self.NDS)):
                    n_on = (kq - s_i + self.NDS - 1) // self.NDS
                    e.wait_ge(self.dsem[name][s_i], 16 * n_on)

        with nc.Block() as block:
            @block.sync
            def _(e):
                run("sp", e)

            @block.scalar
            def _(e):
                run("act", e)

            @block.vector
            def _(e):
                run("dve", e)

            @block.tensor
            def _(e):
                run("pe", e)

            @block.gpsimd
            def _(e):
                run("pool", e)


def build_nc(stage=99, ntiles=NT, cut=99, nblk=96):
    nc = bass.Bass("TRN2", target_bir_lowering=False)
    dt = lambda name, shape, dty, kind: nc.dram_tensor(name, shape, dty, kind=kind).ap()
    x_d = dt("x", [S, D], F32, "ExternalInput")
    prm_d = dt("prm", [128, NPRM], F32, "ExternalInput")
    cst_d = dt("cst", [128, NCST], F32, "ExternalInput")
    bc_d = dt("bc", [128, NBC], F32, "ExternalInput")
    adaw_d = dt("ada_w", [D, 6 * D], F32, "ExternalInput")
    win_d = dt("w_in", [D, INW], F32, "ExternalInput")
    wout_d = dt("w_out", [D, D], F32, "ExternalInput")
    w2_d = dt("w2", [32, 512], F32, "ExternalInput")
    a2_d = dt("a2", [32, 512], F32, "ExternalInput")
    g2_d = dt("g2", [96, 512], F32, "ExternalInput")
    wsT_d = dt("wsT", [128, 8, 128], F32, "ExternalInput")
    wr_d = dt("wr", [D, 36], F32, "ExternalInput")
    fg_d = dt("fg", [128, D], F32, "ExternalInput")
    wall_d = dt("wall", [NE * 128, 12288], F32, "ExternalInput")
    NBLK = 96
    xn2_d = dt("xn2_d", [S, D], BF16, "Internal")
    xs_d = dt("xs_d", [NBLK * 128, D], BF16, "Internal")
    ys_d = dt("ys_d", [NBLK * 128, D], F32, "Internal")
    out_d = dt("out", [S, D], F32, "ExternalOutput")
    x1_d = dt("x1_d", [S, D], F32, "Internal")

    with ExitStack() as top:
        P = Prog(nc, top)
        P.cut = cut
        psum = [top.enter_context(nc.psum_tensor(f"ps{i}", [128, 512], F32)) for i in range(7)]
        psTt = top.enter_context(nc.psum_tensor("psT", [128, 512], F32))
        psT = [psTt[:, 0:256], psum[6][:, 0:256]]
        psTk = ["ps7", "ps6"]
        fst = None
        fmv = None
        pk = lambda i: f"ps{i}"

        def SB(ctx, name, shape, dty=F32):
            return ctx.enter_context(nc.sbuf_tensor(name, shape, dty))

        prm = SB(top, "prm_s", [128, NPRM])
        cst = SB(top, "cst_s", [128, NCST])
        bcs = SB(top, "bc_s", [128, NBC])
        mod = SB(top, "mod", [128, 48])
        wt = SB(top, "wt", [128, 32, 32])
        PABi = SB(top, "PABi", [128, 2, 32], I32)
        WAB = SB(top, "WAB", [128, 2, 32])
        IDXW = SB(top, "IDXW", [128, 4, 96], I32)
        P.add("sp", lambda e: e.dma_start(out=prm[:], in_=prm_d[:, :]), w=["prm"], dma=True)
        P.add("sp", lambda e: e.dma_start(out=cst[:], in_=cst_d[:, :]), w=["cst"], dma=True)
        P.add("sp", lambda e: e.dma_start(out=bcs[:], in_=bc_d[:, :]), w=["bc"], dma=True)

        def pcol(name, j=0, n=1):
            return prm[:, PC[name] + j:PC[name] + j + n]

        ident = cst[:, CC["ident"]:CC["ident"] + 128]
        istack = cst[:, CC["istack"]:CC["istack"] + 64]
        tril = cst[:, CC["tril"]:CC["tril"] + 128]
        bones = cst[:, CC["bones"]:CC["bones"] + 128]
        ones = cst[:, CC["ones"]:CC["ones"] + 128]
        m5 = cst[:, CC["m5"]:CC["m5"] + 320]

        with ExitStack() as c0:
            stg = [SB(c0, f"ada_stg{i}", [128, 6 * D]) for i in range(2)]
            for kc in range(8):
                sb = stg[kc % 2]
                P.add("sp", (lambda sb, kc: lambda e: e.dma_start(out=sb[:], in_=adaw_d[kc * 128:(kc + 1) * 128, :]))(sb, kc),
                      w=[f"ada_stg{kc % 2}"], dma=True)
                for oc in range(48):
                    P.add("pe", (lambda sb, kc, oc: lambda e: e.matmul(psum[0][:, oc:oc + 1], lhsT=sb[:, oc * 128:(oc + 1) * 128],
                                                                   rhs=prm[:, PC["cT"] + kc:PC["cT"] + kc + 1], start=True, stop=True))(sb, kc, oc),
                          r=[f"ada_stg{kc % 2}", "prm"], w=[pk(0)])
                if kc == 0:
                    P.add("dve", lambda e: e.tensor_tensor(out=mod[:], in0=psum[0][:, 0:48], in1=prm[:, PC["ada_b"]:PC["ada_b"] + 48], op=ALU.add),
                          r=[pk(0), "prm"], w=["mod"])
                else:
                    P.add("dve", lambda e: e.tensor_tensor(out=mod[:], in0=psum[0][:, 0:48], in1=mod[:], op=ALU.add),
                          r=[pk(0), "mod"], w=["mod"])
            P.barrier()
        sc = SB(top, "sc", [128, 32])
        P.add("dve", lambda e: e.scalar_tensor_tensor(out=sc[:, 0:8], in0=mod[:, 8:16], scalar=1.0, in1=prm[:, PC["n1g"]:PC["n1g"] + 8],
                                                      op0=ALU.add, op1=ALU.mult), r=["mod", "prm"], w=["sc"])
        P.add("dve", lambda e: e.scalar_tensor_tensor(out=sc[:, 8:16], in0=mod[:, 32:40], scalar=1.0, in1=prm[:, PC["n2g"]:PC["n2g"] + 8],
                                                      op0=ALU.add, op1=ALU.mult), r=["mod", "prm"], w=["sc"])
        gate_bc = SB(top, "gate_bc", [128, D])
        gl = SB(top, "gl", [128, 128])

        def make_gate(dst, dkey, gcol):
            for c in range(8):
                P.add("dve", (lambda c: lambda e: e.tensor_scalar(out=gl[:], in0=ones, scalar1=mod[:, gcol + c:gcol + c + 1], scalar2=None,
                                                                  op0=ALU.mult))(c), r=["mod", "cst"], w=["gl"])
                P.add("pe", (lambda c: lambda e: e.matmul(psum[1][:, (c % 4) * 128:(c % 4 + 1) * 128], lhsT=gl[:], rhs=ident, start=True, stop=True))(c),
                      r=["gl", "cst"], w=[pk(1)])
                P.add("act", (lambda c: lambda e: e.activation(out=dst[:, c * 128:(c + 1) * 128], in_=psum[1][:, (c % 4) * 128:(c % 4 + 1) * 128],
                                                               func=AF.Identity))(c), r=[pk(1)], w=[dkey])
        make_gate(gate_bc, "gate_bc", 16)

        with ExitStack() as ca:
            win = SB(ca, "win", [128, 8, INW], BF16)
            wout = SB(ca, "wout", [128, 8, D], BF16)
            identb = SB(ca, "identb", [128, 128], BF16)
            w2s = SB(ca, "w2s", [32, 512])
            a2s = SB(ca, "a2s", [32, 512])
            g2s = SB(ca, "g2s", [96, 512])
            wrs = SB(ca, "wrs", [128, 8, 36])
            wc = SB(ca, "wc", [128, 8, 128])
            logits = SB(ca, "logits", [128, 32, 36])
            Hst = [SB(ca, f"H{j}", [128, 64]) for j in range(4)]
            Hb_l = [SB(ca, f"Hb{j}", [128, 64], BF16) for j in range(4)]
            istb = SB(ca, "istb", [128, 64], BF16)
            carry = SB(ca, "carry", [128, 16])
            P.add("pool", lambda e: e.tensor_copy(out=identb[:], in_=ident), r=["cst"], w=["identb"])
            P.add("sp", lambda e: e.dma_start(out=w2s[:], in_=w2_d[:, :]), w=["w2s"], dma=True)
            P.add("sp", lambda e: e.dma_start(out=a2s[:], in_=a2_d[:, :]), w=["a2s"], dma=True)
            P.add("sp", lambda e: e.dma_start(out=g2s[:], in_=g2_d[:, :]), w=["g2s"], dma=True)
            P.add("sp", lambda e: e.dma_start(out=wrs[:], in_=wr_d.rearrange("(c p) n -> p c n", p=128)), w=["wrs"], dma=True)
            P.add("sp", lambda e: e.dma_start(out=wc[:], in_=wsT_d[:, :, :]), w=["wc"], dma=True)
            for h in range(8):
                P.add("pool", (lambda h: lambda e: e.tensor_tensor(out=wc[:, h, :], in0=wc[:, h, :], in1=tril, op=ALU.mult))(h),
                      r=["wc", "cst"], w=["wc"])
            P.add("pool", lambda e: e.memset(carry[:], 0.0), w=["carry"])
            for _i in range(int(os.environ.get("KPAD", "0"))):
                P.add("pool", lambda e: e.memset(gl[:, 0:1], 0.0), w=["gl_dummy"])
            for j in range(4):
                P.add("pool", (lambda j: lambda e: e.memset(Hst[j][:], 0.0))(j), w=[f"H{j}"])
                P.add("pool", (lambda j: lambda e: e.memset(Hb_l[j][:], 0.0))(j), w=[f"Hb{j}"])
            P.add("pool", lambda e: e.tensor_copy(out=istb[:], in_=istack), r=["cst"], w=["istb"])
            with ExitStack() as cw:
                wstg = [SB(cw, f"wstg{i}", [128, INW]) for i in range(2)]
                for kc in range(8):
                    sb = wstg[kc % 2]
                    P.add("sp", (lambda sb, kc: lambda e: e.dma_start(out=sb[:], in_=win_d[kc * 128:(kc + 1) * 128, :]))(sb, kc),
                          w=[f"wstg{kc % 2}"], dma=True)
                    P.add("pool", (lambda sb, kc: lambda e: e.tensor_copy(out=win[:, kc, :], in_=sb[:]))(sb, kc),
                          r=[f"wstg{kc % 2}"], w=["win"])
                for kc in range(8):
                    sb = wstg[kc % 2]
                    P.add("sp", (lambda sb, kc: lambda e: e.dma_start(out=sb[:, 0:D], in_=wout_d[kc * 128:(kc + 1) * 128, :]))(sb, kc),
                          w=[f"wstg{kc % 2}"], dma=True)
                    P.add("pool", (lambda sb, kc: lambda e: e.tensor_copy(out=wout[:, kc, :], in_=sb[:, 0:D]))(sb, kc),
                          r=[f"wstg{kc % 2}"], w=["wout"])
                P.barrier()

            ct = ExitStack()
            xt = [SB(ct, "xt0", [128, NSUB, D])] * 2
            xn = SB(ct, "xn", [128, NSUB, D], BF16)
            hT = SB(ct, "hT", [128, 8, TT], BF16)
            st6 = SB(ct, "st6", [128, 4, 6])
            mv = SB(ct, "mv", [128, 8])
            u_s = SB(ct, "u_s", [128, 4, TT], BF16)
            vg = SB(ct, "vg", [128, 512])
            vn = vg
            zraw = [SB(ct, f"zraw{i}", [128, TT + 1]) for i in range(2)]
            rkv = SB(ct, "rkv", [128, 12, TT])
            lor = SB(ct, "lor", [128, 3, TT])
            LW, ASN, BSN, KM, BON, GG = range(6)
            pers = [SB(ct, f"pers{j}", [128, 6, TT]) for j in range(4)]
            AA, KX, RN, PRD = range(4)
            ptmp = SB(ct, "ptmp", [128, 4, TT])
            ybT = SB(ct, "ybT", [128, 4, TT])
            ycat = SB(ct, "ycat", [128, 8, TT], BF16)
            x1t = SB(ct, "x1t", [128, D])
            xn2 = x1t
            h2f = SB(ct, "h2f", [128, 8, 128])
            gtmp = h2f[:, 0:4, :]
            xnb = SB(ct, "xnb", [128, D], BF16)
            CI, CE, EI, EE, EN = range(5)
            sct = [SB(ct, f"sct{j}", [128, 5, 64]) for j in range(4)]
            sb4 = [SB(ct, f"sb4{j}", [128, 4, 64], BF16) for j in range(4)]
            mats = [SB(ct, f"mats{j}", [128, 320], BF16) for j in range(4)]
            trs = [SB(ct, f"trs{j}", [128, 192], BF16) for j in range(4)]
            wzb_l = [SB(ct, f"wzb{j}", [128, 256], BF16) for j in range(4)]
            qq = [[SB(ct, f"qq{j}_{i}", [128, 64], BF16) for i in range(2)] for j in range(4)]
            chn = [SB(ct, f"chn{j}", [128, 3, 64], BF16) for j in range(4)]
            hpc_l = [SB(ct, f"hpc{j}", [128, 64]) for j in range(4)]
            gst = [SB(ct, f"gst{j}", [128, 12]) for j in range(4)]

            def A(eng, fn, r=(), w=()):
                P.add(eng, fn, r=r, w=w)

            def norm_stats(src, srck, eps):
                for hf in range(2):
                    A("dve", (lambda hf: lambda e: e.bn_stats(out=st6[:, hf, :], in_=src(hf)))(hf), r=[srck], w=["st6"])
                A("dve", lambda e: e.bn_aggr(out=mv[:, 0:2], in_=st6[:, 0:2, :].rearrange("p a b -> p (a b)")), r=["st6"], w=["mv"])
                A("dve", lambda e: e.scalar_tensor_tensor(out=mv[:, 2:3], in0=mv[:, 0:1], scalar=mv[:, 0:1], in1=mv[:, 1:2], op0=ALU.mult, op1=ALU.add),
                  r=["mv"], w=["mv"])
                A("act", lambda e: e.activation(out=mv[:, 3:4], in_=mv[:, 2:3], func=AF.Sqrt, bias=float(eps)), r=["mv"], w=["mv"])
                A("dve", lambda e: e.reciprocal(out=mv[:, 3:4], in_=mv[:, 3:4]), r=["mv"], w=["mv"])

            for TI in range(min(NT, ntiles) if stage >= 1 else 0):
                t0 = TI * TT
                xb = xt[TI % 2]
                xk = "xt0"
                P.add("sp", (lambda xb, t0: lambda e: e.dma_start(out=xb[:], in_=x_d[t0:t0 + TT, :].rearrange("(s p) d -> p s d", p=128)))(xb, t0),
                      w=[xk], dma=True)
                P.section(1)
                for sub in range(NSUB):
                    norm_stats((lambda xb, sub: lambda hf: xb[:, sub, hf * 512:(hf + 1) * 512])(xb, sub), xk, 1e-6)
                    A("act", (lambda xb, sub: lambda e: e.activation(out=xn[:, sub, :], in_=xb[:, sub, :], func=AF.Identity, scale=mv[:, 3:4]))(xb, sub),
                      r=[xk, "mv"], w=["xn"])
                P.section(1.5)
                for kc in range(8):
                    pb = kc % 2
                    for sub in range(NSUB):
                        A("pe", (lambda kc, sub, pb: lambda e: e.matmul(psT[pb][:, sub * 128:(sub + 1) * 128], lhsT=xn[:, sub, kc * 128:(kc + 1) * 128],
                                                                       rhs=identb[:], start=True, stop=True))(kc, sub, pb), r=["xn", "identb"], w=[psTk[pb]])
                    if os.environ.get("DVE_EVAC"):
                      A("dve", (lambda kc, pb: lambda e: e.tensor_scalar(out=hT[:, kc, :], in0=psT[pb][:, 0:TT], scalar1=sc[:, kc:kc + 1], scalar2=mod[:, kc:kc + 1],
                                                                         op0=ALU.mult, op1=ALU.add))(kc, pb), r=[psTk[pb], "sc", "mod"], w=["hT"])
                    elif not os.environ.get("SKIP_EVAC"):
                      A("act", (lambda kc, pb: lambda e: e.activation(out=hT[:, kc, :], in_=psT[pb][:, 0:TT], func=AF.Identity,
                                                                    **({} if os.environ.get("NO_SB") else dict(scale=sc[:, kc:kc + 1], bias=mod[:, kc:kc + 1]))))(kc, pb),
                      r=[psTk[pb], "sc", "mod"], w=["hT"])

                P.section(2)
                pcnt = [0]

                def proj(c0, M):
                    pb = pcnt[0] % 2
                    pcnt[0] += 1
                    for kc in range(8):
                        A("pe", (lambda kc, pb: lambda e: e.matmul(psum[pb][0:M, 0:TT], lhsT=win[:, kc, c0:c0 + M], rhs=hT[:, kc, :],
                                                                  start=(kc == 0), stop=(kc == 7)))(kc, pb), r=["win", "hT"], w=[pk(pb)])
                    return pb

                for j in range(4):
                    pb = proj(j * 128, 128)
                    A("act", (lambda j, pb: lambda e: e.activation(out=u_s[:, j, :], in_=psum[pb][:, 0:TT], func=AF.Gelu_apprx_tanh))(j, pb),
                      r=[pk(pb)], w=["u_s"])
                specs = [(1024 + q * 128, 128, PC["mu_rkv"] + q) for q in range(12)] + \
                        [(2560, 32, PC["mu_xw"]), (2592, 32, PC["mu_xa"]), (2624, 96, PC["mu_xg"])]
                for q, (c0, M, mucol) in enumerate(specs):
                    pb = proj(c0, M)
                    zb = zraw[q % 2]
                    zk = f"zraw{q % 2}"
                    dst = rkv[0:M, q, :] if q < 12 else lor[0:M, q - 12, :]
                    dk = f"rkv{q}"
                    A("dve", (lambda zb, q, M: lambda e: e.tensor_copy(out=zb[0:M, 0:1], in_=carry[0:M, q:q + 1]))(zb, q, M), r=["carry"], w=[zk])
                    A("act", (lambda zb, pb, M: lambda e: e.activation(out=zb[0:M, 1:TT + 1], in_=psum[pb][0:M, 0:TT], func=AF.Identity))(zb, pb, M),
                      r=[pk(pb)], w=[zk])
                    A("dve", (lambda zb, q, M: lambda e: e.tensor_copy(out=carry[0:M, q:q + 1], in_=zb[0:M, TT:TT + 1]))(zb, q, M), r=[zk], w=["carry"])
                    A("dve", (lambda zb, dst, M: lambda e: e.tensor_tensor(out=dst, in0=zb[0:M, 0:TT], in1=zb[0:M, 1:TT + 1], op=ALU.subtract))(zb, dst, M),
                      r=[zk], w=[dk])
                    A("dve", (lambda zb, dst, M, mucol: lambda e: e.scalar_tensor_tensor(out=dst, in0=dst, scalar=prm[0:M, mucol:mucol + 1], in1=zb[0:M, 1:TT + 1],
                                                                                      op0=ALU.mult, op1=ALU.add))(zb, dst, M, mucol), r=[zk, dk, "prm"], w=[dk])
                A("act", lambda e: e.activation(out=lor[0:32, 0, :], in_=lor[0:32, 0, :], func=AF.Tanh), r=["rkv12"], w=["rkv12"])
                A("act", lambda e: e.activation(out=lor[0:96, 2, :], in_=lor[0:96, 2, :], func=AF.Sigmoid), r=["rkv14"], w=["rkv14"])

                P.section(3)
                for j in range(4):
                    jc = slice(j * 128, (j + 1) * 128)
                    pj = pers[j]
                    pjk = f"pers{j}"
                    rr = rkv[:, j, :]
                    kr = rkv[:, 4 + j, :]
                    vr = rkv[:, 8 + j, :]
                    A("pe", (lambda jc: lambda e: e.matmul(psum[0][:, 0:TT], lhsT=w2s[0:32, jc], rhs=lor[0:32, 0, :], start=True, stop=True))(jc),
                      r=["w2s", "rkv12"], w=[pk(0)])
                    A("act", (lambda pj, j: lambda e: e.activation(out=pj[:, LW, :], in_=psum[0][:, 0:TT], func=AF.Sigmoid, bias=pcol("w0", j)))(pj, j),
                      r=[pk(0), "prm"], w=[pjk + "LW"])
                    A("pool", (lambda pj: lambda e: e.tensor_scalar(out=pj[:, LW, :], in0=pj[:, LW, :], scalar1=NEG_HALF_E, scalar2=None, op0=ALU.mult))(pj),
                      r=[pjk + "LW"], w=[pjk + "LW"])
                    A("pe", (lambda jc: lambda e: e.matmul(psum[1][:, 0:TT], lhsT=a2s[0:32, jc], rhs=lor[0:32, 1, :], start=True, stop=True))(jc),
                      r=["a2s", "rkv13"], w=[pk(1)])
                    A("act", (lambda j: lambda e: e.activation(out=ptmp[:, AA, :], in_=psum[1][:, 0:TT], func=AF.Sigmoid, bias=pcol("a0", j)))(j),
                      r=[pk(1), "prm"], w=["pAA"])
                    A("pe", (lambda jc: lambda e: e.matmul(psum[0][:, 0:TT], lhsT=g2s[0:96, jc], rhs=lor[0:96, 2, :], start=True, stop=True))(jc),
                      r=["g2s", "rkv14"], w=[pk(0)])
                    A("act", (lambda pj: lambda e: e.activation(out=pj[:, GG, :], in_=psum[0][:, 0:TT], func=AF.Identity))(pj), r=[pk(0)], w=[pjk + "GG"])
                    A("dve", (lambda kr, j: lambda e: e.tensor_scalar(out=ptmp[:, KX, :], in0=kr, scalar1=pcol("k_k", j), scalar2=None, op0=ALU.mult))(kr, j),
                      r=[f"rkv{4 + j}", "prm"], w=["pKX"])
                    A("pool", lambda e: e.tensor_tensor(out=ptmp[:, RN, :], in0=ptmp[:, KX, :], in1=ptmp[:, KX, :], op=ALU.mult), r=["pKX"], w=["pRN"])
                    A("pe", lambda e: e.matmul(psum[1][:, 0:TT], lhsT=bones, rhs=ptmp[:, RN, :], start=True, stop=True), r=["cst", "pRN"], w=[pk(1)])
                    A("act", lambda e: e.activation(out=ptmp[:, RN, :], in_=psum[1][:, 0:TT], func=AF.Sqrt, bias=float(1e-24)), r=[pk(1)], w=["pRN"])
                    A("dve", lambda e: e.reciprocal(out=ptmp[:, RN, :], in_=ptmp[:, RN, :]), r=["pRN"], w=["pRN"])
                    A("dve", (lambda pj: lambda e: e.scalar_tensor_tensor(out=pj[:, ASN, :], in0=ptmp[:, KX, :], scalar=-1.0, in1=ptmp[:, RN, :],
                                                                          op0=ALU.mult, op1=ALU.mult))(pj), r=["pKX", "pRN"], w=[pjk + "ASN"])
                    A("dve", (lambda pj: lambda e: e.scalar_tensor_tensor(out=pj[:, BSN, :], in0=pj[:, ASN, :], scalar=-1.0, in1=ptmp[:, AA, :],
                                                                          op0=ALU.mult, op1=ALU.mult))(pj), r=[pjk + "ASN", "pAA"], w=[pjk + "BSN"])
                    A("dve", (lambda j: lambda e: e.tensor_scalar(out=ptmp[:, PRD, :], in0=ptmp[:, AA, :], scalar1=-1.0, scalar2=pcol("k_a", j),
                                                                  op0=ALU.add, op1=ALU.mult))(j), r=["pAA", "prm"], w=["pPRD"])
                    A("dve", (lambda pj, kr: lambda e: e.scalar_tensor_tensor(out=pj[:, KM, :], in0=ptmp[:, PRD, :], scalar=1.0, in1=kr,
                                                                              op0=ALU.add, op1=ALU.mult))(pj, kr), r=["pPRD", f"rkv{4 + j}"], w=[pjk + "KM"])
                    A("dve", (lambda pj, rr, j: lambda e: e.scalar_tensor_tensor(out=ptmp[:, PRD, :], in0=rr, scalar=pcol("r_k", j), in1=pj[:, KM, :],
                                                                                 op0=ALU.mult, op1=ALU.mult))(pj, rr, j), r=[f"rkv{j}", "prm", pjk + "KM"], w=["pPRD"])
                    A("pe", lambda e: e.matmul(psum[0][:, 0:TT], lhsT=bones, rhs=ptmp[:, PRD, :], start=True, stop=True), r=["cst", "pPRD"], w=[pk(0)])
                    A("dve", (lambda pj, vr: lambda e: e.tensor_tensor(out=pj[:, BON, :], in0=psum[0][:, 0:TT], in1=vr, op=ALU.mult))(pj, vr),
                      r=[pk(0), f"rkv{8 + j}"], w=[pjk + "BON"])

                P.section(4)
                def unit_stages(j, c):
                    col = slice(c * CH, (c + 1) * CH)
                    pj = pers[j]
                    pjk = f"pers{j}"
                    s = sct[j]
                    sk = f"sct{j}"
                    mt = mats[j]
                    mk = f"mats{j}"
                    tr = trs[j]
                    tk = f"trs{j}"
                    cn = chn[j]
                    ck = f"chn{j}"
                    H = Hst[j]
                    Hk = f"H{j}"
                    Hb = Hb_l[j]
                    Hbk = f"Hb{j}"
                    b4 = sb4[j]
                    hpc = hpc_l[j]
                    rr = rkv[:, j, col]
                    vr = rkv[:, 8 + j, col]
                    hp = [slice(0, 64), slice(64, 128)]
                    st = []

                    def s1():
                        A("dve", lambda e: e.tensor_tensor_scan(out=s[:, CI, :], data0=ones[:, 0:64], data1=pj[:, LW, col], initial=0.0, op0=ALU.mult, op1=ALU.add),
                          r=[pjk + "LW", "cst"], w=[sk + "ci"])
                        A("dve", lambda e: e.tensor_tensor(out=s[:, CE, :], in0=s[:, CI, :], in1=pj[:, LW, col], op=ALU.subtract),
                          r=[sk + "ci", pjk + "LW"], w=[sk + "ce"])
                    st.append(s1)

                    def s2():
                        A("act", lambda e: e.activation(out=s[:, EI, :], in_=s[:, CI, :], func=AF.Exp), r=[sk + "ci"], w=[sk + "ei"])
                        A("act", lambda e: e.activation(out=s[:, EE, :], in_=s[:, CE, :], func=AF.Exp), r=[sk + "ce"], w=[sk + "ee"])
                        A("act", lambda e: e.activation(out=s[:, EN, :], in_=s[:, CI, :], func=AF.Exp, scale=-1.0), r=[sk + "ci"], w=[sk + "en"])
                    st.append(s2)

                    def s3():
                        A("dve", lambda e: e.tensor_tensor(out=b4[:, 0, :], in0=pj[:, ASN, col], in1=s[:, EE, :], op=ALU.mult), r=[pjk + "ASN", sk + "ee"], w=[sk + "at"])
                        A("dve", lambda e: e.tensor_tensor(out=b4[:, 1, :], in0=rr, in1=s[:, EI, :], op=ALU.mult), r=[f"rkv{j}", sk + "ei"], w=[sk + "rt"])
                        A("dve", lambda e: e.tensor_tensor(out=b4[:, 2, :], in0=pj[:, BSN, col], in1=s[:, EN, :], op=ALU.mult), r=[pjk + "BSN", sk + "en"], w=[sk + "bt"])
                        A("dve", lambda e: e.tensor_tensor(out=b4[:, 3, :], in0=pj[:, KM, col], in1=s[:, EN, :], op=ALU.mult), r=[pjk + "KM", sk + "en"], w=[sk + "kt"])
                    st.append(s3)

                    def s4():
                        for p in hp:
                            A("pe", (lambda p: lambda e: e.matmul(psum[2][p, 0:128], lhsT=b4[p, 2, :], rhs=b4[p, 0:2, :].rearrange("p a b -> p (a b)"), start=True, stop=True))(p),
                              r=[sk + "bt", sk + "at", sk + "rt"], w=["ps2"])
                            A("pe", (lambda p: lambda e: e.matmul(psum[2][p, 128:256], lhsT=b4[p, 3, :], rhs=b4[p, 0:2, :].rearrange("p a b -> p (a b)"), start=True, stop=True))(p),
                              r=[sk + "kt", sk + "at", sk + "rt"], w=["ps2"])
                            A("pe", (lambda p: lambda e: e.matmul(psum[2][p, 256:320], lhsT=b4[p, 0, :], rhs=b4[p, 2, :], start=True, stop=True))(p),
                              r=[sk + "bt", sk + "at"], w=["ps2"])
                        A("dve", lambda e: e.tensor_tensor(out=mt[:], in0=psum[2][:, 0:320], in1=m5, op=ALU.mult), r=["ps2", "cst"], w=[mk])
                        for p in hp:
                            A("pe", (lambda p: lambda e: e.matmul(psum[3][p, 0:64], lhsT=rkv[p, 8 + j, col], rhs=istack[p, :], start=True, stop=True))(p),
                              r=[f"rkv{8 + j}", "cst"], w=["ps3"])
                            A("pe", (lambda p: lambda e: e.matmul(psum[3][p, 64:128], lhsT=b4[p, 2, :], rhs=istb[p, :], start=True, stop=True))(p),
                              r=[sk + "bt", "istb"], w=["ps3"])
                            A("pe", (lambda p: lambda e: e.matmul(psum[3][p, 128:192], lhsT=b4[p, 3, :], rhs=istb[p, :], start=True, stop=True))(p),
                              r=[sk + "kt", "istb"], w=["ps3"])
                        A("act", lambda e: e.activation(out=tr[:], in_=psum[3][:, 0:192], func=AF.Identity), r=["ps3"], w=[tk])
                        A("dve", lambda e: e.tensor_tensor(out=qq[j][0][:], in0=mt[:, 0:64], in1=istack, op=ALU.add), r=[mk, "cst"], w=[f"qq{j}_0"])
                    st.append(s4)

                    wzb = wzb_l[j]
                    wbk = f"wzb{j}"
                    bm3 = bones.rearrange("p (a b) -> p a b", a=2)

                    def s4b():
                        A("dve", lambda e: e.tensor_tensor(out=wzb[:, 0:128].rearrange("p (a b) -> p a b", a=2),
                                                            in0=mt[:, 0:64].rearrange("p (o t) -> p o t", o=1).to_broadcast([128, 2, 64]), in1=bm3, op=ALU.mult),
                          r=[mk, "cst"], w=[wbk])
                        A("dve", lambda e: e.tensor_tensor(out=wzb[:, 128:256].rearrange("p (a b) -> p a b", a=2),
                                                            in0=mt[:, 256:320].rearrange("p (o t) -> p o t", o=1).to_broadcast([128, 2, 64]), in1=bm3, op=ALU.mult),
                          r=[mk, "cst"], w=[wbk])
                    st.append(s4b)

                    for i in range(5):
                        def lv(i=i):
                            last = (i == 4)
                            qc = qq[j][i % 2]
                            qck = f"qq{j}_{i % 2}"
                            qn = qq[j][(i + 1) % 2]
                            qnk = f"qq{j}_{(i + 1) % 2}"
                            if not last:
                                A("pe", lambda e: e.matmul(psum[4][:, 0:128], lhsT=wzb[:, 128:256], rhs=wzb[:, 0:128], start=True, stop=True), r=[wbk], w=["ps4"])
                            A("pe", lambda e: e.matmul(psum[4][:, 128:256], lhsT=wzb[:, 0:128], rhs=wzb[:, 128:256], start=True, stop=True), r=[wbk], w=["ps4"])
                            if not last:
                                A("act", lambda e: e.activation(out=wzb[:, 0:256], in_=psum[4][:, 0:256], func=AF.Identity), r=["ps4"], w=[wbk])
                            else:
                                A("act", lambda e: e.activation(out=wzb[:, 128:256], in_=psum[4][:, 128:256], func=AF.Identity), r=["ps4"], w=[wbk])
                            A("pe", lambda e: e.matmul(psum[1][:, 128:192], lhsT=wzb[:, 128:256], rhs=qc[:, :], start=True, stop=True), r=[wbk, qck], w=["ps1"])
                            A("dve", lambda e: e.tensor_tensor(out=qn[:], in0=psum[1][:, 128:192], in1=qc[:], op=ALU.add), r=["ps1", qck], w=[qnk])
                        st.append(lv)

                    cbank = [5, 6, 7, 0][j]
                    cps = psum[cbank] if cbank != 7 else psTt
                    cpk = f"ps{cbank}"
                    q5 = qq[j][1]
                    q5k = f"qq{j}_1"
                    g = gst[j]
                    gk = f"gst{j}"

                    def s5a():
                        A("dve", lambda e: e.tensor_scalar(out=hpc[:], in0=H[:], scalar1=s[:, EI, 63:64], scalar2=None, op0=ALU.mult),
                          r=[Hk, sk + "ei"], w=[ck + "hpc"])
                        for p in hp:
                            A("pe", (lambda p: lambda e: e.matmul(cps[p, 0:64], lhsT=b4[p, 0, :], rhs=Hb[p, :], start=True, stop=False))(p), r=[sk + "at", Hbk], w=[cpk])
                            A("pe", (lambda p: lambda e: e.matmul(cps[p, 0:64], lhsT=mt[p, 128:192], rhs=tr[p, 0:64], start=False, stop=True))(p), r=[mk, tk], w=[cpk])
                        A("act", lambda e: e.activation(out=cn[:, 0, :], in_=cps[:, 0:64], func=AF.Identity), r=[cpk], w=[ck + "x"])
                    st.append(s5a)

                    def s5b():
                        for p in hp:
                            A("pe", (lambda p: lambda e: e.matmul(cps[p, 64:128], lhsT=q5[p, :], rhs=cn[p, 0, :], start=True, stop=True))(p), r=[q5k, ck + "x"], w=[cpk])
                        A("act", lambda e: e.activation(out=cn[:, 1, :], in_=cps[:, 64:128], func=AF.Identity), r=[cpk], w=[ck + "u"])
                    st.append(s5b)

                    def s5c():
                        for p in hp:
                            A("pe", (lambda p: lambda e: e.matmul(cps[p, 192:256], lhsT=tr[p, 64:128], rhs=cn[p, 1, :], start=True, stop=False))(p), r=[tk, ck + "u"], w=[cpk])
                            A("pe", (lambda p: lambda e: e.matmul(cps[p, 192:256], lhsT=tr[p, 128:192], rhs=tr[p, 0:64], start=False, stop=True))(p), r=[tk], w=[cpk])
                        for p in hp:
                            A("pe", (lambda p: lambda e: e.matmul(cps[p, 128:192], lhsT=b4[p, 1, :], rhs=Hb[p, :], start=True, stop=False))(p), r=[sk + "rt", Hbk], w=[cpk])
                            A("pe", (lambda p: lambda e: e.matmul(cps[p, 128:192], lhsT=mt[p, 64:128], rhs=cn[p, 1, :], start=False, stop=False))(p), r=[mk, ck + "u"], w=[cpk])
                            A("pe", (lambda p: lambda e: e.matmul(cps[p, 128:192], lhsT=mt[p, 192:256], rhs=tr[p, 0:64], start=False, stop=True))(p), r=[mk, tk], w=[cpk])
                        A("dve", lambda e: e.scalar_tensor_tensor(out=Hb[:], in0=cps[:, 192:256], scalar=s[:, EI, 63:64], in1=hpc[:], op0=ALU.mult, op1=ALU.add),
                          r=[cpk, sk + "ei", ck + "hpc"], w=[Hbk])
                        A("dve", lambda e: e.scalar_tensor_tensor(out=H[:], in0=cps[:, 192:256], scalar=s[:, EI, 63:64], in1=hpc[:], op0=ALU.mult, op1=ALU.add),
                          r=[cpk, sk + "ei", ck + "hpc"], w=[Hk])
                    st.append(s5c)

                    def s5d():
                        A("dve", lambda e: e.bn_stats(out=g[:, 0:6], in_=cps[:, 128:192]), r=[cpk], w=[gk])
                        A("dve", lambda e: e.bn_aggr(out=g[:, 6:8], in_=g[:, 0:6]), r=[gk], w=[gk])
                        A("act", lambda e: e.activation(out=g[:, 8:9], in_=g[:, 7:8], func=AF.Sqrt, bias=float(64e-5)), r=[gk], w=[gk])
                        A("dve", lambda e: e.reciprocal(out=g[:, 8:9], in_=g[:, 8:9]), r=[gk], w=[gk])
                        A("dve", lambda e: e.tensor_scalar(out=cn[:, 2, :], in0=cps[:, 128:192], scalar1=g[:, 6:7], scalar2=g[:, 8:9], op0=ALU.subtract, op1=ALU.mult),
                          r=[cpk, gk], w=[ck + "yn"])
                    st.append(s5d)

                    def s5e():
                        for p in hp:
                            A("pe", (lambda p: lambda e: e.matmul(psum[3][p, 192:256], lhsT=cn[p, 2, :], rhs=istb[p, :], start=True, stop=True))(p), r=[ck + "yn", "istb"], w=["ps3"])
                        A("act", lambda e: e.activation(out=ybT[:, j, col], in_=psum[3][:, 192:256], func=AF.Identity, scale=pcol("gn_w", j), bias=pcol("gn_b", j)),
                          r=["ps3", "prm"], w=[f"ybT{j}"])
                    st.append(s5e)
                    return st

                for c in range(NCH):
                    stl = [unit_stages(j, c) for j in range(4)]
                    for si in range(len(stl[0])):
                        for j in range(4):
                            stl[j][si]()
                P.section(5)
                for j in range(4):
                    pj = pers[j]
                    pjk = f"pers{j}"
                    A("pool", (lambda pj, j: lambda e: e.tensor_tensor(out=ybT[:, j, :], in0=ybT[:, j, :], in1=pj[:, BON, :], op=ALU.add))(pj, j),
                      r=[f"ybT{j}", pjk + "BON"], w=[f"ybT{j}"])
                    A("pool", (lambda pj, j: lambda e: e.tensor_tensor(out=ycat[:, 4 + j, :], in0=ybT[:, j, :], in1=pj[:, GG, :], op=ALU.mult))(pj, j),
                      r=[f"ybT{j}", pjk + "GG"], w=["ycat"])

                P.section(6)
                for sub in range(NSUB):
                    tc_ = slice(sub * 128, (sub + 1) * 128)
                    for kc in range(8):
                        A("pe", (lambda kc, tc_: lambda e: e.matmul(psum[0][:, 0:512], lhsT=hT[:, kc, tc_], rhs=win[:, kc, 512:1024], start=(kc == 0), stop=(kc == 7)))(kc, tc_),
                          r=["hT", "win"], w=[pk(0)])
                    A("act", lambda e: e.activation(out=vg[:], in_=psum[0][:, 0:512], func=AF.Gelu_apprx_tanh), r=[pk(0)], w=["vg"])
                    A("dve", lambda e: e.bn_stats(out=st6[:, 2, :], in_=vg[:]), r=["vg"], w=["st6b"])
                    A("dve", lambda e: e.bn_aggr(out=mv[:, 4:6], in_=st6[:, 2, :]), r=["st6b"], w=["mvb"])
                    A("act", lambda e: e.activation(out=mv[:, 6:7], in_=mv[:, 5:6], func=AF.Sqrt, bias=float(1e-5)), r=["mvb"], w=["mvb"])
                    A("dve", lambda e: e.reciprocal(out=mv[:, 6:7], in_=mv[:, 6:7]), r=["mvb"], w=["mvb"])
                    A("dve", lambda e: e.tensor_scalar(out=vn[:], in0=vg[:], scalar1=mv[:, 4:5], scalar2=mv[:, 6:7], op0=ALU.subtract, op1=ALU.mult), r=["vg", "mvb"], w=["vg"])
                    A("pool", lambda e: e.tensor_tensor(out=vn[:], in0=vn[:], in1=bcs[:, BC["lnw"]:BC["lnw"] + 512], op=ALU.mult), r=["vg", "bc"], w=["vg"])
                    A("pool", lambda e: e.tensor_tensor(out=vn[:], in0=vn[:], in1=bcs[:, BC["lnb"]:BC["lnb"] + 512], op=ALU.add), r=["vg", "bc"], w=["vg"])
                    for h in range(8):
                        A("pe", (lambda h: lambda e: e.matmul(psum[1][(h % 2) * 64:(h % 2) * 64 + 64, (h // 2) * 128:(h // 2 + 1) * 128], lhsT=vn[:, h * 64:(h + 1) * 64],
                                                              rhs=wc[:, h, :], start=True, stop=True))(h), r=["vg", "wc"], w=[pk(1)])
                    A("dve", lambda e: e.tensor_tensor(out=gtmp.rearrange("p a b -> p (a b)"), in0=psum[1][:, 0:512], in1=bcs[:, BC["bsT"]:BC["bsT"] + 512], op=ALU.add),
                      r=[pk(1), "bc"], w=["h2f"])
                    A("dve", (lambda tc_: lambda e: e.tensor_tensor(out=ycat[:, 0:4, tc_], in0=gtmp, in1=u_s[:, :, tc_], op=ALU.mult))(tc_), r=["h2f", "u_s"], w=["ycat"])

                P.section(7)
                for sub in range(NSUB):
                    tc_ = slice(sub * 128, (sub + 1) * 128)
                    ti = TI * NSUB + sub
                    tok0 = t0 + sub * 128
                    for hf in range(2):
                        for cc in range(8):
                            A("pe", (lambda cc, hf, tc_: lambda e: e.matmul(psum[6][:, 0:512], lhsT=ycat[:, cc, tc_], rhs=wout[:, cc, hf * 512:(hf + 1) * 512],
                                                                           start=(cc == 0), stop=(cc == 7)))(cc, hf, tc_), r=["ycat", "wout"], w=[pk(6)])
                        A("dve", (lambda hf: lambda e: e.tensor_tensor(out=x1t[:, hf * 512:(hf + 1) * 512], in0=psum[6][:, 0:512], in1=gate_bc[:, hf * 512:(hf + 1) * 512],
                                                                      op=ALU.mult))(hf), r=[pk(6), "gate_bc"], w=["x1t"])
                        A("pool", (lambda hf, xb, sub: lambda e: e.tensor_tensor(out=x1t[:, hf * 512:(hf + 1) * 512], in0=x1t[:, hf * 512:(hf + 1) * 512],
                                                                                in1=xb[:, sub, hf * 512:(hf + 1) * 512], op=ALU.add))(hf, xb, sub), r=["x1t", xk], w=["x1t"])
                    P.add("sp", (lambda tok0: lambda e: e.dma_start(out=x1_d[tok0:tok0 + 128, :], in_=x1t[:]))(tok0), r=["x1t"], w=["x1_d"], dma=True)
                    norm_stats(lambda hf: x1t[:, hf * 512:(hf + 1) * 512], "x1t", 1e-6)
                    A("act", lambda e: e.activation(out=xn2[:], in_=x1t[:], func=AF.Identity, scale=mv[:, 3:4]), r=["x1t", "mv"], w=["x1t"])
                    for kc in range(8):
                        pb = kc // 4
                        A("pe", (lambda kc, pb: lambda e: e.matmul(psum[pb][:, (kc % 4) * 128:(kc % 4 + 1) * 128], lhsT=xn2[:, kc * 128:(kc + 1) * 128], rhs=ident, start=True, stop=True))(kc, pb),
                          r=["x1t", "cst"], w=[pk(pb)])
                    for kc in range(8):
                        pb = kc // 4
                        A("act", (lambda kc, pb: lambda e: e.activation(out=h2f[:, kc, :], in_=psum[pb][:, (kc % 4) * 128:(kc % 4 + 1) * 128], func=AF.Identity,
                                                                        scale=sc[:, 8 + kc:9 + kc], bias=mod[:, 24 + kc:25 + kc]))(kc, pb), r=[pk(pb), "sc", "mod"], w=["h2f"])
                    A("pool", lambda e: e.tensor_copy(out=xnb[:], in_=xn2[:]), r=["x1t"], w=["xnb"])
                    P.add("sp", (lambda tok0: lambda e: e.dma_start(out=xn2_d[tok0:tok0 + 128, :], in_=xnb[:]))(tok0),
                          r=["xnb"], w=["xn2_d"], dma=True)
                    for kc in range(8):
                        A("pe", (lambda kc: lambda e: e.matmul(psum[6][:, 0:36], lhsT=h2f[:, kc, :], rhs=wrs[:, kc, :], start=(kc == 0), stop=(kc == 7)))(kc),
                          r=["h2f", "wrs"], w=[pk(6)])
                    A("dve", (lambda ti: lambda e: e.tensor_tensor(out=logits[:, ti, :], in0=psum[6][:, 0:36], in1=bcs[:, BC["rb"]:BC["rb"] + 36], op=ALU.add))(ti),
                      r=[pk(6), "bc"], w=["logits"])

            P.muted = False
            P.barrier()
            ct.close()
            if stage >= 2:
                rt = SB(ca, "rt", [128, 32, 48])
                sel = SB(ca, "sel", [128, 32, 8])
                sel2 = SB(ca, "sel2", [128, 32, 8])
                ohg = SB(ca, "ohg", [128, 32, 4])
                tmp48 = SB(ca, "tmp48", [128, 32, 4, 8])
                lg = logits[:, :, 0:4]
                le = logits[:, :, 4:36].rearrange("p t (g e) -> p t g e", g=4)
                MG, SG, M1, M2, P1, W1, W2 = range(7)
                r1 = lambda i: rt[:, :, i:i + 1]
                R = lambda fn, r, w: A("dve", fn, r=r, w=w)
                R(lambda e: e.tensor_reduce(out=rt[:, :, MG], in_=lg, axis=AX.X, op=ALU.max), ["logits"], ["rt"])
                R(lambda e: e.tensor_tensor(out=ohg[:], in0=lg, in1=r1(MG).to_broadcast([128, 32, 4]), op=ALU.is_equal), ["logits", "rt"], ["ohg"])
                R(lambda e: e.tensor_tensor(out=rt[:, :, 8:12], in0=lg, in1=r1(MG).to_broadcast([128, 32, 4]), op=ALU.subtract), ["logits", "rt"], ["rt"])
                A("act", lambda e: e.activation(out=rt[:, :, 8:12], in_=rt[:, :, 8:12], func=AF.Exp), r=["rt"], w=["rt"])
                R(lambda e: e.tensor_reduce(out=rt[:, :, SG], in_=rt[:, :, 8:12], axis=AX.X, op=ALU.add), ["rt"], ["rt"])
                R(lambda e: e.reciprocal(out=rt[:, :, SG], in_=rt[:, :, SG]), ["rt"], ["rt"])
                R(lambda e: e.tensor_tensor(out=tmp48[:], in0=le, in1=ohg[:].rearrange("p t (g o) -> p t g o", o=1).to_broadcast([128, 32, 4, 8]), op=ALU.mult),
                  ["logits", "ohg"], ["tmp48"])
                R(lambda e: e.tensor_reduce(out=sel[:], in_=tmp48[:].rearrange("p t g e -> p t e g"), axis=AX.X, op=ALU.add), ["tmp48"], ["sel"])
                R(lambda e: e.tensor_reduce(out=rt[:, :, M1], in_=sel[:], axis=AX.X, op=ALU.max), ["sel"], ["rt"])
                R(lambda e: e.tensor_tensor(out=sel2[:], in0=sel[:], in1=r1(M1).to_broadcast([128, 32, 8]), op=ALU.is_equal), ["sel", "rt"], ["sel2"])
                R(lambda e: e.scalar_tensor_tensor(out=tmp48[:, :, 0, :], in0=sel2[:], scalar=-1e30, in1=sel[:], op0=ALU.mult, op1=ALU.add), ["sel", "sel2"], ["tmp48"])
                R(lambda e: e.tensor_reduce(out=rt[:, :, M2], in_=tmp48[:, :, 0, :], axis=AX.X, op=ALU.max), ["tmp48"], ["rt"])
                R(lambda e: e.tensor_tensor(out=tmp48[:, :, 1, :], in0=tmp48[:, :, 0, :], in1=r1(M2).to_broadcast([128, 32, 8]), op=ALU.is_equal), ["tmp48", "rt"], ["tmp48"])
                R(lambda e: e.tensor_tensor(out=rt[:, :, P1], in0=rt[:, :, M2], in1=rt[:, :, M1], op=ALU.subtract), ["rt"], ["rt"])
                A("act", lambda e: e.activation(out=rt[:, :, P1], in_=rt[:, :, P1], func=AF.Exp), r=["rt"], w=["rt"])
                R(lambda e: e.tensor_scalar(out=rt[:, :, P1], in0=rt[:, :, P1], scalar1=1.0, scalar2=None, op0=ALU.add), ["rt"], ["rt"])
                R(lambda e: e.reciprocal(out=rt[:, :, P1], in_=rt[:, :, P1]), ["rt"], ["rt"])
                R(lambda e: e.tensor_tensor(out=rt[:, :, W1], in0=rt[:, :, P1], in1=rt[:, :, SG], op=ALU.mult), ["rt"], ["rt"])
                R(lambda e: e.tensor_tensor(out=rt[:, :, W2], in0=rt[:, :, SG], in1=rt[:, :, W1], op=ALU.subtract), ["rt"], ["rt"])
                R(lambda e: e.tensor_tensor(out=sel[:], in0=sel2[:], in1=r1(W1).to_broadcast([128, 32, 8]), op=ALU.mult), ["sel2", "rt"], ["sel"])
                R(lambda e: e.tensor_tensor(out=sel2[:], in0=tmp48[:, :, 1, :], in1=r1(W2).to_broadcast([128, 32, 8]), op=ALU.mult), ["tmp48", "rt"], ["sel2"])
                R(lambda e: e.tensor_tensor(out=sel[:], in0=sel[:], in1=sel2[:], op=ALU.add), ["sel", "sel2"], ["sel"])
                for g in range(4):
                    R((lambda g: lambda e: e.tensor_tensor(out=wt[:, :, g * 8:(g + 1) * 8], in0=sel[:], in1=ohg[:, :, g:g + 1].to_broadcast([128, 32, 8]), op=ALU.mult))(g),
                      ["sel", "ohg"], ["wt"])
            if stage >= 2:
                Mf = SB(ca, "Mf", [128, 1024])
                Mb = SB(ca, "Mb", [128, 1024], BF16)
                Lsb = SB(ca, "Lsb", [128, 128], BF16)
                onesb = SB(ca, "onesb", [128, 128], BF16)
                PRE = SB(ca, "PRE", [128, 1024])
                CNTs = SB(ca, "CNTs", [128, 1024])
                CA_ = SB(ca, "CA", [128, 1024])
                CB_ = SB(ca, "CB", [128, 1024])
                sm = SB(ca, "sm", [128, 8, 32])
                cmpb = SB(ca, "cmpb", [128, 96, 32])
                bev = SB(ca, "bev", [128, 6, 96])
                pabf = SB(ca, "pabf", [128, 2, 32])
                v3 = lambda t: t[:].rearrange("p (a b) -> p a b", a=32)
                wtf = wt[:].rearrange("p t e -> p (t e)")
                R(lambda e: e.tensor_single_scalar(out=Mf[:], in_=wtf, scalar=0.0, op=ALU.is_gt), ["wt"], ["Mf"])
                A("pool", lambda e: e.tensor_copy(out=Mb[:], in_=Mf[:]), r=["Mf"], w=["Mb"])
                A("pool", lambda e: e.tensor_tensor(out=Lsb[:], in0=tril, in1=ident, op=ALU.subtract), r=["cst"], w=["Lsb"])
                A("pool", lambda e: e.tensor_copy(out=onesb[:], in_=ones), r=["cst"], w=["onesb"])
                for h in range(2):
                    A("pe", (lambda h: lambda e: e.matmul(psum[h][:, 0:512], lhsT=Lsb[:], rhs=Mb[:, h * 512:(h + 1) * 512], start=True, stop=True))(h),
                      r=["Lsb", "Mb"], w=[pk(h)])
                    A("pe", (lambda h: lambda e: e.matmul(psum[2 + h][:, 0:512], lhsT=onesb[:], rhs=Mb[:, h * 512:(h + 1) * 512], start=True, stop=True))(h),
                      r=["onesb", "Mb"], w=[pk(2 + h)])
                    A("act", (lambda h: lambda e: e.activation(out=PRE[:, h * 512:(h + 1) * 512], in_=psum[h][:, 0:512], func=AF.Identity))(h), r=[pk(h)], w=["PRE"])
                    A("act", (lambda h: lambda e: e.activation(out=CNTs[:, h * 512:(h + 1) * 512], in_=psum[2 + h][:, 0:512], func=AF.Identity))(h), r=[pk(2 + h)], w=["CNTs"])
                src, srck, dst, dstk = CNTs, "CNTs", CA_, "CA"
                for dd in (1, 2, 4, 8, 16):
                    w_ = dd * 32
                    R((lambda src, dst, w_: lambda e: e.tensor_copy(out=dst[:, 0:w_], in_=src[:, 0:w_]))(src, dst, w_), [srck], [dstk])
                    R((lambda src, dst, w_: lambda e: e.tensor_tensor(out=dst[:, w_:1024], in0=src[:, w_:1024], in1=src[:, 0:1024 - w_], op=ALU.add))(src, dst, w_), [srck], [dstk])
                    src, srck = dst, dstk
                    dst, dstk = (CB_, "CB") if dst is CA_ else (CA_, "CA")
                R(lambda e: e.tensor_tensor(out=CB_[:], in0=CA_[:], in1=CNTs[:], op=ALU.subtract), ["CA", "CNTs"], ["CB"])
                R(lambda e: e.tensor_copy(out=sm[:, 0, :], in_=CA_[:, 31 * 32:32 * 32]), ["CA"], ["sm"])
                R(lambda e: e.tensor_tensor(out=cmpb[:, 0:32, :], in0=sm[:, 0, :].rearrange("p (e o) -> p e o", o=1).to_broadcast([128, 32, 32]),
                                            in1=cst[:, CC["thr"]:CC["thr"] + 32].rearrange("p (o k) -> p o k", o=1).to_broadcast([128, 32, 32]), op=ALU.is_gt),
                  ["sm", "cst"], ["cmpb"])
                R(lambda e: e.tensor_reduce(out=sm[:, 1, :], in_=cmpb[:, 0:32, :], axis=AX.X, op=ALU.add), ["cmpb"], ["sm"])
                R(lambda e: e.tensor_tensor_scan(out=sm[:, 2, :], data0=ones[:, 0:32], data1=sm[:, 1, :], initial=0.0, op0=ALU.mult, op1=ALU.add), ["sm", "cst"], ["sm"])
                R(lambda e: e.tensor_tensor(out=sm[:, 3, :], in0=sm[:, 2, :], in1=sm[:, 1, :], op=ALU.subtract), ["sm"], ["sm"])
                R(lambda e: e.tensor_single_scalar(out=sm[:, 4, :], in_=sm[:, 3, :], scalar=128.0, op=ALU.mult), ["sm"], ["sm"])
                R(lambda e: e.tensor_tensor(out=v3(CB_), in0=v3(CB_), in1=sm[:, 4, :].rearrange("p (o e) -> p o e", o=1).to_broadcast([128, 32, 32]), op=ALU.add),
                  ["CB", "sm"], ["CB"])
                R(lambda e: e.tensor_tensor(out=PRE[:], in0=PRE[:], in1=CB_[:], op=ALU.add), ["PRE", "CB"], ["PRE"])
                R(lambda e: e.tensor_tensor(out=CA_[:], in0=PRE[:], in1=Mf[:], op=ALU.mult), ["PRE", "Mf", "sm"], ["CA"])
                R(lambda e: e.tensor_scalar(out=CB_[:], in0=Mf[:], scalar1=-1e9, scalar2=1e9, op0=ALU.mult, op1=ALU.add), ["Mf", "PRE"], ["CB"])
                R(lambda e: e.tensor_tensor(out=CB_[:], in0=CB_[:], in1=CA_[:], op=ALU.add), ["CB", "CA"], ["CB"])
                R(lambda e: e.tensor_reduce(out=pabf[:, 0, :], in_=v3(CB_), axis=AX.X, op=ALU.min), ["CB"], ["pabf"])
                R(lambda e: e.tensor_reduce(out=pabf[:, 1, :], in_=v3(CA_), axis=AX.X, op=ALU.max), ["CA"], ["pabf"])
                R(lambda e: e.tensor_tensor(out=v3(CB_), in0=v3(CB_), in1=pabf[:, 0, :].rearrange("p (t o) -> p t o", o=1).to_broadcast([128, 32, 32]), op=ALU.is_equal),
                  ["CB", "pabf"], ["CB"])
                R(lambda e: e.tensor_tensor(out=CB_[:], in0=CB_[:], in1=wtf, op=ALU.mult), ["CB", "wt"], ["CB"])
                R(lambda e: e.tensor_reduce(out=WAB[:, 0, :], in_=v3(CB_), axis=AX.X, op=ALU.add), ["CB"], ["WAB"])
                R(lambda e: e.tensor_tensor(out=WAB[:, 1, :], in0=rt[:, :, SG], in1=WAB[:, 0, :], op=ALU.subtract), ["rt", "WAB"], ["WAB"])
                R(lambda e: e.tensor_copy(out=PABi[:], in_=pabf[:]), ["pabf"], ["PABi"])
                R(lambda e: e.tensor_tensor(out=cmpb[:], in0=sm[:, 2, :].rearrange("p (o e) -> p o e", o=1).to_broadcast([128, 96, 32]),
                                            in1=cst[:, CC["iotab"]:CC["iotab"] + 96].rearrange("p (b o) -> p b o", o=1).to_broadcast([128, 96, 32]), op=ALU.is_le),
                  ["sm", "cst", "cmpb"], ["cmpb"])
                R(lambda e: e.tensor_reduce(out=bev[:, 0, :], in_=cmpb[:], axis=AX.X, op=ALU.add), ["cmpb"], ["bev"])
                R(lambda e: e.memset(bev[:, 1, 0:1], 1.0), [], ["bev"])
                R(lambda e: e.tensor_tensor(out=bev[:, 1, 1:96], in0=bev[:, 0, 1:96], in1=bev[:, 0, 0:95], op=ALU.not_equal), ["bev"], ["bev"])
                R(lambda e: e.tensor_scalar(out=bev[:, 2, :], in0=bev[:, 1, :], scalar1=-1e6, scalar2=1e6, op0=ALU.mult, op1=ALU.add), ["bev"], ["bev"])
                R(lambda e: e.scalar_tensor_tensor(out=bev[:, 2, :], in0=bev[:, 0, :], scalar=128.0, in1=bev[:, 2, :], op0=ALU.mult, op1=ALU.add), ["bev"], ["bev"])
                R(lambda e: e.tensor_scalar(out=bev[:, 2, :], in0=bev[:, 2, :], scalar1=cst[:, CC["pidx"]:CC["pidx"] + 1], scalar2=None, op0=ALU.add), ["bev", "cst"], ["bev"])
                R(lambda e: e.tensor_copy(out=IDXW[:, 0:1, :], in_=bev[:, 2:3, :]), ["bev"], ["IDXW"])
            P.barrier()

        if stage >= 3:
            with ExitStack() as cb:
                IOA = bass.IndirectOffsetOnAxis
                NROW = NE * 128
                _bc = {}

                def bc_reg(e):
                    if "r" not in _bc:
                        _bc["r"] = e.to_reg(NROW - 1)
                    return _bc["r"]
                W32 = SB(cb, "W32", [128, 12288])
                idb2 = SB(cb, "idb2", [128, 128], BF16)
                xload = [SB(cb, f"xl{i}", [128, D], BF16) for i in range(2)]
                xsb = [SB(cb, f"xsb{i}", [128, D], BF16) for i in range(2)]
                XgT_l = [SB(cb, f"XgT{i}", [128, 8, 128], BF16) for i in range(2)]
                Wgb = SB(cb, "Wgb", [128, 8, EH], BF16)
                Wub = SB(cb, "Wub", [128, 8, EH], BF16)
                Wdb = SB(cb, "Wdb", [128, 4, D], BF16)
                sg_l = [SB(cb, f"sg{i}", [128, 512]) for i in range(2)]
                actb_l = [SB(cb, f"actb{i}", [128, 512], BF16) for i in range(2)]
                actT_l = [SB(cb, f"actT{i}", [128, 4, 128], BF16) for i in range(2)]
                yblk = [SB(cb, f"yblk{i}", [128, D]) for i in range(2)]
                fgt = SB(cb, "fgt", [128, D])
                fgs = SB(cb, "fgs", [128, D])
                g2bc = SB(cb, "g2bc", [128, D])
                yA = SB(cb, "yA", [128, D])
                yB = SB(cb, "yB", [128, D])
                fst = SB(cb, "fst", [128, 2, 6])
                fmv = SB(cb, "fmv", [128, 4])
                P.add("sp", lambda e: e.dma_start(out=fgs[:], in_=fg_d[:, :]), w=["fgs"], dma=True)
                P.add("dve", lambda e: e.tensor_copy(out=idb2[:], in_=ident), r=["cst"], w=["idb2"])
                make_gate(g2bc, "g2bc", 40)
                zt = SB(cb, "zt", [128, D], BF16)
                P.add("pool", lambda e: e.memset(zt[:], 0.0), w=["zt"])
                xz_keys = []
                for b in range(NBLK):
                    P.add("sp", (lambda b: lambda e: e.dma_start(out=xs_d[b * 128:(b + 1) * 128, :], in_=zt[:]))(b), r=["zt"], w=[f"xz{b}"], dma=True)
                    xz_keys.append(f"xz{b}")
                sc_keys = []
                for i in range(32):
                    xl = xload[i % 2]
                    xlk = f"xl{i % 2}"
                    P.add("sp", (lambda xl, i: lambda e: e.dma_start(out=xl[:], in_=xn2_d[i * 128:(i + 1) * 128, :]))(xl, i), r=["xn2_d"], w=[xlk], dma=True)
                    for k in range(2):
                        key = f"xsc{i}_{k}"
                        P.add("pool", (lambda xl, i, k: lambda e: e.indirect_dma_start(out=xs_d[:, :], out_offset=IOA(ap=PABi[:, k, i:i + 1], axis=0),
                                                                                       in_=xl[:], in_offset=None))(xl, i, k),
                              r=[xlk, "PABi"] + xz_keys, w=[key], dma=True)
                        sc_keys.append(key)
                ys_keys = []
                def do_block(b):
                    P.add("pool", (lambda b: lambda e: e.indirect_dma_start(out=W32[:], out_offset=None, in_=wall_d[:, :],
                                                                           in_offset=IOA(ap=IDXW[:, 0, b:b + 1], axis=0), bounds_check=bc_reg(e), oob_is_err=False))(b),
                          r=["IDXW", "W32"], w=["W32"], dma=True)
                    P.add("act", lambda e: e.activation(out=Wgb[:].rearrange("p a b -> p (a b)"), in_=W32[:, 0:4096], func=AF.Identity), r=["W32"], w=["Wgb"])
                    P.add("dve", lambda e: e.tensor_copy(out=Wub[:].rearrange("p a b -> p (a b)"), in_=W32[:, 4096:8192]), r=["W32"], w=["Wub"])
                    P.add("pool", lambda e: e.tensor_copy(out=Wdb[:, 0:2, :].rearrange("p a b -> p (a b)"), in_=W32[:, 8192:10240]), r=["W32"], w=["Wdb"])
                    P.add("dve", lambda e: e.tensor_copy(out=Wdb[:, 2:4, :].rearrange("p a b -> p (a b)"), in_=W32[:, 10240:12288]), r=["W32"], w=["Wdb"])
                    XgT = XgT_l[b % 2]
                    xgk = f"XgT{b % 2}"
                    sg = sg_l[b % 2]
                    sgk = f"sg{b % 2}"
                    actb = actb_l[b % 2]
                    abk = f"actb{b % 2}"
                    actT = actT_l[b % 2]
                    atk = f"actT{b % 2}"
                    xb_ = xsb[b % 2]
                    xbk = f"xsb{b % 2}"
                    P.add("sp", (lambda xb_, b: lambda e: e.dma_start(out=xb_[:], in_=xs_d[b * 128:(b + 1) * 128, :]))(xb_, b), r=sc_keys, w=[xbk], dma=True)
                    for kc in range(8):
                        P.add("pe", (lambda xb_, kc: lambda e: e.matmul(psum[kc // 4][:, (kc % 4) * 128:(kc % 4 + 1) * 128], lhsT=xb_[:, kc * 128:(kc + 1) * 128],
                                                                       rhs=idb2[:], start=True, stop=True))(xb_, kc), r=[xbk, "idb2"], w=[pk(kc // 4)])
                    for kc in range(8):
                        P.add("act", (lambda kc: lambda e: e.activation(out=XgT[:, kc, :], in_=psum[kc // 4][:, (kc % 4) * 128:(kc % 4 + 1) * 128], func=AF.Identity,
                                                                        scale=sc[:, 8 + kc:9 + kc], bias=mod[:, 24 + kc:25 + kc]))(kc), r=[pk(kc // 4), "sc", "mod"], w=[xgk])
                    for kc in range(8):
                        P.add("pe", (lambda kc: lambda e: e.matmul(psum[2][:, 0:512], lhsT=XgT[:, kc, :], rhs=Wgb[:, kc, :], start=(kc == 0), stop=(kc == 7)))(kc),
                              r=[xgk, "Wgb"], w=[pk(2)])
                    for kc in range(8):
                        P.add("pe", (lambda kc: lambda e: e.matmul(psum[3][:, 0:512], lhsT=XgT[:, kc, :], rhs=Wub[:, kc, :], start=(kc == 0), stop=(kc == 7)))(kc),
                              r=[xgk, "Wub"], w=[pk(3)])
                    P.add("act", lambda e: e.activation(out=sg[:], in_=psum[2][:, 0:512], func=AF.Silu), r=[pk(2)], w=[sgk])
                    P.add("dve", lambda e: e.tensor_tensor(out=actb[:], in0=psum[3][:, 0:512], in1=sg[:], op=ALU.mult), r=[pk(3), sgk], w=[abk])
                    for hc in range(4):
                        P.add("pe", (lambda hc: lambda e: e.matmul(psum[4][:, hc * 128:(hc + 1) * 128], lhsT=actb[:, hc * 128:(hc + 1) * 128], rhs=idb2[:],
                                                                   start=True, stop=True))(hc), r=[abk, "idb2"], w=[pk(4)])
                    P.add("act", lambda e: e.activation(out=actT[:].rearrange("p a b -> p (a b)"), in_=psum[4][:, 0:512], func=AF.Identity), r=[pk(4)], w=[atk])
                    yb_ = yblk[b % 2]
                    ybk = f"yblk{b % 2}"
                    for hf in range(2):
                        for hc in range(4):
                            P.add("pe", (lambda hc, hf: lambda e: e.matmul(psum[5 + hf][:, 0:512], lhsT=actT[:, hc, :], rhs=Wdb[:, hc, hf * 512:(hf + 1) * 512],
                                                                           start=(hc == 0), stop=(hc == 3)))(hc, hf), r=[atk, "Wdb"], w=[pk(5 + hf)])
                        if hf == 0:
                            P.add("act", (lambda yb_: lambda e: e.activation(out=yb_[:, 0:512], in_=psum[5][:, 0:512], func=AF.Identity))(yb_), r=[pk(5)], w=[ybk])
                        else:
                            P.add("dve", (lambda yb_: lambda e: e.tensor_copy(out=yb_[:, 512:1024], in_=psum[6][:, 0:512]))(yb_), r=[pk(6)], w=[ybk])
                    key = f"ys{b}"
                    P.add("sp", (lambda yb_, b: lambda e: e.dma_start(out=ys_d[b * 128:(b + 1) * 128, :], in_=yb_[:]))(yb_, b), r=[ybk], w=[key], dma=True)
                    ys_keys.append(key)

                for b in range(min(NBLK, nblk)):
                    do_block(b)
                for i in range(32):
                    tok0 = i * 128
                    P.add("pool", (lambda i: lambda e: e.indirect_dma_start(out=yA[:], out_offset=None, in_=ys_d[:, :], in_offset=IOA(ap=PABi[:, 0, i:i + 1], axis=0)))(i),
                          r=ys_keys + ["PABi", "yA"], w=["yA"], dma=True)
                    P.add("pool", (lambda i: lambda e: e.indirect_dma_start(out=yB[:], out_offset=None, in_=ys_d[:, :], in_offset=IOA(ap=PABi[:, 1, i:i + 1], axis=0)))(i),
                          r=ys_keys + ["PABi", "yB"], w=["yB"], dma=True)
                    P.add("sp", (lambda tok0: lambda e: e.dma_start(out=fgt[:], in_=x1_d[tok0:tok0 + 128, :]))(tok0), r=["x1_d"], w=["fgt"], dma=True)
                    P.add("dve", (lambda i: lambda e: e.tensor_scalar(out=yA[:], in0=yA[:], scalar1=WAB[:, 0, i:i + 1], scalar2=None, op0=ALU.mult))(i), r=["yA", "WAB"], w=["yA"])
                    P.add("dve", (lambda i: lambda e: e.scalar_tensor_tensor(out=yA[:], in0=yB[:], scalar=WAB[:, 1, i:i + 1], in1=yA[:], op0=ALU.mult, op1=ALU.add))(i),
                          r=["yA", "yB", "WAB"], w=["yA"])
                    P.add("pool", lambda e: e.tensor_tensor(out=yA[:], in0=yA[:], in1=g2bc[:], op=ALU.mult), r=["yA", "g2bc"], w=["yA"])
                    P.add("dve", lambda e: e.tensor_tensor(out=fgt[:], in0=fgt[:], in1=yA[:], op=ALU.add), r=["yA", "fgt"], w=["fgt"])
                    for hf in range(2):
                        P.add("dve", (lambda hf: lambda e: e.bn_stats(out=fst[:, hf, :], in_=fgt[:, hf * 512:(hf + 1) * 512]))(hf), r=["fgt"], w=["fst"])
                    P.add("dve", lambda e: e.bn_aggr(out=fmv[:, 0:2], in_=fst[:, 0:2, :].rearrange("p a b -> p (a b)")), r=["fst"], w=["fmv"])
                    P.add("dve", lambda e: e.scalar_tensor_tensor(out=fmv[:, 2:3], in0=fmv[:, 0:1], scalar=fmv[:, 0:1], in1=fmv[:, 1:2], op0=ALU.mult, op1=ALU.add),
                          r=["fmv"], w=["fmv"])
                    P.add("act", lambda e: e.activation(out=fmv[:, 3:4], in_=fmv[:, 2:3], func=AF.Sqrt, bias=float(1e-6)), r=["fmv"], w=["fmv"])
                    P.add("dve", lambda e: e.reciprocal(out=fmv[:, 3:4], in_=fmv[:, 3:4]), r=["fmv"], w=["fmv"])
                    P.add("dve", lambda e: e.scalar_tensor_tensor(out=fgt[:], in0=fgt[:], scalar=fmv[:, 3:4], in1=fgs[:], op0=ALU.mult, op1=ALU.mult),
                          r=["fgt", "fmv", "fgs"], w=["fgt"])
                    P.add("sp", (lambda tok0: lambda e: e.dma_start(out=out_d[tok0:tok0 + 128, :], in_=fgt[:]))(tok0), r=["fgt"], w=["out_d"], dma=True)
        else:
            with ExitStack() as cb:
                fgt = SB(cb, "fgt", [128, D])
                for ti in range(32):
                    tok0 = ti * 128
                    P.add("sp", (lambda tok0: lambda e: e.dma_start(out=fgt[:], in_=x1_d[tok0:tok0 + 128, :]))(tok0), r=["x1_d"], w=["fgt"], dma=True)
                    P.add("sp", (lambda tok0: lambda e: e.dma_start(out=out_d[tok0:tok0 + 128, :], in_=fgt[:]))(tok0), r=["fgt"], w=["out_d"], dma=True)
        P.emit()
    return nc


_NC_CACHE = {}


def _layouts(inp):
    f = np.float32
    g = lambda k: np.asarray(inp[k], dtype=f)
    col = lambda v, n: np.ascontiguousarray(v.reshape(n, 128).T)
    mu = g("rwkv_mu")[0]
    def pad128(v):
        o = np.zeros((128, 1), f)
        o[:v.shape[0], 0] = v
        return o
    shared = [None, col(g("ada_b")[0], 48), col(g("norm1_g")[0], 8), col(g("norm2_g")[0], 8), col(mu[0:1536], 12),
              pad128(mu[1536:1568]), pad128(mu[1568:1600]), pad128(mu[1600:1696]), col(g("rwkv_w0")[0], 4), col(g("rwkv_a0")[0], 4),
              col(g("rwkv_k_k")[0], 4), col(g("rwkv_k_a")[0], 4), col(g("rwkv_r_k")[0].reshape(512), 4), col(g("rwkv_gn_w")[0], 4),
              col(g("rwkv_gn_b")[0], 4)]
    tt = np.arange(64)
    su = (tt[:, None] < tt[None, :]).astype(f)
    iu = (tt[:, None] <= tt[None, :]).astype(f)
    sl = (tt[None, :] < tt[:, None]).astype(f)
    m5 = np.tile(np.concatenate([su, iu, su, iu, sl], axis=1), (2, 1))
    ident = np.eye(128, dtype=f)
    istack = np.tile(np.eye(64, dtype=f), (2, 1))
    ss = np.arange(128)
    tril = (ss[:, None] <= ss[None, :]).astype(f)
    bones = (ss[:, None] // 64 == ss[None, :] // 64).astype(f)
    ones = np.ones((128, 128), f)
    thr = np.broadcast_to((np.arange(32) * 128).astype(f)[None, :], (128, 32))
    iotab = np.broadcast_to(np.arange(96).astype(f)[None, :], (128, 96))
    pidx = np.arange(128).astype(f)[:, None]
    cst = np.ascontiguousarray(np.concatenate([m5, ident, istack, tril, bones, ones, thr, iotab, pidx], axis=1))
    assert cst.shape[1] == NCST
    rep = lambda v: np.broadcast_to(v[None, :], (128, v.shape[0]))
    bs = g("gmlp_bs")[0]
    bsT = np.zeros((128, 4, 128), f)
    for j in range(4):
        for hh in range(2):
            bsT[hh * 64:(hh + 1) * 64, j, :] = bs[2 * j + hh][None, :]
    bc = np.ascontiguousarray(np.concatenate([rep(g("gmlp_ln_w")[0]), rep(g("gmlp_ln_b")[0]),
                                              rep(np.concatenate([g("router_group_b")[0], g("router_expert_b")[0]])),
                                              bsT.reshape(128, 512)], axis=1))
    assert bc.shape[1] == NBC
    common = dict(cst=cst, bc=bc, fg=np.ascontiguousarray(rep(g("final_norm_g"))), ada_w=np.ascontiguousarray(g("ada_w")[0]), w_in=np.ascontiguousarray(g("w_in")[0]),
                  w_out=np.ascontiguousarray(g("w_out")[0]), w2=np.ascontiguousarray(g("rwkv_w2")[0]),
                  a2=np.ascontiguousarray(g("rwkv_a2")[0]), g2=np.ascontiguousarray(g("rwkv_g2")[0]),
                  wsT=np.ascontiguousarray(g("gmlp_ws")[0].transpose(2, 0, 1)),
                  wr=np.ascontiguousarray(np.concatenate([g("router_group_w")[0], g("router_expert_w")[0]], axis=1)),
                  wall=np.ascontiguousarray(np.concatenate([
                      g("moe_w_gate")[0].reshape(NE, 8, 128, EH).transpose(0, 2, 1, 3).reshape(NE * 128, 4096),
                      g("moe_w_up")[0].reshape(NE, 8, 128, EH).transpose(0, 2, 1, 3).reshape(NE * 128, 4096),
                      g("moe_w_down")[0].reshape(NE, 4, 128, D).transpose(0, 2, 1, 3).reshape(NE * 128, 4096)], axis=1)))
    x = g("x")
    c = g("c")
    maps = []
    for b in range(x.shape[0]):
        cols = [col(c[b], 8)] + shared[1:]
        prm = np.ascontiguousarray(np.concatenate(cols, axis=1))
        assert prm.shape[1] == NPRM
        m = dict(common)
        m["x"] = np.ascontiguousarray(x[b])
        m["prm"] = prm
        maps.append(m)
    return maps


def kernel(**inputs):
    maps = _layouts(inputs)
    if "nc" not in _NC_CACHE:
        _NC_CACHE["nc"] = build_nc()
    nc = _NC_CACHE["nc"]
    res = run_bass_kernel_spmd(nc, maps, core_ids=list(range(len(maps))))
    return np.stack([r["out"] for r in res.results], axis=0).astype(np.float32)
```

```python
import numpy as np
import os
from contextlib import ExitStack
import concourse.bass as bass
import concourse.mybir as mybir
from concourse.bass_utils import run_bass_kernel_spmd

F32 = mybir.dt.float32
BF16 = mybir.dt.bfloat16
I32 = mybir.dt.int32
AF = mybir.ActivationFunctionType
ALU = mybir.AluOpType
AX = mybir.AxisListType

D = 1024
S = 4096
TT = 256
NT = S // TT
NSUB = TT // 128
CH = 64
NCH = TT // CH
INW = 2720
NE = 32
EH = 512
NEG_HALF_E = -0.6065306597126334

PC = {}
_o = 0
for _n, _w in [("cT", 8), ("ada_b", 48), ("n1g", 8), ("n2g", 8), ("mu_rkv", 12), ("mu_xw", 1),
               ("mu_xa", 1), ("mu_xg", 1), ("w0", 4), ("a0", 4), ("k_k", 4), ("k_a", 4), ("r_k", 4),
               ("gn_w", 4), ("gn_b", 4)]:
    PC[_n] = _o
    _o += _w
NPRM = _o
CC = {}
_o = 0
for _n, _w in [("m5", 320), ("ident", 128), ("istack", 64), ("tril", 128), ("bones", 128), ("ones", 128), ("thr", 32), ("iotab", 96), ("pidx", 1)]:
    CC[_n] = _o
    _o += _w
NCST = _o
BC = {}
_o = 0
for _n, _w in [("lnw", 512), ("lnb", 512), ("rb", 36), ("bsT", 512)]:
    BC[_n] = _o
    _o += _w
NBC = _o


ATTACH_WAIT = True
NO_SELF_SYNC = ("pe", "act")


class Prog:
    def __init__(self, nc, ctx):
        self.nc = nc
        self.ops = []
        self.last_w = {}
        self.readers = {}
        self.engs = ["pe", "act", "dve", "pool", "sp"]
        self.count = {e: 0 for e in self.engs}
        self.sem = {e: ctx.enter_context(nc.semaphore("s_" + e)) for e in self.engs}
        self.NDS = 8
        self.dsem = {q: [ctx.enter_context(nc.semaphore(f"d_{q}{i}")) for i in range(self.NDS)]
                     for q in ("sp", "pool")}
        self.dcount = {"sp": 0, "pool": 0}
        self.last_op = {e: None for e in self.engs}
        self.pending = {e: set() for e in self.engs}
        self.recent_dma = {"sp": [], "pool": []}

    def section(self, k):
        self.muted = k > self.cut

    def add(self, eng, fn, r=(), w=(), dma=False):
        if getattr(self, 'muted', False):
            return None
        idx = len(self.ops)
        deps = set(self.pending[eng])
        self.pending[eng] = set()
        for k in r:
            if k in self.last_w:
                deps.add(self.last_w[k])
        for k in w:
            if k in self.last_w:
                deps.add(self.last_w[k])
            deps.update(self.readers.get(k, ()))
        for k in r:
            self.readers.setdefault(k, []).append(idx)
        for k in w:
            self.last_w[k] = idx
            self.readers[k] = []
        if dma:
            q = eng
            kq = self.dcount[q]
            self.dcount[q] += 1
            sem = self.dsem[q][kq % self.NDS]
            val = 16 * (kq // self.NDS + 1)
            prev = (sem, val - 16) if val > 16 else None
            self.recent_dma[q].append(idx)
            self.recent_dma[q] = self.recent_dma[q][-self.NDS:]
        else:
            self.count[eng] += 1
            sem = self.sem[eng]
            val = self.count[eng]
            prev = None
        self.ops.append(dict(eng=eng, fn=fn, deps=deps, dma=dma, sem=sem, val=val, prev=prev))
        self.last_op[eng] = idx
        return idx

    def barrier(self):
        allops = set()
        for e in self.engs:
            if self.last_op[e] is not None:
                allops.add(self.last_op[e])
        for q in ("sp", "pool"):
            allops.update(self.recent_dma[q])
        dmaops = set()
        for q in ("sp", "pool"):
            dmaops.update(self.recent_dma[q])
        for e in self.engs:
            self.pending[e] |= (allops - dmaops) if e == "pe" else allops

    def emit(self):
        nc = self.nc
        per = {e: [] for e in self.engs}
        for i, op in enumerate(self.ops):
            per[op["eng"]].append(i)
        ops = self.ops

        def run(name, e):
            waited = {}
            for i in per[name]:
                op = ops[i]
                need = []
                for j in op["deps"]:
                    d = ops[j]
                    if d["eng"] == name and not d["dma"] and name in NO_SELF_SYNC:
                        continue
                    need.append((d["sem"], d["val"]))
                if op["prev"] is not None:
                    need.append(op["prev"])
                need.sort(key=lambda t: -t[1])
                todo = []
                for sem, val in need:
                    key = id(sem)
                    if waited.get(key, 0) >= val:
                        continue
                    waited[key] = val
                    todo.append((sem, val))
                attach = todo.pop() if (todo and ATTACH_WAIT) else None
                for sem, val in todo:
                    e.wait_ge(sem, val)
                inst = op["fn"](e)
                if attach is not None:
                    inst._wait_ge(attach[0], attach[1])
                inst.then_inc(op["sem"], 16 if op["dma"] else 1)
            if name in ("sp", "pool"):
                kq = self.dcount[name]
                for s_i in range(min(kq, self.NDS)):
                    n_on = (kq - s_i + self.NDS - 1) // self.NDS
                    e.wait_ge(self.dsem[name][s_i], 16 * n_on)

        with nc.Block() as block:
            @block.sync
            def _(e):
                run("sp", e)

            @block.scalar
            def _(e):
                run("act", e)

            @block.vector
            def _(e):
                run("dve", e)

            @block.tensor
            def _(e):
                run("pe", e)

            @block.gpsimd
            def _(e):
                run("pool", e)


def build_nc(stage=99, ntiles=NT, cut=99, nblk=96):
    nc = bass.Bass("TRN2", target_bir_lowering=False)
    dt = lambda name, shape, dty, kind: nc.dram_tensor(name, shape, dty, kind=kind).ap()
    x_d = dt("x", [S, D], F32, "ExternalInput")
    prm_d = dt("prm", [128, NPRM], F32, "ExternalInput")
    cst_d = dt("cst", [128, NCST], F32, "ExternalInput")
    bc_d = dt("bc", [128, NBC], F32, "ExternalInput")
    adaw_d = dt("ada_w", [D, 6 * D], F32, "ExternalInput")
    win_d = dt("w_in", [D, INW], F32, "ExternalInput")
    wout_d = dt("w_out", [D, D], F32, "ExternalInput")
    w2_d = dt("w2", [32, 512], F32, "ExternalInput")
    a2_d = dt("a2", [32, 512], F32, "ExternalInput")
    g2_d = dt("g2", [96, 512], F32, "ExternalInput")
    wsT_d = dt("wsT", [128, 8, 128], F32, "ExternalInput")
    wr_d = dt("wr", [D, 36], F32, "ExternalInput")
    fg_d = dt("fg", [128, D], F32, "ExternalInput")
    wall_d = dt("wall", [NE * 128, 12288], F32, "ExternalInput")
    NBLK = 96
    xn2_d = dt("xn2_d", [S, D], BF16, "Internal")
    xs_d = dt("xs_d", [NBLK * 128, D], BF16, "Internal")
    ys_d = dt("ys_d", [NBLK * 128, D], F32, "Internal")
    out_d = dt("out", [S, D], F32, "ExternalOutput")
    x1_d = dt("x1_d", [S, D], F32, "Internal")

    with ExitStack() as top:
        P = Prog(nc, top)
        P.cut = cut
        psum = [top.enter_context(nc.psum_tensor(f"ps{i}", [128, 512], F32)) for i in range(7)]
        psTt = top.enter_context(nc.psum_tensor("psT", [128, 512], F32))
        psT = [psTt[:, 0:256], psum[6][:, 0:256]]
        psTk = ["ps7", "ps6"]
        fst = None
        fmv = None
        pk = lambda i: f"ps{i}"

        def SB(ctx, name, shape, dty=F32):
            return ctx.enter_context(nc.sbuf_tensor(name, shape, dty))

        prm = SB(top, "prm_s", [128, NPRM])
        cst = SB(top, "cst_s", [128, NCST])
        bcs = SB(top, "bc_s", [128, NBC])
        mod = SB(top, "mod", [128, 48])
        wt = SB(top, "wt", [128, 32, 32])
        PABi = SB(top, "PABi", [128, 2, 32], I32)
        WAB = SB(top, "WAB", [128, 2, 32])
        IDXW = SB(top, "IDXW", [128, 4, 96], I32)
        P.add("sp", lambda e: e.dma_start(out=prm[:], in_=prm_d[:, :]), w=["prm"], dma=True)
        P.add("sp", lambda e: e.dma_start(out=cst[:], in_=cst_d[:, :]), w=["cst"], dma=True)
        P.add("sp", lambda e: e.dma_start(out=bcs[:], in_=bc_d[:, :]), w=["bc"], dma=True)

        def pcol(name, j=0, n=1):
            return prm[:, PC[name] + j:PC[name] + j + n]

        ident = cst[:, CC["ident"]:CC["ident"] + 128]
        istack = cst[:, CC["istack"]:CC["istack"] + 64]
        tril = cst[:, CC["tril"]:CC["tril"] + 128]
        bones = cst[:, CC["bones"]:CC["bones"] + 128]
        ones = cst[:, CC["ones"]:CC["ones"] + 128]
        m5 = cst[:, CC["m5"]:CC["m5"] + 320]

        with ExitStack() as c0:
            stg = [SB(c0, f"ada_stg{i}", [128, 6 * D]) for i in range(2)]
            for kc in range(8):
                sb = stg[kc % 2]
                P.add("sp", (lambda sb, kc: lambda e: e.dma_start(out=sb[:], in_=adaw_d[kc * 128:(kc + 1) * 128, :]))(sb, kc),
                      w=[f"ada_stg{kc % 2}"], dma=True)
                for oc in range(48):
                    P.add("pe", (lambda sb, kc, oc: lambda e: e.matmul(psum[0][:, oc:oc + 1], lhsT=sb[:, oc * 128:(oc + 1) * 128],
                                                                   rhs=prm[:, PC["cT"] + kc:PC["cT"] + kc + 1], start=True, stop=True))(sb, kc, oc),
                          r=[f"ada_stg{kc % 2}", "prm"], w=[pk(0)])
                if kc == 0:
                    P.add("dve", lambda e: e.tensor_tensor(out=mod[:], in0=psum[0][:, 0:48], in1=prm[:, PC["ada_b"]:PC["ada_b"] + 48], op=ALU.add),
                          r=[pk(0), "prm"], w=["mod"])
                else:
                    P.add("dve", lambda e: e.tensor_tensor(out=mod[:], in0=psum[0][:, 0:48], in1=mod[:], op=ALU.add),
                          r=[pk(0), "mod"], w=["mod"])
            P.barrier()
        sc = SB(top, "sc", [128, 32])
        P.add("dve", lambda e: e.scalar_tensor_tensor(out=sc[:, 0:8], in0=mod[:, 8:16], scalar=1.0, in1=prm[:, PC["n1g"]:PC["n1g"] + 8],
                                                      op0=ALU.add, op1=ALU.mult), r=["mod", "prm"], w=["sc"])
        P.add("dve", lambda e: e.scalar_tensor_tensor(out=sc[:, 8:16], in0=mod[:, 32:40], scalar=1.0, in1=prm[:, PC["n2g"]:PC["n2g"] + 8],
                                                      op0=ALU.add, op1=ALU.mult), r=["mod", "prm"], w=["sc"])
        gate_bc = SB(top, "gate_bc", [128, D])
        gl = SB(top, "gl", [128, 128])

        def make_gate(dst, dkey, gcol):
            for c in range(8):
                P.add("dve", (lambda c: lambda e: e.tensor_scalar(out=gl[:], in0=ones, scalar1=mod[:, gcol + c:gcol + c + 1], scalar2=None,
                                                                  op0=ALU.mult))(c), r=["mod", "cst"], w=["gl"])
                P.add("pe", (lambda c: lambda e: e.matmul(psum[1][:, (c % 4) * 128:(c % 4 + 1) * 128], lhsT=gl[:], rhs=ident, start=True, stop=True))(c),
                      r=["gl", "cst"], w=[pk(1)])
                P.add("act", (lambda c: lambda e: e.activation(out=dst[:, c * 128:(c + 1) * 128], in_=psum[1][:, (c % 4) * 128:(c % 4 + 1) * 128],
                                                               func=AF.Identity))(c), r=[pk(1)], w=[dkey])
        make_gate(gate_bc, "gate_bc", 16)

        with ExitStack() as ca:
            win = SB(ca, "win", [128, 8, INW], BF16)
            wout = SB(ca, "wout", [128, 8, D], BF16)
            identb = SB(ca, "identb", [128, 128], BF16)
            w2s = SB(ca, "w2s", [32, 512])
            a2s = SB(ca, "a2s", [32, 512])
            g2s = SB(ca, "g2s", [96, 512])
            wrs = SB(ca, "wrs", [128, 8, 36])
            wc = SB(ca, "wc", [128, 8, 128])
            logits = SB(ca, "logits", [128, 32, 36])
            Hst = [SB(ca, f"H{j}", [128, 64]) for j in range(4)]
            Hb_l = [SB(ca, f"Hb{j}", [128, 64], BF16) for j in range(4)]
            istb = SB(ca, "istb", [128, 64], BF16)
            carry = SB(ca, "carry", [128, 16])
            P.add("pool", lambda e: e.tensor_copy(out=identb[:], in_=ident), r=["cst"], w=["identb"])
            P.add("sp", lambda e: e.dma_start(out=w2s[:], in_=w2_d[:, :]), w=["w2s"], dma=True)
            P.add("sp", lambda e: e.dma_start(out=a2s[:], in_=a2_d[:, :]), w=["a2s"], dma=True)
            P.add("sp", lambda e: e.dma_start(out=g2s[:], in_=g2_d[:, :]), w=["g2s"], dma=True)
            P.add("sp", lambda e: e.dma_start(out=wrs[:], in_=wr_d.rearrange("(c p) n -> p c n", p=128)), w=["wrs"], dma=True)
            P.add("sp", lambda e: e.dma_start(out=wc[:], in_=wsT_d[:, :, :]), w=["wc"], dma=True)
            for h in range(8):
                P.add("pool", (lambda h: lambda e: e.tensor_tensor(out=wc[:, h, :], in0=wc[:, h, :], in1=tril, op=ALU.mult))(h),
                      r=["wc", "cst"], w=["wc"])
            P.add("pool", lambda e: e.memset(carry[:], 0.0), w=["carry"])
            for _i in range(int(os.environ.get("KPAD", "0"))):
                P.add("pool", lambda e: e.memset(gl[:, 0:1], 0.0), w=["gl_dummy"])
            for j in range(4):
                P.add("pool", (lambda j: lambda e: e.memset(Hst[j][:], 0.0))(j), w=[f"H{j}"])
                P.add("pool", (lambda j: lambda e: e.memset(Hb_l[j][:], 0.0))(j), w=[f"Hb{j}"])
            P.add("pool", lambda e: e.tensor_copy(out=istb[:], in_=istack), r=["cst"], w=["istb"])
            with ExitStack() as cw:
                wstg = [SB(cw, f"wstg{i}", [128, INW]) for i in range(2)]
                for kc in range(8):
                    sb = wstg[kc % 2]
                    P.add("sp", (lambda sb, kc: lambda e: e.dma_start(out=sb[:], in_=win_d[kc * 128:(kc + 1) * 128, :]))(sb, kc),
                          w=[f"wstg{kc % 2}"], dma=True)
                    P.add("pool", (lambda sb, kc: lambda e: e.tensor_copy(out=win[:, kc, :], in_=sb[:]))(sb, kc),
                          r=[f"wstg{kc % 2}"], w=["win"])
                for kc in range(8):
                    sb = wstg[kc % 2]
                    P.add("sp", (lambda sb, kc: lambda e: e.dma_start(out=sb[:, 0:D], in_=wout_d[kc * 128:(kc + 1) * 128, :]))(sb, kc),
                          w=[f"wstg{kc % 2}"], dma=True)
                    P.add("pool", (lambda sb, kc: lambda e: e.tensor_copy(out=wout[:, kc, :], in_=sb[:, 0:D]))(sb, kc),
                          r=[f"wstg{kc % 2}"], w=["wout"])
                P.barrier()

            ct = ExitStack()
            xt = [SB(ct, "xt0", [128, NSUB, D])] * 2
            xn = SB(ct, "xn", [128, NSUB, D], BF16)
            hT = SB(ct, "hT", [128, 8, TT], BF16)
            st6 = SB(ct, "st6", [128, 4, 6])
            mv = SB(ct, "mv", [128, 8])
            u_s = SB(ct, "u_s", [128, 4, TT], BF16)
            vg = SB(ct, "vg", [128, 512])
            vn = vg
            zraw = [SB(ct, f"zraw{i}", [128, TT + 1]) for i in range(2)]
            rkv = SB(ct, "rkv", [128, 12, TT])
            lor = SB(ct, "lor", [128, 3, TT])
            LW, ASN, BSN, KM, BON, GG = range(6)
            pers = [SB(ct, f"pers{j}", [128, 6, TT]) for j in range(4)]
            AA, KX, RN, PRD = range(4)
            ptmp_l = [SB(ct, f"ptmp{i}", [128, 4, TT]) for i in range(2)]
            ybT = SB(ct, "ybT", [128, 4, TT])
            ycat = SB(ct, "ycat", [128, 8, TT], BF16)
            x1t = SB(ct, "x1t", [128, D])
            xn2 = x1t
            h2f = SB(ct, "h2f", [128, 8, 128])
            gtmp = h2f[:, 0:4, :]
            xnb = SB(ct, "xnb", [128, D], BF16)
            CI, CE, EI, EE, EN = range(5)
            sct = [SB(ct, f"sct{j}", [128, 5, 64]) for j in range(4)]
            sb4 = [SB(ct, f"sb4{j}", [128, 4, 64], BF16) for j in range(4)]
            mats = [SB(ct, f"mats{j}", [128, 320], BF16) for j in range(4)]
            trs = [SB(ct, f"trs{j}", [128, 192], BF16) for j in range(4)]
            wzb_l = [SB(ct, f"wzb{j}", [128, 256], BF16) for j in range(4)]
            qq = [[SB(ct, f"qq{j}_{i}", [128, 64], BF16) for i in range(2)] for j in range(4)]
            chn = [SB(ct, f"chn{j}", [128, 3, 64], BF16) for j in range(4)]
            hpc_l = [SB(ct, f"hpc{j}", [128, 64]) for j in range(4)]
            gst = [SB(ct, f"gst{j}", [128, 12]) for j in range(4)]

            def A(eng, fn, r=(), w=()):
                P.add(eng, fn, r=r, w=w)

            def norm_stats(src, srck, eps):
                for hf in range(2):
                    A("dve", (lambda hf: lambda e: e.bn_stats(out=st6[:, hf, :], in_=src(hf)))(hf), r=[srck], w=["st6"])
                A("dve", lambda e: e.bn_aggr(out=mv[:, 0:2], in_=st6[:, 0:2, :].rearrange("p a b -> p (a b)")), r=["st6"], w=["mv"])
                A("dve", lambda e: e.scalar_tensor_tensor(out=mv[:, 2:3], in0=mv[:, 0:1], scalar=mv[:, 0:1], in1=mv[:, 1:2], op0=ALU.mult, op1=ALU.add),
                  r=["mv"], w=["mv"])
                A("act", lambda e: e.activation(out=mv[:, 3:4], in_=mv[:, 2:3], func=AF.Sqrt, bias=float(eps)), r=["mv"], w=["mv"])
                A("dve", lambda e: e.reciprocal(out=mv[:, 3:4], in_=mv[:, 3:4]), r=["mv"], w=["mv"])

            for TI in range(min(NT, ntiles) if stage >= 1 else 0):
                t0 = TI * TT
                xb = xt[TI % 2]
                xk = "xt0"
                P.add("sp", (lambda xb, t0: lambda e: e.dma_start(out=xb[:], in_=x_d[t0:t0 + TT, :].rearrange("(s p) d -> p s d", p=128)))(xb, t0),
                      w=[xk], dma=True)
                P.section(1)
                for sub in range(NSUB):
                    norm_stats((lambda xb, sub: lambda hf: xb[:, sub, hf * 512:(hf + 1) * 512])(xb, sub), xk, 1e-6)
                    A("act", (lambda xb, sub: lambda e: e.activation(out=xn[:, sub, :], in_=xb[:, sub, :], func=AF.Identity, scale=mv[:, 3:4]))(xb, sub),
                      r=[xk, "mv"], w=["xn"])
                P.section(1.5)
                for kc in range(8):
                    pb = kc % 2
                    for sub in range(NSUB):
                        A("pe", (lambda kc, sub, pb: lambda e: e.matmul(psT[pb][:, sub * 128:(sub + 1) * 128], lhsT=xn[:, sub, kc * 128:(kc + 1) * 128],
                                                                       rhs=identb[:], start=True, stop=True))(kc, sub, pb), r=["xn", "identb"], w=[psTk[pb]])
                    if os.environ.get("DVE_EVAC"):
                      A("dve", (lambda kc, pb: lambda e: e.tensor_scalar(out=hT[:, kc, :], in0=psT[pb][:, 0:TT], scalar1=sc[:, kc:kc + 1], scalar2=mod[:, kc:kc + 1],
                                                                         op0=ALU.mult, op1=ALU.add))(kc, pb), r=[psTk[pb], "sc", "mod"], w=["hT"])
                    elif not os.environ.get("SKIP_EVAC"):
                      A("act", (lambda kc, pb: lambda e: e.activation(out=hT[:, kc, :], in_=psT[pb][:, 0:TT], func=AF.Identity,
                                                                    **({} if os.environ.get("NO_SB") else dict(scale=sc[:, kc:kc + 1], bias=mod[:, kc:kc + 1]))))(kc, pb),
                      r=[psTk[pb], "sc", "mod"], w=["hT"])

                P.section(2)
                pcnt = [0]

                def proj(c0, M):
                    pb = pcnt[0] % 2
                    pcnt[0] += 1
                    for kc in range(8):
                        A("pe", (lambda kc, pb: lambda e: e.matmul(psum[pb][0:M, 0:TT], lhsT=win[:, kc, c0:c0 + M], rhs=hT[:, kc, :],
                                                                  start=(kc == 0), stop=(kc == 7)))(kc, pb), r=["win", "hT"], w=[pk(pb)])
                    return pb

                for j in range(4):
                    pb = proj(j * 128, 128)
                    A("act", (lambda j, pb: lambda e: e.activation(out=u_s[:, j, :], in_=psum[pb][:, 0:TT], func=AF.Gelu_apprx_tanh))(j, pb),
                      r=[pk(pb)], w=["u_s"])
                specs = [(1024 + q * 128, 128, PC["mu_rkv"] + q) for q in range(12)] + \
                        [(2560, 32, PC["mu_xw"]), (2592, 32, PC["mu_xa"]), (2624, 96, PC["mu_xg"])]
                for q, (c0, M, mucol) in enumerate(specs):
                    pb = proj(c0, M)
                    zb = zraw[q % 2]
                    zk = f"zraw{q % 2}"
                    dst = rkv[0:M, q, :] if q < 12 else lor[0:M, q - 12, :]
                    dk = f"rkv{q}"
                    A("dve", (lambda zb, q, M: lambda e: e.tensor_copy(out=zb[0:M, 0:1], in_=carry[0:M, q:q + 1]))(zb, q, M), r=["carry"], w=[zk])
                    A("act", (lambda zb, pb, M: lambda e: e.activation(out=zb[0:M, 1:TT + 1], in_=psum[pb][0:M, 0:TT], func=AF.Identity))(zb, pb, M),
                      r=[pk(pb)], w=[zk])
                    A("dve", (lambda zb, q, M: lambda e: e.tensor_copy(out=carry[0:M, q:q + 1], in_=zb[0:M, TT:TT + 1]))(zb, q, M), r=[zk], w=["carry"])
                    A("dve", (lambda zb, dst, M: lambda e: e.tensor_tensor(out=dst, in0=zb[0:M, 0:TT], in1=zb[0:M, 1:TT + 1], op=ALU.subtract))(zb, dst, M),
                      r=[zk], w=[dk])
                    A("dve", (lambda zb, dst, M, mucol: lambda e: e.scalar_tensor_tensor(out=dst, in0=dst, scalar=prm[0:M, mucol:mucol + 1], in1=zb[0:M, 1:TT + 1],
                                                                                      op0=ALU.mult, op1=ALU.add))(zb, dst, M, mucol), r=[zk, dk, "prm"], w=[dk])
                A("act", lambda e: e.activation(out=lor[0:32, 0, :], in_=lor[0:32, 0, :], func=AF.Tanh), r=["rkv12"], w=["rkv12"])
                A("act", lambda e: e.activation(out=lor[0:96, 2, :], in_=lor[0:96, 2, :], func=AF.Sigmoid), r=["rkv14"], w=["rkv14"])

                P.section(3)
                def prep_body(j, A):
                    par = j % 2
                    ptmp = ptmp_l[par]
                    pa, pb = (0, 1) if par == 0 else (2, 3)
                    jc = slice(j * 128, (j + 1) * 128)
                    pj = pers[j]
                    pjk = f"pers{j}"
                    rr = rkv[:, j, :]
                    kr = rkv[:, 4 + j, :]
                    vr = rkv[:, 8 + j, :]
                    A("pe", (lambda jc: lambda e: e.matmul(psum[pa][:, 0:TT], lhsT=w2s[0:32, jc], rhs=lor[0:32, 0, :], start=True, stop=True))(jc),
                      r=["w2s", "rkv12"], w=[pk(pa)])
                    A("act", (lambda pj, j: lambda e: e.activation(out=pj[:, LW, :], in_=psum[pa][:, 0:TT], func=AF.Sigmoid, bias=pcol("w0", j)))(pj, j),
                      r=[pk(pa), "prm"], w=[pjk + "LW"])
                    A("pool", (lambda pj: lambda e: e.tensor_scalar(out=pj[:, LW, :], in0=pj[:, LW, :], scalar1=NEG_HALF_E, scalar2=None, op0=ALU.mult))(pj),
                      r=[pjk + "LW"], w=[pjk + "LW"])
                    A("pe", (lambda jc: lambda e: e.matmul(psum[pb][:, 0:TT], lhsT=a2s[0:32, jc], rhs=lor[0:32, 1, :], start=True, stop=True))(jc),
                      r=["a2s", "rkv13"], w=[pk(pb)])
                    A("act", (lambda j: lambda e: e.activation(out=ptmp[:, AA, :], in_=psum[pb][:, 0:TT], func=AF.Sigmoid, bias=pcol("a0", j)))(j),
                      r=[pk(pb), "prm"], w=["pAA" + str(par)])
                    A("pe", (lambda jc: lambda e: e.matmul(psum[pa][:, 0:TT], lhsT=g2s[0:96, jc], rhs=lor[0:96, 2, :], start=True, stop=True))(jc),
                      r=["g2s", "rkv14"], w=[pk(pa)])
                    A("act", (lambda pj: lambda e: e.activation(out=pj[:, GG, :], in_=psum[pa][:, 0:TT], func=AF.Identity))(pj), r=[pk(pa)], w=[pjk + "GG"])
                    A("dve", (lambda kr, j: lambda e: e.tensor_scalar(out=ptmp[:, KX, :], in0=kr, scalar1=pcol("k_k", j), scalar2=None, op0=ALU.mult))(kr, j),
                      r=[f"rkv{4 + j}", "prm"], w=["pKX" + str(par)])
                    A("pool", lambda e: e.tensor_tensor(out=ptmp[:, RN, :], in0=ptmp[:, KX, :], in1=ptmp[:, KX, :], op=ALU.mult), r=["pKX" + str(par)], w=["pRN" + str(par)])
                    A("pe", lambda e: e.matmul(psum[pb][:, 0:TT], lhsT=bones, rhs=ptmp[:, RN, :], start=True, stop=True), r=["cst", "pRN" + str(par)], w=[pk(pb)])
                    A("act", lambda e: e.activation(out=ptmp[:, RN, :], in_=psum[pb][:, 0:TT], func=AF.Sqrt, bias=float(1e-24)), r=[pk(pb)], w=["pRN" + str(par)])
                    A("dve", lambda e: e.reciprocal(out=ptmp[:, RN, :], in_=ptmp[:, RN, :]), r=["pRN" + str(par)], w=["pRN" + str(par)])
                    A("dve", (lambda pj: lambda e: e.scalar_tensor_tensor(out=pj[:, ASN, :], in0=ptmp[:, KX, :], scalar=-1.0, in1=ptmp[:, RN, :],
                                                                          op0=ALU.mult, op1=ALU.mult))(pj), r=["pKX" + str(par), "pRN" + str(par)], w=[pjk + "ASN"])
                    A("dve", (lambda pj: lambda e: e.scalar_tensor_tensor(out=pj[:, BSN, :], in0=pj[:, ASN, :], scalar=-1.0, in1=ptmp[:, AA, :],
                                                                          op0=ALU.mult, op1=ALU.mult))(pj), r=[pjk + "ASN", "pAA" + str(par)], w=[pjk + "BSN"])
                    A("dve", (lambda j: lambda e: e.tensor_scalar(out=ptmp[:, PRD, :], in0=ptmp[:, AA, :], scalar1=-1.0, scalar2=pcol("k_a", j),
                                                                  op0=ALU.add, op1=ALU.mult))(j), r=["pAA" + str(par), "prm"], w=["pPRD" + str(par)])
                    A("dve", (lambda pj, kr: lambda e: e.scalar_tensor_tensor(out=pj[:, KM, :], in0=ptmp[:, PRD, :], scalar=1.0, in1=kr,
                                                                              op0=ALU.add, op1=ALU.mult))(pj, kr), r=["pPRD" + str(par), f"rkv{4 + j}"], w=[pjk + "KM"])
                    A("dve", (lambda pj, rr, j: lambda e: e.scalar_tensor_tensor(out=ptmp[:, PRD, :], in0=rr, scalar=pcol("r_k", j), in1=pj[:, KM, :],
                                                                                 op0=ALU.mult, op1=ALU.mult))(pj, rr, j), r=[f"rkv{j}", "prm", pjk + "KM"], w=["pPRD" + str(par)])
                    A("pe", lambda e: e.matmul(psum[pa][:, 0:TT], lhsT=bones, rhs=ptmp[:, PRD, :], start=True, stop=True), r=["cst", "pPRD" + str(par)], w=[pk(pa)])
                    A("dve", (lambda pj, vr: lambda e: e.tensor_tensor(out=pj[:, BON, :], in0=psum[pa][:, 0:TT], in1=vr, op=ALU.mult))(pj, vr),
                      r=[pk(pa), f"rkv{8 + j}"], w=[pjk + "BON"])


                prep_ops = []
                for j in range(4):
                    rec = []
                    prep_body(j, lambda eng, fn, r=(), w=(): rec.append((eng, fn, r, w)))
                    prep_ops.append(rec)
                for grp in ((0, 1), (2, 3)):
                    for si in range(len(prep_ops[0])):
                        for j in grp:
                            eng_, fn_, r_, w_ = prep_ops[j][si]
                            A(eng_, fn_, r=r_, w=w_)
                P.section(4)
                def unit_stages(j, c):
                    col = slice(c * CH, (c + 1) * CH)
                    pj = pers[j]
                    pjk = f"pers{j}"
                    s = sct[j]
                    sk = f"sct{j}"
                    mt = mats[j]
                    mk = f"mats{j}"
                    tr = trs[j]
                    tk = f"trs{j}"
                    cn = chn[j]
                    ck = f"chn{j}"
                    H = Hst[j]
                    Hk = f"H{j}"
                    Hb = Hb_l[j]
                    Hbk = f"Hb{j}"
                    b4 = sb4[j]
                    hpc = hpc_l[j]
                    rr = rkv[:, j, col]
                    vr = rkv[:, 8 + j, col]
                    hp = [slice(0, 64), slice(64, 128)]
                    st = []

                    def s1():
                        A("dve", lambda e: e.tensor_tensor_scan(out=s[:, CI, :], data0=ones[:, 0:64], data1=pj[:, LW, col], initial=0.0, op0=ALU.mult, op1=ALU.add),
                          r=[pjk + "LW", "cst"], w=[sk + "ci"])
                        A("dve", lambda e: e.tensor_tensor(out=s[:, CE, :], in0=s[:, CI, :], in1=pj[:, LW, col], op=ALU.subtract),
                          r=[sk + "ci", pjk + "LW"], w=[sk + "ce"])
                    st.append(s1)

                    def s2():
                        A("act", lambda e: e.activation(out=s[:, EI, :], in_=s[:, CI, :], func=AF.Exp), r=[sk + "ci"], w=[sk + "ei"])
                        A("act", lambda e: e.activation(out=s[:, EE, :], in_=s[:, CE, :], func=AF.Exp), r=[sk + "ce"], w=[sk + "ee"])
                        A("act", lambda e: e.activation(out=s[:, EN, :], in_=s[:, CI, :], func=AF.Exp, scale=-1.0), r=[sk + "ci"], w=[sk + "en"])
                    st.append(s2)

                    def s3():
                        A("dve", lambda e: e.tensor_tensor(out=b4[:, 0, :], in0=pj[:, ASN, col], in1=s[:, EE, :], op=ALU.mult), r=[pjk + "ASN", sk + "ee"], w=[sk + "at"])
                        A("dve", lambda e: e.tensor_tensor(out=b4[:, 1, :], in0=rr, in1=s[:, EI, :], op=ALU.mult), r=[f"rkv{j}", sk + "ei"], w=[sk + "rt"])
                        A("dve", lambda e: e.tensor_tensor(out=b4[:, 2, :], in0=pj[:, BSN, col], in1=s[:, EN, :], op=ALU.mult), r=[pjk + "BSN", sk + "en"], w=[sk + "bt"])
                        A("dve", lambda e: e.tensor_tensor(out=b4[:, 3, :], in0=pj[:, KM, col], in1=s[:, EN, :], op=ALU.mult), r=[pjk + "KM", sk + "en"], w=[sk + "kt"])
                    st.append(s3)

                    def s4():
                        for p in hp:
                            A("pe", (lambda p: lambda e: e.matmul(psum[2][p, 0:128], lhsT=b4[p, 2, :], rhs=b4[p, 0:2, :].rearrange("p a b -> p (a b)"), start=True, stop=True))(p),
                              r=[sk + "bt", sk + "at", sk + "rt"], w=["ps2"])
                            A("pe", (lambda p: lambda e: e.matmul(psum[2][p, 128:256], lhsT=b4[p, 3, :], rhs=b4[p, 0:2, :].rearrange("p a b -> p (a b)"), start=True, stop=True))(p),
                              r=[sk + "kt", sk + "at", sk + "rt"], w=["ps2"])
                            A("pe", (lambda p: lambda e: e.matmul(psum[2][p, 256:320], lhsT=b4[p, 0, :], rhs=b4[p, 2, :], start=True, stop=True))(p),
                              r=[sk + "bt", sk + "at"], w=["ps2"])
                        A("dve", lambda e: e.tensor_tensor(out=mt[:], in0=psum[2][:, 0:320], in1=m5, op=ALU.mult), r=["ps2", "cst"], w=[mk])
                        for p in hp:
                            A("pe", (lambda p: lambda e: e.matmul(psum[3][p, 0:64], lhsT=rkv[p, 8 + j, col], rhs=istack[p, :], start=True, stop=True))(p),
                              r=[f"rkv{8 + j}", "cst"], w=["ps3"])
                            A("pe", (lambda p: lambda e: e.matmul(psum[3][p, 64:128], lhsT=b4[p, 2, :], rhs=istb[p, :], start=True, stop=True))(p),
                              r=[sk + "bt", "istb"], w=["ps3"])
                            A("pe", (lambda p: lambda e: e.matmul(psum[3][p, 128:192], lhsT=b4[p, 3, :], rhs=istb[p, :], start=True, stop=True))(p),
                              r=[sk + "kt", "istb"], w=["ps3"])
                        A("act", lambda e: e.activation(out=tr[:], in_=psum[3][:, 0:192], func=AF.Identity), r=["ps3"], w=[tk])
                        A("dve", lambda e: e.tensor_tensor(out=qq[j][0][:], in0=mt[:, 0:64], in1=istack, op=ALU.add), r=[mk, "cst"], w=[f"qq{j}_0"])
                    st.append(s4)

                    wzb = wzb_l[j]
                    wbk = f"wzb{j}"
                    bm3 = bones.rearrange("p (a b) -> p a b", a=2)

                    def s4b():
                        A("dve", lambda e: e.tensor_tensor(out=wzb[:, 0:128].rearrange("p (a b) -> p a b", a=2),
                                                            in0=mt[:, 0:64].rearrange("p (o t) -> p o t", o=1).to_broadcast([128, 2, 64]), in1=bm3, op=ALU.mult),
                          r=[mk, "cst"], w=[wbk])
                        A("dve", lambda e: e.tensor_tensor(out=wzb[:, 128:256].rearrange("p (a b) -> p a b", a=2),
                                                            in0=mt[:, 256:320].rearrange("p (o t) -> p o t", o=1).to_broadcast([128, 2, 64]), in1=bm3, op=ALU.mult),
                          r=[mk, "cst"], w=[wbk])
                    st.append(s4b)

                    for i in range(5):
                        def lv(i=i):
                            last = (i == 4)
                            qc = qq[j][i % 2]
                            qck = f"qq{j}_{i % 2}"
                            qn = qq[j][(i + 1) % 2]
                            qnk = f"qq{j}_{(i + 1) % 2}"
                            if not last:
                                A("pe", lambda e: e.matmul(psum[4][:, 0:128], lhsT=wzb[:, 128:256], rhs=wzb[:, 0:128], start=True, stop=True), r=[wbk], w=["ps4"])
                            A("pe", lambda e: e.matmul(psum[4][:, 128:256], lhsT=wzb[:, 0:128], rhs=wzb[:, 128:256], start=True, stop=True), r=[wbk], w=["ps4"])
                            if not last:
                                A("act", lambda e: e.activation(out=wzb[:, 0:256], in_=psum[4][:, 0:256], func=AF.Identity), r=["ps4"], w=[wbk])
                            else:
                                A("act", lambda e: e.activation(out=wzb[:, 128:256], in_=psum[4][:, 128:256], func=AF.Identity), r=["ps4"], w=[wbk])
                            A("pe", lambda e: e.matmul(psum[1][:, 128:192], lhsT=wzb[:, 128:256], rhs=qc[:, :], start=True, stop=True), r=[wbk, qck], w=["ps1"])
                            A("dve", lambda e: e.tensor_tensor(out=qn[:], in0=psum[1][:, 128:192], in1=qc[:], op=ALU.add), r=["ps1", qck], w=[qnk])
                        st.append(lv)

                    cbank = [5, 6, 7, 0][j]
                    cps = psum[cbank] if cbank != 7 else psTt
                    cpk = f"ps{cbank}"
                    q5 = qq[j][1]
                    q5k = f"qq{j}_1"
                    g = gst[j]
                    gk = f"gst{j}"

                    def s5a():
                        A("dve", lambda e: e.tensor_scalar(out=hpc[:], in0=H[:], scalar1=s[:, EI, 63:64], scalar2=None, op0=ALU.mult),
                          r=[Hk, sk + "ei"], w=[ck + "hpc"])
                        for p in hp:
                            A("pe", (lambda p: lambda e: e.matmul(cps[p, 0:64], lhsT=b4[p, 0, :], rhs=Hb[p, :], start=True, stop=False))(p), r=[sk + "at", Hbk], w=[cpk])
                            A("pe", (lambda p: lambda e: e.matmul(cps[p, 0:64], lhsT=mt[p, 128:192], rhs=tr[p, 0:64], start=False, stop=True))(p), r=[mk, tk], w=[cpk])
                        A("act", lambda e: e.activation(out=cn[:, 0, :], in_=cps[:, 0:64], func=AF.Identity), r=[cpk], w=[ck + "x"])
                    st.append(s5a)

                    def s5b():
                        for p in hp:
                            A("pe", (lambda p: lambda e: e.matmul(cps[p, 64:128], lhsT=q5[p, :], rhs=cn[p, 0, :], start=True, stop=True))(p), r=[q5k, ck + "x"], w=[cpk])
                        A("act", lambda e: e.activation(out=cn[:, 1, :], in_=cps[:, 64:128], func=AF.Identity), r=[cpk], w=[ck + "u"])
                    st.append(s5b)

                    def s5c():
                        for p in hp:
                            A("pe", (lambda p: lambda e: e.matmul(cps[p, 192:256], lhsT=tr[p, 64:128], rhs=cn[p, 1, :], start=True, stop=False))(p), r=[tk, ck + "u"], w=[cpk])
                            A("pe", (lambda p: lambda e: e.matmul(cps[p, 192:256], lhsT=tr[p, 128:192], rhs=tr[p, 0:64], start=False, stop=True))(p), r=[tk], w=[cpk])
                        for p in hp:
                            A("pe", (lambda p: lambda e: e.matmul(cps[p, 128:192], lhsT=b4[p, 1, :], rhs=Hb[p, :], start=True, stop=False))(p), r=[sk + "rt", Hbk], w=[cpk])
                            A("pe", (lambda p: lambda e: e.matmul(cps[p, 128:192], lhsT=mt[p, 64:128], rhs=cn[p, 1, :], start=False, stop=False))(p), r=[mk, ck + "u"], w=[cpk])
                            A("pe", (lambda p: lambda e: e.matmul(cps[p, 128:192], lhsT=mt[p, 192:256], rhs=tr[p, 0:64], start=False, stop=True))(p), r=[mk, tk], w=[cpk])
                        A("dve", lambda e: e.scalar_tensor_tensor(out=Hb[:], in0=cps[:, 192:256], scalar=s[:, EI, 63:64], in1=hpc[:], op0=ALU.mult, op1=ALU.add),
                          r=[cpk, sk + "ei", ck + "hpc"], w=[Hbk])
                        A("dve", lambda e: e.scalar_tensor_tensor(out=H[:], in0=cps[:, 192:256], scalar=s[:, EI, 63:64], in1=hpc[:], op0=ALU.mult, op1=ALU.add),
                          r=[cpk, sk + "ei", ck + "hpc"], w=[Hk])
                    st.append(s5c)

                    def s5d():
                        A("dve", lambda e: e.bn_stats(out=g[:, 0:6], in_=cps[:, 128:192]), r=[cpk], w=[gk])
                        A("dve", lambda e: e.bn_aggr(out=g[:, 6:8], in_=g[:, 0:6]), r=[gk], w=[gk])
                        A("act", lambda e: e.activation(out=g[:, 8:9], in_=g[:, 7:8], func=AF.Sqrt, bias=float(64e-5)), r=[gk], w=[gk])
                        A("dve", lambda e: e.reciprocal(out=g[:, 8:9], in_=g[:, 8:9]), r=[gk], w=[gk])
                        A("dve", lambda e: e.tensor_scalar(out=cn[:, 2, :], in0=cps[:, 128:192], scalar1=g[:, 6:7], scalar2=g[:, 8:9], op0=ALU.subtract, op1=ALU.mult),
                          r=[cpk, gk], w=[ck + "yn"])
                    st.append(s5d)

                    def s5e():
                        for p in hp:
                            A("pe", (lambda p: lambda e: e.matmul(psum[3][p, 192:256], lhsT=cn[p, 2, :], rhs=istb[p, :], start=True, stop=True))(p), r=[ck + "yn", "istb"], w=["ps3"])
                        A("act", lambda e: e.activation(out=ybT[:, j, col], in_=psum[3][:, 192:256], func=AF.Identity, scale=pcol("gn_w", j), bias=pcol("gn_b", j)),
                          r=["ps3", "prm"], w=[f"ybT{j}"])
                    st.append(s5e)
                    return st

                for c in range(NCH):
                    stl = [unit_stages(j, c) for j in range(4)]
                    for si in range(len(stl[0])):
                        for j in range(4):
                            stl[j][si]()
                P.section(5)
                for j in range(4):
                    pj = pers[j]
                    pjk = f"pers{j}"
                    A("pool", (lambda pj, j: lambda e: e.tensor_tensor(out=ybT[:, j, :], in0=ybT[:, j, :], in1=pj[:, BON, :], op=ALU.add))(pj, j),
                      r=[f"ybT{j}", pjk + "BON"], w=[f"ybT{j}"])
                    A("pool", (lambda pj, j: lambda e: e.tensor_tensor(out=ycat[:, 4 + j, :], in0=ybT[:, j, :], in1=pj[:, GG, :], op=ALU.mult))(pj, j),
                      r=[f"ybT{j}", pjk + "GG"], w=["ycat"])

                P.section(6)
                for sub in range(NSUB):
                    tc_ = slice(sub * 128, (sub + 1) * 128)
                    for kc in range(8):
                        A("pe", (lambda kc, tc_: lambda e: e.matmul(psum[0][:, 0:512], lhsT=hT[:, kc, tc_], rhs=win[:, kc, 512:1024], start=(kc == 0), stop=(kc == 7)))(kc, tc_),
                          r=["hT", "win"], w=[pk(0)])
                    A("act", lambda e: e.activation(out=vg[:], in_=psum[0][:, 0:512], func=AF.Gelu_apprx_tanh), r=[pk(0)], w=["vg"])
                    A("dve", lambda e: e.bn_stats(out=st6[:, 2, :], in_=vg[:]), r=["vg"], w=["st6b"])
                    A("dve", lambda e: e.bn_aggr(out=mv[:, 4:6], in_=st6[:, 2, :]), r=["st6b"], w=["mvb"])
                    A("act", lambda e: e.activation(out=mv[:, 6:7], in_=mv[:, 5:6], func=AF.Sqrt, bias=float(1e-5)), r=["mvb"], w=["mvb"])
                    A("dve", lambda e: e.reciprocal(out=mv[:, 6:7], in_=mv[:, 6:7]), r=["mvb"], w=["mvb"])
                    A("dve", lambda e: e.tensor_scalar(out=vn[:], in0=vg[:], scalar1=mv[:, 4:5], scalar2=mv[:, 6:7], op0=ALU.subtract, op1=ALU.mult), r=["vg", "mvb"], w=["vg"])
                    A("pool", lambda e: e.tensor_tensor(out=vn[:], in0=vn[:], in1=bcs[:, BC["lnw"]:BC["lnw"] + 512], op=ALU.mult), r=["vg", "bc"], w=["vg"])
                    A("pool", lambda e: e.tensor_tensor(out=vn[:], in0=vn[:], in1=bcs[:, BC["lnb"]:BC["lnb"] + 512], op=ALU.add), r=["vg", "bc"], w=["vg"])
                    for h in range(8):
                        A("pe", (lambda h: lambda e: e.matmul(psum[1][(h % 2) * 64:(h % 2) * 64 + 64, (h // 2) * 128:(h // 2 + 1) * 128], lhsT=vn[:, h * 64:(h + 1) * 64],
                                                              rhs=wc[:, h, :], start=True, stop=True))(h), r=["vg", "wc"], w=[pk(1)])
                    A("dve", lambda e: e.tensor_tensor(out=gtmp.rearrange("p a b -> p (a b)"), in0=psum[1][:, 0:512], in1=bcs[:, BC["bsT"]:BC["bsT"] + 512], op=ALU.add),
                      r=[pk(1), "bc"], w=["h2f"])
                    A("dve", (lambda tc_: lambda e: e.tensor_tensor(out=ycat[:, 0:4, tc_], in0=gtmp, in1=u_s[:, :, tc_], op=ALU.mult))(tc_), r=["h2f", "u_s"], w=["ycat"])

                P.section(7)
                for sub in range(NSUB):
                    tc_ = slice(sub * 128, (sub + 1) * 128)
                    ti = TI * NSUB + sub
                    tok0 = t0 + sub * 128
                    for hf in range(2):
                        for cc in range(8):
                            A("pe", (lambda cc, hf, tc_: lambda e: e.matmul(psum[6][:, 0:512], lhsT=ycat[:, cc, tc_], rhs=wout[:, cc, hf * 512:(hf + 1) * 512],
                                                                           start=(cc == 0), stop=(cc == 7)))(cc, hf, tc_), r=["ycat", "wout"], w=[pk(6)])
                        A("dve", (lambda hf: lambda e: e.tensor_tensor(out=x1t[:, hf * 512:(hf + 1) * 512], in0=psum[6][:, 0:512], in1=gate_bc[:, hf * 512:(hf + 1) * 512],
                                                                      op=ALU.mult))(hf), r=[pk(6), "gate_bc"], w=["x1t"])
                        A("pool", (lambda hf, xb, sub: lambda e: e.tensor_tensor(out=x1t[:, hf * 512:(hf + 1) * 512], in0=x1t[:, hf * 512:(hf + 1) * 512],
                                                                                in1=xb[:, sub, hf * 512:(hf + 1) * 512], op=ALU.add))(hf, xb, sub), r=["x1t", xk], w=["x1t"])
                    P.add("sp", (lambda tok0: lambda e: e.dma_start(out=x1_d[tok0:tok0 + 128, :], in_=x1t[:]))(tok0), r=["x1t"], w=["x1_d"], dma=True)
                    norm_stats(lambda hf: x1t[:, hf * 512:(hf + 1) * 512], "x1t", 1e-6)
                    A("act", lambda e: e.activation(out=xn2[:], in_=x1t[:], func=AF.Identity, scale=mv[:, 3:4]), r=["x1t", "mv"], w=["x1t"])
                    for kc in range(8):
                        pb = kc // 4
                        A("pe", (lambda kc, pb: lambda e: e.matmul(psum[pb][:, (kc % 4) * 128:(kc % 4 + 1) * 128], lhsT=xn2[:, kc * 128:(kc + 1) * 128], rhs=ident, start=True, stop=True))(kc, pb),
                          r=["x1t", "cst"], w=[pk(pb)])
                    for kc in range(8):
                        pb = kc // 4
                        A("act", (lambda kc, pb: lambda e: e.activation(out=h2f[:, kc, :], in_=psum[pb][:, (kc % 4) * 128:(kc % 4 + 1) * 128], func=AF.Identity,
                                                                        scale=sc[:, 8 + kc:9 + kc], bias=mod[:, 24 + kc:25 + kc]))(kc, pb), r=[pk(pb), "sc", "mod"], w=["h2f"])
                    A("pool", lambda e: e.tensor_copy(out=xnb[:], in_=xn2[:]), r=["x1t"], w=["xnb"])
                    P.add("sp", (lambda tok0: lambda e: e.dma_start(out=xn2_d[tok0:tok0 + 128, :], in_=xnb[:]))(tok0),
                          r=["xnb"], w=["xn2_d"], dma=True)
                    for kc in range(8):
                        A("pe", (lambda kc: lambda e: e.matmul(psum[6][:, 0:36], lhsT=h2f[:, kc, :], rhs=wrs[:, kc, :], start=(kc == 0), stop=(kc == 7)))(kc),
                          r=["h2f", "wrs"], w=[pk(6)])
                    A("dve", (lambda ti: lambda e: e.tensor_tensor(out=logits[:, ti, :], in0=psum[6][:, 0:36], in1=bcs[:, BC["rb"]:BC["rb"] + 36], op=ALU.add))(ti),
                      r=[pk(6), "bc"], w=["logits"])

            P.muted = False
            P.barrier()
            ct.close()
            if stage >= 2:
                rt = SB(ca, "rt", [128, 32, 48])
                sel = SB(ca, "sel", [128, 32, 8])
                sel2 = SB(ca, "sel2", [128, 32, 8])
                ohg = SB(ca, "ohg", [128, 32, 4])
                tmp48 = SB(ca, "tmp48", [128, 32, 4, 8])
                lg = logits[:, :, 0:4]
                le = logits[:, :, 4:36].rearrange("p t (g e) -> p t g e", g=4)
                MG, SG, M1, M2, P1, W1, W2 = range(7)
                r1 = lambda i: rt[:, :, i:i + 1]
                R = lambda fn, r, w: A("dve", fn, r=r, w=w)
                R(lambda e: e.tensor_reduce(out=rt[:, :, MG], in_=lg, axis=AX.X, op=ALU.max), ["logits"], ["rt"])
                R(lambda e: e.tensor_tensor(out=ohg[:], in0=lg, in1=r1(MG).to_broadcast([128, 32, 4]), op=ALU.is_equal), ["logits", "rt"], ["ohg"])
                R(lambda e: e.tensor_tensor(out=rt[:, :, 8:12], in0=lg, in1=r1(MG).to_broadcast([128, 32, 4]), op=ALU.subtract), ["logits", "rt"], ["rt"])
                A("act", lambda e: e.activation(out=rt[:, :, 8:12], in_=rt[:, :, 8:12], func=AF.Exp), r=["rt"], w=["rt"])
                R(lambda e: e.tensor_reduce(out=rt[:, :, SG], in_=rt[:, :, 8:12], axis=AX.X, op=ALU.add), ["rt"], ["rt"])
                R(lambda e: e.reciprocal(out=rt[:, :, SG], in_=rt[:, :, SG]), ["rt"], ["rt"])
                R(lambda e: e.tensor_tensor(out=tmp48[:], in0=le, in1=ohg[:].rearrange("p t (g o) -> p t g o", o=1).to_broadcast([128, 32, 4, 8]), op=ALU.mult),
                  ["logits", "ohg"], ["tmp48"])
                R(lambda e: e.tensor_reduce(out=sel[:], in_=tmp48[:].rearrange("p t g e -> p t e g"), axis=AX.X, op=ALU.add), ["tmp48"], ["sel"])
                R(lambda e: e.tensor_reduce(out=rt[:, :, M1], in_=sel[:], axis=AX.X, op=ALU.max), ["sel"], ["rt"])
                R(lambda e: e.tensor_tensor(out=sel2[:], in0=sel[:], in1=r1(M1).to_broadcast([128, 32, 8]), op=ALU.is_equal), ["sel", "rt"], ["sel2"])
                R(lambda e: e.scalar_tensor_tensor(out=tmp48[:, :, 0, :], in0=sel2[:], scalar=-1e30, in1=sel[:], op0=ALU.mult, op1=ALU.add), ["sel", "sel2"], ["tmp48"])
                R(lambda e: e.tensor_reduce(out=rt[:, :, M2], in_=tmp48[:, :, 0, :], axis=AX.X, op=ALU.max), ["tmp48"], ["rt"])
                R(lambda e: e.tensor_tensor(out=tmp48[:, :, 1, :], in0=tmp48[:, :, 0, :], in1=r1(M2).to_broadcast([128, 32, 8]), op=ALU.is_equal), ["tmp48", "rt"], ["tmp48"])
                R(lambda e: e.tensor_tensor(out=rt[:, :, P1], in0=rt[:, :, M2], in1=rt[:, :, M1], op=ALU.subtract), ["rt"], ["rt"])
                A("act", lambda e: e.activation(out=rt[:, :, P1], in_=rt[:, :, P1], func=AF.Exp), r=["rt"], w=["rt"])
                R(lambda e: e.tensor_scalar(out=rt[:, :, P1], in0=rt[:, :, P1], scalar1=1.0, scalar2=None, op0=ALU.add), ["rt"], ["rt"])
                R(lambda e: e.reciprocal(out=rt[:, :, P1], in_=rt[:, :, P1]), ["rt"], ["rt"])
                R(lambda e: e.tensor_tensor(out=rt[:, :, W1], in0=rt[:, :, P1], in1=rt[:, :, SG], op=ALU.mult), ["rt"], ["rt"])
                R(lambda e: e.tensor_tensor(out=rt[:, :, W2], in0=rt[:, :, SG], in1=rt[:, :, W1], op=ALU.subtract), ["rt"], ["rt"])
                R(lambda e: e.tensor_tensor(out=sel[:], in0=sel2[:], in1=r1(W1).to_broadcast([128, 32, 8]), op=ALU.mult), ["sel2", "rt"], ["sel"])
                R(lambda e: e.tensor_tensor(out=sel2[:], in0=tmp48[:, :, 1, :], in1=r1(W2).to_broadcast([128, 32, 8]), op=ALU.mult), ["tmp48", "rt"], ["sel2"])
                R(lambda e: e.tensor_tensor(out=sel[:], in0=sel[:], in1=sel2[:], op=ALU.add), ["sel", "sel2"], ["sel"])
                for g in range(4):
                    R((lambda g: lambda e: e.tensor_tensor(out=wt[:, :, g * 8:(g + 1) * 8], in0=sel[:], in1=ohg[:, :, g:g + 1].to_broadcast([128, 32, 8]), op=ALU.mult))(g),
                      ["sel", "ohg"], ["wt"])
            if stage >= 2:
                Mf = SB(ca, "Mf", [128, 1024])
                Mb = SB(ca, "Mb", [128, 1024], BF16)
                Lsb = SB(ca, "Lsb", [128, 128], BF16)
                onesb = SB(ca, "onesb", [128, 128], BF16)
                PRE = SB(ca, "PRE", [128, 1024])
                CNTs = SB(ca, "CNTs", [128, 1024])
                CA_ = SB(ca, "CA", [128, 1024])
                CB_ = SB(ca, "CB", [128, 1024])
                sm = SB(ca, "sm", [128, 8, 32])
                cmpb = SB(ca, "cmpb", [128, 96, 32])
                bev = SB(ca, "bev", [128, 6, 96])
                pabf = SB(ca, "pabf", [128, 2, 32])
                v3 = lambda t: t[:].rearrange("p (a b) -> p a b", a=32)
                wtf = wt[:].rearrange("p t e -> p (t e)")
                R(lambda e: e.tensor_single_scalar(out=Mf[:], in_=wtf, scalar=0.0, op=ALU.is_gt), ["wt"], ["Mf"])
                A("pool", lambda e: e.tensor_copy(out=Mb[:], in_=Mf[:]), r=["Mf"], w=["Mb"])
                A("pool", lambda e: e.tensor_tensor(out=Lsb[:], in0=tril, in1=ident, op=ALU.subtract), r=["cst"], w=["Lsb"])
                A("pool", lambda e: e.tensor_copy(out=onesb[:], in_=ones), r=["cst"], w=["onesb"])
                for h in range(2):
                    A("pe", (lambda h: lambda e: e.matmul(psum[h][:, 0:512], lhsT=Lsb[:], rhs=Mb[:, h * 512:(h + 1) * 512], start=True, stop=True))(h),
                      r=["Lsb", "Mb"], w=[pk(h)])
                    A("pe", (lambda h: lambda e: e.matmul(psum[2 + h][:, 0:512], lhsT=onesb[:], rhs=Mb[:, h * 512:(h + 1) * 512], start=True, stop=True))(h),
                      r=["onesb", "Mb"], w=[pk(2 + h)])
                    A("act", (lambda h: lambda e: e.activation(out=PRE[:, h * 512:(h + 1) * 512], in_=psum[h][:, 0:512], func=AF.Identity))(h), r=[pk(h)], w=["PRE"])
                    A("act", (lambda h: lambda e: e.activation(out=CNTs[:, h * 512:(h + 1) * 512], in_=psum[2 + h][:, 0:512], func=AF.Identity))(h), r=[pk(2 + h)], w=["CNTs"])
                src, srck, dst, dstk = CNTs, "CNTs", CA_, "CA"
                for dd in (1, 2, 4, 8, 16):
                    w_ = dd * 32
                    R((lambda src, dst, w_: lambda e: e.tensor_copy(out=dst[:, 0:w_], in_=src[:, 0:w_]))(src, dst, w_), [srck], [dstk])
                    R((lambda src, dst, w_: lambda e: e.tensor_tensor(out=dst[:, w_:1024], in0=src[:, w_:1024], in1=src[:, 0:1024 - w_], op=ALU.add))(src, dst, w_), [srck], [dstk])
                    src, srck = dst, dstk
                    dst, dstk = (CB_, "CB") if dst is CA_ else (CA_, "CA")
                R(lambda e: e.tensor_tensor(out=CB_[:], in0=CA_[:], in1=CNTs[:], op=ALU.subtract), ["CA", "CNTs"], ["CB"])
                R(lambda e: e.tensor_copy(out=sm[:, 0, :], in_=CA_[:, 31 * 32:32 * 32]), ["CA"], ["sm"])
                R(lambda e: e.tensor_tensor(out=cmpb[:, 0:32, :], in0=sm[:, 0, :].rearrange("p (e o) -> p e o", o=1).to_broadcast([128, 32, 32]),
                                            in1=cst[:, CC["thr"]:CC["thr"] + 32].rearrange("p (o k) -> p o k", o=1).to_broadcast([128, 32, 32]), op=ALU.is_gt),
                  ["sm", "cst"], ["cmpb"])
                R(lambda e: e.tensor_reduce(out=sm[:, 1, :], in_=cmpb[:, 0:32, :], axis=AX.X, op=ALU.add), ["cmpb"], ["sm"])
                R(lambda e: e.tensor_tensor_scan(out=sm[:, 2, :], data0=ones[:, 0:32], data1=sm[:, 1, :], initial=0.0, op0=ALU.mult, op1=ALU.add), ["sm", "cst"], ["sm"])
                R(lambda e: e.tensor_tensor(out=sm[:, 3, :], in0=sm[:, 2, :], in1=sm[:, 1, :], op=ALU.subtract), ["sm"], ["sm"])
                R(lambda e: e.tensor_single_scalar(out=sm[:, 4, :], in_=sm[:, 3, :], scalar=128.0, op=ALU.mult), ["sm"], ["sm"])
                R(lambda e: e.tensor_tensor(out=v3(CB_), in0=v3(CB_), in1=sm[:, 4, :].rearrange("p (o e) -> p o e", o=1).to_broadcast([128, 32, 32]), op=ALU.add),
                  ["CB", "sm"], ["CB"])
                R(lambda e: e.tensor_tensor(out=PRE[:], in0=PRE[:], in1=CB_[:], op=ALU.add), ["PRE", "CB"], ["PRE"])
                R(lambda e: e.tensor_tensor(out=CA_[:], in0=PRE[:], in1=Mf[:], op=ALU.mult), ["PRE", "Mf", "sm"], ["CA"])
                R(lambda e: e.tensor_scalar(out=CB_[:], in0=Mf[:], scalar1=-1e9, scalar2=1e9, op0=ALU.mult, op1=ALU.add), ["Mf", "PRE"], ["CB"])
                R(lambda e: e.tensor_tensor(out=CB_[:], in0=CB_[:], in1=CA_[:], op=ALU.add), ["CB", "CA"], ["CB"])
                R(lambda e: e.tensor_reduce(out=pabf[:, 0, :], in_=v3(CB_), axis=AX.X, op=ALU.min), ["CB"], ["pabf"])
                R(lambda e: e.tensor_reduce(out=pabf[:, 1, :], in_=v3(CA_), axis=AX.X, op=ALU.max), ["CA"], ["pabf"])
                R(lambda e: e.tensor_tensor(out=v3(CB_), in0=v3(CB_), in1=pabf[:, 0, :].rearrange("p (t o) -> p t o", o=1).to_broadcast([128, 32, 32]), op=ALU.is_equal),
                  ["CB", "pabf"], ["CB"])
                R(lambda e: e.tensor_tensor(out=CB_[:], in0=CB_[:], in1=wtf, op=ALU.mult), ["CB", "wt"], ["CB"])
                R(lambda e: e.tensor_reduce(out=WAB[:, 0, :], in_=v3(CB_), axis=AX.X, op=ALU.add), ["CB"], ["WAB"])
                R(lambda e: e.tensor_tensor(out=WAB[:, 1, :], in0=rt[:, :, SG], in1=WAB[:, 0, :], op=ALU.subtract), ["rt", "WAB"], ["WAB"])
                R(lambda e: e.tensor_copy(out=PABi[:], in_=pabf[:]), ["pabf"], ["PABi"])
                R(lambda e: e.tensor_tensor(out=cmpb[:], in0=sm[:, 2, :].rearrange("p (o e) -> p o e", o=1).to_broadcast([128, 96, 32]),
                                            in1=cst[:, CC["iotab"]:CC["iotab"] + 96].rearrange("p (b o) -> p b o", o=1).to_broadcast([128, 96, 32]), op=ALU.is_le),
                  ["sm", "cst", "cmpb"], ["cmpb"])
                R(lambda e: e.tensor_reduce(out=bev[:, 0, :], in_=cmpb[:], axis=AX.X, op=ALU.add), ["cmpb"], ["bev"])
                R(lambda e: e.memset(bev[:, 1, 0:1], 1.0), [], ["bev"])
                R(lambda e: e.tensor_tensor(out=bev[:, 1, 1:96], in0=bev[:, 0, 1:96], in1=bev[:, 0, 0:95], op=ALU.not_equal), ["bev"], ["bev"])
                R(lambda e: e.tensor_scalar(out=bev[:, 2, :], in0=bev[:, 1, :], scalar1=-1e6, scalar2=1e6, op0=ALU.mult, op1=ALU.add), ["bev"], ["bev"])
                R(lambda e: e.scalar_tensor_tensor(out=bev[:, 2, :], in0=bev[:, 0, :], scalar=128.0, in1=bev[:, 2, :], op0=ALU.mult, op1=ALU.add), ["bev"], ["bev"])
                R(lambda e: e.tensor_scalar(out=bev[:, 2, :], in0=bev[:, 2, :], scalar1=cst[:, CC["pidx"]:CC["pidx"] + 1], scalar2=None, op0=ALU.add), ["bev", "cst"], ["bev"])
                R(lambda e: e.tensor_copy(out=IDXW[:, 0:1, :], in_=bev[:, 2:3, :]), ["bev"], ["IDXW"])
            P.barrier()

        if stage >= 3:
            with ExitStack() as cb:
                IOA = bass.IndirectOffsetOnAxis
                NROW = NE * 128
                _bc = {}

                def bc_reg(e):
                    if "r" not in _bc:
                        _bc["r"] = e.to_reg(NROW - 1)
                    return _bc["r"]
                W32 = SB(cb, "W32", [128, 12288])
                idb2 = SB(cb, "idb2", [128, 128], BF16)
                xload = [SB(cb, f"xl{i}", [128, D], BF16) for i in range(2)]
                xsb = [SB(cb, f"xsb{i}", [128, D], BF16) for i in range(2)]
                XgT_l = [SB(cb, f"XgT{i}", [128, 8, 128], BF16) for i in range(2)]
                Wgb = SB(cb, "Wgb", [128, 8, EH], BF16)
                Wub = SB(cb, "Wub", [128, 8, EH], BF16)
                Wdb = SB(cb, "Wdb", [128, 4, D], BF16)
                sg_l = [SB(cb, f"sg{i}", [128, 512]) for i in range(2)]
                actb_l = [SB(cb, f"actb{i}", [128, 512], BF16) for i in range(2)]
                actT_l = [SB(cb, f"actT{i}", [128, 4, 128], BF16) for i in range(2)]
                yblk = [SB(cb, f"yblk{i}", [128, D]) for i in range(2)]
                fgt = SB(cb, "fgt", [128, D])
                fgs = SB(cb, "fgs", [128, D])
                g2bc = SB(cb, "g2bc", [128, D])
                yA = SB(cb, "yA", [128, D])
                yB = SB(cb, "yB", [128, D])
                fst = SB(cb, "fst", [128, 2, 6])
                fmv = SB(cb, "fmv", [128, 4])
                P.add("sp", lambda e: e.dma_start(out=fgs[:], in_=fg_d[:, :]), w=["fgs"], dma=True)
                P.add("dve", lambda e: e.tensor_copy(out=idb2[:], in_=ident), r=["cst"], w=["idb2"])
                make_gate(g2bc, "g2bc", 40)
                zt = SB(cb, "zt", [128, D], BF16)
                P.add("pool", lambda e: e.memset(zt[:], 0.0), w=["zt"])
                xz_keys = []
                for b in range(NBLK):
                    P.add("sp", (lambda b: lambda e: e.dma_start(out=xs_d[b * 128:(b + 1) * 128, :], in_=zt[:]))(b), r=["zt"], w=[f"xz{b}"], dma=True)
                    xz_keys.append(f"xz{b}")
                sc_keys = []
                for i in range(32):
                    xl = xload[i % 2]
                    xlk = f"xl{i % 2}"
                    P.add("sp", (lambda xl, i: lambda e: e.dma_start(out=xl[:], in_=xn2_d[i * 128:(i + 1) * 128, :]))(xl, i), r=["xn2_d"], w=[xlk], dma=True)
                    for k in range(2):
                        key = f"xsc{i}_{k}"
                        P.add("pool", (lambda xl, i, k: lambda e: e.indirect_dma_start(out=xs_d[:, :], out_offset=IOA(ap=PABi[:, k, i:i + 1], axis=0),
                                                                                       in_=xl[:], in_offset=None))(xl, i, k),
                              r=[xlk, "PABi"] + xz_keys, w=[key], dma=True)
                        sc_keys.append(key)
                ys_keys = []
                def do_block(b):
                    P.add("pool", (lambda b: lambda e: e.indirect_dma_start(out=W32[:], out_offset=None, in_=wall_d[:, :],
                                                                           in_offset=IOA(ap=IDXW[:, 0, b:b + 1], axis=0), bounds_check=bc_reg(e), oob_is_err=False))(b),
                          r=["IDXW", "W32"], w=["W32"], dma=True)
                    P.add("act", lambda e: e.activation(out=Wgb[:].rearrange("p a b -> p (a b)"), in_=W32[:, 0:4096], func=AF.Identity), r=["W32"], w=["Wgb"])
                    P.add("dve", lambda e: e.tensor_copy(out=Wub[:].rearrange("p a b -> p (a b)"), in_=W32[:, 4096:8192]), r=["W32"], w=["Wub"])
                    P.add("pool", lambda e: e.tensor_copy(out=Wdb[:, 0:2, :].rearrange("p a b -> p (a b)"), in_=W32[:, 8192:10240]), r=["W32"], w=["Wdb"])
                    P.add("dve", lambda e: e.tensor_copy(out=Wdb[:, 2:4, :].rearrange("p a b -> p (a b)"), in_=W32[:, 10240:12288]), r=["W32"], w=["Wdb"])
                    XgT = XgT_l[b % 2]
                    xgk = f"XgT{b % 2}"
                    sg = sg_l[b % 2]
                    sgk = f"sg{b % 2}"
                    actb = actb_l[b % 2]
                    abk = f"actb{b % 2}"
                    actT = actT_l[b % 2]
                    atk = f"actT{b % 2}"
                    xb_ = xsb[b % 2]
                    xbk = f"xsb{b % 2}"
                    P.add("sp", (lambda xb_, b: lambda e: e.dma_start(out=xb_[:], in_=xs_d[b * 128:(b + 1) * 128, :]))(xb_, b), r=sc_keys, w=[xbk], dma=True)
                    for kc in range(8):
                        P.add("pe", (lambda xb_, kc: lambda e: e.matmul(psum[kc // 4][:, (kc % 4) * 128:(kc % 4 + 1) * 128], lhsT=xb_[:, kc * 128:(kc + 1) * 128],
                                                                       rhs=idb2[:], start=True, stop=True))(xb_, kc), r=[xbk, "idb2"], w=[pk(kc // 4)])
                    for kc in range(8):
                        P.add("act", (lambda kc: lambda e: e.activation(out=XgT[:, kc, :], in_=psum[kc // 4][:, (kc % 4) * 128:(kc % 4 + 1) * 128], func=AF.Identity,
                                                                        scale=sc[:, 8 + kc:9 + kc], bias=mod[:, 24 + kc:25 + kc]))(kc), r=[pk(kc // 4), "sc", "mod"], w=[xgk])
                    for kc in range(8):
                        P.add("pe", (lambda kc: lambda e: e.matmul(psum[2][:, 0:512], lhsT=XgT[:, kc, :], rhs=Wgb[:, kc, :], start=(kc == 0), stop=(kc == 7)))(kc),
                              r=[xgk, "Wgb"], w=[pk(2)])
                    for kc in range(8):
                        P.add("pe", (lambda kc: lambda e: e.matmul(psum[3][:, 0:512], lhsT=XgT[:, kc, :], rhs=Wub[:, kc, :], start=(kc == 0), stop=(kc == 7)))(kc),
                              r=[xgk, "Wub"], w=[pk(3)])
                    P.add("act", lambda e: e.activation(out=sg[:], in_=psum[2][:, 0:512], func=AF.Silu), r=[pk(2)], w=[sgk])
                    P.add("dve", lambda e: e.tensor_tensor(out=actb[:], in0=psum[3][:, 0:512], in1=sg[:], op=ALU.mult), r=[pk(3), sgk], w=[abk])
                    for hc in range(4):
                        P.add("pe", (lambda hc: lambda e: e.matmul(psum[4][:, hc * 128:(hc + 1) * 128], lhsT=actb[:, hc * 128:(hc + 1) * 128], rhs=idb2[:],
                                                                   start=True, stop=True))(hc), r=[abk, "idb2"], w=[pk(4)])
                    P.add("act", lambda e: e.activation(out=actT[:].rearrange("p a b -> p (a b)"), in_=psum[4][:, 0:512], func=AF.Identity), r=[pk(4)], w=[atk])
                    yb_ = yblk[b % 2]
                    ybk = f"yblk{b % 2}"
                    for hf in range(2):
                        for hc in range(4):
                            P.add("pe", (lambda hc, hf: lambda e: e.matmul(psum[5 + hf][:, 0:512], lhsT=actT[:, hc, :], rhs=Wdb[:, hc, hf * 512:(hf + 1) * 512],
                                                                           start=(hc == 0), stop=(hc == 3)))(hc, hf), r=[atk, "Wdb"], w=[pk(5 + hf)])
                        if hf == 0:
                            P.add("act", (lambda yb_: lambda e: e.activation(out=yb_[:, 0:512], in_=psum[5][:, 0:512], func=AF.Identity))(yb_), r=[pk(5)], w=[ybk])
                        else:
                            P.add("dve", (lambda yb_: lambda e: e.tensor_copy(out=yb_[:, 512:1024], in_=psum[6][:, 0:512]))(yb_), r=[pk(6)], w=[ybk])
                    key = f"ys{b}"
                    P.add("sp", (lambda yb_, b: lambda e: e.dma_start(out=ys_d[b * 128:(b + 1) * 128, :], in_=yb_[:]))(yb_, b), r=[ybk], w=[key], dma=True)
                    ys_keys.append(key)

                for b in range(min(NBLK, nblk)):
                    do_block(b)
                for i in range(32):
                    tok0 = i * 128
                    P.add("pool", (lambda i: lambda e: e.indirect_dma_start(out=yA[:], out_offset=None, in_=ys_d[:, :], in_offset=IOA(ap=PABi[:, 0, i:i + 1], axis=0)))(i),
                          r=ys_keys + ["PABi", "yA"], w=["yA"], dma=True)
                    P.add("pool", (lambda i: lambda e: e.indirect_dma_start(out=yB[:], out_offset=None, in_=ys_d[:, :], in_offset=IOA(ap=PABi[:, 1, i:i + 1], axis=0)))(i),
                          r=ys_keys + ["PABi", "yB"], w=["yB"], dma=True)
                    P.add("sp", (lambda tok0: lambda e: e.dma_start(out=fgt[:], in_=x1_d[tok0:tok0 + 128, :]))(tok0), r=["x1_d"], w=["fgt"], dma=True)
                    P.add("dve", (lambda i: lambda e: e.tensor_scalar(out=yA[:], in0=yA[:], scalar1=WAB[:, 0, i:i + 1], scalar2=None, op0=ALU.mult))(i), r=["yA", "WAB"], w=["yA"])
                    P.add("dve", (lambda i: lambda e: e.scalar_tensor_tensor(out=yA[:], in0=yB[:], scalar=WAB[:, 1, i:i + 1], in1=yA[:], op0=ALU.mult, op1=ALU.add))(i),
                          r=["yA", "yB", "WAB"], w=["yA"])
                    P.add("pool", lambda e: e.tensor_tensor(out=yA[:], in0=yA[:], in1=g2bc[:], op=ALU.mult), r=["yA", "g2bc"], w=["yA"])
                    P.add("dve", lambda e: e.tensor_tensor(out=fgt[:], in0=fgt[:], in1=yA[:], op=ALU.add), r=["yA", "fgt"], w=["fgt"])
                    for hf in range(2):
                        P.add("dve", (lambda hf: lambda e: e.bn_stats(out=fst[:, hf, :], in_=fgt[:, hf * 512:(hf + 1) * 512]))(hf), r=["fgt"], w=["fst"])
                    P.add("dve", lambda e: e.bn_aggr(out=fmv[:, 0:2], in_=fst[:, 0:2, :].rearrange("p a b -> p (a b)")), r=["fst"], w=["fmv"])
                    P.add("dve", lambda e: e.scalar_tensor_tensor(out=fmv[:, 2:3], in0=fmv[:, 0:1], scalar=fmv[:, 0:1], in1=fmv[:, 1:2], op0=ALU.mult, op1=ALU.add),
                          r=["fmv"], w=["fmv"])
                    P.add("act", lambda e: e.activation(out=fmv[:, 3:4], in_=fmv[:, 2:3], func=AF.Sqrt, bias=float(1e-6)), r=["fmv"], w=["fmv"])
                    P.add("dve", lambda e: e.reciprocal(out=fmv[:, 3:4], in_=fmv[:, 3:4]), r=["fmv"], w=["fmv"])
                    P.add("dve", lambda e: e.scalar_tensor_tensor(out=fgt[:], in0=fgt[:], scalar=fmv[:, 3:4], in1=fgs[:], op0=ALU.mult, op1=ALU.mult),
                          r=["fgt", "fmv", "fgs"], w=["fgt"])
                    P.add("sp", (lambda tok0: lambda e: e.dma_start(out=out_d[tok0:tok0 + 128, :], in_=fgt[:]))(tok0), r=["fgt"], w=["out_d"], dma=True)
        else:
            with ExitStack() as cb:
                fgt = SB(cb, "fgt", [128, D])
                for ti in range(32):
                    tok0 = ti * 128
                    P.add("sp", (lambda tok0: lambda e: e.dma_start(out=fgt[:], in_=x1_d[tok0:tok0 + 128, :]))(tok0), r=["x1_d"], w=["fgt"], dma=True)
                    P.add("sp", (lambda tok0: lambda e: e.dma_start(out=out_d[tok0:tok0 + 128, :], in_=fgt[:]))(tok0), r=["fgt"], w=["out_d"], dma=True)
        P.emit()
    return nc


_NC_CACHE = {}


def _layouts(inp):
    f = np.float32
    g = lambda k: np.asarray(inp[k], dtype=f)
    col = lambda v, n: np.ascontiguousarray(v.reshape(n, 128).T)
    mu = g("rwkv_mu")[0]
    def pad128(v):
        o = np.zeros((128, 1), f)
        o[:v.shape[0], 0] = v
        return o
    shared = [None, col(g("ada_b")[0], 48), col(g("norm1_g")[0], 8), col(g("norm2_g")[0], 8), col(mu[0:1536], 12),
              pad128(mu[1536:1568]), pad128(mu[1568:1600]), pad128(mu[1600:1696]), col(g("rwkv_w0")[0], 4), col(g("rwkv_a0")[0], 4),
              col(g("rwkv_k_k")[0], 4), col(g("rwkv_k_a")[0], 4), col(g("rwkv_r_k")[0].reshape(512), 4), col(g("rwkv_gn_w")[0], 4),
              col(g("rwkv_gn_b")[0], 4)]
    tt = np.arange(64)
    su = (tt[:, None] < tt[None, :]).astype(f)
    iu = (tt[:, None] <= tt[None, :]).astype(f)
    sl = (tt[None, :] < tt[:, None]).astype(f)
    m5 = np.tile(np.concatenate([su, iu, su, iu, sl], axis=1), (2, 1))
    ident = np.eye(128, dtype=f)
    istack = np.tile(np.eye(64, dtype=f), (2, 1))
    ss = np.arange(128)
    tril = (ss[:, None] <= ss[None, :]).astype(f)
    bones = (ss[:, None] // 64 == ss[None, :] // 64).astype(f)
    ones = np.ones((128, 128), f)
    thr = np.broadcast_to((np.arange(32) * 128).astype(f)[None, :], (128, 32))
    iotab = np.broadcast_to(np.arange(96).astype(f)[None, :], (128, 96))
    pidx = np.arange(128).astype(f)[:, None]
    cst = np.ascontiguousarray(np.concatenate([m5, ident, istack, tril, bones, ones, thr, iotab, pidx], axis=1))
    assert cst.shape[1] == NCST
    rep = lambda v: np.broadcast_to(v[None, :], (128, v.shape[0]))
    bs = g("gmlp_bs")[0]
    bsT = np.zeros((128, 4, 128), f)
    for j in range(4):
        for hh in range(2):
            bsT[hh * 64:(hh + 1) * 64, j, :] = bs[2 * j + hh][None, :]
    bc = np.ascontiguousarray(np.concatenate([rep(g("gmlp_ln_w")[0]), rep(g("gmlp_ln_b")[0]),
                                              rep(np.concatenate([g("router_group_b")[0], g("router_expert_b")[0]])),
                                              bsT.reshape(128, 512)], axis=1))
    assert bc.shape[1] == NBC
    common = dict(cst=cst, bc=bc, fg=np.ascontiguousarray(rep(g("final_norm_g"))), ada_w=np.ascontiguousarray(g("ada_w")[0]), w_in=np.ascontiguousarray(g("w_in")[0]),
                  w_out=np.ascontiguousarray(g("w_out")[0]), w2=np.ascontiguousarray(g("rwkv_w2")[0]),
                  a2=np.ascontiguousarray(g("rwkv_a2")[0]), g2=np.ascontiguousarray(g("rwkv_g2")[0]),
                  wsT=np.ascontiguousarray(g("gmlp_ws")[0].transpose(2, 0, 1)),
                  wr=np.ascontiguousarray(np.concatenate([g("router_group_w")[0], g("router_expert_w")[0]], axis=1)),
                  wall=np.ascontiguousarray(np.concatenate([
                      g("moe_w_gate")[0].reshape(NE, 8, 128, EH).transpose(0, 2, 1, 3).reshape(NE * 128, 4096),
                      g("moe_w_up")[0].reshape(NE, 8, 128, EH).transpose(0, 2, 1, 3).reshape(NE * 128, 4096),
                      g("moe_w_down")[0].reshape(NE, 4, 128, D).transpose(0, 2, 1, 3).reshape(NE * 128, 4096)], axis=1)))
    x = g("x")
    c = g("c")
    maps = []
    for b in range(x.shape[0]):
        cols = [col(c[b], 8)] + shared[1:]
        prm = np.ascontiguousarray(np.concatenate(cols, axis=1))
        assert prm.shape[1] == NPRM
        m = dict(common)
        m["x"] = np.ascontiguousarray(x[b])
        m["prm"] = prm
        maps.append(m)
    return maps


def kernel(**inputs):
    maps = _layouts(inputs)
    if "nc" not in _NC_CACHE:
        _NC_CACHE["nc"] = build_nc()
    nc = _NC_CACHE["nc"]
    res = run_bass_kernel_spmd(nc, maps, core_ids=list(range(len(maps))))
    return np.stack([r["out"] for r in res.results], axis=0).astype(np.float32)
```

```python
import numpy as np
import os
from contextlib import ExitStack
import concourse.bass as bass
import concourse.mybir as mybir
from concourse.bass_utils import run_bass_kernel_spmd

F32 = mybir.dt.float32
BF16 = mybir.dt.bfloat16
I32 = mybir.dt.int32
AF = mybir.ActivationFunctionType
ALU = mybir.AluOpType
AX = mybir.AxisListType

D = 1024
S = 4096
TT = 256
NT = S // TT
NSUB = TT // 128
CH = 64
NCH = TT // CH
INW = 2720
NE = 32
EH = 512
NEG_HALF_E = -0.6065306597126334

PC = {}
_o = 0
for _n, _w in [("cT", 8), ("ada_b", 48), ("n1g", 8), ("n2g", 8), ("mu_rkv", 12), ("mu_xw", 1),
               ("mu_xa", 1), ("mu_xg", 1), ("w0", 4), ("a0", 4), ("k_k", 4), ("k_a", 4), ("r_k", 4),
               ("gn_w", 4), ("gn_b", 4)]:
    PC[_n] = _o
    _o += _w
NPRM = _o
CC = {}
_o = 0
for _n, _w in [("m5", 320), ("ident", 128), ("istack", 64), ("tril", 128), ("bones", 128), ("ones", 128), ("thr", 32), ("iotab", 96), ("pidx", 1)]:
    CC[_n] = _o
    _o += _w
NCST = _o
BC = {}
_o = 0
for _n, _w in [("lnw", 512), ("lnb", 512), ("rb", 36), ("bsT", 512)]:
    BC[_n] = _o
    _o += _w
NBC = _o


ATTACH_WAIT = True
NO_SELF_SYNC = ("pe", "act")


class Prog:
    def __init__(self, nc, ctx):
        self.nc = nc
        self.ops = []
        self.last_w = {}
        self.readers = {}
        self.engs = ["pe", "act", "dve", "pool", "sp"]
        self.count = {e: 0 for e in self.engs}
        self.sem = {e: ctx.enter_context(nc.semaphore("s_" + e)) for e in self.engs}
        self.NDS = 8
        self.dsem = {q: [ctx.enter_context(nc.semaphore(f"d_{q}{i}")) for i in range(self.NDS)]
                     for q in ("sp", "pool")}
        self.dcount = {"sp": 0, "pool": 0}
        self.last_op = {e: None for e in self.engs}
        self.pending = {e: set() for e in self.engs}
        self.recent_dma = {"sp": [], "pool": []}

    def section(self, k):
        self.muted = k > self.cut

    def add(self, eng, fn, r=(), w=(), dma=False):
        if getattr(self, 'muted', False):
            return None
        idx = len(self.ops)
        deps = set(self.pending[eng])
        self.pending[eng] = set()
        for k in r:
            if k in self.last_w:
                deps.add(self.last_w[k])
        for k in w:
            if k in self.last_w:
                deps.add(self.last_w[k])
            deps.update(self.readers.get(k, ()))
        for k in r:
            self.readers.setdefault(k, []).append(idx)
        for k in w:
            self.last_w[k] = idx
            self.readers[k] = []
        if dma:
            q = eng
            kq = self.dcount[q]
            self.dcount[q] += 1
            sem = self.dsem[q][kq % self.NDS]
            val = 16 * (kq // self.NDS + 1)
            prev = (sem, val - 16) if val > 16 else None
            self.recent_dma[q].append(idx)
            self.recent_dma[q] = self.recent_dma[q][-self.NDS:]
        else:
            self.count[eng] += 1
            sem = self.sem[eng]
            val = self.count[eng]
            prev = None
        self.ops.append(dict(eng=eng, fn=fn, deps=deps, dma=dma, sem=sem, val=val, prev=prev))
        self.last_op[eng] = idx
        return idx

    def barrier(self):
        allops = set()
        for e in self.engs:
            if self.last_op[e] is not None:
                allops.add(self.last_op[e])
        for q in ("sp", "pool"):
            allops.update(self.recent_dma[q])
        dmaops = set()
        for q in ("sp", "pool"):
            dmaops.update(self.recent_dma[q])
        for e in self.engs:
            self.pending[e] |= (allops - dmaops) if e == "pe" else allops

    def emit(self):
        nc = self.nc
        per = {e: [] for e in self.engs}
        for i, op in enumerate(self.ops):
            per[op["eng"]].append(i)
        ops = self.ops

        def run(name, e):
            waited = {}
            for i in per[name]:
                op = ops[i]
                need = []
                for j in op["deps"]:
                    d = ops[j]
                    if d["eng"] == name and not d["dma"] and name in NO_SELF_SYNC:
                        continue
                    need.append((d["sem"], d["val"]))
                if op["prev"] is not None:
                    need.append(op["prev"])
                need.sort(key=lambda t: -t[1])
                todo = []
                for sem, val in need:
                    key = id(sem)
                    if waited.get(key, 0) >= val:
                        continue
                    waited[key] = val
                    todo.append((sem, val))
                attach = todo.pop() if (todo and ATTACH_WAIT) else None
                for sem, val in todo:
                    e.wait_ge(sem, val)
                inst = op["fn"](e)
                if attach is not None:
                    inst._wait_ge(attach[0], attach[1])
                inst.then_inc(op["sem"], 16 if op["dma"] else 1)
            if name in ("sp", "pool"):
                kq = self.dcount[name]
                for s_i in range(min(kq, self.NDS)):
                    n_on = (kq - s_i + self.NDS - 1) // self.NDS
                    e.wait_ge(self.dsem[name][s_i], 16 * n_on)

        with nc.Block() as block:
            @block.sync
            def _(e):
                run("sp", e)

            @block.scalar
            def _(e):
                run("act", e)

            @block.vector
            def _(e):
                run("dve", e)

            @block.tensor
            def _(e):
                run("pe", e)

            @block.gpsimd
            def _(e):
                run("pool", e)


def build_nc(stage=99, ntiles=NT, cut=99, nblk=96):
    nc = bass.Bass("TRN2", target_bir_lowering=False)
    dt = lambda name, shape, dty, kind: nc.dram_tensor(name, shape, dty, kind=kind).ap()
    x_d = dt("x", [S, D], F32, "ExternalInput")
    prm_d = dt("prm", [128, NPRM], F32, "ExternalInput")
    cst_d = dt("cst", [128, NCST], F32, "ExternalInput")
    bc_d = dt("bc", [128, NBC], F32, "ExternalInput")
    adaw_d = dt("ada_w", [D, 6 * D], F32, "ExternalInput")
    win_d = dt("w_in", [D, INW], F32, "ExternalInput")
    wout_d = dt("w_out", [D, D], F32, "ExternalInput")
    w2_d = dt("w2", [32, 512], F32, "ExternalInput")
    a2_d = dt("a2", [32, 512], F32, "ExternalInput")
    g2_d = dt("g2", [96, 512], F32, "ExternalInput")
    wsT_d = dt("wsT", [128, 8, 128], F32, "ExternalInput")
    wr_d = dt("wr", [D, 36], F32, "ExternalInput")
    fg_d = dt("fg", [128, D], F32, "ExternalInput")
    wall_d = dt("wall", [NE * 128, 12288], F32, "ExternalInput")
    NBLK = 96
    xn2_d = dt("xn2_d", [S, D], BF16, "Internal")
    xs_d = dt("xs_d", [NBLK * 128, D], BF16, "Internal")
    ys_d = dt("ys_d", [NBLK * 128, D], F32, "Internal")
    out_d = dt("out", [S, D], F32, "ExternalOutput")
    x1_d = dt("x1_d", [S, D], F32, "Internal")

    with ExitStack() as top:
        P = Prog(nc, top)
        P.cut = cut
        psum = [top.enter_context(nc.psum_tensor(f"ps{i}", [128, 512], F32)) for i in range(7)]
        psTt = top.enter_context(nc.psum_tensor("psT", [128, 512], F32))
        psT = [psTt[:, 0:256], psum[6][:, 0:256]]
        psTk = ["ps7", "ps6"]
        fst = None
        fmv = None
        pk = lambda i: f"ps{i}"

        def SB(ctx, name, shape, dty=F32):
            return ctx.enter_context(nc.sbuf_tensor(name, shape, dty))

        prm = SB(top, "prm_s", [128, NPRM])
        cst = SB(top, "cst_s", [128, NCST])
        bcs = SB(top, "bc_s", [128, NBC])
        mod = SB(top, "mod", [128, 48])
        wt = SB(top, "wt", [128, 32, 32])
        PABi = SB(top, "PABi", [128, 2, 32], I32)
        WAB = SB(top, "WAB", [128, 2, 32])
        IDXW = SB(top, "IDXW", [128, 4, 96], I32)
        P.add("sp", lambda e: e.dma_start(out=prm[:], in_=prm_d[:, :]), w=["prm"], dma=True)
        P.add("sp", lambda e: e.dma_start(out=cst[:], in_=cst_d[:, :]), w=["cst"], dma=True)
        P.add("sp", lambda e: e.dma_start(out=bcs[:], in_=bc_d[:, :]), w=["bc"], dma=True)

        def pcol(name, j=0, n=1):
            return prm[:, PC[name] + j:PC[name] + j + n]

        ident = cst[:, CC["ident"]:CC["ident"] + 128]
        istack = cst[:, CC["istack"]:CC["istack"] + 64]
        tril = cst[:, CC["tril"]:CC["tril"] + 128]
        bones = cst[:, CC["bones"]:CC["bones"] + 128]
        ones = cst[:, CC["ones"]:CC["ones"] + 128]
        m5 = cst[:, CC["m5"]:CC["m5"] + 320]

        with ExitStack() as c0:
            stg = [SB(c0, f"ada_stg{i}", [128, 6 * D]) for i in range(2)]
            for kc in range(8):
                sb = stg[kc % 2]
                P.add("sp", (lambda sb, kc: lambda e: e.dma_start(out=sb[:], in_=adaw_d[kc * 128:(kc + 1) * 128, :]))(sb, kc),
                      w=[f"ada_stg{kc % 2}"], dma=True)
                for oc in range(48):
                    P.add("pe", (lambda sb, kc, oc: lambda e: e.matmul(psum[0][:, oc:oc + 1], lhsT=sb[:, oc * 128:(oc + 1) * 128],
                                                                   rhs=prm[:, PC["cT"] + kc:PC["cT"] + kc + 1], start=True, stop=True))(sb, kc, oc),
                          r=[f"ada_stg{kc % 2}", "prm"], w=[pk(0)])
                if kc == 0:
                    P.add("dve", lambda e: e.tensor_tensor(out=mod[:], in0=psum[0][:, 0:48], in1=prm[:, PC["ada_b"]:PC["ada_b"] + 48], op=ALU.add),
                          r=[pk(0), "prm"], w=["mod"])
                else:
                    P.add("dve", lambda e: e.tensor_tensor(out=mod[:], in0=psum[0][:, 0:48], in1=mod[:], op=ALU.add),
                          r=[pk(0), "mod"], w=["mod"])
            P.barrier()
        sc = SB(top, "sc", [128, 32])
        P.add("dve", lambda e: e.scalar_tensor_tensor(out=sc[:, 0:8], in0=mod[:, 8:16], scalar=1.0, in1=prm[:, PC["n1g"]:PC["n1g"] + 8],
                                                      op0=ALU.add, op1=ALU.mult), r=["mod", "prm"], w=["sc"])
        P.add("dve", lambda e: e.scalar_tensor_tensor(out=sc[:, 8:16], in0=mod[:, 32:40], scalar=1.0, in1=prm[:, PC["n2g"]:PC["n2g"] + 8],
                                                      op0=ALU.add, op1=ALU.mult), r=["mod", "prm"], w=["sc"])
        gate_bc = SB(top, "gate_bc", [128, D])
        gl = SB(top, "gl", [128, 128])

        def make_gate(dst, dkey, gcol):
            for c in range(8):
                P.add("dve", (lambda c: lambda e: e.tensor_scalar(out=gl[:], in0=ones, scalar1=mod[:, gcol + c:gcol + c + 1], scalar2=None,
                                                                  op0=ALU.mult))(c), r=["mod", "cst"], w=["gl"])
                P.add("pe", (lambda c: lambda e: e.matmul(psum[1][:, (c % 4) * 128:(c % 4 + 1) * 128], lhsT=gl[:], rhs=ident, start=True, stop=True))(c),
                      r=["gl", "cst"], w=[pk(1)])
                P.add("act", (lambda c: lambda e: e.activation(out=dst[:, c * 128:(c + 1) * 128], in_=psum[1][:, (c % 4) * 128:(c % 4 + 1) * 128],
                                                               func=AF.Identity))(c), r=[pk(1)], w=[dkey])
        make_gate(gate_bc, "gate_bc", 16)

        with ExitStack() as ca:
            win = SB(ca, "win", [128, 8, INW], BF16)
            wout = SB(ca, "wout", [128, 8, D], BF16)
            identb = SB(ca, "identb", [128, 128], BF16)
            w2s = SB(ca, "w2s", [32, 512])
            a2s = SB(ca, "a2s", [32, 512])
            g2s = SB(ca, "g2s", [96, 512])
            wrs = SB(ca, "wrs", [128, 8, 36])
            wc = SB(ca, "wc", [128, 8, 128])
            logits = SB(ca, "logits", [128, 32, 36])
            Hst = [SB(ca, f"H{j}", [128, 64]) for j in range(4)]
            Hb_l = [SB(ca, f"Hb{j}", [128, 64], BF16) for j in range(4)]
            istb = SB(ca, "istb", [128, 64], BF16)
            carry = SB(ca, "carry", [128, 16])
            P.add("pool", lambda e: e.tensor_copy(out=identb[:], in_=ident), r=["cst"], w=["identb"])
            P.add("sp", lambda e: e.dma_start(out=w2s[:], in_=w2_d[:, :]), w=["w2s"], dma=True)
            P.add("sp", lambda e: e.dma_start(out=a2s[:], in_=a2_d[:, :]), w=["a2s"], dma=True)
            P.add("sp", lambda e: e.dma_start(out=g2s[:], in_=g2_d[:, :]), w=["g2s"], dma=True)
            P.add("sp", lambda e: e.dma_start(out=wrs[:], in_=wr_d.rearrange("(c p) n -> p c n", p=128)), w=["wrs"], dma=True)
            P.add("sp", lambda e: e.dma_start(out=wc[:], in_=wsT_d[:, :, :]), w=["wc"], dma=True)
            for h in range(8):
                P.add("pool", (lambda h: lambda e: e.tensor_tensor(out=wc[:, h, :], in0=wc[:, h, :], in1=tril, op=ALU.mult))(h),
                      r=["wc", "cst"], w=["wc"])
            P.add("pool", lambda e: e.memset(carry[:], 0.0), w=["carry"])
            zt = SB(ca, "zt", [128, D], BF16)
            P.add("pool", lambda e: e.memset(zt[:], 0.0), w=["zt"])
            xz_keys = [f"xz{b}" for b in range(NBLK)]
            for _i in range(int(os.environ.get("KPAD", "0"))):
                P.add("pool", lambda e: e.memset(gl[:, 0:1], 0.0), w=["gl_dummy"])
            for j in range(4):
                P.add("pool", (lambda j: lambda e: e.memset(Hst[j][:], 0.0))(j), w=[f"H{j}"])
                P.add("pool", (lambda j: lambda e: e.memset(Hb_l[j][:], 0.0))(j), w=[f"Hb{j}"])
            P.add("pool", lambda e: e.tensor_copy(out=istb[:], in_=istack), r=["cst"], w=["istb"])
            with ExitStack() as cw:
                wstg = [SB(cw, f"wstg{i}", [128, INW]) for i in range(2)]
                for kc in range(8):
                    sb = wstg[kc % 2]
                    P.add("sp", (lambda sb, kc: lambda e: e.dma_start(out=sb[:], in_=win_d[kc * 128:(kc + 1) * 128, :]))(sb, kc),
                          w=[f"wstg{kc % 2}"], dma=True)
                    P.add("pool", (lambda sb, kc: lambda e: e.tensor_copy(out=win[:, kc, :], in_=sb[:]))(sb, kc),
                          r=[f"wstg{kc % 2}"], w=["win"])
                for kc in range(8):
                    sb = wstg[kc % 2]
                    P.add("sp", (lambda sb, kc: lambda e: e.dma_start(out=sb[:, 0:D], in_=wout_d[kc * 128:(kc + 1) * 128, :]))(sb, kc),
                          w=[f"wstg{kc % 2}"], dma=True)
                    P.add("pool", (lambda sb, kc: lambda e: e.tensor_copy(out=wout[:, kc, :], in_=sb[:, 0:D]))(sb, kc),
                          r=[f"wstg{kc % 2}"], w=["wout"])
                P.barrier()

            ct = ExitStack()
            xt = [SB(ct, "xt0", [128, NSUB, D])] * 2
            xn = SB(ct, "xn", [128, NSUB, D], BF16)
            hT = SB(ct, "hT", [128, 8, TT], BF16)
            st6 = SB(ct, "st6", [128, 4, 6])
            mv = SB(ct, "mv", [128, 8])
            u_s = SB(ct, "u_s", [128, 4, TT], BF16)
            vg = SB(ct, "vg", [128, 512])
            vn = vg
            zraw = [SB(ct, f"zraw{i}", [128, TT + 1]) for i in range(2)]
            rkv = SB(ct, "rkv", [128, 12, TT])
            lor = SB(ct, "lor", [128, 3, TT])
            LW, ASN, BSN, KM, BON, GG = range(6)
            pers = [SB(ct, f"pers{j}", [128, 6, TT]) for j in range(4)]
            AA, KX, RN, PRD = range(4)
            ptmp_l = [SB(ct, f"ptmp{i}", [128, 4, TT]) for i in range(2)]
            ybT = SB(ct, "ybT", [128, 4, TT])
            ycat = SB(ct, "ycat", [128, 8, TT], BF16)
            x1t = SB(ct, "x1t", [128, D])
            xn2 = x1t
            h2f = SB(ct, "h2f", [128, 8, 128])
            gtmp = h2f[:, 0:4, :]
            xnb = SB(ct, "xnb", [128, D], BF16)
            CI, CE, EI, EE, EN = range(5)
            sct = [SB(ct, f"sct{j}", [128, 5, 64]) for j in range(4)]
            sb4 = [SB(ct, f"sb4{j}", [128, 4, 64], BF16) for j in range(4)]
            mats = [SB(ct, f"mats{j}", [128, 320], BF16) for j in range(4)]
            trs = [SB(ct, f"trs{j}", [128, 192], BF16) for j in range(4)]
            wzb_l = [SB(ct, f"wzb{j}", [128, 256], BF16) for j in range(4)]
            qq = [[SB(ct, f"qq{j}_{i}", [128, 64], BF16) for i in range(2)] for j in range(4)]
            chn = [SB(ct, f"chn{j}", [128, 3, 64], BF16) for j in range(4)]
            hpc_l = [SB(ct, f"hpc{j}", [128, 64]) for j in range(4)]
            gst = [SB(ct, f"gst{j}", [128, 12]) for j in range(4)]

            def A(eng, fn, r=(), w=()):
                P.add(eng, fn, r=r, w=w)

            def norm_stats(src, srck, eps):
                for hf in range(2):
                    A("dve", (lambda hf: lambda e: e.bn_stats(out=st6[:, hf, :], in_=src(hf)))(hf), r=[srck], w=["st6"])
                A("dve", lambda e: e.bn_aggr(out=mv[:, 0:2], in_=st6[:, 0:2, :].rearrange("p a b -> p (a b)")), r=["st6"], w=["mv"])
                A("dve", lambda e: e.scalar_tensor_tensor(out=mv[:, 2:3], in0=mv[:, 0:1], scalar=mv[:, 0:1], in1=mv[:, 1:2], op0=ALU.mult, op1=ALU.add),
                  r=["mv"], w=["mv"])
                A("act", lambda e: e.activation(out=mv[:, 3:4], in_=mv[:, 2:3], func=AF.Sqrt, bias=float(eps)), r=["mv"], w=["mv"])
                A("dve", lambda e: e.reciprocal(out=mv[:, 3:4], in_=mv[:, 3:4]), r=["mv"], w=["mv"])

            for TI in range(min(NT, ntiles) if stage >= 1 else 0):
                t0 = TI * TT
                xb = xt[TI % 2]
                for b in range(TI * 6, TI * 6 + 6):
                    P.add("pool", (lambda b: lambda e: e.dma_start(out=xs_d[b * 128:(b + 1) * 128, :], in_=zt[:]))(b), r=["zt"], w=[f"xz{b}"], dma=True)
                xk = "xt0"
                P.add("sp", (lambda xb, t0: lambda e: e.dma_start(out=xb[:], in_=x_d[t0:t0 + TT, :].rearrange("(s p) d -> p s d", p=128)))(xb, t0),
                      w=[xk], dma=True)
                P.section(1)
                for sub in range(NSUB):
                    norm_stats((lambda xb, sub: lambda hf: xb[:, sub, hf * 512:(hf + 1) * 512])(xb, sub), xk, 1e-6)
                    A("act", (lambda xb, sub: lambda e: e.activation(out=xn[:, sub, :], in_=xb[:, sub, :], func=AF.Identity, scale=mv[:, 3:4]))(xb, sub),
                      r=[xk, "mv"], w=["xn"])
                P.section(1.5)
                for kc in range(8):
                    pb = kc % 2
                    for sub in range(NSUB):
                        A("pe", (lambda kc, sub, pb: lambda e: e.matmul(psT[pb][:, sub * 128:(sub + 1) * 128], lhsT=xn[:, sub, kc * 128:(kc + 1) * 128],
                                                                       rhs=identb[:], start=True, stop=True))(kc, sub, pb), r=["xn", "identb"], w=[psTk[pb]])
                    if os.environ.get("DVE_EVAC"):
                      A("dve", (lambda kc, pb: lambda e: e.tensor_scalar(out=hT[:, kc, :], in0=psT[pb][:, 0:TT], scalar1=sc[:, kc:kc + 1], scalar2=mod[:, kc:kc + 1],
                                                                         op0=ALU.mult, op1=ALU.add))(kc, pb), r=[psTk[pb], "sc", "mod"], w=["hT"])
                    elif not os.environ.get("SKIP_EVAC"):
                      A("act", (lambda kc, pb: lambda e: e.activation(out=hT[:, kc, :], in_=psT[pb][:, 0:TT], func=AF.Identity,
                                                                    **({} if os.environ.get("NO_SB") else dict(scale=sc[:, kc:kc + 1], bias=mod[:, kc:kc + 1]))))(kc, pb),
                      r=[psTk[pb], "sc", "mod"], w=["hT"])

                P.section(2)
                pcnt = [0]

                def proj(c0, M):
                    pb = pcnt[0] % 2
                    pcnt[0] += 1
                    for kc in range(8):
                        A("pe", (lambda kc, pb: lambda e: e.matmul(psum[pb][0:M, 0:TT], lhsT=win[:, kc, c0:c0 + M], rhs=hT[:, kc, :],
                                                                  start=(kc == 0), stop=(kc == 7)))(kc, pb), r=["win", "hT"], w=[pk(pb)])
                    return pb

                for j in range(4):
                    pb = proj(j * 128, 128)
                    A("act", (lambda j, pb: lambda e: e.activation(out=u_s[:, j, :], in_=psum[pb][:, 0:TT], func=AF.Gelu_apprx_tanh))(j, pb),
                      r=[pk(pb)], w=["u_s"])
                specs = [(1024 + q * 128, 128, PC["mu_rkv"] + q) for q in range(12)] + \
                        [(2560, 32, PC["mu_xw"]), (2592, 32, PC["mu_xa"]), (2624, 96, PC["mu_xg"])]
                for q, (c0, M, mucol) in enumerate(specs):
                    pb = proj(c0, M)
                    zb = zraw[q % 2]
                    zk = f"zraw{q % 2}"
                    dst = rkv[0:M, q, :] if q < 12 else lor[0:M, q - 12, :]
                    dk = f"rkv{q}"
                    A("dve", (lambda zb, q, M: lambda e: e.tensor_copy(out=zb[0:M, 0:1], in_=carry[0:M, q:q + 1]))(zb, q, M), r=["carry"], w=[zk])
                    A("act", (lambda zb, pb, M: lambda e: e.activation(out=zb[0:M, 1:TT + 1], in_=psum[pb][0:M, 0:TT], func=AF.Identity))(zb, pb, M),
                      r=[pk(pb)], w=[zk])
                    A("dve", (lambda zb, q, M: lambda e: e.tensor_copy(out=carry[0:M, q:q + 1], in_=zb[0:M, TT:TT + 1]))(zb, q, M), r=[zk], w=["carry"])
                    A("dve", (lambda zb, dst, M: lambda e: e.tensor_tensor(out=dst, in0=zb[0:M, 0:TT], in1=zb[0:M, 1:TT + 1], op=ALU.subtract))(zb, dst, M),
                      r=[zk], w=[dk])
                    A("dve", (lambda zb, dst, M, mucol: lambda e: e.scalar_tensor_tensor(out=dst, in0=dst, scalar=prm[0:M, mucol:mucol + 1], in1=zb[0:M, 1:TT + 1],
                                                                                      op0=ALU.mult, op1=ALU.add))(zb, dst, M, mucol), r=[zk, dk, "prm"], w=[dk])
                A("act", lambda e: e.activation(out=lor[0:32, 0, :], in_=lor[0:32, 0, :], func=AF.Tanh), r=["rkv12"], w=["rkv12"])
                A("act", lambda e: e.activation(out=lor[0:96, 2, :], in_=lor[0:96, 2, :], func=AF.Sigmoid), r=["rkv14"], w=["rkv14"])

                P.section(3)
                def prep_body(j, A):
                    par = j % 2
                    ptmp = ptmp_l[par]
                    pa, pb = (0, 1) if par == 0 else (2, 3)
                    jc = slice(j * 128, (j + 1) * 128)
                    pj = pers[j]
                    pjk = f"pers{j}"
                    rr = rkv[:, j, :]
                    kr = rkv[:, 4 + j, :]
                    vr = rkv[:, 8 + j, :]
                    A("pe", (lambda jc: lambda e: e.matmul(psum[pa][:, 0:TT], lhsT=w2s[0:32, jc], rhs=lor[0:32, 0, :], start=True, stop=True))(jc),
                      r=["w2s", "rkv12"], w=[pk(pa)])
                    A("act", (lambda pj, j: lambda e: e.activation(out=pj[:, LW, :], in_=psum[pa][:, 0:TT], func=AF.Sigmoid, bias=pcol("w0", j)))(pj, j),
                      r=[pk(pa), "prm"], w=[pjk + "LW"])
                    A("pool", (lambda pj: lambda e: e.tensor_scalar(out=pj[:, LW, :], in0=pj[:, LW, :], scalar1=NEG_HALF_E, scalar2=None, op0=ALU.mult))(pj),
                      r=[pjk + "LW"], w=[pjk + "LW"])
                    A("pe", (lambda jc: lambda e: e.matmul(psum[pb][:, 0:TT], lhsT=a2s[0:32, jc], rhs=lor[0:32, 1, :], start=True, stop=True))(jc),
                      r=["a2s", "rkv13"], w=[pk(pb)])
                    A("act", (lambda j: lambda e: e.activation(out=ptmp[:, AA, :], in_=psum[pb][:, 0:TT], func=AF.Sigmoid, bias=pcol("a0", j)))(j),
                      r=[pk(pb), "prm"], w=["pAA" + str(par)])
                    A("pe", (lambda jc: lambda e: e.matmul(psum[pa][:, 0:TT], lhsT=g2s[0:96, jc], rhs=lor[0:96, 2, :], start=True, stop=True))(jc),
                      r=["g2s", "rkv14"], w=[pk(pa)])
                    A("act", (lambda pj: lambda e: e.activation(out=pj[:, GG, :], in_=psum[pa][:, 0:TT], func=AF.Identity))(pj), r=[pk(pa)], w=[pjk + "GG"])
                    A("dve", (lambda kr, j: lambda e: e.tensor_scalar(out=ptmp[:, KX, :], in0=kr, scalar1=pcol("k_k", j), scalar2=None, op0=ALU.mult))(kr, j),
                      r=[f"rkv{4 + j}", "prm"], w=["pKX" + str(par)])
                    A("pool", lambda e: e.tensor_tensor(out=ptmp[:, RN, :], in0=ptmp[:, KX, :], in1=ptmp[:, KX, :], op=ALU.mult), r=["pKX" + str(par)], w=["pRN" + str(par)])
                    A("pe", lambda e: e.matmul(psum[pb][:, 0:TT], lhsT=bones, rhs=ptmp[:, RN, :], start=True, stop=True), r=["cst", "pRN" + str(par)], w=[pk(pb)])
                    A("act", lambda e: e.activation(out=ptmp[:, RN, :], in_=psum[pb][:, 0:TT], func=AF.Sqrt, bias=float(1e-24)), r=[pk(pb)], w=["pRN" + str(par)])
                    A("dve", lambda e: e.reciprocal(out=ptmp[:, RN, :], in_=ptmp[:, RN, :]), r=["pRN" + str(par)], w=["pRN" + str(par)])
                    A("dve", (lambda pj: lambda e: e.scalar_tensor_tensor(out=pj[:, ASN, :], in0=ptmp[:, KX, :], scalar=-1.0, in1=ptmp[:, RN, :],
                                                                          op0=ALU.mult, op1=ALU.mult))(pj), r=["pKX" + str(par), "pRN" + str(par)], w=[pjk + "ASN"])
                    A("dve", (lambda pj: lambda e: e.scalar_tensor_tensor(out=pj[:, BSN, :], in0=pj[:, ASN, :], scalar=-1.0, in1=ptmp[:, AA, :],
                                                                          op0=ALU.mult, op1=ALU.mult))(pj), r=[pjk + "ASN", "pAA" + str(par)], w=[pjk + "BSN"])
                    A("dve", (lambda j: lambda e: e.tensor_scalar(out=ptmp[:, PRD, :], in0=ptmp[:, AA, :], scalar1=-1.0, scalar2=pcol("k_a", j),
                                                                  op0=ALU.add, op1=ALU.mult))(j), r=["pAA" + str(par), "prm"], w=["pPRD" + str(par)])
                    A("dve", (lambda pj, kr: lambda e: e.scalar_tensor_tensor(out=pj[:, KM, :], in0=ptmp[:, PRD, :], scalar=1.0, in1=kr,
                                                                              op0=ALU.add, op1=ALU.mult))(pj, kr), r=["pPRD" + str(par), f"rkv{4 + j}"], w=[pjk + "KM"])
                    A("dve", (lambda pj, rr, j: lambda e: e.scalar_tensor_tensor(out=ptmp[:, PRD, :], in0=rr, scalar=pcol("r_k", j), in1=pj[:, KM, :],
                                                                                 op0=ALU.mult, op1=ALU.mult))(pj, rr, j), r=[f"rkv{j}", "prm", pjk + "KM"], w=["pPRD" + str(par)])
                    A("pe", lambda e: e.matmul(psum[pa][:, 0:TT], lhsT=bones, rhs=ptmp[:, PRD, :], start=True, stop=True), r=["cst", "pPRD" + str(par)], w=[pk(pa)])
                    A("dve", (lambda pj, vr: lambda e: e.tensor_tensor(out=pj[:, BON, :], in0=psum[pa][:, 0:TT], in1=vr, op=ALU.mult))(pj, vr),
                      r=[pk(pa), f"rkv{8 + j}"], w=[pjk + "BON"])


                prep_ops = []
                for j in range(4):
                    rec = []
                    prep_body(j, lambda eng, fn, r=(), w=(): rec.append((eng, fn, r, w)))
                    prep_ops.append(rec)
                for grp in ((0, 1), (2, 3)):
                    for si in range(len(prep_ops[0])):
                        for j in grp:
                            eng_, fn_, r_, w_ = prep_ops[j][si]
                            A(eng_, fn_, r=r_, w=w_)
                P.section(4)
                def unit_stages(j, c):
                    col = slice(c * CH, (c + 1) * CH)
                    pj = pers[j]
                    pjk = f"pers{j}"
                    s = sct[j]
                    sk = f"sct{j}"
                    mt = mats[j]
                    mk = f"mats{j}"
                    tr = trs[j]
                    tk = f"trs{j}"
                    cn = chn[j]
                    ck = f"chn{j}"
                    H = Hst[j]
                    Hk = f"H{j}"
                    Hb = Hb_l[j]
                    Hbk = f"Hb{j}"
                    b4 = sb4[j]
                    hpc = hpc_l[j]
                    rr = rkv[:, j, col]
                    vr = rkv[:, 8 + j, col]
                    hp = [slice(0, 64), slice(64, 128)]
                    st = []

                    def s1():
                        A("dve", lambda e: e.tensor_tensor_scan(out=s[:, CI, :], data0=ones[:, 0:64], data1=pj[:, LW, col], initial=0.0, op0=ALU.mult, op1=ALU.add),
                          r=[pjk + "LW", "cst"], w=[sk + "ci"])
                        A("dve", lambda e: e.tensor_tensor(out=s[:, CE, :], in0=s[:, CI, :], in1=pj[:, LW, col], op=ALU.subtract),
                          r=[sk + "ci", pjk + "LW"], w=[sk + "ce"])
                    st.append(s1)

                    def s2():
                        A("act", lambda e: e.activation(out=s[:, EI, :], in_=s[:, CI, :], func=AF.Exp), r=[sk + "ci"], w=[sk + "ei"])
                        A("act", lambda e: e.activation(out=s[:, EE, :], in_=s[:, CE, :], func=AF.Exp), r=[sk + "ce"], w=[sk + "ee"])
                        A("act", lambda e: e.activation(out=s[:, EN, :], in_=s[:, CI, :], func=AF.Exp, scale=-1.0), r=[sk + "ci"], w=[sk + "en"])
                    st.append(s2)

                    def s3():
                        A("dve", lambda e: e.tensor_tensor(out=b4[:, 0, :], in0=pj[:, ASN, col], in1=s[:, EE, :], op=ALU.mult), r=[pjk + "ASN", sk + "ee"], w=[sk + "at"])
                        A("dve", lambda e: e.tensor_tensor(out=b4[:, 1, :], in0=rr, in1=s[:, EI, :], op=ALU.mult), r=[f"rkv{j}", sk + "ei"], w=[sk + "rt"])
                        A("dve", lambda e: e.tensor_tensor(out=b4[:, 2, :], in0=pj[:, BSN, col], in1=s[:, EN, :], op=ALU.mult), r=[pjk + "BSN", sk + "en"], w=[sk + "bt"])
                        A("dve", lambda e: e.tensor_tensor(out=b4[:, 3, :], in0=pj[:, KM, col], in1=s[:, EN, :], op=ALU.mult), r=[pjk + "KM", sk + "en"], w=[sk + "kt"])
                    st.append(s3)

                    def s4():
                        for p in hp:
                            A("pe", (lambda p: lambda e: e.matmul(psum[2][p, 0:128], lhsT=b4[p, 2, :], rhs=b4[p, 0:2, :].rearrange("p a b -> p (a b)"), start=True, stop=True))(p),
                              r=[sk + "bt", sk + "at", sk + "rt"], w=["ps2"])
                            A("pe", (lambda p: lambda e: e.matmul(psum[2][p, 128:256], lhsT=b4[p, 3, :], rhs=b4[p, 0:2, :].rearrange("p a b -> p (a b)"), start=True, stop=True))(p),
                              r=[sk + "kt", sk + "at", sk + "rt"], w=["ps2"])
                            A("pe", (lambda p: lambda e: e.matmul(psum[2][p, 256:320], lhsT=b4[p, 0, :], rhs=b4[p, 2, :], start=True, stop=True))(p),
                              r=[sk + "bt", sk + "at"], w=["ps2"])
                        A("dve", lambda e: e.tensor_tensor(out=mt[:], in0=psum[2][:, 0:320], in1=m5, op=ALU.mult), r=["ps2", "cst"], w=[mk])
                        for p in hp:
                            A("pe", (lambda p: lambda e: e.matmul(psum[3][p, 0:64], lhsT=rkv[p, 8 + j, col], rhs=istack[p, :], start=True, stop=True))(p),
                              r=[f"rkv{8 + j}", "cst"], w=["ps3"])
                            A("pe", (lambda p: lambda e: e.matmul(psum[3][p, 64:128], lhsT=b4[p, 2, :], rhs=istb[p, :], start=True, stop=True))(p),
                              r=[sk + "bt", "istb"], w=["ps3"])
                            A("pe", (lambda p: lambda e: e.matmul(psum[3][p, 128:192], lhsT=b4[p, 3, :], rhs=istb[p, :], start=True, stop=True))(p),
                              r=[sk + "kt", "istb"], w=["ps3"])
                        A("act", lambda e: e.activation(out=tr[:], in_=psum[3][:, 0:192], func=AF.Identity), r=["ps3"], w=[tk])
                        A("dve", lambda e: e.tensor_tensor(out=qq[j][0][:], in0=mt[:, 0:64], in1=istack, op=ALU.add), r=[mk, "cst"], w=[f"qq{j}_0"])
                    st.append(s4)

                    wzb = wzb_l[j]
                    wbk = f"wzb{j}"
                    bm3 = bones.rearrange("p (a b) -> p a b", a=2)

                    def s4b():
                        A("dve", lambda e: e.tensor_tensor(out=wzb[:, 0:128].rearrange("p (a b) -> p a b", a=2),
                                                            in0=mt[:, 0:64].rearrange("p (o t) -> p o t", o=1).to_broadcast([128, 2, 64]), in1=bm3, op=ALU.mult),
                          r=[mk, "cst"], w=[wbk])
                        A("dve", lambda e: e.tensor_tensor(out=wzb[:, 128:256].rearrange("p (a b) -> p a b", a=2),
                                                            in0=mt[:, 256:320].rearrange("p (o t) -> p o t", o=1).to_broadcast([128, 2, 64]), in1=bm3, op=ALU.mult),
                          r=[mk, "cst"], w=[wbk])
                    st.append(s4b)

                    for i in range(5):
                        def lv(i=i):
                            last = (i == 4)
                            qc = qq[j][i % 2]
                            qck = f"qq{j}_{i % 2}"
                            qn = qq[j][(i + 1) % 2]
                            qnk = f"qq{j}_{(i + 1) % 2}"
                            if not last:
                                A("pe", lambda e: e.matmul(psum[4][:, 0:128], lhsT=wzb[:, 128:256], rhs=wzb[:, 0:128], start=True, stop=True), r=[wbk], w=["ps4"])
                            A("pe", lambda e: e.matmul(psum[4][:, 128:256], lhsT=wzb[:, 0:128], rhs=wzb[:, 128:256], start=True, stop=True), r=[wbk], w=["ps4"])
                            if not last:
                                A("act", lambda e: e.activation(out=wzb[:, 0:256], in_=psum[4][:, 0:256], func=AF.Identity), r=["ps4"], w=[wbk])
                            else:
                                A("act", lambda e: e.activation(out=wzb[:, 128:256], in_=psum[4][:, 128:256], func=AF.Identity), r=["ps4"], w=[wbk])
                            A("pe", lambda e: e.matmul(psum[1][:, 128:192], lhsT=wzb[:, 128:256], rhs=qc[:, :], start=True, stop=True), r=[wbk, qck], w=["ps1"])
                            A("dve", lambda e: e.tensor_tensor(out=qn[:], in0=psum[1][:, 128:192], in1=qc[:], op=ALU.add), r=["ps1", qck], w=[qnk])
                        st.append(lv)

                    cbank = [5, 6, 7, 0][j]
                    cps = psum[cbank] if cbank != 7 else psTt
                    cpk = f"ps{cbank}"
                    q5 = qq[j][1]
                    q5k = f"qq{j}_1"
                    g = gst[j]
                    gk = f"gst{j}"

                    def s5a():
                        A("dve", lambda e: e.tensor_scalar(out=hpc[:], in0=H[:], scalar1=s[:, EI, 63:64], scalar2=None, op0=ALU.mult),
                          r=[Hk, sk + "ei"], w=[ck + "hpc"])
                        for p in hp:
                            A("pe", (lambda p: lambda e: e.matmul(cps[p, 0:64], lhsT=b4[p, 0, :], rhs=Hb[p, :], start=True, stop=False))(p), r=[sk + "at", Hbk], w=[cpk])
                            A("pe", (lambda p: lambda e: e.matmul(cps[p, 0:64], lhsT=mt[p, 128:192], rhs=tr[p, 0:64], start=False, stop=True))(p), r=[mk, tk], w=[cpk])
                        A("act", lambda e: e.activation(out=cn[:, 0, :], in_=cps[:, 0:64], func=AF.Identity), r=[cpk], w=[ck + "x"])
                    st.append(s5a)

                    def s5b():
                        for p in hp:
                            A("pe", (lambda p: lambda e: e.matmul(cps[p, 64:128], lhsT=q5[p, :], rhs=cn[p, 0, :], start=True, stop=True))(p), r=[q5k, ck + "x"], w=[cpk])
                        A("act", lambda e: e.activation(out=cn[:, 1, :], in_=cps[:, 64:128], func=AF.Identity), r=[cpk], w=[ck + "u"])
                    st.append(s5b)

                    def s5c():
                        for p in hp:
                            A("pe", (lambda p: lambda e: e.matmul(cps[p, 192:256], lhsT=tr[p, 64:128], rhs=cn[p, 1, :], start=True, stop=False))(p), r=[tk, ck + "u"], w=[cpk])
                            A("pe", (lambda p: lambda e: e.matmul(cps[p, 192:256], lhsT=tr[p, 128:192], rhs=tr[p, 0:64], start=False, stop=True))(p), r=[tk], w=[cpk])
                        for p in hp:
                            A("pe", (lambda p: lambda e: e.matmul(cps[p, 128:192], lhsT=b4[p, 1, :], rhs=Hb[p, :], start=True, stop=False))(p), r=[sk + "rt", Hbk], w=[cpk])
                            A("pe", (lambda p: lambda e: e.matmul(cps[p, 128:192], lhsT=mt[p, 64:128], rhs=cn[p, 1, :], start=False, stop=False))(p), r=[mk, ck + "u"], w=[cpk])
                            A("pe", (lambda p: lambda e: e.matmul(cps[p, 128:192], lhsT=mt[p, 192:256], rhs=tr[p, 0:64], start=False, stop=True))(p), r=[mk, tk], w=[cpk])
                        A("dve", lambda e: e.scalar_tensor_tensor(out=Hb[:], in0=cps[:, 192:256], scalar=s[:, EI, 63:64], in1=hpc[:], op0=ALU.mult, op1=ALU.add),
                          r=[cpk, sk + "ei", ck + "hpc"], w=[Hbk])
                        A("dve", lambda e: e.scalar_tensor_tensor(out=H[:], in0=cps[:, 192:256], scalar=s[:, EI, 63:64], in1=hpc[:], op0=ALU.mult, op1=ALU.add),
                          r=[cpk, sk + "ei", ck + "hpc"], w=[Hk])
                    st.append(s5c)

                    def s5d():
                        A("dve", lambda e: e.bn_stats(out=g[:, 0:6], in_=cps[:, 128:192]), r=[cpk], w=[gk])
                        A("dve", lambda e: e.bn_aggr(out=g[:, 6:8], in_=g[:, 0:6]), r=[gk], w=[gk])
                        A("act", lambda e: e.activation(out=g[:, 8:9], in_=g[:, 7:8], func=AF.Sqrt, bias=float(64e-5)), r=[gk], w=[gk])
                        A("dve", lambda e: e.reciprocal(out=g[:, 8:9], in_=g[:, 8:9]), r=[gk], w=[gk])
                        A("dve", lambda e: e.tensor_scalar(out=cn[:, 2, :], in0=cps[:, 128:192], scalar1=g[:, 6:7], scalar2=g[:, 8:9], op0=ALU.subtract, op1=ALU.mult),
                          r=[cpk, gk], w=[ck + "yn"])
                    st.append(s5d)

                    def s5e():
                        for p in hp:
                            A("pe", (lambda p: lambda e: e.matmul(psum[3][p, 192:256], lhsT=cn[p, 2, :], rhs=istb[p, :], start=True, stop=True))(p), r=[ck + "yn", "istb"], w=["ps3"])
                        A("act", lambda e: e.activation(out=ybT[:, j, col], in_=psum[3][:, 192:256], func=AF.Identity, scale=pcol("gn_w", j), bias=pcol("gn_b", j)),
                          r=["ps3", "prm"], w=[f"ybT{j}"])
                    st.append(s5e)
                    return st

                for c in range(NCH):
                    stl = [unit_stages(j, c) for j in range(4)]
                    for si in range(len(stl[0])):
                        for j in range(4):
                            stl[j][si]()
                P.section(5)
                for j in range(4):
                    pj = pers[j]
                    pjk = f"pers{j}"
                    A("pool", (lambda pj, j: lambda e: e.tensor_tensor(out=ybT[:, j, :], in0=ybT[:, j, :], in1=pj[:, BON, :], op=ALU.add))(pj, j),
                      r=[f"ybT{j}", pjk + "BON"], w=[f"ybT{j}"])
                    A("pool", (lambda pj, j: lambda e: e.tensor_tensor(out=ycat[:, 4 + j, :], in0=ybT[:, j, :], in1=pj[:, GG, :], op=ALU.mult))(pj, j),
                      r=[f"ybT{j}", pjk + "GG"], w=["ycat"])

                P.section(6)
                for sub in range(NSUB):
                    tc_ = slice(sub * 128, (sub + 1) * 128)
                    for kc in range(8):
                        A("pe", (lambda kc, tc_: lambda e: e.matmul(psum[0][:, 0:512], lhsT=hT[:, kc, tc_], rhs=win[:, kc, 512:1024], start=(kc == 0), stop=(kc == 7)))(kc, tc_),
                          r=["hT", "win"], w=[pk(0)])
                    A("act", lambda e: e.activation(out=vg[:], in_=psum[0][:, 0:512], func=AF.Gelu_apprx_tanh), r=[pk(0)], w=["vg"])
                    A("dve", lambda e: e.bn_stats(out=st6[:, 2, :], in_=vg[:]), r=["vg"], w=["st6b"])
                    A("dve", lambda e: e.bn_aggr(out=mv[:, 4:6], in_=st6[:, 2, :]), r=["st6b"], w=["mvb"])
                    A("act", lambda e: e.activation(out=mv[:, 6:7], in_=mv[:, 5:6], func=AF.Sqrt, bias=float(1e-5)), r=["mvb"], w=["mvb"])
                    A("dve", lambda e: e.reciprocal(out=mv[:, 6:7], in_=mv[:, 6:7]), r=["mvb"], w=["mvb"])
                    A("dve", lambda e: e.tensor_scalar(out=vn[:], in0=vg[:], scalar1=mv[:, 4:5], scalar2=mv[:, 6:7], op0=ALU.subtract, op1=ALU.mult), r=["vg", "mvb"], w=["vg"])
                    A("pool", lambda e: e.tensor_tensor(out=vn[:], in0=vn[:], in1=bcs[:, BC["lnw"]:BC["lnw"] + 512], op=ALU.mult), r=["vg", "bc"], w=["vg"])
                    A("pool", lambda e: e.tensor_tensor(out=vn[:], in0=vn[:], in1=bcs[:, BC["lnb"]:BC["lnb"] + 512], op=ALU.add), r=["vg", "bc"], w=["vg"])
                    for h in range(8):
                        A("pe", (lambda h: lambda e: e.matmul(psum[1][(h % 2) * 64:(h % 2) * 64 + 64, (h // 2) * 128:(h // 2 + 1) * 128], lhsT=vn[:, h * 64:(h + 1) * 64],
                                                              rhs=wc[:, h, :], start=True, stop=True))(h), r=["vg", "wc"], w=[pk(1)])
                    A("dve", lambda e: e.tensor_tensor(out=gtmp.rearrange("p a b -> p (a b)"), in0=psum[1][:, 0:512], in1=bcs[:, BC["bsT"]:BC["bsT"] + 512], op=ALU.add),
                      r=[pk(1), "bc"], w=["h2f"])
                    A("dve", (lambda tc_: lambda e: e.tensor_tensor(out=ycat[:, 0:4, tc_], in0=gtmp, in1=u_s[:, :, tc_], op=ALU.mult))(tc_), r=["h2f", "u_s"], w=["ycat"])

                P.section(7)
                for sub in range(NSUB):
                    tc_ = slice(sub * 128, (sub + 1) * 128)
                    ti = TI * NSUB + sub
                    tok0 = t0 + sub * 128
                    for hf in range(2):
                        for cc in range(8):
                            A("pe", (lambda cc, hf, tc_: lambda e: e.matmul(psum[6][:, 0:512], lhsT=ycat[:, cc, tc_], rhs=wout[:, cc, hf * 512:(hf + 1) * 512],
                                                                           start=(cc == 0), stop=(cc == 7)))(cc, hf, tc_), r=["ycat", "wout"], w=[pk(6)])
                        A("dve", (lambda hf: lambda e: e.tensor_tensor(out=x1t[:, hf * 512:(hf + 1) * 512], in0=psum[6][:, 0:512], in1=gate_bc[:, hf * 512:(hf + 1) * 512],
                                                                      op=ALU.mult))(hf), r=[pk(6), "gate_bc"], w=["x1t"])
                        A("pool", (lambda hf, xb, sub: lambda e: e.tensor_tensor(out=x1t[:, hf * 512:(hf + 1) * 512], in0=x1t[:, hf * 512:(hf + 1) * 512],
                                                                                in1=xb[:, sub, hf * 512:(hf + 1) * 512], op=ALU.add))(hf, xb, sub), r=["x1t", xk], w=["x1t"])
                    P.add("sp", (lambda tok0: lambda e: e.dma_start(out=x1_d[tok0:tok0 + 128, :], in_=x1t[:]))(tok0), r=["x1t"], w=["x1_d"], dma=True)
                    norm_stats(lambda hf: x1t[:, hf * 512:(hf + 1) * 512], "x1t", 1e-6)
                    A("act", lambda e: e.activation(out=xn2[:], in_=x1t[:], func=AF.Identity, scale=mv[:, 3:4]), r=["x1t", "mv"], w=["x1t"])
                    for kc in range(8):
                        pb = kc // 4
                        A("pe", (lambda kc, pb: lambda e: e.matmul(psum[pb][:, (kc % 4) * 128:(kc % 4 + 1) * 128], lhsT=xn2[:, kc * 128:(kc + 1) * 128], rhs=ident, start=True, stop=True))(kc, pb),
                          r=["x1t", "cst"], w=[pk(pb)])
                    for kc in range(8):
                        pb = kc // 4
                        A("act", (lambda kc, pb: lambda e: e.activation(out=h2f[:, kc, :], in_=psum[pb][:, (kc % 4) * 128:(kc % 4 + 1) * 128], func=AF.Identity,
                                                                        scale=sc[:, 8 + kc:9 + kc], bias=mod[:, 24 + kc:25 + kc]))(kc, pb), r=[pk(pb), "sc", "mod"], w=["h2f"])
                    A("pool", lambda e: e.tensor_copy(out=xnb[:], in_=xn2[:]), r=["x1t"], w=["xnb"])
                    P.add("sp", (lambda tok0: lambda e: e.dma_start(out=xn2_d[tok0:tok0 + 128, :], in_=xnb[:]))(tok0),
                          r=["xnb"], w=["xn2_d"], dma=True)
                    for kc in range(8):
                        A("pe", (lambda kc: lambda e: e.matmul(psum[6][:, 0:36], lhsT=h2f[:, kc, :], rhs=wrs[:, kc, :], start=(kc == 0), stop=(kc == 7)))(kc),
                          r=["h2f", "wrs"], w=[pk(6)])
                    A("dve", (lambda ti: lambda e: e.tensor_tensor(out=logits[:, ti, :], in0=psum[6][:, 0:36], in1=bcs[:, BC["rb"]:BC["rb"] + 36], op=ALU.add))(ti),
                      r=[pk(6), "bc"], w=["logits"])

            P.muted = False
            P.barrier()
            ct.close()
            if stage >= 2:
                rt = SB(ca, "rt", [128, 32, 48])
                sel = SB(ca, "sel", [128, 32, 8])
                sel2 = SB(ca, "sel2", [128, 32, 8])
                ohg = SB(ca, "ohg", [128, 32, 4])
                tmp48 = SB(ca, "tmp48", [128, 32, 4, 8])
                lg = logits[:, :, 0:4]
                le = logits[:, :, 4:36].rearrange("p t (g e) -> p t g e", g=4)
                MG, SG, M1, M2, P1, W1, W2 = range(7)
                r1 = lambda i: rt[:, :, i:i + 1]
                R = lambda fn, r, w: A("dve", fn, r=r, w=w)
                R(lambda e: e.tensor_reduce(out=rt[:, :, MG], in_=lg, axis=AX.X, op=ALU.max), ["logits"], ["rt"])
                R(lambda e: e.tensor_tensor(out=ohg[:], in0=lg, in1=r1(MG).to_broadcast([128, 32, 4]), op=ALU.is_equal), ["logits", "rt"], ["ohg"])
                R(lambda e: e.tensor_tensor(out=rt[:, :, 8:12], in0=lg, in1=r1(MG).to_broadcast([128, 32, 4]), op=ALU.subtract), ["logits", "rt"], ["rt"])
                A("act", lambda e: e.activation(out=rt[:, :, 8:12], in_=rt[:, :, 8:12], func=AF.Exp), r=["rt"], w=["rt"])
                R(lambda e: e.tensor_reduce(out=rt[:, :, SG], in_=rt[:, :, 8:12], axis=AX.X, op=ALU.add), ["rt"], ["rt"])
                R(lambda e: e.reciprocal(out=rt[:, :, SG], in_=rt[:, :, SG]), ["rt"], ["rt"])
                R(lambda e: e.tensor_tensor(out=tmp48[:], in0=le, in1=ohg[:].rearrange("p t (g o) -> p t g o", o=1).to_broadcast([128, 32, 4, 8]), op=ALU.mult),
                  ["logits", "ohg"], ["tmp48"])
                R(lambda e: e.tensor_reduce(out=sel[:], in_=tmp48[:].rearrange("p t g e -> p t e g"), axis=AX.X, op=ALU.add), ["tmp48"], ["sel"])
                R(lambda e: e.tensor_reduce(out=rt[:, :, M1], in_=sel[:], axis=AX.X, op=ALU.max), ["sel"], ["rt"])
                R(lambda e: e.tensor_tensor(out=sel2[:], in0=sel[:], in1=r1(M1).to_broadcast([128, 32, 8]), op=ALU.is_equal), ["sel", "rt"], ["sel2"])
                R(lambda e: e.scalar_tensor_tensor(out=tmp48[:, :, 0, :], in0=sel2[:], scalar=-1e30, in1=sel[:], op0=ALU.mult, op1=ALU.add), ["sel", "sel2"], ["tmp48"])
                R(lambda e: e.tensor_reduce(out=rt[:, :, M2], in_=tmp48[:, :, 0, :], axis=AX.X, op=ALU.max), ["tmp48"], ["rt"])
                R(lambda e: e.tensor_tensor(out=tmp48[:, :, 1, :], in0=tmp48[:, :, 0, :], in1=r1(M2).to_broadcast([128, 32, 8]), op=ALU.is_equal), ["tmp48", "rt"], ["tmp48"])
                R(lambda e: e.tensor_tensor(out=rt[:, :, P1], in0=rt[:, :, M2], in1=rt[:, :, M1], op=ALU.subtract), ["rt"], ["rt"])
                A("act", lambda e: e.activation(out=rt[:, :, P1], in_=rt[:, :, P1], func=AF.Exp), r=["rt"], w=["rt"])
                R(lambda e: e.tensor_scalar(out=rt[:, :, P1], in0=rt[:, :, P1], scalar1=1.0, scalar2=None, op0=ALU.add), ["rt"], ["rt"])
                R(lambda e: e.reciprocal(out=rt[:, :, P1], in_=rt[:, :, P1]), ["rt"], ["rt"])
                R(lambda e: e.tensor_tensor(out=rt[:, :, W1], in0=rt[:, :, P1], in1=rt[:, :, SG], op=ALU.mult), ["rt"], ["rt"])
                R(lambda e: e.tensor_tensor(out=rt[:, :, W2], in0=rt[:, :, SG], in1=rt[:, :, W1], op=ALU.subtract), ["rt"], ["rt"])
                R(lambda e: e.tensor_tensor(out=sel[:], in0=sel2[:], in1=r1(W1).to_broadcast([128, 32, 8]), op=ALU.mult), ["sel2", "rt"], ["sel"])
                R(lambda e: e.tensor_tensor(out=sel2[:], in0=tmp48[:, :, 1, :], in1=r1(W2).to_broadcast([128, 32, 8]), op=ALU.mult), ["tmp48", "rt"], ["sel2"])
                R(lambda e: e.tensor_tensor(out=sel[:], in0=sel[:], in1=sel2[:], op=ALU.add), ["sel", "sel2"], ["sel"])
                for g in range(4):
                    R((lambda g: lambda e: e.tensor_tensor(out=wt[:, :, g * 8:(g + 1) * 8], in0=sel[:], in1=ohg[:, :, g:g + 1].to_broadcast([128, 32, 8]), op=ALU.mult))(g),
                      ["sel", "ohg"], ["wt"])
            if stage >= 2:
                Mf = SB(ca, "Mf", [128, 1024])
                Mb = SB(ca, "Mb", [128, 1024], BF16)
                Lsb = SB(ca, "Lsb", [128, 128], BF16)
                onesb = SB(ca, "onesb", [128, 128], BF16)
                PRE = SB(ca, "PRE", [128, 1024])
                CNTs = SB(ca, "CNTs", [128, 1024])
                CA_ = SB(ca, "CA", [128, 1024])
                CB_ = SB(ca, "CB", [128, 1024])
                sm = SB(ca, "sm", [128, 8, 32])
                cmpb = SB(ca, "cmpb", [128, 96, 32])
                bev = SB(ca, "bev", [128, 6, 96])
                pabf = SB(ca, "pabf", [128, 2, 32])
                v3 = lambda t: t[:].rearrange("p (a b) -> p a b", a=32)
                wtf = wt[:].rearrange("p t e -> p (t e)")
                R(lambda e: e.tensor_single_scalar(out=Mf[:], in_=wtf, scalar=0.0, op=ALU.is_gt), ["wt"], ["Mf"])
                A("pool", lambda e: e.tensor_copy(out=Mb[:], in_=Mf[:]), r=["Mf"], w=["Mb"])
                A("pool", lambda e: e.tensor_tensor(out=Lsb[:], in0=tril, in1=ident, op=ALU.subtract), r=["cst"], w=["Lsb"])
                A("pool", lambda e: e.tensor_copy(out=onesb[:], in_=ones), r=["cst"], w=["onesb"])
                for h in range(2):
                    A("pe", (lambda h: lambda e: e.matmul(psum[h][:, 0:512], lhsT=Lsb[:], rhs=Mb[:, h * 512:(h + 1) * 512], start=True, stop=True))(h),
                      r=["Lsb", "Mb"], w=[pk(h)])
                    A("pe", (lambda h: lambda e: e.matmul(psum[2 + h][:, 0:512], lhsT=onesb[:], rhs=Mb[:, h * 512:(h + 1) * 512], start=True, stop=True))(h),
                      r=["onesb", "Mb"], w=[pk(2 + h)])
                    A("act", (lambda h: lambda e: e.activation(out=PRE[:, h * 512:(h + 1) * 512], in_=psum[h][:, 0:512], func=AF.Identity))(h), r=[pk(h)], w=["PRE"])
                    A("act", (lambda h: lambda e: e.activation(out=CNTs[:, h * 512:(h + 1) * 512], in_=psum[2 + h][:, 0:512], func=AF.Identity))(h), r=[pk(2 + h)], w=["CNTs"])
                src, srck, dst, dstk = CNTs, "CNTs", CA_, "CA"
                for dd in (1, 2, 4, 8, 16):
                    w_ = dd * 32
                    R((lambda src, dst, w_: lambda e: e.tensor_copy(out=dst[:, 0:w_], in_=src[:, 0:w_]))(src, dst, w_), [srck], [dstk])
                    R((lambda src, dst, w_: lambda e: e.tensor_tensor(out=dst[:, w_:1024], in0=src[:, w_:1024], in1=src[:, 0:1024 - w_], op=ALU.add))(src, dst, w_), [srck], [dstk])
                    src, srck = dst, dstk
                    dst, dstk = (CB_, "CB") if dst is CA_ else (CA_, "CA")
                R(lambda e: e.tensor_tensor(out=CB_[:], in0=CA_[:], in1=CNTs[:], op=ALU.subtract), ["CA", "CNTs"], ["CB"])
                R(lambda e: e.tensor_copy(out=sm[:, 0, :], in_=CA_[:, 31 * 32:32 * 32]), ["CA"], ["sm"])
                R(lambda e: e.tensor_tensor(out=cmpb[:, 0:32, :], in0=sm[:, 0, :].rearrange("p (e o) -> p e o", o=1).to_broadcast([128, 32, 32]),
                                            in1=cst[:, CC["thr"]:CC["thr"] + 32].rearrange("p (o k) -> p o k", o=1).to_broadcast([128, 32, 32]), op=ALU.is_gt),
                  ["sm", "cst"], ["cmpb"])
                R(lambda e: e.tensor_reduce(out=sm[:, 1, :], in_=cmpb[:, 0:32, :], axis=AX.X, op=ALU.add), ["cmpb"], ["sm"])
                R(lambda e: e.tensor_tensor_scan(out=sm[:, 2, :], data0=ones[:, 0:32], data1=sm[:, 1, :], initial=0.0, op0=ALU.mult, op1=ALU.add), ["sm", "cst"], ["sm"])
                R(lambda e: e.tensor_tensor(out=sm[:, 3, :], in0=sm[:, 2, :], in1=sm[:, 1, :], op=ALU.subtract), ["sm"], ["sm"])
                R(lambda e: e.tensor_single_scalar(out=sm[:, 4, :], in_=sm[:, 3, :], scalar=128.0, op=ALU.mult), ["sm"], ["sm"])
                R(lambda e: e.tensor_tensor(out=v3(CB_), in0=v3(CB_), in1=sm[:, 4, :].rearrange("p (o e) -> p o e", o=1).to_broadcast([128, 32, 32]), op=ALU.add),
                  ["CB", "sm"], ["CB"])
                R(lambda e: e.tensor_tensor(out=PRE[:], in0=PRE[:], in1=CB_[:], op=ALU.add), ["PRE", "CB"], ["PRE"])
                R(lambda e: e.tensor_tensor(out=CA_[:], in0=PRE[:], in1=Mf[:], op=ALU.mult), ["PRE", "Mf", "sm"], ["CA"])
                R(lambda e: e.tensor_scalar(out=CB_[:], in0=Mf[:], scalar1=-1e9, scalar2=1e9, op0=ALU.mult, op1=ALU.add), ["Mf", "PRE"], ["CB"])
                R(lambda e: e.tensor_tensor(out=CB_[:], in0=CB_[:], in1=CA_[:], op=ALU.add), ["CB", "CA"], ["CB"])
                R(lambda e: e.tensor_reduce(out=pabf[:, 0, :], in_=v3(CB_), axis=AX.X, op=ALU.min), ["CB"], ["pabf"])
                R(lambda e: e.tensor_reduce(out=pabf[:, 1, :], in_=v3(CA_), axis=AX.X, op=ALU.max), ["CA"], ["pabf"])
                R(lambda e: e.tensor_tensor(out=v3(CB_), in0=v3(CB_), in1=pabf[:, 0, :].rearrange("p (t o) -> p t o", o=1).to_broadcast([128, 32, 32]), op=ALU.is_equal),
                  ["CB", "pabf"], ["CB"])
                R(lambda e: e.tensor_tensor(out=CB_[:], in0=CB_[:], in1=wtf, op=ALU.mult), ["CB", "wt"], ["CB"])
                R(lambda e: e.tensor_reduce(out=WAB[:, 0, :], in_=v3(CB_), axis=AX.X, op=ALU.add), ["CB"], ["WAB"])
                R(lambda e: e.tensor_tensor(out=WAB[:, 1, :], in0=rt[:, :, SG], in1=WAB[:, 0, :], op=ALU.subtract), ["rt", "WAB"], ["WAB"])
                R(lambda e: e.tensor_copy(out=PABi[:], in_=pabf[:]), ["pabf"], ["PABi"])
                R(lambda e: e.tensor_tensor(out=cmpb[:], in0=sm[:, 2, :].rearrange("p (o e) -> p o e", o=1).to_broadcast([128, 96, 32]),
                                            in1=cst[:, CC["iotab"]:CC["iotab"] + 96].rearrange("p (b o) -> p b o", o=1).to_broadcast([128, 96, 32]), op=ALU.is_le),
                  ["sm", "cst", "cmpb"], ["cmpb"])
                R(lambda e: e.tensor_reduce(out=bev[:, 0, :], in_=cmpb[:], axis=AX.X, op=ALU.add), ["cmpb"], ["bev"])
                R(lambda e: e.memset(bev[:, 1, 0:1], 1.0), [], ["bev"])
                R(lambda e: e.tensor_tensor(out=bev[:, 1, 1:96], in0=bev[:, 0, 1:96], in1=bev[:, 0, 0:95], op=ALU.not_equal), ["bev"], ["bev"])
                R(lambda e: e.tensor_scalar(out=bev[:, 2, :], in0=bev[:, 1, :], scalar1=-1e6, scalar2=1e6, op0=ALU.mult, op1=ALU.add), ["bev"], ["bev"])
                R(lambda e: e.scalar_tensor_tensor(out=bev[:, 2, :], in0=bev[:, 0, :], scalar=128.0, in1=bev[:, 2, :], op0=ALU.mult, op1=ALU.add), ["bev"], ["bev"])
                R(lambda e: e.tensor_scalar(out=bev[:, 2, :], in0=bev[:, 2, :], scalar1=cst[:, CC["pidx"]:CC["pidx"] + 1], scalar2=None, op0=ALU.add), ["bev", "cst"], ["bev"])
                R(lambda e: e.tensor_copy(out=IDXW[:, 0:1, :], in_=bev[:, 2:3, :]), ["bev"], ["IDXW"])
            P.barrier()

        if stage >= 3:
            with ExitStack() as cb:
                IOA = bass.IndirectOffsetOnAxis
                NROW = NE * 128
                _bc = {}

                def bc_reg(e):
                    if "r" not in _bc:
                        _bc["r"] = e.to_reg(NROW - 1)
                    return _bc["r"]
                W32 = SB(cb, "W32", [128, 12288])
                idb2 = SB(cb, "idb2", [128, 128], BF16)
                xload = [SB(cb, f"xl{i}", [128, D], BF16) for i in range(2)]
                xsb = [SB(cb, f"xsb{i}", [128, D], BF16) for i in range(2)]
                XgT_l = [SB(cb, f"XgT{i}", [128, 8, 128], BF16) for i in range(2)]
                Wgb = SB(cb, "Wgb", [128, 8, EH], BF16)
                Wub = SB(cb, "Wub", [128, 8, EH], BF16)
                Wdb = SB(cb, "Wdb", [128, 4, D], BF16)
                sg_l = [SB(cb, f"sg{i}", [128, 512]) for i in range(2)]
                actb_l = [SB(cb, f"actb{i}", [128, 512], BF16) for i in range(2)]
                actT_l = [SB(cb, f"actT{i}", [128, 4, 128], BF16) for i in range(2)]
                yblk = [SB(cb, f"yblk{i}", [128, D]) for i in range(2)]
                fgt = SB(cb, "fgt", [128, D])
                fgs = SB(cb, "fgs", [128, D])
                g2bc = SB(cb, "g2bc", [128, D])
                yA = SB(cb, "yA", [128, D])
                yB = SB(cb, "yB", [128, D])
                fst = SB(cb, "fst", [128, 2, 6])
                fmv = SB(cb, "fmv", [128, 4])
                P.add("sp", lambda e: e.dma_start(out=fgs[:], in_=fg_d[:, :]), w=["fgs"], dma=True)
                P.add("dve", lambda e: e.tensor_copy(out=idb2[:], in_=ident), r=["cst"], w=["idb2"])
                make_gate(g2bc, "g2bc", 40)
                sc_keys = []
                for i in range(32):
                    xl = xload[i % 2]
                    xlk = f"xl{i % 2}"
                    P.add("sp", (lambda xl, i: lambda e: e.dma_start(out=xl[:], in_=xn2_d[i * 128:(i + 1) * 128, :]))(xl, i), r=["xn2_d"], w=[xlk], dma=True)
                    for k in range(2):
                        key = f"xsc{i}_{k}"
                        P.add("pool", (lambda xl, i, k: lambda e: e.indirect_dma_start(out=xs_d[:, :], out_offset=IOA(ap=PABi[:, k, i:i + 1], axis=0),
                                                                                       in_=xl[:], in_offset=None))(xl, i, k),
                              r=[xlk, "PABi"] + xz_keys, w=[key], dma=True)
                        sc_keys.append(key)
                ys_keys = []
                def do_block(b):
                    P.add("pool", (lambda b: lambda e: e.indirect_dma_start(out=W32[:], out_offset=None, in_=wall_d[:, :],
                                                                           in_offset=IOA(ap=IDXW[:, 0, b:b + 1], axis=0), bounds_check=bc_reg(e), oob_is_err=False))(b),
                          r=["IDXW", "W32"], w=["W32"], dma=True)
                    P.add("act", lambda e: e.activation(out=Wgb[:].rearrange("p a b -> p (a b)"), in_=W32[:, 0:4096], func=AF.Identity), r=["W32"], w=["Wgb"])
                    P.add("dve", lambda e: e.tensor_copy(out=Wub[:].rearrange("p a b -> p (a b)"), in_=W32[:, 4096:8192]), r=["W32"], w=["Wub"])
                    P.add("pool", lambda e: e.tensor_copy(out=Wdb[:, 0:2, :].rearrange("p a b -> p (a b)"), in_=W32[:, 8192:10240]), r=["W32"], w=["Wdb"])
                    P.add("dve", lambda e: e.tensor_copy(out=Wdb[:, 2:4, :].rearrange("p a b -> p (a b)"), in_=W32[:, 10240:12288]), r=["W32"], w=["Wdb"])
                    XgT = XgT_l[b % 2]
                    xgk = f"XgT{b % 2}"
                    sg = sg_l[b % 2]
                    sgk = f"sg{b % 2}"
                    actb = actb_l[b % 2]
                    abk = f"actb{b % 2}"
                    actT = actT_l[b % 2]
                    atk = f"actT{b % 2}"
                    xb_ = xsb[b % 2]
                    xbk = f"xsb{b % 2}"
                    P.add("sp", (lambda xb_, b: lambda e: e.dma_start(out=xb_[:], in_=xs_d[b * 128:(b + 1) * 128, :]))(xb_, b), r=sc_keys, w=[xbk], dma=True)
                    for kc in range(8):
                        P.add("pe", (lambda xb_, kc: lambda e: e.matmul(psum[kc // 4][:, (kc % 4) * 128:(kc % 4 + 1) * 128], lhsT=xb_[:, kc * 128:(kc + 1) * 128],
                                                                       rhs=idb2[:], start=True, stop=True))(xb_, kc), r=[xbk, "idb2"], w=[pk(kc // 4)])
                    for kc in range(8):
                        P.add("act", (lambda kc: lambda e: e.activation(out=XgT[:, kc, :], in_=psum[kc // 4][:, (kc % 4) * 128:(kc % 4 + 1) * 128], func=AF.Identity,
                                                                        scale=sc[:, 8 + kc:9 + kc], bias=mod[:, 24 + kc:25 + kc]))(kc), r=[pk(kc // 4), "sc", "mod"], w=[xgk])
                    for kc in range(8):
                        P.add("pe", (lambda kc: lambda e: e.matmul(psum[2][:, 0:512], lhsT=XgT[:, kc, :], rhs=Wgb[:, kc, :], start=(kc == 0), stop=(kc == 7)))(kc),
                              r=[xgk, "Wgb"], w=[pk(2)])
                    for kc in range(8):
                        P.add("pe", (lambda kc: lambda e: e.matmul(psum[3][:, 0:512], lhsT=XgT[:, kc, :], rhs=Wub[:, kc, :], start=(kc == 0), stop=(kc == 7)))(kc),
                              r=[xgk, "Wub"], w=[pk(3)])
                    P.add("act", lambda e: e.activation(out=sg[:], in_=psum[2][:, 0:512], func=AF.Silu), r=[pk(2)], w=[sgk])
                    P.add("dve", lambda e: e.tensor_tensor(out=actb[:], in0=psum[3][:, 0:512], in1=sg[:], op=ALU.mult), r=[pk(3), sgk], w=[abk])
                    for hc in range(4):
                        P.add("pe", (lambda hc: lambda e: e.matmul(psum[4][:, hc * 128:(hc + 1) * 128], lhsT=actb[:, hc * 128:(hc + 1) * 128], rhs=idb2[:],
                                                                   start=True, stop=True))(hc), r=[abk, "idb2"], w=[pk(4)])
                    P.add("act", lambda e: e.activation(out=actT[:].rearrange("p a b -> p (a b)"), in_=psum[4][:, 0:512], func=AF.Identity), r=[pk(4)], w=[atk])
                    yb_ = yblk[b % 2]
                    ybk = f"yblk{b % 2}"
                    for hf in range(2):
                        for hc in range(4):
                            P.add("pe", (lambda hc, hf: lambda e: e.matmul(psum[5 + hf][:, 0:512], lhsT=actT[:, hc, :], rhs=Wdb[:, hc, hf * 512:(hf + 1) * 512],
                                                                           start=(hc == 0), stop=(hc == 3)))(hc, hf), r=[atk, "Wdb"], w=[pk(5 + hf)])
                        if hf == 0:
                            P.add("act", (lambda yb_: lambda e: e.activation(out=yb_[:, 0:512], in_=psum[5][:, 0:512], func=AF.Identity))(yb_), r=[pk(5)], w=[ybk])
                        else:
                            P.add("dve", (lambda yb_: lambda e: e.tensor_copy(out=yb_[:, 512:1024], in_=psum[6][:, 0:512]))(yb_), r=[pk(6)], w=[ybk])
                    key = f"ys{b}"
                    P.add("sp", (lambda yb_, b: lambda e: e.dma_start(out=ys_d[b * 128:(b + 1) * 128, :], in_=yb_[:]))(yb_, b), r=[ybk], w=[key], dma=True)
                    ys_keys.append(key)

                for b in range(min(NBLK, nblk)):
                    do_block(b)
                for i in range(32):
                    tok0 = i * 128
                    P.add("pool", (lambda i: lambda e: e.indirect_dma_start(out=yA[:], out_offset=None, in_=ys_d[:, :], in_offset=IOA(ap=PABi[:, 0, i:i + 1], axis=0)))(i),
                          r=ys_keys + ["PABi", "yA"], w=["yA"], dma=True)
                    P.add("pool", (lambda i: lambda e: e.indirect_dma_start(out=yB[:], out_offset=None, in_=ys_d[:, :], in_offset=IOA(ap=PABi[:, 1, i:i + 1], axis=0)))(i),
                          r=ys_keys + ["PABi", "yB"], w=["yB"], dma=True)
                    P.add("sp", (lambda tok0: lambda e: e.dma_start(out=fgt[:], in_=x1_d[tok0:tok0 + 128, :]))(tok0), r=["x1_d"], w=["fgt"], dma=True)
                    P.add("dve", (lambda i: lambda e: e.tensor_scalar(out=yA[:], in0=yA[:], scalar1=WAB[:, 0, i:i + 1], scalar2=None, op0=ALU.mult))(i), r=["yA", "WAB"], w=["yA"])
                    P.add("dve", (lambda i: lambda e: e.scalar_tensor_tensor(out=yA[:], in0=yB[:], scalar=WAB[:, 1, i:i + 1], in1=yA[:], op0=ALU.mult, op1=ALU.add))(i),
                          r=["yA", "yB", "WAB"], w=["yA"])
                    P.add("pool", lambda e: e.tensor_tensor(out=yA[:], in0=yA[:], in1=g2bc[:], op=ALU.mult), r=["yA", "g2bc"], w=["yA"])
                    P.add("dve", lambda e: e.tensor_tensor(out=fgt[:], in0=fgt[:], in1=yA[:], op=ALU.add), r=["yA", "fgt"], w=["fgt"])
                    for hf in range(2):
                        P.add("dve", (lambda hf: lambda e: e.bn_stats(out=fst[:, hf, :], in_=fgt[:, hf * 512:(hf + 1) * 512]))(hf), r=["fgt"], w=["fst"])
                    P.add("dve", lambda e: e.bn_aggr(out=fmv[:, 0:2], in_=fst[:, 0:2, :].rearrange("p a b -> p (a b)")), r=["fst"], w=["fmv"])
                    P.add("dve", lambda e: e.scalar_tensor_tensor(out=fmv[:, 2:3], in0=fmv[:, 0:1], scalar=fmv[:, 0:1], in1=fmv[:, 1:2], op0=ALU.mult, op1=ALU.add),
                          r=["fmv"], w=["fmv"])
                    P.add("act", lambda e: e.activation(out=fmv[:, 3:4], in_=fmv[:, 2:3], func=AF.Sqrt, bias=float(1e-6)), r=["fmv"], w=["fmv"])
                    P.add("dve", lambda e: e.reciprocal(out=fmv[:, 3:4], in_=fmv[:, 3:4]), r=["fmv"], w=["fmv"])
                    P.add("dve", lambda e: e.scalar_tensor_tensor(out=fgt[:], in0=fgt[:], scalar=fmv[:, 3:4], in1=fgs[:], op0=ALU.mult, op1=ALU.mult),
                          r=["fgt", "fmv", "fgs"], w=["fgt"])
                    P.add("sp", (lambda tok0: lambda e: e.dma_start(out=out_d[tok0:tok0 + 128, :], in_=fgt[:]))(tok0), r=["fgt"], w=["out_d"], dma=True)
        else:
            with ExitStack() as cb:
                fgt = SB(cb, "fgt", [128, D])
                for ti in range(32):
                    tok0 = ti * 128
                    P.add("sp", (lambda tok0: lambda e: e.dma_start(out=fgt[:], in_=x1_d[tok0:tok0 + 128, :]))(tok0), r=["x1_d"], w=["fgt"], dma=True)
                    P.add("sp", (lambda tok0: lambda e: e.dma_start(out=out_d[tok0:tok0 + 128, :], in_=fgt[:]))(tok0), r=["fgt"], w=["out_d"], dma=True)
        P.emit()
    return nc


_NC_CACHE = {}


def _layouts(inp):
    f = np.float32
    g = lambda k: np.asarray(inp[k], dtype=f)
    col = lambda v, n: np.ascontiguousarray(v.reshape(n, 128).T)
    mu = g("rwkv_mu")[0]
    def pad128(v):
        o = np.zeros((128, 1), f)
        o[:v.shape[0], 0] = v
        return o
    shared = [None, col(g("ada_b")[0], 48), col(g("norm1_g")[0], 8), col(g("norm2_g")[0], 8), col(mu[0:1536], 12),
              pad128(mu[1536:1568]), pad128(mu[1568:1600]), pad128(mu[1600:1696]), col(g("rwkv_w0")[0], 4), col(g("rwkv_a0")[0], 4),
              col(g("rwkv_k_k")[0], 4), col(g("rwkv_k_a")[0], 4), col(g("rwkv_r_k")[0].reshape(512), 4), col(g("rwkv_gn_w")[0], 4),
              col(g("rwkv_gn_b")[0], 4)]
    tt = np.arange(64)
    su = (tt[:, None] < tt[None, :]).astype(f)
    iu = (tt[:, None] <= tt[None, :]).astype(f)
    sl = (tt[None, :] < tt[:, None]).astype(f)
    m5 = np.tile(np.concatenate([su, iu, su, iu, sl], axis=1), (2, 1))
    ident = np.eye(128, dtype=f)
    istack = np.tile(np.eye(64, dtype=f), (2, 1))
    ss = np.arange(128)
    tril = (ss[:, None] <= ss[None, :]).astype(f)
    bones = (ss[:, None] // 64 == ss[None, :] // 64).astype(f)
    ones = np.ones((128, 128), f)
    thr = np.broadcast_to((np.arange(32) * 128).astype(f)[None, :], (128, 32))
    iotab = np.broadcast_to(np.arange(96).astype(f)[None, :], (128, 96))
    pidx = np.arange(128).astype(f)[:, None]
    cst = np.ascontiguousarray(np.concatenate([m5, ident, istack, tril, bones, ones, thr, iotab, pidx], axis=1))
    assert cst.shape[1] == NCST
    rep = lambda v: np.broadcast_to(v[None, :], (128, v.shape[0]))
    bs = g("gmlp_bs")[0]
    bsT = np.zeros((128, 4, 128), f)
    for j in range(4):
        for hh in range(2):
            bsT[hh * 64:(hh + 1) * 64, j, :] = bs[2 * j + hh][None, :]
    bc = np.ascontiguousarray(np.concatenate([rep(g("gmlp_ln_w")[0]), rep(g("gmlp_ln_b")[0]),
                                              rep(np.concatenate([g("router_group_b")[0], g("router_expert_b")[0]])),
                                              bsT.reshape(128, 512)], axis=1))
    assert bc.shape[1] == NBC
    common = dict(cst=cst, bc=bc, fg=np.ascontiguousarray(rep(g("final_norm_g"))), ada_w=np.ascontiguousarray(g("ada_w")[0]), w_in=np.ascontiguousarray(g("w_in")[0]),
                  w_out=np.ascontiguousarray(g("w_out")[0]), w2=np.ascontiguousarray(g("rwkv_w2")[0]),
                  a2=np.ascontiguousarray(g("rwkv_a2")[0]), g2=np.ascontiguousarray(g("rwkv_g2")[0]),
                  wsT=np.ascontiguousarray(g("gmlp_ws")[0].transpose(2, 0, 1)),
                  wr=np.ascontiguousarray(np.concatenate([g("router_group_w")[0], g("router_expert_w")[0]], axis=1)),
                  wall=np.ascontiguousarray(np.concatenate([
                      g("moe_w_gate")[0].reshape(NE, 8, 128, EH).transpose(0, 2, 1, 3).reshape(NE * 128, 4096),
                      g("moe_w_up")[0].reshape(NE, 8, 128, EH).transpose(0, 2, 1, 3).reshape(NE * 128, 4096),
                      g("moe_w_down")[0].reshape(NE, 4, 128, D).transpose(0, 2, 1, 3).reshape(NE * 128, 4096)], axis=1)))
    x = g("x")
    c = g("c")
    maps = []
    for b in range(x.shape[0]):
        cols = [col(c[b], 8)] + shared[1:]
        prm = np.ascontiguousarray(np.concatenate(cols, axis=1))
        assert prm.shape[1] == NPRM
        m = dict(common)
        m["x"] = np.ascontiguousarray(x[b])
        m["prm"] = prm
        maps.append(m)
    return maps


def kernel(**inputs):
    maps = _layouts(inputs)
    if "nc" not in _NC_CACHE:
        _NC_CACHE["nc"] = build_nc()
    nc = _NC_CACHE["nc"]
    res = run_bass_kernel_spmd(nc, maps, core_ids=list(range(len(maps))))
    return np.stack([r["out"] for r in res.results], axis=0).astype(np.float32)
```

```python
import numpy as np
import os
from contextlib import ExitStack
import concourse.bass as bass
import concourse.mybir as mybir
from concourse.bass_utils import run_bass_kernel_spmd

F32 = mybir.dt.float32
BF16 = mybir.dt.bfloat16
I32 = mybir.dt.int32
AF = mybir.ActivationFunctionType
ALU = mybir.AluOpType
AX = mybir.AxisListType

D = 1024
S = 4096
TT = 256
NT = S // TT
NSUB = TT // 128
CH = 64
NCH = TT // CH
INW = 2720
NE = 32
EH = 512
NEG_HALF_E = -0.6065306597126334

PC = {}
_o = 0
for _n, _w in [("cT", 8), ("ada_b", 48), ("n1g", 8), ("n2g", 8), ("mu_rkv", 12), ("mu_xw", 1),
               ("mu_xa", 1), ("mu_xg", 1), ("w0", 4), ("a0", 4), ("k_k", 4), ("k_a", 4), ("r_k", 4),
               ("gn_w", 4), ("gn_b", 4)]:
    PC[_n] = _o
    _o += _w
NPRM = _o
CC = {}
_o = 0
for _n, _w in [("m5", 320), ("ident", 128), ("istack", 64), ("tril", 128), ("bones", 128), ("ones", 128), ("thr", 32), ("iotab", 96), ("pidx", 1)]:
    CC[_n] = _o
    _o += _w
NCST = _o
BC = {}
_o = 0
for _n, _w in [("lnw", 512), ("lnb", 512), ("rb", 36), ("bsT", 512)]:
    BC[_n] = _o
    _o += _w
NBC = _o


ATTACH_WAIT = True
NO_SELF_SYNC = ("pe", "act")


class Prog:
    def __init__(self, nc, ctx):
        self.nc = nc
        self.ops = []
        self.last_w = {}
        self.readers = {}
        self.engs = ["pe", "act", "dve", "pool", "sp"]
        self.count = {e: 0 for e in self.engs}
        self.sem = {e: ctx.enter_context(nc.semaphore("s_" + e)) for e in self.engs}
        self.NDS = 8
        self.dsem = {q: [ctx.enter_context(nc.semaphore(f"d_{q}{i}")) for i in range(self.NDS)]
                     for q in ("sp", "pool")}
        self.dcount = {"sp": 0, "pool": 0}
        self.last_op = {e: None for e in self.engs}
        self.pending = {e: set() for e in self.engs}
        self.recent_dma = {"sp": [], "pool": []}

    def section(self, k):
        self.muted = k > self.cut

    def add(self, eng, fn, r=(), w=(), dma=False):
        if getattr(self, 'muted', False):
            return None
        idx = len(self.ops)
        deps = set(self.pending[eng])
        self.pending[eng] = set()
        for k in r:
            if k in self.last_w:
                deps.add(self.last_w[k])
        for k in w:
            if k in self.last_w:
                deps.add(self.last_w[k])
            deps.update(self.readers.get(k, ()))
        for k in r:
            self.readers.setdefault(k, []).append(idx)
        for k in w:
            self.last_w[k] = idx
            self.readers[k] = []
        if dma:
            q = eng
            kq = self.dcount[q]
            self.dcount[q] += 1
            sem = self.dsem[q][kq % self.NDS]
            val = 16 * (kq // self.NDS + 1)
            prev = (sem, val - 16) if val > 16 else None
            self.recent_dma[q].append(idx)
            self.recent_dma[q] = self.recent_dma[q][-self.NDS:]
        else:
            self.count[eng] += 1
            sem = self.sem[eng]
            val = self.count[eng]
            prev = None
        self.ops.append(dict(eng=eng, fn=fn, deps=deps, dma=dma, sem=sem, val=val, prev=prev))
        self.last_op[eng] = idx
        return idx

    def barrier(self):
        allops = set()
        for e in self.engs:
            if self.last_op[e] is not None:
                allops.add(self.last_op[e])
        for q in ("sp", "pool"):
            allops.update(self.recent_dma[q])
        dmaops = set()
        for q in ("sp", "pool"):
            dmaops.update(self.recent_dma[q])
        for e in self.engs:
            self.pending[e] |= (allops - dmaops) if e == "pe" else allops

    def emit(self):
        nc = self.nc
        per = {e: [] for e in self.engs}
        for i, op in enumerate(self.ops):
            per[op["eng"]].append(i)
        ops = self.ops

        def run(name, e):
            waited = {}
            for i in per[name]:
                op = ops[i]
                need = []
                for j in op["deps"]:
                    d = ops[j]
                    if d["eng"] == name and not d["dma"] and name in NO_SELF_SYNC:
                        continue
                    need.append((d["sem"], d["val"]))
                if op["prev"] is not None:
                    need.append(op["prev"])
                need.sort(key=lambda t: -t[1])
                todo = []
                for sem, val in need:
                    key = id(sem)
                    if waited.get(key, 0) >= val:
                        continue
                    waited[key] = val
                    todo.append((sem, val))
                attach = todo.pop() if (todo and ATTACH_WAIT) else None
                for sem, val in todo:
                    e.wait_ge(sem, val)
                inst = op["fn"](e)
                if attach is not None:
                    inst._wait_ge(attach[0], attach[1])
                inst.then_inc(op["sem"], 16 if op["dma"] else 1)
            if name in ("sp", "pool"):
                kq = self.dcount[name]
                for s_i in range(min(kq, self.NDS)):
                    n_on = (kq - s_i + self.NDS - 1) // self.NDS
                    e.wait_ge(self.dsem[name][s_i], 16 * n_on)

        with nc.Block() as block:
            @block.sync
            def _(e):
                run("sp", e)

            @block.scalar
            def _(e):
                run("act", e)

            @block.vector
            def _(e):
                run("dve", e)

            @block.tensor
            def _(e):
                run("pe", e)

            @block.gpsimd
            def _(e):
                run("pool", e)


def build_nc(stage=99, ntiles=NT, cut=99, nblk=96):
    nc = bass.Bass("TRN2", target_bir_lowering=False)
    dt = lambda name, shape, dty, kind: nc.dram_tensor(name, shape, dty, kind=kind).ap()
    x_d = dt("x", [S, D], F32, "ExternalInput")
    prm_d = dt("prm", [128, NPRM], F32, "ExternalInput")
    cst_d = dt("cst", [128, NCST], F32, "ExternalInput")
    bc_d = dt("bc", [128, NBC], F32, "ExternalInput")
    adaw_d = dt("ada_w", [D, 6 * D], F32, "ExternalInput")
    win_d = dt("w_in", [D, INW], F32, "ExternalInput")
    wout_d = dt("w_out", [D, D], F32, "ExternalInput")
    w2_d = dt("w2", [32, 512], F32, "ExternalInput")
    a2_d = dt("a2", [32, 512], F32, "ExternalInput")
    g2_d = dt("g2", [96, 512], F32, "ExternalInput")
    wsT_d = dt("wsT", [128, 8, 128], F32, "ExternalInput")
    wr_d = dt("wr", [D, 36], F32, "ExternalInput")
    fg_d = dt("fg", [128, D], F32, "ExternalInput")
    wall_d = dt("wall", [NE * 128, 12288], F32, "ExternalInput")
    NBLK = 96
    xn2_d = dt("xn2_d", [S, D], BF16, "Internal")
    xs_d = dt("xs_d", [NBLK * 128, D], BF16, "Internal")
    ys_d = dt("ys_d", [NBLK * 128, D], F32, "Internal")
    out_d = dt("out", [S, D], F32, "ExternalOutput")
    x1_d = dt("x1_d", [S, D], F32, "Internal")

    with ExitStack() as top:
        P = Prog(nc, top)
        P.cut = cut
        psum = [top.enter_context(nc.psum_tensor(f"ps{i}", [128, 512], F32)) for i in range(7)]
        psTt = top.enter_context(nc.psum_tensor("psT", [128, 512], F32))
        psT = [psTt[:, 0:256], psum[6][:, 0:256]]
        psTk = ["ps7", "ps6"]
        fst = None
        fmv = None
        pk = lambda i: f"ps{i}"

        def SB(ctx, name, shape, dty=F32):
            return ctx.enter_context(nc.sbuf_tensor(name, shape, dty))

        prm = SB(top, "prm_s", [128, NPRM])
        cst = SB(top, "cst_s", [128, NCST])
        bcs = SB(top, "bc_s", [128, NBC])
        mod = SB(top, "mod", [128, 48])
        wt = SB(top, "wt", [128, 32, 32])
        PABi = SB(top, "PABi", [128, 2, 32], I32)
        WAB = SB(top, "WAB", [128, 2, 32])
        IDXW = SB(top, "IDXW", [128, 4, 96], I32)
        P.add("sp", lambda e: e.dma_start(out=prm[:], in_=prm_d[:, :]), w=["prm"], dma=True)
        P.add("sp", lambda e: e.dma_start(out=cst[:], in_=cst_d[:, :]), w=["cst"], dma=True)
        P.add("sp", lambda e: e.dma_start(out=bcs[:], in_=bc_d[:, :]), w=["bc"], dma=True)

        def pcol(name, j=0, n=1):
            return prm[:, PC[name] + j:PC[name] + j + n]

        ident = cst[:, CC["ident"]:CC["ident"] + 128]
        istack = cst[:, CC["istack"]:CC["istack"] + 64]
        tril = cst[:, CC["tril"]:CC["tril"] + 128]
        bones = cst[:, CC["bones"]:CC["bones"] + 128]
        ones = cst[:, CC["ones"]:CC["ones"] + 128]
        m5 = cst[:, CC["m5"]:CC["m5"] + 320]

        with ExitStack() as c0:
            stg = [SB(c0, f"ada_stg{i}", [128, 6 * D]) for i in range(2)]
            for kc in range(8):
                sb = stg[kc % 2]
                P.add("sp", (lambda sb, kc: lambda e: e.dma_start(out=sb[:], in_=adaw_d[kc * 128:(kc + 1) * 128, :]))(sb, kc),
                      w=[f"ada_stg{kc % 2}"], dma=True)
                for oc in range(48):
                    P.add("pe", (lambda sb, kc, oc: lambda e: e.matmul(psum[0][:, oc:oc + 1], lhsT=sb[:, oc * 128:(oc + 1) * 128],
                                                                   rhs=prm[:, PC["cT"] + kc:PC["cT"] + kc + 1], start=True, stop=True))(sb, kc, oc),
                          r=[f"ada_stg{kc % 2}", "prm"], w=[pk(0)])
                if kc == 0:
                    P.add("dve", lambda e: e.tensor_tensor(out=mod[:], in0=psum[0][:, 0:48], in1=prm[:, PC["ada_b"]:PC["ada_b"] + 48], op=ALU.add),
                          r=[pk(0), "prm"], w=["mod"])
                else:
                    P.add("dve", lambda e: e.tensor_tensor(out=mod[:], in0=psum[0][:, 0:48], in1=mod[:], op=ALU.add),
                          r=[pk(0), "mod"], w=["mod"])
            P.barrier()
        sc = SB(top, "sc", [128, 32])
        P.add("dve", lambda e: e.scalar_tensor_tensor(out=sc[:, 0:8], in0=mod[:, 8:16], scalar=1.0, in1=prm[:, PC["n1g"]:PC["n1g"] + 8],
                                                      op0=ALU.add, op1=ALU.mult), r=["mod", "prm"], w=["sc"])
        P.add("dve", lambda e: e.scalar_tensor_tensor(out=sc[:, 8:16], in0=mod[:, 32:40], scalar=1.0, in1=prm[:, PC["n2g"]:PC["n2g"] + 8],
                                                      op0=ALU.add, op1=ALU.mult), r=["mod", "prm"], w=["sc"])
        gate_bc = SB(top, "gate_bc", [128, D])
        gl = SB(top, "gl", [128, 128])

        def make_gate(dst, dkey, gcol):
            for c in range(8):
                P.add("dve", (lambda c: lambda e: e.tensor_scalar(out=gl[:], in0=ones, scalar1=mod[:, gcol + c:gcol + c + 1], scalar2=None,
                                                                  op0=ALU.mult))(c), r=["mod", "cst"], w=["gl"])
                P.add("pe", (lambda c: lambda e: e.matmul(psum[1][:, (c % 4) * 128:(c % 4 + 1) * 128], lhsT=gl[:], rhs=ident, start=True, stop=True))(c),
                      r=["gl", "cst"], w=[pk(1)])
                P.add("act", (lambda c: lambda e: e.activation(out=dst[:, c * 128:(c + 1) * 128], in_=psum[1][:, (c % 4) * 128:(c % 4 + 1) * 128],
                                                               func=AF.Identity))(c), r=[pk(1)], w=[dkey])
        make_gate(gate_bc, "gate_bc", 16)

        with ExitStack() as ca:
            win = SB(ca, "win", [128, 8, INW], BF16)
            wout = SB(ca, "wout", [128, 8, D], BF16)
            identb = SB(ca, "identb", [128, 128], BF16)
            w2s = SB(ca, "w2s", [32, 512])
            a2s = SB(ca, "a2s", [32, 512])
            g2s = SB(ca, "g2s", [96, 512])
            wrs = SB(ca, "wrs", [128, 8, 36])
            wc = SB(ca, "wc", [128, 8, 128])
            logits = SB(ca, "logits", [128, 32, 36])
            Hst = [SB(ca, f"H{j}", [128, 64]) for j in range(4)]
            Hb_l = [SB(ca, f"Hb{j}", [128, 64], BF16) for j in range(4)]
            istb = SB(ca, "istb", [128, 64], BF16)
            carry = SB(ca, "carry", [128, 16])
            P.add("pool", lambda e: e.tensor_copy(out=identb[:], in_=ident), r=["cst"], w=["identb"])
            P.add("sp", lambda e: e.dma_start(out=w2s[:], in_=w2_d[:, :]), w=["w2s"], dma=True)
            P.add("sp", lambda e: e.dma_start(out=a2s[:], in_=a2_d[:, :]), w=["a2s"], dma=True)
            P.add("sp", lambda e: e.dma_start(out=g2s[:], in_=g2_d[:, :]), w=["g2s"], dma=True)
            P.add("sp", lambda e: e.dma_start(out=wrs[:], in_=wr_d.rearrange("(c p) n -> p c n", p=128)), w=["wrs"], dma=True)
            P.add("sp", lambda e: e.dma_start(out=wc[:], in_=wsT_d[:, :, :]), w=["wc"], dma=True)
            for h in range(8):
                P.add("pool", (lambda h: lambda e: e.tensor_tensor(out=wc[:, h, :], in0=wc[:, h, :], in1=tril, op=ALU.mult))(h),
                      r=["wc", "cst"], w=["wc"])
            P.add("pool", lambda e: e.memset(carry[:], 0.0), w=["carry"])
            zt = SB(ca, "zt", [128, D], BF16)
            P.add("pool", lambda e: e.memset(zt[:], 0.0), w=["zt"])
            xz_keys = [f"xz{b}" for b in range(NBLK)]
            for _i in range(int(os.environ.get("KPAD", "0"))):
                P.add("pool", lambda e: e.memset(gl[:, 0:1], 0.0), w=["gl_dummy"])
            for j in range(4):
                P.add("pool", (lambda j: lambda e: e.memset(Hst[j][:], 0.0))(j), w=[f"H{j}"])
                P.add("pool", (lambda j: lambda e: e.memset(Hb_l[j][:], 0.0))(j), w=[f"Hb{j}"])
            P.add("pool", lambda e: e.tensor_copy(out=istb[:], in_=istack), r=["cst"], w=["istb"])
            with ExitStack() as cw:
                wstg = [SB(cw, f"wstg{i}", [128, INW]) for i in range(2)]
                for kc in range(8):
                    sb = wstg[kc % 2]
                    P.add("sp", (lambda sb, kc: lambda e: e.dma_start(out=sb[:], in_=win_d[kc * 128:(kc + 1) * 128, :]))(sb, kc),
                          w=[f"wstg{kc % 2}"], dma=True)
                    P.add("pool", (lambda sb, kc: lambda e: e.tensor_copy(out=win[:, kc, :], in_=sb[:]))(sb, kc),
                          r=[f"wstg{kc % 2}"], w=["win"])
                for kc in range(8):
                    sb = wstg[kc % 2]
                    P.add("sp", (lambda sb, kc: lambda e: e.dma_start(out=sb[:, 0:D], in_=wout_d[kc * 128:(kc + 1) * 128, :]))(sb, kc),
                          w=[f"wstg{kc % 2}"], dma=True)
                    P.add("pool", (lambda sb, kc: lambda e: e.tensor_copy(out=wout[:, kc, :], in_=sb[:, 0:D]))(sb, kc),
                          r=[f"wstg{kc % 2}"], w=["wout"])
                P.barrier()

            ct = ExitStack()
            xt = [SB(ct, "xt0", [128, NSUB, D])] * 2
            xn = SB(ct, "xn", [128, NSUB, D], BF16)
            hT = SB(ct, "hT", [128, 8, TT], BF16)
            st6 = SB(ct, "st6", [128, 4, 6])
            mv = SB(ct, "mv", [128, 8])
            u_s = SB(ct, "u_s", [128, 4, TT], BF16)
            vg = SB(ct, "vg", [128, 512])
            vn = vg
            zraw = [SB(ct, f"zraw{i}", [128, TT + 1]) for i in range(2)]
            rkv = SB(ct, "rkv", [128, 12, TT])
            lor = SB(ct, "lor", [128, 3, TT])
            LW, ASN, BSN, KM, BON, GG = range(6)
            pers = [SB(ct, f"pers{j}", [128, 6, TT]) for j in range(4)]
            AA, KX, RN, PRD = range(4)
            ptmp_l = [SB(ct, f"ptmp{i}", [128, 4, TT]) for i in range(2)]
            ybT = SB(ct, "ybT", [128, 4, TT])
            ycat = SB(ct, "ycat", [128, 8, TT], BF16)
            x1t = SB(ct, "x1t", [128, D])
            xn2 = x1t
            h2f = SB(ct, "h2f", [128, 8, 128])
            gtmp = h2f[:, 0:4, :]
            xnb = SB(ct, "xnb", [128, D], BF16)
            CI, CE, EI, EE, EN = range(5)
            sct = [SB(ct, f"sct{j}", [128, 5, 64]) for j in range(4)]
            sb4 = [SB(ct, f"sb4{j}", [128, 4, 64], BF16) for j in range(4)]
            mats = [SB(ct, f"mats{j}", [128, 320], BF16) for j in range(4)]
            trs = [SB(ct, f"trs{j}", [128, 192], BF16) for j in range(4)]
            wzb_l = [SB(ct, f"wzb{j}", [128, 256], BF16) for j in range(4)]
            qq = [[SB(ct, f"qq{j}_{i}", [128, 64], BF16) for i in range(2)] for j in range(4)]
            chn = [SB(ct, f"chn{j}", [128, 3, 64], BF16) for j in range(4)]
            hpc_l = [SB(ct, f"hpc{j}", [128, 64]) for j in range(4)]
            gst = [SB(ct, f"gst{j}", [128, 12]) for j in range(4)]

            def A(eng, fn, r=(), w=()):
                P.add(eng, fn, r=r, w=w)

            def norm_stats(src, srck, eps):
                for hf in range(2):
                    A("dve", (lambda hf: lambda e: e.bn_stats(out=st6[:, hf, :], in_=src(hf)))(hf), r=[srck], w=["st6"])
                A("dve", lambda e: e.bn_aggr(out=mv[:, 0:2], in_=st6[:, 0:2, :].rearrange("p a b -> p (a b)")), r=["st6"], w=["mv"])
                A("dve", lambda e: e.scalar_tensor_tensor(out=mv[:, 2:3], in0=mv[:, 0:1], scalar=mv[:, 0:1], in1=mv[:, 1:2], op0=ALU.mult, op1=ALU.add),
                  r=["mv"], w=["mv"])
                A("act", lambda e: e.activation(out=mv[:, 3:4], in_=mv[:, 2:3], func=AF.Sqrt, bias=float(eps)), r=["mv"], w=["mv"])
                A("dve", lambda e: e.reciprocal(out=mv[:, 3:4], in_=mv[:, 3:4]), r=["mv"], w=["mv"])

            for TI in range(min(NT, ntiles) if stage >= 1 else 0):
                t0 = TI * TT
                xb = xt[TI % 2]
                for b in range(TI * 6, TI * 6 + 6):
                    P.add("pool", (lambda b: lambda e: e.dma_start(out=xs_d[b * 128:(b + 1) * 128, :], in_=zt[:]))(b), r=["zt"], w=[f"xz{b}"], dma=True)
                xk = "xt0"
                P.add("sp", (lambda xb, t0: lambda e: e.dma_start(out=xb[:], in_=x_d[t0:t0 + TT, :].rearrange("(s p) d -> p s d", p=128)))(xb, t0),
                      w=[xk], dma=True)
                P.section(1)
                for sub in range(NSUB):
                    norm_stats((lambda xb, sub: lambda hf: xb[:, sub, hf * 512:(hf + 1) * 512])(xb, sub), xk, 1e-6)
                    A("act", (lambda xb, sub: lambda e: e.activation(out=xn[:, sub, :], in_=xb[:, sub, :], func=AF.Identity, scale=mv[:, 3:4]))(xb, sub),
                      r=[xk, "mv"], w=["xn"])
                P.section(1.5)
                for kc in range(8):
                    pb = kc % 2
                    for sub in range(NSUB):
                        A("pe", (lambda kc, sub, pb: lambda e: e.matmul(psT[pb][:, sub * 128:(sub + 1) * 128], lhsT=xn[:, sub, kc * 128:(kc + 1) * 128],
                                                                       rhs=identb[:], start=True, stop=True))(kc, sub, pb), r=["xn", "identb"], w=[psTk[pb]])
                    if os.environ.get("DVE_EVAC"):
                      A("dve", (lambda kc, pb: lambda e: e.tensor_scalar(out=hT[:, kc, :], in0=psT[pb][:, 0:TT], scalar1=sc[:, kc:kc + 1], scalar2=mod[:, kc:kc + 1],
                                                                         op0=ALU.mult, op1=ALU.add))(kc, pb), r=[psTk[pb], "sc", "mod"], w=["hT"])
                    elif not os.environ.get("SKIP_EVAC"):
                      A("act", (lambda kc, pb: lambda e: e.activation(out=hT[:, kc, :], in_=psT[pb][:, 0:TT], func=AF.Identity,
                                                                    **({} if os.environ.get("NO_SB") else dict(scale=sc[:, kc:kc + 1], bias=mod[:, kc:kc + 1]))))(kc, pb),
                      r=[psTk[pb], "sc", "mod"], w=["hT"])

                P.section(2)
                pcnt = [0]

                def proj(c0, M):
                    pb = pcnt[0] % 2
                    pcnt[0] += 1
                    for kc in range(8):
                        A("pe", (lambda kc, pb: lambda e: e.matmul(psum[pb][0:M, 0:TT], lhsT=win[:, kc, c0:c0 + M], rhs=hT[:, kc, :],
                                                                  start=(kc == 0), stop=(kc == 7)))(kc, pb), r=["win", "hT"], w=[pk(pb)])
                    return pb

                for j in range(4):
                    pb = proj(j * 128, 128)
                    A("act", (lambda j, pb: lambda e: e.activation(out=u_s[:, j, :], in_=psum[pb][:, 0:TT], func=AF.Gelu_apprx_tanh))(j, pb),
                      r=[pk(pb)], w=["u_s"])
                specs = [(1024 + q * 128, 128, PC["mu_rkv"] + q) for q in range(12)] + \
                        [(2560, 32, PC["mu_xw"]), (2592, 32, PC["mu_xa"]), (2624, 96, PC["mu_xg"])]
                for q, (c0, M, mucol) in enumerate(specs):
                    pb = proj(c0, M)
                    zb = zraw[q % 2]
                    zk = f"zraw{q % 2}"
                    dst = rkv[0:M, q, :] if q < 12 else lor[0:M, q - 12, :]
                    dk = f"rkv{q}"
                    A("dve", (lambda zb, q, M: lambda e: e.tensor_copy(out=zb[0:M, 0:1], in_=carry[0:M, q:q + 1]))(zb, q, M), r=["carry"], w=[zk])
                    A("act", (lambda zb, pb, M: lambda e: e.activation(out=zb[0:M, 1:TT + 1], in_=psum[pb][0:M, 0:TT], func=AF.Identity))(zb, pb, M),
                      r=[pk(pb)], w=[zk])
                    A("dve", (lambda zb, q, M: lambda e: e.tensor_copy(out=carry[0:M, q:q + 1], in_=zb[0:M, TT:TT + 1]))(zb, q, M), r=[zk], w=["carry"])
                    A("dve", (lambda zb, dst, M: lambda e: e.tensor_tensor(out=dst, in0=zb[0:M, 0:TT], in1=zb[0:M, 1:TT + 1], op=ALU.subtract))(zb, dst, M),
                      r=[zk], w=[dk])
                    A("dve", (lambda zb, dst, M, mucol: lambda e: e.scalar_tensor_tensor(out=dst, in0=dst, scalar=prm[0:M, mucol:mucol + 1], in1=zb[0:M, 1:TT + 1],
                                                                                      op0=ALU.mult, op1=ALU.add))(zb, dst, M, mucol), r=[zk, dk, "prm"], w=[dk])
                A("act", lambda e: e.activation(out=lor[0:32, 0, :], in_=lor[0:32, 0, :], func=AF.Tanh), r=["rkv12"], w=["rkv12"])
                A("act", lambda e: e.activation(out=lor[0:96, 2, :], in_=lor[0:96, 2, :], func=AF.Sigmoid), r=["rkv14"], w=["rkv14"])

                P.section(3)
                def prep_body(j, A):
                    par = j % 2
                    ptmp = ptmp_l[par]
                    pa, pb = (0, 1) if par == 0 else (2, 3)
                    jc = slice(j * 128, (j + 1) * 128)
                    pj = pers[j]
                    pjk = f"pers{j}"
                    rr = rkv[:, j, :]
                    kr = rkv[:, 4 + j, :]
                    vr = rkv[:, 8 + j, :]
                    A("pe", (lambda jc: lambda e: e.matmul(psum[pa][:, 0:TT], lhsT=w2s[0:32, jc], rhs=lor[0:32, 0, :], start=True, stop=True))(jc),
                      r=["w2s", "rkv12"], w=[pk(pa)])
                    A("act", (lambda pj, j: lambda e: e.activation(out=pj[:, LW, :], in_=psum[pa][:, 0:TT], func=AF.Sigmoid, bias=pcol("w0", j)))(pj, j),
                      r=[pk(pa), "prm"], w=[pjk + "LW"])
                    A("pool", (lambda pj: lambda e: e.tensor_scalar(out=pj[:, LW, :], in0=pj[:, LW, :], scalar1=NEG_HALF_E, scalar2=None, op0=ALU.mult))(pj),
                      r=[pjk + "LW"], w=[pjk + "LW"])
                    A("pe", (lambda jc: lambda e: e.matmul(psum[pb][:, 0:TT], lhsT=a2s[0:32, jc], rhs=lor[0:32, 1, :], start=True, stop=True))(jc),
                      r=["a2s", "rkv13"], w=[pk(pb)])
                    A("act", (lambda j: lambda e: e.activation(out=ptmp[:, AA, :], in_=psum[pb][:, 0:TT], func=AF.Sigmoid, bias=pcol("a0", j)))(j),
                      r=[pk(pb), "prm"], w=["pAA" + str(par)])
                    A("pe", (lambda jc: lambda e: e.matmul(psum[pa][:, 0:TT], lhsT=g2s[0:96, jc], rhs=lor[0:96, 2, :], start=True, stop=True))(jc),
                      r=["g2s", "rkv14"], w=[pk(pa)])
                    A("act", (lambda pj: lambda e: e.activation(out=pj[:, GG, :], in_=psum[pa][:, 0:TT], func=AF.Identity))(pj), r=[pk(pa)], w=[pjk + "GG"])
                    A("dve", (lambda kr, j: lambda e: e.tensor_scalar(out=ptmp[:, KX, :], in0=kr, scalar1=pcol("k_k", j), scalar2=None, op0=ALU.mult))(kr, j),
                      r=[f"rkv{4 + j}", "prm"], w=["pKX" + str(par)])
                    A("pool", lambda e: e.tensor_tensor(out=ptmp[:, RN, :], in0=ptmp[:, KX, :], in1=ptmp[:, KX, :], op=ALU.mult), r=["pKX" + str(par)], w=["pRN" + str(par)])
                    A("pe", lambda e: e.matmul(psum[pb][:, 0:TT], lhsT=bones, rhs=ptmp[:, RN, :], start=True, stop=True), r=["cst", "pRN" + str(par)], w=[pk(pb)])
                    A("act", lambda e: e.activation(out=ptmp[:, RN, :], in_=psum[pb][:, 0:TT], func=AF.Sqrt, bias=float(1e-24)), r=[pk(pb)], w=["pRN" + str(par)])
                    A("dve", lambda e: e.reciprocal(out=ptmp[:, RN, :], in_=ptmp[:, RN, :]), r=["pRN" + str(par)], w=["pRN" + str(par)])
                    A("dve", (lambda pj: lambda e: e.scalar_tensor_tensor(out=pj[:, ASN, :], in0=ptmp[:, KX, :], scalar=-1.0, in1=ptmp[:, RN, :],
                                                                          op0=ALU.mult, op1=ALU.mult))(pj), r=["pKX" + str(par), "pRN" + str(par)], w=[pjk + "ASN"])
                    A("dve", (lambda pj: lambda e: e.scalar_tensor_tensor(out=pj[:, BSN, :], in0=pj[:, ASN, :], scalar=-1.0, in1=ptmp[:, AA, :],
                                                                          op0=ALU.mult, op1=ALU.mult))(pj), r=[pjk + "ASN", "pAA" + str(par)], w=[pjk + "BSN"])
                    A("dve", (lambda j: lambda e: e.tensor_scalar(out=ptmp[:, PRD, :], in0=ptmp[:, AA, :], scalar1=-1.0, scalar2=pcol("k_a", j),
                                                                  op0=ALU.add, op1=ALU.mult))(j), r=["pAA" + str(par), "prm"], w=["pPRD" + str(par)])
                    A("dve", (lambda pj, kr: lambda e: e.scalar_tensor_tensor(out=pj[:, KM, :], in0=ptmp[:, PRD, :], scalar=1.0, in1=kr,
                                                                              op0=ALU.add, op1=ALU.mult))(pj, kr), r=["pPRD" + str(par), f"rkv{4 + j}"], w=[pjk + "KM"])
                    A("dve", (lambda pj, rr, j: lambda e: e.scalar_tensor_tensor(out=ptmp[:, PRD, :], in0=rr, scalar=pcol("r_k", j), in1=pj[:, KM, :],
                                                                                 op0=ALU.mult, op1=ALU.mult))(pj, rr, j), r=[f"rkv{j}", "prm", pjk + "KM"], w=["pPRD" + str(par)])
                    A("pe", lambda e: e.matmul(psum[pa][:, 0:TT], lhsT=bones, rhs=ptmp[:, PRD, :], start=True, stop=True), r=["cst", "pPRD" + str(par)], w=[pk(pa)])
                    A("dve", (lambda pj, vr: lambda e: e.tensor_tensor(out=pj[:, BON, :], in0=psum[pa][:, 0:TT], in1=vr, op=ALU.mult))(pj, vr),
                      r=[pk(pa), f"rkv{8 + j}"], w=[pjk + "BON"])


                prep_ops = []
                for j in range(4):
                    rec = []
                    prep_body(j, lambda eng, fn, r=(), w=(): rec.append((eng, fn, r, w)))
                    prep_ops.append(rec)
                for grp in ((0, 1), (2, 3)):
                    for si in range(len(prep_ops[0])):
                        for j in grp:
                            eng_, fn_, r_, w_ = prep_ops[j][si]
                            A(eng_, fn_, r=r_, w=w_)
                P.section(4)
                def unit_stages(j, c):
                    col = slice(c * CH, (c + 1) * CH)
                    pj = pers[j]
                    pjk = f"pers{j}"
                    s = sct[j]
                    sk = f"sct{j}"
                    mt = mats[j]
                    mk = f"mats{j}"
                    tr = trs[j]
                    tk = f"trs{j}"
                    cn = chn[j]
                    ck = f"chn{j}"
                    H = Hst[j]
                    Hk = f"H{j}"
                    Hb = Hb_l[j]
                    Hbk = f"Hb{j}"
                    b4 = sb4[j]
                    hpc = hpc_l[j]
                    rr = rkv[:, j, col]
                    vr = rkv[:, 8 + j, col]
                    hp = [slice(0, 64), slice(64, 128)]
                    st = []

                    def s1():
                        A("dve", lambda e: e.tensor_tensor_scan(out=s[:, CI, :], data0=ones[:, 0:64], data1=pj[:, LW, col], initial=0.0, op0=ALU.mult, op1=ALU.add),
                          r=[pjk + "LW", "cst"], w=[sk + "ci"])
                        A("dve", lambda e: e.tensor_tensor(out=s[:, CE, :], in0=s[:, CI, :], in1=pj[:, LW, col], op=ALU.subtract),
                          r=[sk + "ci", pjk + "LW"], w=[sk + "ce"])
                    st.append(s1)

                    def s2():
                        A("act", lambda e: e.activation(out=s[:, EI, :], in_=s[:, CI, :], func=AF.Exp), r=[sk + "ci"], w=[sk + "ei"])
                        A("act", lambda e: e.activation(out=s[:, EE, :], in_=s[:, CE, :], func=AF.Exp), r=[sk + "ce"], w=[sk + "ee"])
                        A("act", lambda e: e.activation(out=s[:, EN, :], in_=s[:, CI, :], func=AF.Exp, scale=-1.0), r=[sk + "ci"], w=[sk + "en"])
                    st.append(s2)

                    def s3():
                        A("dve", lambda e: e.tensor_tensor(out=b4[:, 0, :], in0=pj[:, ASN, col], in1=s[:, EE, :], op=ALU.mult), r=[pjk + "ASN", sk + "ee"], w=[sk + "at"])
                        A("dve", lambda e: e.tensor_tensor(out=b4[:, 1, :], in0=rr, in1=s[:, EI, :], op=ALU.mult), r=[f"rkv{j}", sk + "ei"], w=[sk + "rt"])
                        A("dve", lambda e: e.tensor_tensor(out=b4[:, 2, :], in0=pj[:, BSN, col], in1=s[:, EN, :], op=ALU.mult), r=[pjk + "BSN", sk + "en"], w=[sk + "bt"])
                        A("dve", lambda e: e.tensor_tensor(out=b4[:, 3, :], in0=pj[:, KM, col], in1=s[:, EN, :], op=ALU.mult), r=[pjk + "KM", sk + "en"], w=[sk + "kt"])
                    st.append(s3)

                    def s4():
                        for p in hp:
                            A("pe", (lambda p: lambda e: e.matmul(psum[2][p, 0:128], lhsT=b4[p, 2, :], rhs=b4[p, 0:2, :].rearrange("p a b -> p (a b)"), start=True, stop=True))(p),
                              r=[sk + "bt", sk + "at", sk + "rt"], w=["ps2"])
                            A("pe", (lambda p: lambda e: e.matmul(psum[2][p, 128:256], lhsT=b4[p, 3, :], rhs=b4[p, 0:2, :].rearrange("p a b -> p (a b)"), start=True, stop=True))(p),
                              r=[sk + "kt", sk + "at", sk + "rt"], w=["ps2"])
                            A("pe", (lambda p: lambda e: e.matmul(psum[2][p, 256:320], lhsT=b4[p, 0, :], rhs=b4[p, 2, :], start=True, stop=True))(p),
                              r=[sk + "bt", sk + "at"], w=["ps2"])
                        A("dve", lambda e: e.tensor_tensor(out=mt[:], in0=psum[2][:, 0:320], in1=m5, op=ALU.mult), r=["ps2", "cst"], w=[mk])
                        for p in hp:
                            A("pe", (lambda p: lambda e: e.matmul(psum[3][p, 0:64], lhsT=rkv[p, 8 + j, col], rhs=istack[p, :], start=True, stop=True))(p),
                              r=[f"rkv{8 + j}", "cst"], w=["ps3"])
                            A("pe", (lambda p: lambda e: e.matmul(psum[3][p, 64:128], lhsT=b4[p, 2, :], rhs=istb[p, :], start=True, stop=True))(p),
                              r=[sk + "bt", "istb"], w=["ps3"])
                            A("pe", (lambda p: lambda e: e.matmul(psum[3][p, 128:192], lhsT=b4[p, 3, :], rhs=istb[p, :], start=True, stop=True))(p),
                              r=[sk + "kt", "istb"], w=["ps3"])
                        A("act", lambda e: e.activation(out=tr[:], in_=psum[3][:, 0:192], func=AF.Identity), r=["ps3"], w=[tk])
                        A("dve", lambda e: e.tensor_tensor(out=qq[j][0][:], in0=mt[:, 0:64], in1=istack, op=ALU.add), r=[mk, "cst"], w=[f"qq{j}_0"])
                    st.append(s4)

                    wzb = wzb_l[j]
                    wbk = f"wzb{j}"
                    bm3 = bones.rearrange("p (a b) -> p a b", a=2)

                    def s4b():
                        A("dve", lambda e: e.tensor_tensor(out=wzb[:, 0:128].rearrange("p (a b) -> p a b", a=2),
                                                            in0=mt[:, 0:64].rearrange("p (o t) -> p o t", o=1).to_broadcast([128, 2, 64]), in1=bm3, op=ALU.mult),
                          r=[mk, "cst"], w=[wbk])
                        A("dve", lambda e: e.tensor_tensor(out=wzb[:, 128:256].rearrange("p (a b) -> p a b", a=2),
                                                            in0=mt[:, 256:320].rearrange("p (o t) -> p o t", o=1).to_broadcast([128, 2, 64]), in1=bm3, op=ALU.mult),
                          r=[mk, "cst"], w=[wbk])
                    st.append(s4b)

                    for i in range(5):
                        def lv(i=i):
                            last = (i == 4)
                            qc = qq[j][i % 2]
                            qck = f"qq{j}_{i % 2}"
                            qn = qq[j][(i + 1) % 2]
                            qnk = f"qq{j}_{(i + 1) % 2}"
                            if not last:
                                A("pe", lambda e: e.matmul(psum[4][:, 0:128], lhsT=wzb[:, 128:256], rhs=wzb[:, 0:128], start=True, stop=True), r=[wbk], w=["ps4"])
                            A("pe", lambda e: e.matmul(psum[4][:, 128:256], lhsT=wzb[:, 0:128], rhs=wzb[:, 128:256], start=True, stop=True), r=[wbk], w=["ps4"])
                            if not last:
                                A("act", lambda e: e.activation(out=wzb[:, 0:256], in_=psum[4][:, 0:256], func=AF.Identity), r=["ps4"], w=[wbk])
                            else:
                                A("act", lambda e: e.activation(out=wzb[:, 128:256], in_=psum[4][:, 128:256], func=AF.Identity), r=["ps4"], w=[wbk])
                            A("pe", lambda e: e.matmul(psum[1][:, 128:192], lhsT=wzb[:, 128:256], rhs=qc[:, :], start=True, stop=True), r=[wbk, qck], w=["ps1"])
                            A("dve", lambda e: e.tensor_tensor(out=qn[:], in0=psum[1][:, 128:192], in1=qc[:], op=ALU.add), r=["ps1", qck], w=[qnk])
                        st.append(lv)

                    cbank = [5, 6, 7, 0][j]
                    cps = psum[cbank] if cbank != 7 else psTt
                    cpk = f"ps{cbank}"
                    q5 = qq[j][1]
                    q5k = f"qq{j}_1"
                    g = gst[j]
                    gk = f"gst{j}"

                    def s5a():
                        A("dve", lambda e: e.tensor_scalar(out=hpc[:], in0=H[:], scalar1=s[:, EI, 63:64], scalar2=None, op0=ALU.mult),
                          r=[Hk, sk + "ei"], w=[ck + "hpc"])
                        for p in hp:
                            A("pe", (lambda p: lambda e: e.matmul(cps[p, 0:64], lhsT=b4[p, 0, :], rhs=Hb[p, :], start=True, stop=False))(p), r=[sk + "at", Hbk], w=[cpk])
                            A("pe", (lambda p: lambda e: e.matmul(cps[p, 0:64], lhsT=mt[p, 128:192], rhs=tr[p, 0:64], start=False, stop=True))(p), r=[mk, tk], w=[cpk])
                        A("act", lambda e: e.activation(out=cn[:, 0, :], in_=cps[:, 0:64], func=AF.Identity), r=[cpk], w=[ck + "x"])
                    st.append(s5a)

                    def s5b():
                        for p in hp:
                            A("pe", (lambda p: lambda e: e.matmul(cps[p, 64:128], lhsT=q5[p, :], rhs=cn[p, 0, :], start=True, stop=True))(p), r=[q5k, ck + "x"], w=[cpk])
                        A("act", lambda e: e.activation(out=cn[:, 1, :], in_=cps[:, 64:128], func=AF.Identity), r=[cpk], w=[ck + "u"])
                    st.append(s5b)

                    def s5c():
                        for p in hp:
                            A("pe", (lambda p: lambda e: e.matmul(cps[p, 192:256], lhsT=tr[p, 64:128], rhs=cn[p, 1, :], start=True, stop=False))(p), r=[tk, ck + "u"], w=[cpk])
                            A("pe", (lambda p: lambda e: e.matmul(cps[p, 192:256], lhsT=tr[p, 128:192], rhs=tr[p, 0:64], start=False, stop=True))(p), r=[tk], w=[cpk])
                        for p in hp:
                            A("pe", (lambda p: lambda e: e.matmul(cps[p, 128:192], lhsT=b4[p, 1, :], rhs=Hb[p, :], start=True, stop=False))(p), r=[sk + "rt", Hbk], w=[cpk])
                            A("pe", (lambda p: lambda e: e.matmul(cps[p, 128:192], lhsT=mt[p, 64:128], rhs=cn[p, 1, :], start=False, stop=False))(p), r=[mk, ck + "u"], w=[cpk])
                            A("pe", (lambda p: lambda e: e.matmul(cps[p, 128:192], lhsT=mt[p, 192:256], rhs=tr[p, 0:64], start=False, stop=True))(p), r=[mk, tk], w=[cpk])
                        A("dve", lambda e: e.scalar_tensor_tensor(out=Hb[:], in0=cps[:, 192:256], scalar=s[:, EI, 63:64], in1=hpc[:], op0=ALU.mult, op1=ALU.add),
                          r=[cpk, sk + "ei", ck + "hpc"], w=[Hbk])
                        A("dve", lambda e: e.scalar_tensor_tensor(out=H[:], in0=cps[:, 192:256], scalar=s[:, EI, 63:64], in1=hpc[:], op0=ALU.mult, op1=ALU.add),
                          r=[cpk, sk + "ei", ck + "hpc"], w=[Hk])
                    st.append(s5c)

                    def s5d():
                        A("dve", lambda e: e.bn_stats(out=g[:, 0:6], in_=cps[:, 128:192]), r=[cpk], w=[gk])
                        A("dve", lambda e: e.bn_aggr(out=g[:, 6:8], in_=g[:, 0:6]), r=[gk], w=[gk])
                        A("act", lambda e: e.activation(out=g[:, 8:9], in_=g[:, 7:8], func=AF.Sqrt, bias=float(64e-5)), r=[gk], w=[gk])
                        A("dve", lambda e: e.reciprocal(out=g[:, 8:9], in_=g[:, 8:9]), r=[gk], w=[gk])
                        A("dve", lambda e: e.tensor_scalar(out=cn[:, 2, :], in0=cps[:, 128:192], scalar1=g[:, 6:7], scalar2=g[:, 8:9], op0=ALU.subtract, op1=ALU.mult),
                          r=[cpk, gk], w=[ck + "yn"])
                    st.append(s5d)

                    def s5e():
                        for p in hp:
                            A("pe", (lambda p: lambda e: e.matmul(psum[3][p, 192:256], lhsT=cn[p, 2, :], rhs=istb[p, :], start=True, stop=True))(p), r=[ck + "yn", "istb"], w=["ps3"])
                        A("act", lambda e: e.activation(out=ybT[:, j, col], in_=psum[3][:, 192:256], func=AF.Identity, scale=pcol("gn_w", j), bias=pcol("gn_b", j)),
                          r=["ps3", "prm"], w=[f"ybT{j}"])
                    st.append(s5e)
                    return st

                for c in range(NCH):
                    stl = [unit_stages(j, c) for j in range(4)]
                    for si in range(len(stl[0])):
                        for j in range(4):
                            stl[j][si]()
                P.section(5)
                for j in range(4):
                    pj = pers[j]
                    pjk = f"pers{j}"
                    A("pool", (lambda pj, j: lambda e: e.tensor_tensor(out=ybT[:, j, :], in0=ybT[:, j, :], in1=pj[:, BON, :], op=ALU.add))(pj, j),
                      r=[f"ybT{j}", pjk + "BON"], w=[f"ybT{j}"])
                    A("pool", (lambda pj, j: lambda e: e.tensor_tensor(out=ycat[:, 4 + j, :], in0=ybT[:, j, :], in1=pj[:, GG, :], op=ALU.mult))(pj, j),
                      r=[f"ybT{j}", pjk + "GG"], w=["ycat"])

                P.section(6)
                for sub in range(NSUB):
                    tc_ = slice(sub * 128, (sub + 1) * 128)
                    for kc in range(8):
                        A("pe", (lambda kc, tc_: lambda e: e.matmul(psum[0][:, 0:512], lhsT=hT[:, kc, tc_], rhs=win[:, kc, 512:1024], start=(kc == 0), stop=(kc == 7)))(kc, tc_),
                          r=["hT", "win"], w=[pk(0)])
                    A("act", lambda e: e.activation(out=vg[:], in_=psum[0][:, 0:512], func=AF.Gelu_apprx_tanh), r=[pk(0)], w=["vg"])
                    A("dve", lambda e: e.bn_stats(out=st6[:, 2, :], in_=vg[:]), r=["vg"], w=["st6b"])
                    A("dve", lambda e: e.bn_aggr(out=mv[:, 4:6], in_=st6[:, 2, :]), r=["st6b"], w=["mvb"])
                    A("act", lambda e: e.activation(out=mv[:, 6:7], in_=mv[:, 5:6], func=AF.Sqrt, bias=float(1e-5)), r=["mvb"], w=["mvb"])
                    A("dve", lambda e: e.reciprocal(out=mv[:, 6:7], in_=mv[:, 6:7]), r=["mvb"], w=["mvb"])
                    A("dve", lambda e: e.tensor_scalar(out=vn[:], in0=vg[:], scalar1=mv[:, 4:5], scalar2=mv[:, 6:7], op0=ALU.subtract, op1=ALU.mult), r=["vg", "mvb"], w=["vg"])
                    A("pool", lambda e: e.tensor_tensor(out=vn[:], in0=vn[:], in1=bcs[:, BC["lnw"]:BC["lnw"] + 512], op=ALU.mult), r=["vg", "bc"], w=["vg"])
                    A("pool", lambda e: e.tensor_tensor(out=vn[:], in0=vn[:], in1=bcs[:, BC["lnb"]:BC["lnb"] + 512], op=ALU.add), r=["vg", "bc"], w=["vg"])
                    for h in range(8):
                        A("pe", (lambda h: lambda e: e.matmul(psum[1][(h % 2) * 64:(h % 2) * 64 + 64, (h // 2) * 128:(h // 2 + 1) * 128], lhsT=vn[:, h * 64:(h + 1) * 64],
                                                              rhs=wc[:, h, :], start=True, stop=True))(h), r=["vg", "wc"], w=[pk(1)])
                    A("dve", lambda e: e.tensor_tensor(out=gtmp.rearrange("p a b -> p (a b)"), in0=psum[1][:, 0:512], in1=bcs[:, BC["bsT"]:BC["bsT"] + 512], op=ALU.add),
                      r=[pk(1), "bc"], w=["h2f"])
                    A("dve", (lambda tc_: lambda e: e.tensor_tensor(out=ycat[:, 0:4, tc_], in0=gtmp, in1=u_s[:, :, tc_], op=ALU.mult))(tc_), r=["h2f", "u_s"], w=["ycat"])

                P.section(7)
                for sub in range(NSUB):
                    tc_ = slice(sub * 128, (sub + 1) * 128)
                    ti = TI * NSUB + sub
                    tok0 = t0 + sub * 128
                    for hf in range(2):
                        for cc in range(8):
                            A("pe", (lambda cc, hf, tc_: lambda e: e.matmul(psum[6][:, 0:512], lhsT=ycat[:, cc, tc_], rhs=wout[:, cc, hf * 512:(hf + 1) * 512],
                                                                           start=(cc == 0), stop=(cc == 7)))(cc, hf, tc_), r=["ycat", "wout"], w=[pk(6)])
                        A("dve", (lambda hf: lambda e: e.tensor_tensor(out=x1t[:, hf * 512:(hf + 1) * 512], in0=psum[6][:, 0:512], in1=gate_bc[:, hf * 512:(hf + 1) * 512],
                                                                      op=ALU.mult))(hf), r=[pk(6), "gate_bc"], w=["x1t"])
                        A("pool", (lambda hf, xb, sub: lambda e: e.tensor_tensor(out=x1t[:, hf * 512:(hf + 1) * 512], in0=x1t[:, hf * 512:(hf + 1) * 512],
                                                                                in1=xb[:, sub, hf * 512:(hf + 1) * 512], op=ALU.add))(hf, xb, sub), r=["x1t", xk], w=["x1t"])
                    P.add("sp", (lambda tok0: lambda e: e.dma_start(out=x1_d[tok0:tok0 + 128, :], in_=x1t[:]))(tok0), r=["x1t"], w=["x1_d"], dma=True)
                    norm_stats(lambda hf: x1t[:, hf * 512:(hf + 1) * 512], "x1t", 1e-6)
                    A("act", lambda e: e.activation(out=xn2[:], in_=x1t[:], func=AF.Identity, scale=mv[:, 3:4]), r=["x1t", "mv"], w=["x1t"])
                    for kc in range(8):
                        pb = kc // 4
                        A("pe", (lambda kc, pb: lambda e: e.matmul(psum[pb][:, (kc % 4) * 128:(kc % 4 + 1) * 128], lhsT=xn2[:, kc * 128:(kc + 1) * 128], rhs=ident, start=True, stop=True))(kc, pb),
                          r=["x1t", "cst"], w=[pk(pb)])
                    for kc in range(8):
                        pb = kc // 4
                        A("act", (lambda kc, pb: lambda e: e.activation(out=h2f[:, kc, :], in_=psum[pb][:, (kc % 4) * 128:(kc % 4 + 1) * 128], func=AF.Identity,
                                                                        scale=sc[:, 8 + kc:9 + kc], bias=mod[:, 24 + kc:25 + kc]))(kc, pb), r=[pk(pb), "sc", "mod"], w=["h2f"])
                    A("pool", lambda e: e.tensor_copy(out=xnb[:], in_=xn2[:]), r=["x1t"], w=["xnb"])
                    P.add("sp", (lambda tok0: lambda e: e.dma_start(out=xn2_d[tok0:tok0 + 128, :], in_=xnb[:]))(tok0),
                          r=["xnb"], w=["xn2_d"], dma=True)
                    for kc in range(8):
                        A("pe", (lambda kc: lambda e: e.matmul(psum[6][:, 0:36], lhsT=h2f[:, kc, :], rhs=wrs[:, kc, :], start=(kc == 0), stop=(kc == 7)))(kc),
                          r=["h2f", "wrs"], w=[pk(6)])
                    A("dve", (lambda ti: lambda e: e.tensor_tensor(out=logits[:, ti, :], in0=psum[6][:, 0:36], in1=bcs[:, BC["rb"]:BC["rb"] + 36], op=ALU.add))(ti),
                      r=[pk(6), "bc"], w=["logits"])

            P.muted = False
            P.barrier()
            ct.close()
            if stage >= 2:
                rt = SB(ca, "rt", [128, 32, 48])
                sel = SB(ca, "sel", [128, 32, 8])
                sel2 = SB(ca, "sel2", [128, 32, 8])
                ohg = SB(ca, "ohg", [128, 32, 4])
                tmp48 = SB(ca, "tmp48", [128, 32, 4, 8])
                lg = logits[:, :, 0:4]
                le = logits[:, :, 4:36].rearrange("p t (g e) -> p t g e", g=4)
                MG, SG, M1, M2, P1, W1, W2 = range(7)
                r1 = lambda i: rt[:, :, i:i + 1]
                R = lambda fn, r, w: A("dve", fn, r=r, w=w)
                R(lambda e: e.tensor_reduce(out=rt[:, :, MG], in_=lg, axis=AX.X, op=ALU.max), ["logits"], ["rt"])
                R(lambda e: e.tensor_tensor(out=ohg[:], in0=lg, in1=r1(MG).to_broadcast([128, 32, 4]), op=ALU.is_equal), ["logits", "rt"], ["ohg"])
                R(lambda e: e.tensor_tensor(out=rt[:, :, 8:12], in0=lg, in1=r1(MG).to_broadcast([128, 32, 4]), op=ALU.subtract), ["logits", "rt"], ["rt"])
                A("act", lambda e: e.activation(out=rt[:, :, 8:12], in_=rt[:, :, 8:12], func=AF.Exp), r=["rt"], w=["rt"])
                R(lambda e: e.tensor_reduce(out=rt[:, :, SG], in_=rt[:, :, 8:12], axis=AX.X, op=ALU.add), ["rt"], ["rt"])
                R(lambda e: e.reciprocal(out=rt[:, :, SG], in_=rt[:, :, SG]), ["rt"], ["rt"])
                R(lambda e: e.tensor_tensor(out=tmp48[:], in0=le, in1=ohg[:].rearrange("p t (g o) -> p t g o", o=1).to_broadcast([128, 32, 4, 8]), op=ALU.mult),
                  ["logits", "ohg"], ["tmp48"])
                R(lambda e: e.tensor_reduce(out=sel[:], in_=tmp48[:].rearrange("p t g e -> p t e g"), axis=AX.X, op=ALU.add), ["tmp48"], ["sel"])
                R(lambda e: e.tensor_reduce(out=rt[:, :, M1], in_=sel[:], axis=AX.X, op=ALU.max), ["sel"], ["rt"])
                R(lambda e: e.tensor_tensor(out=sel2[:], in0=sel[:], in1=r1(M1).to_broadcast([128, 32, 8]), op=ALU.is_equal), ["sel", "rt"], ["sel2"])
                R(lambda e: e.scalar_tensor_tensor(out=tmp48[:, :, 0, :], in0=sel2[:], scalar=-1e30, in1=sel[:], op0=ALU.mult, op1=ALU.add), ["sel", "sel2"], ["tmp48"])
                R(lambda e: e.tensor_reduce(out=rt[:, :, M2], in_=tmp48[:, :, 0, :], axis=AX.X, op=ALU.max), ["tmp48"], ["rt"])
                R(lambda e: e.tensor_tensor(out=tmp48[:, :, 1, :], in0=tmp48[:, :, 0, :], in1=r1(M2).to_broadcast([128, 32, 8]), op=ALU.is_equal), ["tmp48", "rt"], ["tmp48"])
                R(lambda e: e.tensor_tensor(out=rt[:, :, P1], in0=rt[:, :, M2], in1=rt[:, :, M1], op=ALU.subtract), ["rt"], ["rt"])
                A("act", lambda e: e.activation(out=rt[:, :, P1], in_=rt[:, :, P1], func=AF.Exp), r=["rt"], w=["rt"])
                R(lambda e: e.tensor_scalar(out=rt[:, :, P1], in0=rt[:, :, P1], scalar1=1.0, scalar2=None, op0=ALU.add), ["rt"], ["rt"])
                R(lambda e: e.reciprocal(out=rt[:, :, P1], in_=rt[:, :, P1]), ["rt"], ["rt"])
                R(lambda e: e.tensor_tensor(out=rt[:, :, W1], in0=rt[:, :, P1], in1=rt[:, :, SG], op=ALU.mult), ["rt"], ["rt"])
                R(lambda e: e.tensor_tensor(out=rt[:, :, W2], in0=rt[:, :, SG], in1=rt[:, :, W1], op=ALU.subtract), ["rt"], ["rt"])
                R(lambda e: e.tensor_tensor(out=sel[:], in0=sel2[:], in1=r1(W1).to_broadcast([128, 32, 8]), op=ALU.mult), ["sel2", "rt"], ["sel"])
                R(lambda e: e.tensor_tensor(out=sel2[:], in0=tmp48[:, :, 1, :], in1=r1(W2).to_broadcast([128, 32, 8]), op=ALU.mult), ["tmp48", "rt"], ["sel2"])
                R(lambda e: e.tensor_tensor(out=sel[:], in0=sel[:], in1=sel2[:], op=ALU.add), ["sel", "sel2"], ["sel"])
                for g in range(4):
                    R((lambda g: lambda e: e.tensor_tensor(out=wt[:, :, g * 8:(g + 1) * 8], in0=sel[:], in1=ohg[:, :, g:g + 1].to_broadcast([128, 32, 8]), op=ALU.mult))(g),
                      ["sel", "ohg"], ["wt"])
            if stage >= 2:
                Mf = SB(ca, "Mf", [128, 1024])
                Mb = SB(ca, "Mb", [128, 1024], BF16)
                Lsb = SB(ca, "Lsb", [128, 128], BF16)
                onesb = SB(ca, "onesb", [128, 128], BF16)
                PRE = SB(ca, "PRE", [128, 1024])
                CNTs = SB(ca, "CNTs", [128, 1024])
                CA_ = SB(ca, "CA", [128, 1024])
                CB_ = SB(ca, "CB", [128, 1024])
                sm = SB(ca, "sm", [128, 8, 32])
                cmpb = SB(ca, "cmpb", [128, 96, 32])
                bev = SB(ca, "bev", [128, 6, 96])
                pabf = SB(ca, "pabf", [128, 2, 32])
                v3 = lambda t: t[:].rearrange("p (a b) -> p a b", a=32)
                wtf = wt[:].rearrange("p t e -> p (t e)")
                R(lambda e: e.tensor_single_scalar(out=Mf[:], in_=wtf, scalar=0.0, op=ALU.is_gt), ["wt"], ["Mf"])
                A("pool", lambda e: e.tensor_copy(out=Mb[:], in_=Mf[:]), r=["Mf"], w=["Mb"])
                A("pool", lambda e: e.tensor_tensor(out=Lsb[:], in0=tril, in1=ident, op=ALU.subtract), r=["cst"], w=["Lsb"])
                A("pool", lambda e: e.tensor_copy(out=onesb[:], in_=ones), r=["cst"], w=["onesb"])
                for h in range(2):
                    A("pe", (lambda h: lambda e: e.matmul(psum[h][:, 0:512], lhsT=Lsb[:], rhs=Mb[:, h * 512:(h + 1) * 512], start=True, stop=True))(h),
                      r=["Lsb", "Mb"], w=[pk(h)])
                    A("pe", (lambda h: lambda e: e.matmul(psum[2 + h][:, 0:512], lhsT=onesb[:], rhs=Mb[:, h * 512:(h + 1) * 512], start=True, stop=True))(h),
                      r=["onesb", "Mb"], w=[pk(2 + h)])
                    A("act", (lambda h: lambda e: e.activation(out=PRE[:, h * 512:(h + 1) * 512], in_=psum[h][:, 0:512], func=AF.Identity))(h), r=[pk(h)], w=["PRE"])
                    A("act", (lambda h: lambda e: e.activation(out=CNTs[:, h * 512:(h + 1) * 512], in_=psum[2 + h][:, 0:512], func=AF.Identity))(h), r=[pk(2 + h)], w=["CNTs"])
                src, srck, dst, dstk = CNTs, "CNTs", CA_, "CA"
                for dd in (1, 2, 4, 8, 16):
                    w_ = dd * 32
                    R((lambda src, dst, w_: lambda e: e.tensor_copy(out=dst[:, 0:w_], in_=src[:, 0:w_]))(src, dst, w_), [srck], [dstk])
                    R((lambda src, dst, w_: lambda e: e.tensor_tensor(out=dst[:, w_:1024], in0=src[:, w_:1024], in1=src[:, 0:1024 - w_], op=ALU.add))(src, dst, w_), [srck], [dstk])
                    src, srck = dst, dstk
                    dst, dstk = (CB_, "CB") if dst is CA_ else (CA_, "CA")
                R(lambda e: e.tensor_tensor(out=CB_[:], in0=CA_[:], in1=CNTs[:], op=ALU.subtract), ["CA", "CNTs"], ["CB"])
                R(lambda e: e.tensor_copy(out=sm[:, 0, :], in_=CA_[:, 31 * 32:32 * 32]), ["CA"], ["sm"])
                R(lambda e: e.tensor_tensor(out=cmpb[:, 0:32, :], in0=sm[:, 0, :].rearrange("p (e o) -> p e o", o=1).to_broadcast([128, 32, 32]),
                                            in1=cst[:, CC["thr"]:CC["thr"] + 32].rearrange("p (o k) -> p o k", o=1).to_broadcast([128, 32, 32]), op=ALU.is_gt),
                  ["sm", "cst"], ["cmpb"])
                R(lambda e: e.tensor_reduce(out=sm[:, 1, :], in_=cmpb[:, 0:32, :], axis=AX.X, op=ALU.add), ["cmpb"], ["sm"])
                R(lambda e: e.tensor_tensor_scan(out=sm[:, 2, :], data0=ones[:, 0:32], data1=sm[:, 1, :], initial=0.0, op0=ALU.mult, op1=ALU.add), ["sm", "cst"], ["sm"])
                R(lambda e: e.tensor_tensor(out=sm[:, 3, :], in0=sm[:, 2, :], in1=sm[:, 1, :], op=ALU.subtract), ["sm"], ["sm"])
                R(lambda e: e.tensor_single_scalar(out=sm[:, 4, :], in_=sm[:, 3, :], scalar=128.0, op=ALU.mult), ["sm"], ["sm"])
                R(lambda e: e.tensor_tensor(out=v3(CB_), in0=v3(CB_), in1=sm[:, 4, :].rearrange("p (o e) -> p o e", o=1).to_broadcast([128, 32, 32]), op=ALU.add),
                  ["CB", "sm"], ["CB"])
                R(lambda e: e.tensor_tensor(out=PRE[:], in0=PRE[:], in1=CB_[:], op=ALU.add), ["PRE", "CB"], ["PRE"])
                R(lambda e: e.tensor_tensor(out=CA_[:], in0=PRE[:], in1=Mf[:], op=ALU.mult), ["PRE", "Mf", "sm"], ["CA"])
                R(lambda e: e.tensor_scalar(out=CB_[:], in0=Mf[:], scalar1=-1e9, scalar2=1e9, op0=ALU.mult, op1=ALU.add), ["Mf", "PRE"], ["CB"])
                R(lambda e: e.tensor_tensor(out=CB_[:], in0=CB_[:], in1=CA_[:], op=ALU.add), ["CB", "CA"], ["CB"])
                R(lambda e: e.tensor_reduce(out=pabf[:, 0, :], in_=v3(CB_), axis=AX.X, op=ALU.min), ["CB"], ["pabf"])
                R(lambda e: e.tensor_reduce(out=pabf[:, 1, :], in_=v3(CA_), axis=AX.X, op=ALU.max), ["CA"], ["pabf"])
                R(lambda e: e.tensor_tensor(out=v3(CB_), in0=v3(CB_), in1=pabf[:, 0, :].rearrange("p (t o) -> p t o", o=1).to_broadcast([128, 32, 32]), op=ALU.is_equal),
                  ["CB", "pabf"], ["CB"])
                R(lambda e: e.tensor_tensor(out=CB_[:], in0=CB_[:], in1=wtf, op=ALU.mult), ["CB", "wt"], ["CB"])
                R(lambda e: e.tensor_reduce(out=WAB[:, 0, :], in_=v3(CB_), axis=AX.X, op=ALU.add), ["CB"], ["WAB"])
                R(lambda e: e.tensor_tensor(out=WAB[:, 1, :], in0=rt[:, :, SG], in1=WAB[:, 0, :], op=ALU.subtract), ["rt", "WAB"], ["WAB"])
                R(lambda e: e.tensor_copy(out=PABi[:], in_=pabf[:]), ["pabf"], ["PABi"])
                R(lambda e: e.tensor_tensor(out=cmpb[:], in0=sm[:, 2, :].rearrange("p (o e) -> p o e", o=1).to_broadcast([128, 96, 32]),
                                            in1=cst[:, CC["iotab"]:CC["iotab"] + 96].rearrange("p (b o) -> p b o", o=1).to_broadcast([128, 96, 32]), op=ALU.is_le),
                  ["sm", "cst", "cmpb"], ["cmpb"])
                R(lambda e: e.tensor_reduce(out=bev[:, 0, :], in_=cmpb[:], axis=AX.X, op=ALU.add), ["cmpb"], ["bev"])
                R(lambda e: e.memset(bev[:, 1, 0:1], 1.0), [], ["bev"])
                R(lambda e: e.tensor_tensor(out=bev[:, 1, 1:96], in0=bev[:, 0, 1:96], in1=bev[:, 0, 0:95], op=ALU.not_equal), ["bev"], ["bev"])
                R(lambda e: e.tensor_scalar(out=bev[:, 2, :], in0=bev[:, 1, :], scalar1=-1e6, scalar2=1e6, op0=ALU.mult, op1=ALU.add), ["bev"], ["bev"])
                R(lambda e: e.scalar_tensor_tensor(out=bev[:, 2, :], in0=bev[:, 0, :], scalar=128.0, in1=bev[:, 2, :], op0=ALU.mult, op1=ALU.add), ["bev"], ["bev"])
                R(lambda e: e.tensor_scalar(out=bev[:, 2, :], in0=bev[:, 2, :], scalar1=cst[:, CC["pidx"]:CC["pidx"] + 1], scalar2=None, op0=ALU.add), ["bev", "cst"], ["bev"])
                R(lambda e: e.tensor_copy(out=IDXW[:, 0:1, :], in_=bev[:, 2:3, :]), ["bev"], ["IDXW"])
            P.barrier()

        if stage >= 3:
            with ExitStack() as cb:
                IOA = bass.IndirectOffsetOnAxis
                NROW = NE * 128
                _bc = {}

                def bc_reg(e):
                    if "r" not in _bc:
                        _bc["r"] = e.to_reg(NROW - 1)
                    return _bc["r"]
                W32 = SB(cb, "W32", [128, 12288])
                idb2 = SB(cb, "idb2", [128, 128], BF16)
                xload = [SB(cb, f"xl{i}", [128, D], BF16) for i in range(2)]
                xsb = [SB(cb, f"xsb{i}", [128, D], BF16) for i in range(2)]
                XgT_l = [SB(cb, f"XgT{i}", [128, 8, 128], BF16) for i in range(2)]
                Wgb = SB(cb, "Wgb", [128, 8, EH], BF16)
                Wub = SB(cb, "Wub", [128, 8, EH], BF16)
                Wdb = SB(cb, "Wdb", [128, 4, D], BF16)
                sg_l = [SB(cb, f"sg{i}", [128, 512]) for i in range(2)]
                actb_l = [SB(cb, f"actb{i}", [128, 512], BF16) for i in range(2)]
                actT_l = [SB(cb, f"actT{i}", [128, 4, 128], BF16) for i in range(2)]
                yblk = [SB(cb, f"yblk{i}", [128, D]) for i in range(2)]
                fgt_l = [SB(cb, f"fgt{i}", [128, D]) for i in range(2)]
                fgs = SB(cb, "fgs", [128, D])
                g2bc = SB(cb, "g2bc", [128, D])
                yA_l = [SB(cb, f"yA{i}", [128, D]) for i in range(2)]
                yB_l = [SB(cb, f"yB{i}", [128, D]) for i in range(2)]
                fst_l = [SB(cb, f"fst{i}", [128, 2, 6]) for i in range(2)]
                fmv_l = [SB(cb, f"fmv{i}", [128, 4]) for i in range(2)]
                P.add("sp", lambda e: e.dma_start(out=fgs[:], in_=fg_d[:, :]), w=["fgs"], dma=True)
                P.add("dve", lambda e: e.tensor_copy(out=idb2[:], in_=ident), r=["cst"], w=["idb2"])
                make_gate(g2bc, "g2bc", 40)
                sc_keys = []
                for i in range(32):
                    xl = xload[i % 2]
                    xlk = f"xl{i % 2}"
                    P.add("sp", (lambda xl, i: lambda e: e.dma_start(out=xl[:], in_=xn2_d[i * 128:(i + 1) * 128, :]))(xl, i), r=["xn2_d"], w=[xlk], dma=True)
                    for k in range(2):
                        key = f"xsc{i}_{k}"
                        P.add("pool", (lambda xl, i, k: lambda e: e.indirect_dma_start(out=xs_d[:, :], out_offset=IOA(ap=PABi[:, k, i:i + 1], axis=0),
                                                                                       in_=xl[:], in_offset=None))(xl, i, k),
                              r=[xlk, "PABi"] + xz_keys, w=[key], dma=True)
                        sc_keys.append(key)
                ys_keys = []
                def do_block(b):
                    P.add("pool", (lambda b: lambda e: e.indirect_dma_start(out=W32[:], out_offset=None, in_=wall_d[:, :],
                                                                           in_offset=IOA(ap=IDXW[:, 0, b:b + 1], axis=0), bounds_check=bc_reg(e), oob_is_err=False))(b),
                          r=["IDXW", "W32"], w=["W32"], dma=True)
                    P.add("act", lambda e: e.activation(out=Wgb[:].rearrange("p a b -> p (a b)"), in_=W32[:, 0:4096], func=AF.Identity), r=["W32"], w=["Wgb"])
                    P.add("dve", lambda e: e.tensor_copy(out=Wub[:].rearrange("p a b -> p (a b)"), in_=W32[:, 4096:8192]), r=["W32"], w=["Wub"])
                    P.add("pool", lambda e: e.tensor_copy(out=Wdb[:, 0:2, :].rearrange("p a b -> p (a b)"), in_=W32[:, 8192:10240]), r=["W32"], w=["Wdb"])
                    P.add("dve", lambda e: e.tensor_copy(out=Wdb[:, 2:4, :].rearrange("p a b -> p (a b)"), in_=W32[:, 10240:12288]), r=["W32"], w=["Wdb"])
                    XgT = XgT_l[b % 2]
                    xgk = f"XgT{b % 2}"
                    sg = sg_l[b % 2]
                    sgk = f"sg{b % 2}"
                    actb = actb_l[b % 2]
                    abk = f"actb{b % 2}"
                    actT = actT_l[b % 2]
                    atk = f"actT{b % 2}"
                    xb_ = xsb[b % 2]
                    xbk = f"xsb{b % 2}"
                    P.add("sp", (lambda xb_, b: lambda e: e.dma_start(out=xb_[:], in_=xs_d[b * 128:(b + 1) * 128, :]))(xb_, b), r=sc_keys, w=[xbk], dma=True)
                    for kc in range(8):
                        P.add("pe", (lambda xb_, kc: lambda e: e.matmul(psum[kc // 4][:, (kc % 4) * 128:(kc % 4 + 1) * 128], lhsT=xb_[:, kc * 128:(kc + 1) * 128],
                                                                       rhs=idb2[:], start=True, stop=True))(xb_, kc), r=[xbk, "idb2"], w=[pk(kc // 4)])
                    for kc in range(8):
                        P.add("act", (lambda kc: lambda e: e.activation(out=XgT[:, kc, :], in_=psum[kc // 4][:, (kc % 4) * 128:(kc % 4 + 1) * 128], func=AF.Identity,
                                                                        scale=sc[:, 8 + kc:9 + kc], bias=mod[:, 24 + kc:25 + kc]))(kc), r=[pk(kc // 4), "sc", "mod"], w=[xgk])
                    for kc in range(8):
                        P.add("pe", (lambda kc: lambda e: e.matmul(psum[2][:, 0:512], lhsT=XgT[:, kc, :], rhs=Wgb[:, kc, :], start=(kc == 0), stop=(kc == 7)))(kc),
                              r=[xgk, "Wgb"], w=[pk(2)])
                    for kc in range(8):
                        P.add("pe", (lambda kc: lambda e: e.matmul(psum[3][:, 0:512], lhsT=XgT[:, kc, :], rhs=Wub[:, kc, :], start=(kc == 0), stop=(kc == 7)))(kc),
                              r=[xgk, "Wub"], w=[pk(3)])
                    P.add("act", lambda e: e.activation(out=sg[:], in_=psum[2][:, 0:512], func=AF.Silu), r=[pk(2)], w=[sgk])
                    P.add("dve", lambda e: e.tensor_tensor(out=actb[:], in0=psum[3][:, 0:512], in1=sg[:], op=ALU.mult), r=[pk(3), sgk], w=[abk])
                    for hc in range(4):
                        P.add("pe", (lambda hc: lambda e: e.matmul(psum[4][:, hc * 128:(hc + 1) * 128], lhsT=actb[:, hc * 128:(hc + 1) * 128], rhs=idb2[:],
                                                                   start=True, stop=True))(hc), r=[abk, "idb2"], w=[pk(4)])
                    P.add("act", lambda e: e.activation(out=actT[:].rearrange("p a b -> p (a b)"), in_=psum[4][:, 0:512], func=AF.Identity), r=[pk(4)], w=[atk])
                    yb_ = yblk[b % 2]
                    ybk = f"yblk{b % 2}"
                    for hf in range(2):
                        for hc in range(4):
                            P.add("pe", (lambda hc, hf: lambda e: e.matmul(psum[5 + hf][:, 0:512], lhsT=actT[:, hc, :], rhs=Wdb[:, hc, hf * 512:(hf + 1) * 512],
                                                                           start=(hc == 0), stop=(hc == 3)))(hc, hf), r=[atk, "Wdb"], w=[pk(5 + hf)])
                        if hf == 0:
                            P.add("act", (lambda yb_: lambda e: e.activation(out=yb_[:, 0:512], in_=psum[5][:, 0:512], func=AF.Identity))(yb_), r=[pk(5)], w=[ybk])
                        else:
                            P.add("dve", (lambda yb_: lambda e: e.tensor_copy(out=yb_[:, 512:1024], in_=psum[6][:, 0:512]))(yb_), r=[pk(6)], w=[ybk])
                    key = f"ys{b}"
                    P.add("sp", (lambda yb_, b: lambda e: e.dma_start(out=ys_d[b * 128:(b + 1) * 128, :], in_=yb_[:]))(yb_, b), r=[ybk], w=[key], dma=True)
                    ys_keys.append(key)

                for b in range(min(NBLK, nblk)):
                    do_block(b)
                def do_final(i):
                    tok0 = i * 128
                    q = i % 2
                    yA, yB, fgt, fst, fmv = yA_l[q], yB_l[q], fgt_l[q], fst_l[q], fmv_l[q]
                    kA, kB, kF, kS, kM = f"yA{q}", f"yB{q}", f"fgt{q}", f"fst{q}", f"fmv{q}"
                    P.add("pool", (lambda i: lambda e: e.indirect_dma_start(out=yA[:], out_offset=None, in_=ys_d[:, :], in_offset=IOA(ap=PABi[:, 0, i:i + 1], axis=0)))(i),
                          r=ys_keys + ["PABi", kA], w=[kA], dma=True)
                    P.add("pool", (lambda i: lambda e: e.indirect_dma_start(out=yB[:], out_offset=None, in_=ys_d[:, :], in_offset=IOA(ap=PABi[:, 1, i:i + 1], axis=0)))(i),
                          r=ys_keys + ["PABi", kB], w=[kB], dma=True)
                    P.add("sp", (lambda tok0: lambda e: e.dma_start(out=fgt[:], in_=x1_d[tok0:tok0 + 128, :]))(tok0), r=["x1_d"], w=[kF], dma=True)
                    P.add("dve", (lambda i: lambda e: e.tensor_scalar(out=yA[:], in0=yA[:], scalar1=WAB[:, 0, i:i + 1], scalar2=None, op0=ALU.mult))(i), r=[kA, "WAB"], w=[kA])
                    P.add("dve", (lambda i: lambda e: e.scalar_tensor_tensor(out=yA[:], in0=yB[:], scalar=WAB[:, 1, i:i + 1], in1=yA[:], op0=ALU.mult, op1=ALU.add))(i),
                          r=[kA, kB, "WAB"], w=[kA])
                    P.add("dve", lambda e: e.tensor_tensor(out=yA[:], in0=yA[:], in1=g2bc[:], op=ALU.mult), r=[kA, "g2bc"], w=[kA])
                    P.add("dve", lambda e: e.tensor_tensor(out=fgt[:], in0=fgt[:], in1=yA[:], op=ALU.add), r=[kA, kF], w=[kF])
                    for hf in range(2):
                        P.add("dve", (lambda hf: lambda e: e.bn_stats(out=fst[:, hf, :], in_=fgt[:, hf * 512:(hf + 1) * 512]))(hf), r=[kF], w=[kS])
                    P.add("dve", lambda e: e.bn_aggr(out=fmv[:, 0:2], in_=fst[:, 0:2, :].rearrange("p a b -> p (a b)")), r=[kS], w=[kM])
                    P.add("dve", lambda e: e.scalar_tensor_tensor(out=fmv[:, 2:3], in0=fmv[:, 0:1], scalar=fmv[:, 0:1], in1=fmv[:, 1:2], op0=ALU.mult, op1=ALU.add),
                          r=[kM], w=[kM])
                    P.add("act", lambda e: e.activation(out=fmv[:, 3:4], in_=fmv[:, 2:3], func=AF.Sqrt, bias=float(1e-6)), r=[kM], w=[kM])
                    P.add("dve", lambda e: e.reciprocal(out=fmv[:, 3:4], in_=fmv[:, 3:4]), r=[kM], w=[kM])
                    P.add("dve", lambda e: e.scalar_tensor_tensor(out=fgt[:], in0=fgt[:], scalar=fmv[:, 3:4], in1=fgs[:], op0=ALU.mult, op1=ALU.mult),
                          r=[kF, kM, "fgs"], w=[kF])
                    P.add("sp", (lambda tok0: lambda e: e.dma_start(out=out_d[tok0:tok0 + 128, :], in_=fgt[:]))(tok0), r=[kF], w=[f"out{i}"], dma=True)

                for i in range(32):
                    do_final(i)
        else:
            with ExitStack() as cb:
                fgt_l = [SB(cb, f"fgt{i}", [128, D]) for i in range(2)]
                for ti in range(32):
                    tok0 = ti * 128
                    P.add("sp", (lambda tok0: lambda e: e.dma_start(out=fgt[:], in_=x1_d[tok0:tok0 + 128, :]))(tok0), r=["x1_d"], w=["fgt"], dma=True)
                    P.add("sp", (lambda tok0: lambda e: e.dma_start(out=out_d[tok0:tok0 + 128, :], in_=fgt[:]))(tok0), r=["fgt"], w=["out_d"], dma=True)
        P.emit()
    return nc


_NC_CACHE = {}


def _layouts(inp):
    f = np.float32
    g = lambda k: np.asarray(inp[k], dtype=f)
    col = lambda v, n: np.ascontiguousarray(v.reshape(n, 128).T)
    mu = g("rwkv_mu")[0]
    def pad128(v):
        o = np.zeros((128, 1), f)
        o[:v.shape[0], 0] = v
        return o
    shared = [None, col(g("ada_b")[0], 48), col(g("norm1_g")[0], 8), col(g("norm2_g")[0], 8), col(mu[0:1536], 12),
              pad128(mu[1536:1568]), pad128(mu[1568:1600]), pad128(mu[1600:1696]), col(g("rwkv_w0")[0], 4), col(g("rwkv_a0")[0], 4),
              col(g("rwkv_k_k")[0], 4), col(g("rwkv_k_a")[0], 4), col(g("rwkv_r_k")[0].reshape(512), 4), col(g("rwkv_gn_w")[0], 4),
              col(g("rwkv_gn_b")[0], 4)]
    tt = np.arange(64)
    su = (tt[:, None] < tt[None, :]).astype(f)
    iu = (tt[:, None] <= tt[None, :]).astype(f)
    sl = (tt[None, :] < tt[:, None]).astype(f)
    m5 = np.tile(np.concatenate([su, iu, su, iu, sl], axis=1), (2, 1))
    ident = np.eye(128, dtype=f)
    istack = np.tile(np.eye(64, dtype=f), (2, 1))
    ss = np.arange(128)
    tril = (ss[:, None] <= ss[None, :]).astype(f)
    bones = (ss[:, None] // 64 == ss[None, :] // 64).astype(f)
    ones = np.ones((128, 128), f)
    thr = np.broadcast_to((np.arange(32) * 128).astype(f)[None, :], (128, 32))
    iotab = np.broadcast_to(np.arange(96).astype(f)[None, :], (128, 96))
    pidx = np.arange(128).astype(f)[:, None]
    cst = np.ascontiguousarray(np.concatenate([m5, ident, istack, tril, bones, ones, thr, iotab, pidx], axis=1))
    assert cst.shape[1] == NCST
    rep = lambda v: np.broadcast_to(v[None, :], (128, v.shape[0]))
    bs = g("gmlp_bs")[0]
    bsT = np.zeros((128, 4, 128), f)
    for j in range(4):
        for hh in range(2):
            bsT[hh * 64:(hh + 1) * 64, j, :] = bs[2 * j + hh][None, :]
    bc = np.ascontiguousarray(np.concatenate([rep(g("gmlp_ln_w")[0]), rep(g("gmlp_ln_b")[0]),
                                              rep(np.concatenate([g("router_group_b")[0], g("router_expert_b")[0]])),
                                              bsT.reshape(128, 512)], axis=1))
    assert bc.shape[1] == NBC
    common = dict(cst=cst, bc=bc, fg=np.ascontiguousarray(rep(g("final_norm_g"))), ada_w=np.ascontiguousarray(g("ada_w")[0]), w_in=np.ascontiguousarray(g("w_in")[0]),
                  w_out=np.ascontiguousarray(g("w_out")[0]), w2=np.ascontiguousarray(g("rwkv_w2")[0]),
                  a2=np.ascontiguousarray(g("rwkv_a2")[0]), g2=np.ascontiguousarray(g("rwkv_g2")[0]),
                  wsT=np.ascontiguousarray(g("gmlp_ws")[0].transpose(2, 0, 1)),
                  wr=np.ascontiguousarray(np.concatenate([g("router_group_w")[0], g("router_expert_w")[0]], axis=1)),
                  wall=np.ascontiguousarray(np.concatenate([
                      g("moe_w_gate")[0].reshape(NE, 8, 128, EH).transpose(0, 2, 1, 3).reshape(NE * 128, 4096),
                      g("moe_w_up")[0].reshape(NE, 8, 128, EH).transpose(0, 2, 1, 3).reshape(NE * 128, 4096),
                      g("moe_w_down")[0].reshape(NE, 4, 128, D).transpose(0, 2, 1, 3).reshape(NE * 128, 4096)], axis=1)))
    x = g("x")
    c = g("c")
    maps = []
    for b in range(x.shape[0]):
        cols = [col(c[b], 8)] + shared[1:]
        prm = np.ascontiguousarray(np.concatenate(cols, axis=1))
        assert prm.shape[1] == NPRM
        m = dict(common)
        m["x"] = np.ascontiguousarray(x[b])
        m["prm"] = prm
        maps.append(m)
    return maps


def kernel(**inputs):
    maps = _layouts(inputs)
    if "nc" not in _NC_CACHE:
        _NC_CACHE["nc"] = build_nc()
    nc = _NC_CACHE["nc"]
    res = run_bass_kernel_spmd(nc, maps, core_ids=list(range(len(maps))))
    return np.stack([r["out"] for r in res.results], axis=0).astype(np.float32)
```
